# Optimizing a Trainium2 kernel written in Bass

```python
import jax, jax.numpy as jnp
from jax import lax
import numpy as np

D_MODEL = 1024
BATCH = 2
SEQ = 8192
DEPTH = 2

N_MIXERS = 2
N_FOX = (DEPTH + 1) // 2
N_HGRN = DEPTH // 2
FOX_HEADS = 16
FOX_HEAD_DIM = D_MODEL // FOX_HEADS
FOX_BLOCK_Q = 128
HGRN_EXPAND = 128
HGRN_HEADS = D_MODEL // HGRN_EXPAND
HGRN_CHUNK = 64
D_FF = ((8 * D_MODEL // 3 + 127) // 128) * 128
N_SUB = 3
EPS = 1e-6

kernel_name = "fox_hgrn2_macaron_adaln_hybrid"


def _normal(k, shape, scale):
    return jax.random.normal(k, shape, jnp.float32) * scale


def setup_inputs(seed: int = 0) -> dict:
    key = jax.random.key(seed)
    ks = jax.random.split(key, 16)
    D, F = D_MODEL, D_FF
    return {
        "x": _normal(ks[0], (BATCH, SEQ, D), 1.0),
        "c": _normal(ks[1], (BATCH, D), 1.0),
        "ada_w": _normal(ks[2], (DEPTH, D, N_SUB * 3 * D), D ** -0.5),
        "ada_b": _normal(ks[3], (DEPTH, N_SUB * 3 * D), 0.02),
        "norm_g": 1.0 + _normal(ks[4], (DEPTH, N_SUB, D), 0.02),
        "ffn_w_up": _normal(ks[5], (DEPTH, 2, D, 2 * F), D ** -0.5),
        "ffn_w_down": _normal(ks[6], (DEPTH, 2, F, D), F ** -0.5),
        "fox_w_in": _normal(ks[7], (N_FOX, D, 4 * D + FOX_HEADS), D ** -0.5),
        "fox_b_f": 2.0 + _normal(ks[8], (N_FOX, FOX_HEADS), 0.5),
        "fox_w_out": _normal(ks[9], (N_FOX, D, D), D ** -0.5),
        "hgrn_w_in": _normal(ks[10], (N_HGRN, D, 4 * D), D ** -0.5),
        "hgrn_norm_g": 1.0 + _normal(ks[11], (N_HGRN, D), 0.02),
        "hgrn_w_out": _normal(ks[12], (N_HGRN, D, D), D ** -0.5),
        "hgrn_lb_logits": _normal(ks[13], (DEPTH, D), 0.5),
        "final_norm_g": 1.0 + _normal(ks[14], (D,), 0.02),
    }


def rms_norm(x, g):
    xf = x.astype(jnp.float32)
    y = xf * lax.rsqrt(jnp.mean(xf * xf, axis=-1, keepdims=True) + EPS)
    return (y * g.astype(jnp.float32)).astype(x.dtype)


def swiglu(h, w_up, w_down):
    a, b = jnp.split(h @ w_up, 2, axis=-1)
    return (jax.nn.silu(a) * b) @ w_down


def fox_attention(h, w_in, b_f, w_out):
    B, S, D = h.shape
    H, Dh, BQ = FOX_HEADS, FOX_HEAD_DIM, FOX_BLOCK_Q
    proj = h @ w_in
    heads = lambda t: t.reshape(B, S, H, Dh).transpose(0, 2, 1, 3)
    q = heads(proj[..., 0:D])
    k = heads(proj[..., D:2 * D])
    v = heads(proj[..., 2 * D:3 * D])
    g = proj[..., 3 * D:4 * D]
    logf = jax.nn.log_sigmoid((proj[..., 4 * D:] + b_f).astype(jnp.float32))
    cum = jnp.cumsum(logf, axis=1).transpose(0, 2, 1)
    nb = S // BQ
    q_blocks = q.reshape(B, H, nb, BQ, Dh).transpose(2, 0, 1, 3, 4)
    c_blocks = cum.reshape(B, H, nb, BQ).transpose(2, 0, 1, 3)
    k_pos = jnp.arange(S)
    scale = Dh ** -0.5

    def block(args):
        qb, cb, start = args
        s = (jnp.einsum('bhqd,bhkd->bhqk', qb, k).astype(jnp.float32) * scale
             + cb[..., None] - cum[:, :, None, :])
        q_pos = start + jnp.arange(BQ)
        s = jnp.where(k_pos[None, :] <= q_pos[:, None], s, -jnp.inf)
        p = jax.nn.softmax(s, axis=-1)
        return jnp.einsum('bhqk,bhkd->bhqd', p.astype(v.dtype), v)

    o = lax.map(block, (q_blocks, c_blocks, jnp.arange(nb) * BQ))
    o = o.transpose(1, 0, 3, 2, 4).reshape(B, S, D)
    o = o * jax.nn.sigmoid(g)
    return o @ w_out


def hgrn2(h, w_in, lb, g_norm, w_out):
    B, S, D = h.shape
    H, K, C = HGRN_HEADS, HGRN_EXPAND, HGRN_CHUNK
    nc = S // C
    proj = (h @ w_in).astype(jnp.float32)
    q = proj[..., 0:D]
    f = lb + (1.0 - lb) * jax.nn.sigmoid(proj[..., D:2 * D])
    v = jax.nn.silu(proj[..., 2 * D:3 * D])
    g = proj[..., 3 * D:4 * D]
    logf = jnp.log(f)
    k = 1.0 - f
    to_chunks = lambda t: t.reshape(B, nc, C, H, K).transpose(1, 0, 3, 2, 4)
    tri = jnp.tril(jnp.ones((C, C), dtype=bool))

    def step(state, inp):
        qc, kc, vc, gc = inp
        G = jnp.cumsum(gc, axis=2)
        o_inter = jnp.einsum('bhtk,bhkv->bhtv', qc * jnp.exp(G), state)
        diff = G[:, :, :, None, :] - G[:, :, None, :, :]
        decay = jnp.exp(jnp.where(tri[:, :, None], diff, -jnp.inf))
        A = jnp.einsum('bhtk,bhsk,bhtsk->bhts', qc, kc, decay)
        o_intra = jnp.einsum('bhts,bhsv->bhtv', A, vc)
        G_last = G[:, :, -1, :]
        state = (jnp.exp(G_last)[..., None] * state
                 + jnp.einsum('bhsk,bhsv->bhkv', kc * jnp.exp(G_last[:, :, None, :] - G), vc))
        return state, o_inter + o_intra

    state0 = jnp.zeros((B, H, K, K), jnp.float32)
    _, o = lax.scan(step, state0, (to_chunks(q), to_chunks(k), to_chunks(v), to_chunks(logf)))
    o = o.transpose(1, 0, 3, 2, 4)
    o = o * lax.rsqrt(jnp.mean(o * o, axis=-1, keepdims=True) + EPS)
    o = o * g_norm.astype(jnp.float32).reshape(H, K)
    o = o.reshape(B, S, D) * jax.nn.silu(g)
    return o.astype(h.dtype) @ w_out


def reference(x, c, ada_w, ada_b, norm_g, ffn_w_up, ffn_w_down, fox_w_in, fox_b_f,
              fox_w_out, hgrn_w_in, hgrn_norm_g, hgrn_w_out, hgrn_lb_logits, final_norm_g):
    B, S, D = x.shape
    cond = jax.nn.silu(c)
    sm = jax.nn.softmax(hgrn_lb_logits.astype(jnp.float32), axis=0)
    lower_bounds = jnp.cumsum(sm, axis=0) - sm[0]
    for i in range(DEPTH):
        mod = (cond @ ada_w[i] + ada_b[i]).reshape(B, N_SUB, 3, 1, D)
        h = rms_norm(x, norm_g[i, 0]) * (1.0 + mod[:, 0, 1]) + mod[:, 0, 0]
        x = x + 0.5 * mod[:, 0, 2] * swiglu(h, ffn_w_up[i, 0], ffn_w_down[i, 0])
        h = rms_norm(x, norm_g[i, 1]) * (1.0 + mod[:, 1, 1]) + mod[:, 1, 0]
        j = i // N_MIXERS
        if i % N_MIXERS == 0:
            y = fox_attention(h, fox_w_in[j], fox_b_f[j], fox_w_out[j])
        else:
            y = hgrn2(h, hgrn_w_in[j], lower_bounds[i], hgrn_norm_g[j], hgrn_w_out[j])
        x = x + mod[:, 1, 2] * y
        h = rms_norm(x, norm_g[i, 2]) * (1.0 + mod[:, 2, 1]) + mod[:, 2, 0]
        x = x + 0.5 * mod[:, 2, 2] * swiglu(h, ffn_w_up[i, 1], ffn_w_down[i, 1])
    return rms_norm(x, final_norm_g)
```

```python
from contextlib import ExitStack
import numpy as np
import ml_dtypes
import concourse.bass as bass
import concourse.mybir as mybir
from concourse.bass_utils import run_bass_kernel_spmd

F32 = mybir.dt.float32
BF16 = mybir.dt.bfloat16
AF = mybir.ActivationFunctionType
ALU = mybir.AluOpType
NPBF = ml_dtypes.bfloat16

D = 1024
B = 2
S = 8192
DFF = 2816
NF = 22
EPS = 1e-6
NCORES = 8
T = 2048
FH = 16
FD = 64
HH = 8
CH = 64


class Buf:
    __slots__ = ("w", "r", "t", "key")

    def __init__(self, t=None):
        self.w = None
        self.r = []
        self.t = t
        self.key = None


class Prog:
    ENG = ["pe", "act", "dve", "pool", "sp"]

    def __init__(self, nc):
        self.nc = nc
        self.ops = {e: [] for e in self.ENG}
        self.clock = {e: {} for e in self.ENG}
        self.snaps = {}
        self.count = {}
        self.needed = set()
        self.nkey = 0
        self.final = None

    def sb(self, es, name, shape, dtype):
        t = es.enter_context(self.nc.sbuf_tensor(name, list(shape), dtype))
        return Buf(t)

    def ps(self, es, name, shape, dtype):
        t = es.enter_context(self.nc.psum_tensor(name, list(shape), dtype))
        return Buf(t)

    def op(self, eng, fn, reads=(), writes=(), dma=None, pe_acc=False):
        need = {}

        def req(ev):
            if ev is None:
                return
            k, s = ev
            if s > need.get(k, 0):
                need[k] = s

        for b in reads:
            req(b.w)
        for b in writes:
            if not (pe_acc and b.w is not None and b.w[0] == "pe"):
                req(b.w)
            for r in b.r:
                req(r)
        key = dma or eng
        if fn is None:
            idx = 0
        else:
            idx = self.count.get(key, 0) + 1
            self.count[key] = idx
        ck = self.clock[eng]
        waits = []
        for k, s in need.items():
            if ck.get(k, 0) < s:
                waits.append((k, s))
        for k, s in waits:
            sn = self.snaps[(k, s)]
            for kk, ss in sn.items():
                if ck.get(kk, 0) < ss:
                    ck[kk] = ss
            if ck.get(k, 0) < s:
                ck[k] = s
            self.needed.add((k, s))
        self.ops[eng].append((fn, waits, key, idx))
        if fn is None:
            return None
        self.snaps[(key, idx)] = dict(ck)
        ev = (key, idx)
        for b in reads:
            b.r.append(ev)
        for b in writes:
            b.w = ev
            b.r = []
        return ev

    def dma(self, eng, out, in_, reads=(), writes=(), ow=None):
        wb = ow if ow is not None else writes[0]
        if wb.key is None:
            self.nkey += 1
            wb.key = "d%d" % self.nkey
        ev = self.op(eng, lambda e: e.dma_start(out=out, in_=in_), reads=reads, writes=writes, dma=wb.key)
        if ow is not None:
            ow.w = ev
        return ev

    def finish(self, outs, eng="sp"):
        self.op(eng, None, reads=list(outs))

    def barrier(self):
        need = dict(self.count)
        for e in self.ENG:
            self.op(e, None, extra=need)

    def emit(self):
        nc = self.nc
        keys = list(self.count.keys())
        for e in self.ENG:
            if e not in keys:
                keys.append(e)
        rank = {}
        for k in keys:
            if k in self.ENG:
                idxs = sorted(s for (kk, s) in self.needed if kk == k)
                rank[k] = {s: i + 1 for i, s in enumerate(idxs)}
        with ExitStack() as es:
            sems = {k: es.enter_context(nc.semaphore("s_" + k)) for k in keys}
            block = es.enter_context(nc.Block())

            def run(eng_name):
                def body(e):
                    for fn, waits, key, idx in self.ops[eng_name]:
                        for k, s in waits:
                            v = rank[k][s] if k in rank else 16 * s
                            e.wait_ge(sems[k], v)
                        if fn is None:
                            continue
                        ins = fn(e)
                        if key in rank:
                            if (key, idx) in self.needed:
                                ins.then_inc(sems[key], 1)
                        else:
                            ins.then_inc(sems[key], 16)
                return body

            block.tensor(run("pe"))
            block.scalar(run("act"))
            block.vector(run("dve"))
            block.gpsimd(run("pool"))
            block.sync(run("sp"))


def _patch_op():
    base = Prog.op

    def op(self, eng, fn, reads=(), writes=(), dma=None, pe_acc=False, extra=None):
        if extra:
            dummy = []
            for k, s in extra.items():
                if s > 0:
                    b = Buf()
                    b.w = (k, s)
                    dummy.append(b)
            reads = list(reads) + dummy
            ev = base(self, eng, fn, reads=reads, writes=writes, dma=dma, pe_acc=pe_acc)
            return ev
        return base(self, eng, fn, reads=reads, writes=writes, dma=dma, pe_acc=pe_acc)

    Prog.op = op


_patch_op()


SQD = float(np.sqrt(D))


class Ctx:
    pass


def mcol(l, v, ch):
    return (l * 9 + v) * 8 + ch


def build_F(cfg):
    nc = bass.Bass("TRN2", target_bir_lowering=False)
    P = Prog(nc)
    c = Ctx()
    c.P, c.nc = P, nc
    dr = {}

    def din(name, shape, dt=F32):
        dr[name] = nc.dram_tensor(name, list(shape), dt, kind="ExternalInput").ap()
        return dr[name]

    def dout(name, shape, dt=F32):
        dr[name] = nc.dram_tensor(name, list(shape), dt, kind="ExternalOutput").ap()
        return dr[name]

    xT = din("xT", [D, T])
    modT = din("modT", [128, 144])
    gT = din("gT", [128, 48])
    outs = []
    with ExitStack() as es:
        X = es.enter_context(nc.sbuf_tensor("X", [128, 8, T], F32))
        c.X = X
        c.Xb = [[Buf(X) for _ in range(4)] for _ in range(8)]
        c.modt = P.sb(es, "modt", [128, 144], F32)
        c.gt = P.sb(es, "gt", [128, 48], F32)
        c.der = P.sb(es, "der", [128, 96], F32)
        c.ones = P.sb(es, "ones", [128, 128], BF16)
        c.bank = [P.ps(es, "bank%d" % i, [128, 512], F32) for i in range(8)]
        sqt = es.enter_context(nc.sbuf_tensor("sq", [128, 8, 512], BF16))
        c.sq = [Buf(sqt) for _ in range(8)]
        c.rstd = P.sb(es, "rstd", [128, 512], F32)
        c.tmp = [P.sb(es, "tmp%d" % i, [128, 512], F32) for i in range(2)]

        xv = xT.rearrange("(c p) t -> p c t", p=128)
        for kc in range(8):
            P.dma("sp", X[:, kc, :], xv[:, kc, :], writes=[c.Xb[kc][tt] for tt in range(4)])
        P.dma("sp", c.modt.t[:, :], modT, writes=[c.modt])
        P.dma("sp", c.gt.t[:, :], gT, writes=[c.gt])
        P.op("pool", lambda e: e.memset(c.ones.t[:, :], 1.0), writes=[c.ones])
        for l in range(2):
            for sub in range(3):
                base = ((l * 3 + sub) * 2) * 8
                sc0 = mcol(l, sub * 3 + 1, 0)
                g0 = (l * 3 + sub) * 8
                ga0 = mcol(l, sub * 3 + 2, 0)
                P.op("dve", lambda e, base=base, sc0=sc0, g0=g0: e.scalar_tensor_tensor(
                    c.der.t[:, base:base + 8], c.modt.t[:, sc0:sc0 + 8], 1.0, c.gt.t[:, g0:g0 + 8], ALU.add, ALU.mult),
                    reads=[c.modt, c.gt], writes=[c.der])
                P.op("dve", lambda e, base=base: e.tensor_scalar(
                    c.der.t[:, base:base + 8], c.der.t[:, base:base + 8], SQD, None, ALU.mult),
                    reads=[c.der], writes=[c.der])
                P.op("dve", lambda e, base=base, ga0=ga0, sub=sub: e.tensor_scalar(
                    c.der.t[:, base + 8:base + 16], c.modt.t[:, ga0:ga0 + 8], (1.0 if sub == 1 else 0.5), None, ALU.mult),
                    reads=[c.modt], writes=[c.der])

        def Acol(l, sub, ch):
            j = ((l * 3 + sub) * 2) * 8 + ch
            return c.der.t[:, j:j + 1]

        def Gcol(l, sub, ch):
            j = ((l * 3 + sub) * 2 + 1) * 8 + ch
            return c.der.t[:, j:j + 1]

        def Scol(l, sub, ch):
            j = mcol(l, sub * 3 + 0, ch)
            return c.modt.t[:, j:j + 1]

        def rstd_tile(src_fn, src_bufs, epsk):
            for kc in range(8):
                P.op("act", lambda e, kc=kc: e.activation(sqt[:, kc, :], src_fn(kc), AF.Square),
                     reads=[src_bufs[kc]], writes=[c.sq[kc]])
            for kc in range(8):
                P.op("pe", lambda e, kc=kc: e.matmul(c.bank[6].t[:, :], c.ones.t[:, :], sqt[:, kc, :],
                                                      start=(kc == 0), stop=(kc == 7)),
                     reads=[c.ones, c.sq[kc]], writes=[c.bank[6]], pe_acc=(kc > 0))
            P.op("dve", lambda e: e.tensor_scalar(c.rstd.t[:, :], c.bank[6].t[:, :], epsk, None, ALU.add),
                 reads=[c.bank[6]], writes=[c.rstd])
            P.op("act", lambda e: e.activation(c.rstd.t[:, :], c.rstd.t[:, :], AF.Sqrt), reads=[c.rstd], writes=[c.rstd])
            P.op("dve", lambda e: e.reciprocal(c.rstd.t[:, :], c.rstd.t[:, :]), reads=[c.rstd], writes=[c.rstd])

        def modnorm_tile(l, sub, tt, hdst, hbuf):
            t0 = tt * 512
            rstd_tile(lambda kc: X[:, kc, t0:t0 + 512], [c.Xb[kc][tt] for kc in range(8)], EPS * D)
            for kc in range(8):
                tb = c.tmp[kc % 2]
                P.op("dve", lambda e, kc=kc, tb=tb: e.tensor_tensor(tb.t[:, :], X[:, kc, t0:t0 + 512], c.rstd.t[:, :], ALU.mult),
                     reads=[c.Xb[kc][tt], c.rstd], writes=[tb])
                P.op("act", lambda e, kc=kc, tb=tb: e.activation(hdst(kc), tb.t[:, :], AF.Identity,
                                                               bias=Scol(l, sub, kc), scale=Acol(l, sub, kc)),
                     reads=[tb, c.der, c.modt], writes=[hbuf(kc)])

        def epilogue(kind, l):
            oT = din("oT", [D, T])
            sgd = din("sg", [D, T], BF16)
            wod = din("wo", [128, 8192])
            ov = oT.rearrange("(c p) t -> p c t", p=128)
            sv = sgd.rearrange("(c p) t -> p c t", p=128)
            with ExitStack() as e2:
                wo = P.sb(e2, "wo_sb", [128, 8, 1024], BF16)
                P.dma("pool", wo.t[:, :, :], wod.rearrange("p (k d) -> p k d", k=8), writes=[wo])
                ot = [P.sb(e2, "ot%d" % i, [128, 8, 512], F32) for i in range(2)]
                st = [P.sb(e2, "st%d" % i, [128, 8, 512], BF16) for i in range(2)]
                ogt = [e2.enter_context(nc.sbuf_tensor("og%d" % i, [128, 8, 512], BF16)) for i in range(2)]
                ogb = [[Buf(ogt[i]) for _ in range(8)] for i in range(2)]
                if kind == "hgrn":
                    hgd = din("hgn", [128, 8])
                    hg = P.sb(e2, "hg", [128, 8], F32)
                    P.dma("sp", hg.t[:, :], hgd, writes=[hg])
                    P.op("dve", lambda e: e.tensor_scalar(hg.t[:, :], hg.t[:, :], float(np.sqrt(128.0)), None, ALU.mult),
                         reads=[hg], writes=[hg])
                    sq1 = P.sb(e2, "sq1", [128, 512], BF16)
                    r1 = P.sb(e2, "r1", [128, 512], F32)
                    t1 = P.sb(e2, "t1", [128, 512], F32)
                for tt in range(4):
                    t0 = tt * 512
                    o_, s_, og_ = ot[tt % 2], st[tt % 2], ogt[tt % 2]
                    P.dma("sp", o_.t[:, :, :], ov[:, :, t0:t0 + 512], writes=[o_])
                    P.dma("sp", s_.t[:, :, :], sv[:, :, t0:t0 + 512], writes=[s_])
                    if kind == "fox":
                        for kc in range(8):
                            P.op("dve", lambda e, kc=kc, o_=o_, s_=s_, og_=og_: e.tensor_tensor(
                                og_[:, kc, :], o_.t[:, kc, :], s_.t[:, kc, :], ALU.mult),
                                reads=[o_, s_], writes=[ogb[tt % 2][kc]])
                    else:
                        for kc in range(8):
                            P.op("act", lambda e, kc=kc, o_=o_: e.activation(sq1.t[:, :], o_.t[:, kc, :], AF.Square),
                                 reads=[o_], writes=[sq1])
                            P.op("pe", lambda e: e.matmul(c.bank[7].t[:, :], c.ones.t[:, :], sq1.t[:, :], start=True, stop=True),
                                 reads=[c.ones, sq1], writes=[c.bank[7]])
                            P.op("dve", lambda e: e.tensor_scalar(r1.t[:, :], c.bank[7].t[:, :], EPS * 128.0, None, ALU.add),
                                 reads=[c.bank[7]], writes=[r1])
                            P.op("act", lambda e: e.activation(r1.t[:, :], r1.t[:, :], AF.Sqrt), reads=[r1], writes=[r1])
                            P.op("dve", lambda e: e.reciprocal(r1.t[:, :], r1.t[:, :]), reads=[r1], writes=[r1])
                            P.op("dve", lambda e, kc=kc, o_=o_: e.tensor_tensor(t1.t[:, :], o_.t[:, kc, :], r1.t[:, :], ALU.mult),
                                 reads=[o_, r1], writes=[t1])
                            P.op("dve", lambda e, kc=kc, s_=s_, og_=og_: e.scalar_tensor_tensor(
                                og_[:, kc, :], t1.t[:, :], hg.t[:, kc:kc + 1], s_.t[:, kc, :], ALU.mult, ALU.mult),
                                reads=[t1, hg, s_], writes=[ogb[tt % 2][kc]])
                    for dc in range(8):
                        bk = c.bank[4 + dc % 2]
                        for kc in range(8):
                            P.op("pe", lambda e, kc=kc, dc=dc, bk=bk, og_=og_: e.matmul(
                                bk.t[:, :], wo.t[:, kc, dc * 128:(dc + 1) * 128], og_[:, kc, :],
                                start=(kc == 0), stop=(kc == 7)),
                                reads=[wo, ogb[tt % 2][kc]], writes=[bk], pe_acc=(kc > 0))
                        P.op("dve", lambda e, dc=dc, bk=bk, t0=t0: e.scalar_tensor_tensor(
                            X[:, dc, t0:t0 + 512], bk.t[:, :], Gcol(l, 1, dc), X[:, dc, t0:t0 + 512], ALU.mult, ALU.add),
                            reads=[bk, c.der, c.Xb[dc][tt]], writes=[c.Xb[dc][tt]])
            P.barrier()

        def ffn(j, l, sub):
            wupd = din("wup%d" % j, [11, 128, 4096])
            wdnd = din("wdn%d" % j, [8, 128, 2816])
            with ExitStack() as e2:
                hbt = e2.enter_context(nc.sbuf_tensor("hb_%d" % j, [128, 8, 1024], BF16))
                hbb = [[Buf(hbt) for _ in range(2)] for _ in range(8)]
                actt = e2.enter_context(nc.sbuf_tensor("actb_%d" % j, [128, NF, 1024], BF16))
                actb = [[Buf(actt) for _ in range(2)] for _ in range(NF)]
                wu = [P.sb(e2, "wu%d_%d" % (j, i), [128, 2, 8, 256], BF16) for i in range(2)]
                wd = [P.sb(e2, "wd%d_%d" % (j, i), [128, NF, 128], BF16) for i in range(2)]
                sa = [P.sb(e2, "sa%d_%d" % (j, i), [128, 512], F32) for i in range(2)]
                for half in range(2):
                    for t2 in range(2):
                        tt = half * 2 + t2
                        modnorm_tile(l, sub, tt, lambda kc, t2=t2: hbt[:, kc, t2 * 512:(t2 + 1) * 512],
                                     lambda kc, t2=t2: hbb[kc][t2])
                    it = 0
                    for g in range(11):
                        w_ = wu[g % 2]
                        P.dma("pool", w_.t[:, :, :, :], wupd[g].rearrange("p (a k f) -> p a k f", a=2, k=8), writes=[w_])
                        for jf in range(2):
                            fc = 2 * g + jf
                            for t2 in range(2):
                                bA, bB = c.bank[it % 2], c.bank[2 + it % 2]
                                s_ = sa[it % 2]
                                it += 1
                                for kc in range(8):
                                    P.op("pe", lambda e, kc=kc, w_=w_, jf=jf, t2=t2, bA=bA: e.matmul(
                                        bA.t[:, :], w_.t[:, 0, kc, jf * 128:(jf + 1) * 128], hbt[:, kc, t2 * 512:(t2 + 1) * 512],
                                        start=(kc == 0), stop=(kc == 7)),
                                        reads=[w_, hbb[kc][t2]], writes=[bA], pe_acc=(kc > 0))
                                for kc in range(8):
                                    P.op("pe", lambda e, kc=kc, w_=w_, jf=jf, t2=t2, bB=bB: e.matmul(
                                        bB.t[:, :], w_.t[:, 1, kc, jf * 128:(jf + 1) * 128], hbt[:, kc, t2 * 512:(t2 + 1) * 512],
                                        start=(kc == 0), stop=(kc == 7)),
                                        reads=[w_, hbb[kc][t2]], writes=[bB], pe_acc=(kc > 0))
                                P.op("act", lambda e, s_=s_, bA=bA: e.activation(s_.t[:, :], bA.t[:, :], AF.Silu),
                                     reads=[bA], writes=[s_])
                                P.op("dve", lambda e, s_=s_, bB=bB, fc=fc, t2=t2: e.tensor_tensor(
                                    actt[:, fc, t2 * 512:(t2 + 1) * 512], bB.t[:, :], s_.t[:, :], ALU.mult),
                                    reads=[bB, s_], writes=[actb[fc][t2]])
                    for dc in range(8):
                        w_ = wd[dc % 2]
                        P.dma("pool", w_.t[:, :, :], wdnd[dc].rearrange("p (f d) -> p f d", f=NF), writes=[w_])
                        for t2 in range(2):
                            tt = half * 2 + t2
                            t0 = tt * 512
                            bk = c.bank[4 + (dc * 2 + t2) % 2]
                            for fc in range(NF):
                                P.op("pe", lambda e, fc=fc, w_=w_, t2=t2, bk=bk: e.matmul(
                                    bk.t[:, :], w_.t[:, fc, :], actt[:, fc, t2 * 512:(t2 + 1) * 512],
                                    start=(fc == 0), stop=(fc == NF - 1)),
                                    reads=[w_, actb[fc][t2]], writes=[bk], pe_acc=(fc > 0))
                            P.op("dve", lambda e, dc=dc, bk=bk, t0=t0: e.scalar_tensor_tensor(
                                X[:, dc, t0:t0 + 512], bk.t[:, :], Gcol(l, sub, dc), X[:, dc, t0:t0 + 512], ALU.mult, ALU.add),
                                reads=[bk, c.der, c.Xb[dc][tt]], writes=[c.Xb[dc][tt]])
            P.barrier()

        def proj_fm(wname, hbt, hbb, evac, n_oc=8):
            wd_ = din(wname, [n_oc, 128, 1024])
            with ExitStack() as e3:
                wp = [P.sb(e3, wname + "_sb%d" % i, [128, 8, 128], BF16) for i in range(2)]
                it = 0
                for oc in range(n_oc):
                    w_ = wp[oc % 2]
                    P.dma("pool", w_.t[:, :, :], wd_[oc].rearrange("p (k f) -> p k f", k=8), writes=[w_])
                    for tt in range(4):
                        bk = c.bank[it % 4]
                        it += 1
                        for kc in range(8):
                            P.op("pe", lambda e, kc=kc, w_=w_, tt=tt, bk=bk: e.matmul(
                                bk.t[:, :], w_.t[:, kc, :], hbt[:, kc, tt * 512:(tt + 1) * 512],
                                start=(kc == 0), stop=(kc == 7)),
                                reads=[w_, hbb[kc][tt]], writes=[bk], pe_acc=(kc > 0))
                        evac(oc, tt, bk)
                P.barrier()

        def proj_tm(wname, hbt, hbb, vout, vob, func):
            wd_ = din(wname, [2, 128, 4096])
            with ExitStack() as e3:
                wv = P.sb(e3, wname + "_sb", [128, 2, 8, 512], BF16)
                for cg in range(2):
                    P.dma("pool", wv.t[:, cg, :, :], wd_[cg].rearrange("p (k f) -> p k f", k=8), writes=[wv])
                vt = [P.sb(e3, "vt%d" % i, [128, 512], BF16) for i in range(2)]
                it = 0
                for tk in range(16):
                    for cg in range(2):
                        bk = c.bank[it % 4]
                        v_ = vt[it % 2]
                        it += 1
                        for kc in range(8):
                            P.op("pe", lambda e, kc=kc, tk=tk, cg=cg, bk=bk: e.matmul(
                                bk.t[:, :], hbt[:, kc, tk * 128:(tk + 1) * 128], wv.t[:, cg, kc, :],
                                start=(kc == 0), stop=(kc == 7)),
                                reads=[wv, hbb[kc][tk // 4]], writes=[bk], pe_acc=(kc > 0))
                        P.op("act", lambda e, bk=bk, v_=v_: e.activation(v_.t[:, :], bk.t[:, :], func),
                             reads=[bk], writes=[v_])
                        P.dma("sp", vout[tk * 128:(tk + 1) * 128, cg * 512:(cg + 1) * 512], v_.t[:, :], reads=[v_], ow=vob)
                P.barrier()

        def stage_out(e3, name, shape, dt):
            return [P.sb(e3, name + "%d" % i, shape, dt) for i in range(2)]

        def projections(kind, l):
            with ExitStack() as e2:
                hbt = e2.enter_context(nc.sbuf_tensor("hb2", [128, 8, T], BF16))
                hbb = [[Buf(hbt) for _ in range(4)] for _ in range(8)]
                for tt in range(4):
                    modnorm_tile(l, 1, tt, lambda kc, tt=tt: hbt[:, kc, tt * 512:(tt + 1) * 512],
                                 lambda kc, tt=tt: hbb[kc][tt])
                qo = dout("qT", [D, T], BF16)
                qob = Buf()
                outs.append(qob)
                sgo = dout("sgo", [D, T], BF16)
                sgob = Buf()
                outs.append(sgob)
                vo = dout("v", [T, D], BF16)
                vob = Buf()
                outs.append(vob)
                cnt = [0]

                def simple_evac(od, ob, func, scale, st, dt_eng="act"):
                    def evac(oc, tt, bk):
                        s_ = st[cnt[0] % 2]
                        cnt[0] += 1
                        P.op("act", lambda e, s_=s_, bk=bk: e.activation(s_.t[:, :], bk.t[:, :], func, scale=scale),
                             reads=[bk], writes=[s_])
                        P.dma("sp", od[oc * 128:(oc + 1) * 128, tt * 512:(tt + 1) * 512], s_.t[:, :], reads=[s_], ow=ob)
                    return evac

                stb = stage_out(e2, "stb", [128, 512], BF16)
                if kind == "fox":
                    ko = dout("kT", [D, T], BF16)
                    kob = Buf()
                    outs.append(kob)
                    lfo = dout("lf", [16, T], F32)
                    lfob = Buf()
                    outs.append(lfob)
                    proj_fm("wq", hbt, hbb, simple_evac(qo, qob, AF.Copy, float(FD ** -0.5), stb))
                    proj_fm("wk", hbt, hbb, simple_evac(ko, kob, AF.Copy, 1.0, stb))
                    proj_fm("wg", hbt, hbb, simple_evac(sgo, sgob, AF.Sigmoid, 1.0, stb))
                    proj_tm("wv", hbt, hbb, vo, vob, AF.Copy)
                    wfd = din("wf", [128, 128])
                    bfd = din("bf", [16, 1])
                    wf = P.sb(e2, "wf_sb", [128, 8, 16], BF16)
                    P.dma("pool", wf.t[:, :, :], wfd.rearrange("p (k f) -> p k f", k=8), writes=[wf])
                    nbf = P.sb(e2, "nbf", [16, 1], F32)
                    P.dma("sp", nbf.t[:, :], bfd, writes=[nbf])
                    P.op("dve", lambda e: e.tensor_scalar(nbf.t[:, :], nbf.t[:, :], -1.0, None, ALU.mult), reads=[nbf], writes=[nbf])
                    e1 = P.sb(e2, "e1", [16, 512], F32)
                    l1 = [P.sb(e2, "l1_%d" % i, [16, 512], F32) for i in range(2)]
                    for tt in range(4):
                        bk = c.bank[tt % 4]
                        for kc in range(8):
                            P.op("pe", lambda e, kc=kc, tt=tt, bk=bk: e.matmul(
                                bk.t[0:16, :], wf.t[:, kc, :], hbt[:, kc, tt * 512:(tt + 1) * 512],
                                start=(kc == 0), stop=(kc == 7)),
                                reads=[wf, hbb[kc][tt]], writes=[bk], pe_acc=(kc > 0))
                        l_ = l1[tt % 2]
                        P.op("act", lambda e, bk=bk: e.activation(e1.t[:, :], bk.t[0:16, :], AF.Exp, bias=nbf.t[:, 0:1], scale=-1.0),
                             reads=[bk, nbf], writes=[e1])
                        P.op("act", lambda e, l_=l_: e.activation(l_.t[:, :], e1.t[:, :], AF.Ln, bias=1.0, scale=1.0),
                             reads=[e1], writes=[l_])
                        P.op("dve", lambda e, l_=l_: e.tensor_scalar(l_.t[:, :], l_.t[:, :], -1.0, None, ALU.mult),
                             reads=[l_], writes=[l_])
                        P.dma("sp", lfo[:, tt * 512:(tt + 1) * 512], l_.t[:, :], reads=[l_], ow=lfob)
                else:
                    ko = dout("kT", [D, T], F32)
                    kob = Buf()
                    outs.append(kob)
                    lfo = dout("lfT", [D, T], F32)
                    lfob = Buf()
                    outs.append(lfob)
                    lbd = din("lbl", [128, 16])
                    lbl = P.sb(e2, "lbl_sb", [128, 16], F32)
                    lb = P.sb(e2, "lb", [128, 8], F32)
                    oml = P.sb(e2, "oml", [128, 8], F32)
                    P.dma("sp", lbl.t[:, :], lbd, writes=[lbl])
                    P.op("dve", lambda e: e.tensor_tensor(lb.t[:, :], lbl.t[:, 8:16], lbl.t[:, 0:8], ALU.subtract), reads=[lbl], writes=[lb])
                    P.op("act", lambda e: e.activation(lb.t[:, :], lb.t[:, :], AF.Sigmoid), reads=[lb], writes=[lb])
                    P.op("dve", lambda e: e.tensor_scalar(oml.t[:, :], lb.t[:, :], -1.0, 1.0, ALU.mult, ALU.add), reads=[lb], writes=[oml])
                    proj_fm("wq", hbt, hbb, simple_evac(qo, qob, AF.Copy, 1.0, stb))
                    proj_fm("wg", hbt, hbb, simple_evac(sgo, sgob, AF.Silu, 1.0, stb))
                    proj_tm("wv", hbt, hbb, vo, vob, AF.Silu)
                    sg1 = P.sb(e2, "sg1", [128, 512], F32)
                    ff = stage_out(e2, "ff", [128, 512], F32)
                    lff = stage_out(e2, "lff", [128, 512], F32)
                    kk = stage_out(e2, "kk", [128, 512], F32)

                    def f_evac(oc, tt, bk):
                        i = cnt[0] % 2
                        cnt[0] += 1
                        f_, l_, k_ = ff[i], lff[i], kk[i]
                        P.op("act", lambda e, bk=bk: e.activation(sg1.t[:, :], bk.t[:, :], AF.Sigmoid), reads=[bk], writes=[sg1])
                        P.op("dve", lambda e, f_=f_, oc=oc: e.tensor_scalar(f_.t[:, :], sg1.t[:, :], oml.t[:, oc:oc + 1], lb.t[:, oc:oc + 1], ALU.mult, ALU.add),
                             reads=[sg1, oml, lb], writes=[f_])
                        P.op("act", lambda e, f_=f_, l_=l_: e.activation(l_.t[:, :], f_.t[:, :], AF.Ln), reads=[f_], writes=[l_])
                        P.op("dve", lambda e, f_=f_, k_=k_: e.tensor_scalar(k_.t[:, :], f_.t[:, :], -1.0, 1.0, ALU.mult, ALU.add),
                             reads=[f_], writes=[k_])
                        P.dma("sp", lfo[oc * 128:(oc + 1) * 128, tt * 512:(tt + 1) * 512], l_.t[:, :], reads=[l_], ow=lfob)
                        P.dma("sp", ko[oc * 128:(oc + 1) * 128, tt * 512:(tt + 1) * 512], k_.t[:, :], reads=[k_], ow=kob)
                    proj_fm("wf", hbt, hbb, f_evac)
            P.barrier()

        if cfg.get("epi"):
            epilogue(cfg["epi"][0], cfg["epi"][1])
        for j, (l, sub) in enumerate(cfg["ffns"]):
            ffn(j, l, sub)
        if cfg.get("proj"):
            projections(cfg["proj"][0], cfg["proj"][1])
        xo = dout("xo", [D, T])
        xob = Buf()
        outs.append(xob)
        xov = xo.rearrange("(c p) t -> p c t", p=128)
        if cfg.get("final"):
            fgd = din("fg", [128, 8])
            fg = P.sb(es, "fg_sb", [128, 8], F32)
            P.dma("sp", fg.t[:, :], fgd, writes=[fg])
            P.op("dve", lambda e: e.tensor_scalar(fg.t[:, :], fg.t[:, :], SQD, None, ALU.mult), reads=[fg], writes=[fg])
            yo = [P.sb(es, "yo%d" % i, [128, 512], F32) for i in range(2)]
            it = 0
            for tt in range(4):
                t0 = tt * 512
                rstd_tile(lambda kc, t0=t0: X[:, kc, t0:t0 + 512], [c.Xb[kc][tt] for kc in range(8)], EPS * D)
                for kc in range(8):
                    y_ = yo[it % 2]
                    it += 1
                    P.op("dve", lambda e, kc=kc, y_=y_, t0=t0: e.scalar_tensor_tensor(
                        y_.t[:, :], X[:, kc, t0:t0 + 512], fg.t[:, kc:kc + 1], c.rstd.t[:, :], ALU.mult, ALU.mult),
                        reads=[c.Xb[kc][tt], fg, c.rstd], writes=[y_])
                    P.dma("sp", xov[:, kc, t0:t0 + 512], y_.t[:, :], reads=[y_], ow=xob)
        else:
            for kc in range(8):
                P.dma("sp", xov[:, kc, :], X[:, kc, :], reads=[c.Xb[kc][tt] for tt in range(4)], ow=xob)
        P.finish(outs)
        P.emit()
    return nc


MC = 2304


def build_mod():
    nc = bass.Bass("TRN2", target_bir_lowering=False)
    P = Prog(nc)
    cT = nc.dram_tensor("cT", [128, 16], F32, kind="ExternalInput").ap()
    w = nc.dram_tensor("w", [128, 8 * MC], F32, kind="ExternalInput").ap()
    bias = nc.dram_tensor("bias", [2, MC], F32, kind="ExternalInput").ap()
    mo = nc.dram_tensor("mo", [2, MC], F32, kind="ExternalOutput").ap()
    with ExitStack() as es:
        ct = P.sb(es, "ct", [128, 8, 2], F32)
        wt = [P.sb(es, "wt%d" % i, [128, 8, 384], F32) for i in range(6)]
        bt = P.sb(es, "bt", [2, MC], F32)
        ot = P.sb(es, "ot", [2, MC], F32)
        banks = [P.ps(es, "bk%d" % i, [128, 512], F32) for i in range(2)]
        P.dma("sp", ct.t[:, :, :], cT.rearrange("p (k b) -> p k b", k=8), writes=[ct])
        P.dma("sp", bt.t[:, :], bias, writes=[bt])
        wv = w.rearrange("p (k n) -> p k n", k=8)
        for i in range(6):
            P.dma("sp" if i % 2 == 0 else "pool", wt[i].t[:, :, :], wv[:, :, i * 384:(i + 1) * 384], writes=[wt[i]])
        P.op("act", lambda e: e.activation(ct.t[:, :, :], ct.t[:, :, :], AF.Silu), reads=[ct], writes=[ct])
        for i in range(6):
            bk = banks[i % 2]
            for kc in range(8):
                P.op("pe", lambda e, kc=kc, i=i, bk=bk: e.matmul(bk.t[0:2, 0:384], ct.t[:, kc, :], wt[i].t[:, kc, :],
                                                                 start=(kc == 0), stop=(kc == 7)),
                     reads=[ct, wt[i]], writes=[bk], pe_acc=(kc > 0))
            P.op("dve", lambda e, i=i, bk=bk: e.tensor_tensor(ot.t[:, i * 384:(i + 1) * 384], bk.t[0:2, 0:384],
                                                             bt.t[:, i * 384:(i + 1) * 384], ALU.add),
                 reads=[bk, bt], writes=[ot])
        ob = Buf()
        P.dma("sp", mo, ot.t[:, :], reads=[ot], ow=ob)
        P.finish([ob])
        P.emit()
    return nc


def run_mod(c, ada_w, ada_b):
    nc = build_mod()
    cT = np.ascontiguousarray(c.T.reshape(8, 128, B).transpose(1, 0, 2)).reshape(128, 16)
    wall = np.concatenate([ada_w[0], ada_w[1]], axis=1)
    ball = np.concatenate([ada_b[0], ada_b[1]], axis=0)
    maps = []
    for j in range(NCORES):
        wj = wall[:, j * MC:(j + 1) * MC].reshape(8, 128, MC).transpose(1, 0, 2)
        maps.append({"cT": cT, "w": np.ascontiguousarray(wj).reshape(128, 8 * MC),
                     "bias": np.ascontiguousarray(np.broadcast_to(ball[j * MC:(j + 1) * MC], (2, MC)))})
    res = run_bass_kernel_spmd(nc, maps, core_ids=list(range(NCORES)))
    mod = np.concatenate([r["mo"] for r in res.results], axis=1)
    return mod.reshape(B, 2, 9, D)


def fm_cols(v):
    lead = int(np.prod(v.shape[:-1])) if v.ndim > 1 else 1
    a = v.reshape(lead, 8, 128).transpose(2, 0, 1)
    return np.ascontiguousarray(a).reshape(128, lead * 8)


def tile_w_fm(w):
    n = w.shape[1] // 128
    a = w.reshape(8, 128, n, 128).transpose(2, 1, 0, 3)
    return np.ascontiguousarray(a).reshape(n, 128, 1024)


def tile_w_tm(w):
    a = w.reshape(8, 128, 2, 512).transpose(2, 1, 0, 3)
    return np.ascontiguousarray(a).reshape(2, 128, 4096)


def tile_wup(w):
    a = w.reshape(8, 128, 2, 11, 256).transpose(3, 1, 2, 0, 4)
    return np.ascontiguousarray(a).reshape(11, 128, 4096)


def tile_wdn(w):
    a = w.reshape(NF, 128, 8, 128).transpose(2, 1, 0, 3)
    return np.ascontiguousarray(a).reshape(8, 128, NF * 128)


def tile_wo(w):
    a = w.reshape(8, 128, D).transpose(1, 0, 2)
    return np.ascontiguousarray(a).reshape(128, 8 * D)


NEG = -30000.0


def build_fox():
    nc = bass.Bass("TRN2", target_bir_lowering=False)
    P = Prog(nc)
    qd = nc.dram_tensor("q", [4, 64, S], BF16, kind="ExternalInput").ap()
    kd = nc.dram_tensor("k", [4, 64, S], BF16, kind="ExternalInput").ap()
    vd = nc.dram_tensor("v", [4, 128, 64 * 64], BF16, kind="ExternalInput").ap()
    ltd = nc.dram_tensor("lt", [128, 256], F32, kind="ExternalInput").ap()
    lqd = nc.dram_tensor("lq", [16, 2048], F32, kind="ExternalInput").ap()
    Ud = nc.dram_tensor("U", [128, 128], F32, kind="ExternalInput").ap()
    seld = nc.dram_tensor("sel", [128, 128], F32, kind="ExternalInput").ap()
    mkd = nc.dram_tensor("mk", [128, 128], F32, kind="ExternalInput").ap()
    od = nc.dram_tensor("o", [4, 64, S], F32, kind="ExternalOutput").ap()
    shi = nc.dram_tensor("shi", [4, S], BF16).ap()
    slo = nc.dram_tensor("slo", [4, S], BF16).ap()
    with ExitStack() as es:
        bank = [P.ps(es, "bank%d" % i, [128, 512], F32) for i in range(8)]
        U = P.sb(es, "U_sb", [128, 128], F32)
        sel = P.sb(es, "sel_sb", [128, 128], F32)
        mk = P.sb(es, "mk_sb", [128, 128], F32)
        onesf = P.sb(es, "onesf", [128, 128], F32)
        lt = P.sb(es, "lt_sb", [128, 256], F32)
        lq = P.sb(es, "lq_sb", [16, 2048], F32)
        within = P.sb(es, "within", [128, 256], F32)
        tot = P.sb(es, "tot", [128, 256], F32)
        inc = P.sb(es, "inc", [128, 256], F32)
        GT = P.sb(es, "GT", [128, 256], F32)
        gend = P.sb(es, "gend", [128, 256], F32)
        negB = P.sb(es, "negB", [128, 4 * 16 * 64], F32)
        cl = P.sb(es, "cl", [16, 2048], F32)
        Aa = P.sb(es, "Aa", [16, 2048], F32)
        ahi = P.sb(es, "ahi", [16, 2048], BF16)
        ahf = P.sb(es, "ahf", [16, 2048], F32)
        alo = P.sb(es, "alo", [16, 2048], BF16)
        qa = [P.sb(es, "qa%d" % i, [66, S], BF16) for i in range(2)]
        ka = [P.sb(es, "ka%d" % i, [66, S], BF16) for i in range(2)]
        va = [P.sb(es, "va%d" % i, [128, 64, 65], BF16) for i in range(2)]
        pt = [P.sb(es, "pt%d" % i, [128, 512], BF16) for i in range(3)]
        drow = P.sb(es, "drow", [65, 512], F32)
        rec = P.sb(es, "rec", [64, 512], F32)
        oo = [P.sb(es, "oo%d" % i, [64, 512], F32) for i in range(2)]
        ob = Buf()

        for t_, d_ in ((U, Ud), (sel, seld), (mk, mkd), (lt, ltd), (lq, lqd)):
            P.dma("sp", t_.t[:, :], d_, writes=[t_])
        P.op("pool", lambda e: e.memset(onesf.t[:, :], 1.0), writes=[onesf])
        P.op("pe", lambda e: e.matmul(bank[6].t[:, 0:256], U.t[:, :], lt.t[:, :], start=True, stop=True), reads=[U, lt], writes=[bank[6]])
        P.op("pe", lambda e: e.matmul(bank[7].t[:, 0:256], onesf.t[:, :], lt.t[:, :], start=True, stop=True), reads=[onesf, lt], writes=[bank[7]])
        P.op("dve", lambda e: e.tensor_copy(within.t[:, :], bank[6].t[:, 0:256]), reads=[bank[6]], writes=[within])
        P.op("dve", lambda e: e.tensor_copy(tot.t[:, :], bank[7].t[:, 0:256]), reads=[bank[7]], writes=[tot])
        for h in range(4):
            P.op("dve", lambda e, h=h: e.tensor_tensor_scan(inc.t[:, h * 64:(h + 1) * 64], onesf.t[:, 0:64], tot.t[:, h * 64:(h + 1) * 64],
                                                            0.0, ALU.mult, ALU.add), reads=[onesf, tot], writes=[inc])
        P.op("dve", lambda e: e.tensor_tensor(GT.t[:, :], within.t[:, :], inc.t[:, :], ALU.add), reads=[within, inc], writes=[GT])
        P.op("dve", lambda e: e.tensor_tensor(GT.t[:, :], GT.t[:, :], tot.t[:, :], ALU.subtract), reads=[GT, tot], writes=[GT])
        P.op("pe", lambda e: e.matmul(bank[6].t[:, 0:256], sel.t[:, :], GT.t[:, :], start=True, stop=True), reads=[sel, GT], writes=[bank[6]])
        P.op("dve", lambda e: e.tensor_copy(gend.t[:, :], bank[6].t[:, 0:256]), reads=[bank[6]], writes=[gend])
        for h in range(4):
            for Q in range(16):
                j0 = (h * 16 + Q) * 64
                gc = h * 64 + 4 * Q + 3
                P.op("dve", lambda e, h=h, j0=j0, gc=gc: e.tensor_scalar(
                    negB.t[:, j0:j0 + 64], GT.t[:, h * 64:(h + 1) * 64], -1.0, gend.t[:, gc:gc + 1], ALU.mult, ALU.add),
                    reads=[GT, gend], writes=[negB])
        ones16 = P.sb(es, "ones16", [16, 512], F32)
        P.op("pool", lambda e: e.memset(ones16.t[:, :], 1.0), writes=[ones16])
        for h in range(4):
            P.op("dve", lambda e, h=h: e.tensor_tensor_scan(cl.t[:, h * 512:(h + 1) * 512], ones16.t[:, :], lq.t[:, h * 512:(h + 1) * 512],
                                                            0.0, ALU.mult, ALU.add), reads=[lq, ones16], writes=[cl])
        for h in range(4):
            P.op("dve", lambda e, h=h: e.tensor_scalar(Aa.t[:, h * 512:(h + 1) * 512], cl.t[:, h * 512:(h + 1) * 512],
                                                       cl.t[:, h * 512 + 511:h * 512 + 512], None, ALU.subtract),
                 reads=[cl], writes=[Aa])
        P.op("dve", lambda e: e.tensor_copy(ahi.t[:, :], Aa.t[:, :]), reads=[Aa], writes=[ahi])
        P.op("dve", lambda e: e.tensor_copy(ahf.t[:, :], ahi.t[:, :]), reads=[ahi], writes=[ahf])
        P.op("dve", lambda e: e.tensor_tensor(alo.t[:, :], Aa.t[:, :], ahf.t[:, :], ALU.subtract), reads=[Aa, ahf], writes=[alo])
        shb, slb = Buf(), Buf()
        P.dma("sp", shi.rearrange("h (q m) -> q h m", q=16), ahi.t[:, :].rearrange("q (h m) -> q h m", h=4), reads=[ahi], writes=[shb])
        P.dma("sp", slo.rearrange("h (q m) -> q h m", q=16), alo.t[:, :].rearrange("q (h m) -> q h m", h=4), reads=[alo], writes=[slb])

        for i in range(2):
            P.op("pool", lambda e, i=i: e.memset(ka[i].t[64:66, :], 1.0), writes=[ka[i]])
            P.op("pool", lambda e, i=i: e.memset(va[i].t[:, :, 64:65], 1.0), writes=[va[i]])

        def load_head(h):
            q_, k_, v_ = qa[h % 2], ka[h % 2], va[h % 2]
            P.dma("sp", q_.t[0:64, :], qd[h], writes=[q_])
            P.dma("sp", q_.t[64:65, :], shi[h:h + 1, :], reads=[shb], writes=[q_])
            P.dma("sp", q_.t[65:66, :], slo[h:h + 1, :], reads=[slb], writes=[q_])
            P.dma("pool", k_.t[0:64, :], kd[h], writes=[k_])
            P.dma("pool", v_.t[:, :, 0:64], vd[h].rearrange("p (t d) -> p t d", d=64), writes=[v_])

        load_head(0)

        def do_head(h, q_, k_, v_, nit):
            items = [(Q, kt) for Q in range(16) for kt in range(4 * Q + 4)]

            def emit_S(idx, it_no):
                Q, kt = items[idx]
                d = kt - 4 * Q
                c0 = 128 * d if d >= 0 else 0
                bk = bank[it_no % 3]
                p_ = pt[it_no % 3]
                P.op("pe", lambda e: e.matmul(bk.t[:, c0:512], k_.t[0:66, kt * 128:(kt + 1) * 128],
                                              q_.t[0:66, Q * 512 + c0:(Q + 1) * 512], start=True, stop=True),
                     reads=[k_, q_], writes=[bk])
                if d >= 0:
                    P.op("dve", lambda e: e.tensor_tensor(bk.t[:, c0:c0 + 128], bk.t[:, c0:c0 + 128], mk.t[:, :], ALU.add),
                         reads=[bk, mk], writes=[bk])
                jb = (h * 16 + Q) * 64 + kt
                P.op("act", lambda e: e.activation(p_.t[:, c0:512], bk.t[:, c0:512], AF.Exp, bias=negB.t[:, jb:jb + 1], scale=1.0),
                     reads=[bk, negB], writes=[p_])

            def emit_PV(idx, it_no):
                Q, kt = items[idx]
                d = kt - 4 * Q
                c0 = 128 * d if d >= 0 else 0
                p_ = pt[it_no % 3]
                ob_ = bank[3 + Q % 2]
                last = (kt == 4 * Q + 3)
                P.op("pe", lambda e: e.matmul(ob_.t[0:65, c0:512], v_.t[:, kt, :], p_.t[:, c0:512], start=(kt == 0), stop=last),
                     reads=[v_, p_], writes=[ob_], pe_acc=(kt > 0))
                if last:
                    o_ = oo[Q % 2]
                    P.op("act", lambda e: e.activation(drow.t[64:65, :], ob_.t[64:65, :], AF.Copy), reads=[ob_], writes=[drow])
                    P.op("pe", lambda e: e.matmul(bank[5].t[0:64, :], onesf.t[64:65, 0:64], drow.t[64:65, :], start=True, stop=True),
                         reads=[onesf, drow], writes=[bank[5]])
                    P.op("dve", lambda e: e.reciprocal(rec.t[:, :], bank[5].t[0:64, :]), reads=[bank[5]], writes=[rec])
                    P.op("dve", lambda e: e.tensor_tensor(o_.t[:, :], ob_.t[0:64, :], rec.t[:, :], ALU.mult), reads=[ob_, rec], writes=[o_])
                    P.dma("sp", od[h][:, Q * 512:(Q + 1) * 512], o_.t[:, :], reads=[o_], ow=ob)

            n = len(items)
            emit_S(0, nit)
            for idx in range(n):
                if idx + 1 < n:
                    emit_S(idx + 1, nit + idx + 1)
                emit_PV(idx, nit + idx)
            return nit + n

        nit = 0
        for h in range(4):
            if h + 1 < 4:
                load_head(h + 1)
            nit = do_head(h, qa[h % 2], ka[h % 2], va[h % 2], nit)
        P.finish([ob])
        P.emit()
    return nc


def fox_consts():
    k = np.arange(128)
    U = (k[:, None] <= k[None, :]).astype(np.float32)
    sel = np.zeros((128, 128), np.float32)
    sel[127, :] = 1.0
    mk = np.where(k[None, :] >= k[:, None], 0.0, NEG).astype(np.float32)
    return U, sel, mk


def build_hgrn():
    nc = bass.Bass("TRN2", target_bir_lowering=False)
    P = Prog(nc)
    qd = nc.dram_tensor("q", [2, 128, S], BF16, kind="ExternalInput").ap()
    kd = nc.dram_tensor("k", [2, 128, S], F32, kind="ExternalInput").ap()
    lfd = nc.dram_tensor("lf", [2, 128, S], F32, kind="ExternalInput").ap()
    vd = nc.dram_tensor("v", [2, 128, 64 * 128], BF16, kind="ExternalInput").ap()
    m01d = nc.dram_tensor("m01", [128, 64], F32, kind="ExternalInput").ap()
    rmd = nc.dram_tensor("rm", [128, 2048], F32, kind="ExternalInput").ap()
    idd = nc.dram_tensor("ident", [128, 128], BF16, kind="ExternalInput").ap()
    od = nc.dram_tensor("o", [2, 128, S], F32, kind="ExternalOutput").ap()
    NB = 2048
    with ExitStack() as es:
        bankA = [P.ps(es, "bankA%d" % i, [128, 512], F32) for i in range(2)]
        bankO = [P.ps(es, "bankO%d" % i, [128, 512], F32) for i in range(2)]
        bankU = [P.ps(es, "bankU%d" % i, [128, 512], F32) for i in range(2)]
        bankT = P.ps(es, "bankT", [128, 1024], BF16)
        m01 = P.sb(es, "m01_sb", [128, 64], F32)
        rm = P.sb(es, "rm_sb", [128, NB], F32)
        ident = P.sb(es, "ident_sb", [128, 128], BF16)
        P.dma("sp", m01.t[:, :], m01d, writes=[m01])
        P.dma("sp", rm.t[:, :], rmd, writes=[rm])
        P.dma("sp", ident.t[:, :], idd, writes=[ident])
        ob = Buf()
        hs = []
        for h in range(2):
            o = Ctx()
            o.qb = P.sb(es, "qb%d" % h, [128, NB], BF16)
            o.kb = P.sb(es, "kb%d" % h, [128, NB], F32)
            o.lf = P.sb(es, "lf%d" % h, [128, NB], F32)
            o.G = P.sb(es, "G%d" % h, [128, NB], F32)
            o.tmp = P.sb(es, "tmp%d" % h, [128, NB], F32)
            o.tmp2 = P.sb(es, "tmp2%d" % h, [128, NB], F32)
            o.qd = P.sb(es, "qd%d" % h, [128, NB], BF16)
            o.kdd = P.sb(es, "kdd%d" % h, [128, NB], BF16)
            o.kend = P.sb(es, "kend%d" % h, [128, NB], BF16)
            o.kT = P.sb(es, "kT%d" % h, [128, 16, 128], BF16)
            o.vb = P.sb(es, "vb%d" % h, [128, 16, 128], BF16)
            o.egl = P.sb(es, "egl%d" % h, [128, 32], F32)
            o.S32 = P.sb(es, "S32_%d" % h, [128, 128], F32)
            o.Sbf = [P.sb(es, "Sbf%d_%d" % (h, i), [128, 128], BF16) for i in range(2)]
            o.am = [P.sb(es, "am%d_%d" % (h, i), [128, 64], BF16) for i in range(2)]
            o.osb = [P.sb(es, "osb%d_%d" % (h, i), [128, 512], F32) for i in range(2)]
            P.op("pool", lambda e, o=o: e.memset(o.S32.t[:, :], 0.0), writes=[o.S32])
            P.op("pool", lambda e, o=o: e.memset(o.Sbf[0].t[:, :], 0.0), writes=[o.Sbf[0]])
            o.si = 0
            hs.append(o)

        def prep(h, blk):
            o = hs[h]
            t0 = blk * NB
            P.dma("sp", o.qb.t[:, :], qd[h][:, t0:t0 + NB], writes=[o.qb])
            P.dma("sp", o.kb.t[:, :], kd[h][:, t0:t0 + NB], writes=[o.kb])
            P.dma("sp", o.lf.t[:, :], lfd[h][:, t0:t0 + NB], writes=[o.lf])
            P.dma("pool", o.vb.t[:, :, :], vd[h][:, blk * 2048:(blk + 1) * 2048].rearrange("p (t d) -> p t d", d=128), writes=[o.vb])
            P.op("dve", lambda e: e.tensor_tensor_scan(o.G.t[:, :], rm.t[:, :], o.lf.t[:, :], 0.0, ALU.mult, ALU.add),
                 reads=[rm, o.lf], writes=[o.G])
            P.op("act", lambda e: e.activation(o.tmp.t[:, :], o.G.t[:, :], AF.Exp), reads=[o.G], writes=[o.tmp])
            P.op("dve", lambda e: e.tensor_tensor(o.qd.t[:, :], o.qb.t[:, :], o.tmp.t[:, :], ALU.mult), reads=[o.qb, o.tmp], writes=[o.qd])
            P.op("act", lambda e: e.activation(o.tmp2.t[:, :], o.G.t[:, :], AF.Exp, scale=-1.0), reads=[o.G], writes=[o.tmp2])
            P.op("dve", lambda e: e.tensor_tensor(o.tmp2.t[:, :], o.kb.t[:, :], o.tmp2.t[:, :], ALU.mult), reads=[o.kb, o.tmp2], writes=[o.tmp2])
            P.op("dve", lambda e: e.tensor_copy(o.kdd.t[:, :], o.tmp2.t[:, :]), reads=[o.tmp2], writes=[o.kdd])
            G3 = o.G.t[:, :].rearrange("p (c s) -> p c s", s=64)
            P.op("act", lambda e: e.activation(o.egl.t[:, :], G3[:, :, 63], AF.Exp), reads=[o.G], writes=[o.egl])
            for cc in range(32):
                P.op("dve", lambda e, cc=cc: e.tensor_scalar(o.kend.t[:, cc * 64:(cc + 1) * 64], o.tmp2.t[:, cc * 64:(cc + 1) * 64],
                                                             o.egl.t[:, cc:cc + 1], None, ALU.mult),
                     reads=[o.tmp2, o.egl], writes=[o.kend])
            for grp in range(2):
                for j in range(8):
                    tk = grp * 8 + j
                    P.op("pe", lambda e, tk=tk, j=j: e.transpose(bankT.t[:, j * 128:(j + 1) * 128], o.kend.t[:, tk * 128:(tk + 1) * 128], ident.t[:, :]),
                         reads=[o.kend, ident], writes=[bankT], pe_acc=(j > 0))
                P.op("act", lambda e, grp=grp: e.activation(o.kT.t[:, grp * 8:(grp + 1) * 8, :],
                                                           bankT.t[:, :].rearrange("p (t k) -> p t k", k=128), AF.Copy),
                     reads=[bankT], writes=[o.kT])

        nA = [0]

        def chunk(h, blk, cc):
            o = hs[h]
            tk, half = cc // 2, cc % 2
            pb = 64 * half
            gc = blk * 32 + cc
            cs = slice(cc * 64, (cc + 1) * 64)
            bA = bankA[nA[0] % 2]
            am = o.am[nA[0] % 2]
            nA[0] += 1
            bO = bankO[h]
            bU = bankU[h]
            oc0 = (gc % 8) * 64
            Sb = o.Sbf[o.si % 2]
            Sn = o.Sbf[(o.si + 1) % 2]
            o.si += 1
            P.op("pe", lambda e: e.matmul(bA.t[pb:pb + 64, 0:64], o.kdd.t[:, cs], o.qd.t[:, cs], start=True, stop=True),
                 reads=[o.kdd, o.qd], writes=[bA])
            P.op("dve", lambda e: e.tensor_tensor(am.t[pb:pb + 64, :], bA.t[pb:pb + 64, 0:64], m01.t[pb:pb + 64, :], ALU.mult),
                 reads=[bA, m01], writes=[am])
            P.op("pe", lambda e: e.matmul(bO.t[:, oc0:oc0 + 64], Sb.t[:, :], o.qd.t[:, cs], start=True, stop=False),
                 reads=[Sb, o.qd], writes=[bO], pe_acc=(gc % 8 != 0))
            P.op("pe", lambda e: e.matmul(bO.t[:, oc0:oc0 + 64], o.vb.t[pb:pb + 64, tk, :], am.t[pb:pb + 64, :], start=False, stop=True),
                 reads=[o.vb, am], writes=[bO], pe_acc=True)
            P.op("pe", lambda e: e.matmul(bU.t[:, 0:128], o.kT.t[pb:pb + 64, tk, :], o.vb.t[pb:pb + 64, tk, :], start=True, stop=True),
                 reads=[o.kT, o.vb], writes=[bU])
            P.op("dve", lambda e: e.scalar_tensor_tensor(o.S32.t[:, :], o.S32.t[:, :], o.egl.t[:, cc:cc + 1], bU.t[:, 0:128], ALU.mult, ALU.add),
                 reads=[o.S32, o.egl, bU], writes=[o.S32])
            P.op("act", lambda e: e.activation(Sn.t[:, :], o.S32.t[:, :], AF.Copy), reads=[o.S32], writes=[Sn])
            if gc % 8 == 7:
                os_ = o.osb[(gc // 8) % 2]
                P.op("act", lambda e: e.activation(os_.t[:, :], bO.t[:, :], AF.Copy), reads=[bO], writes=[os_])
                tok0 = (gc - 7) * 64
                P.dma("sp", od[h][:, tok0:tok0 + 512], os_.t[:, :], reads=[os_], ow=ob)

        for blk in range(4):
            for h in range(2):
                prep(h, blk)
            for cc in range(32):
                for h in range(2):
                    chunk(h, blk, cc)
        P.finish([ob])
        P.emit()
    return nc


def hgrn_consts():
    p = np.arange(128)
    t = np.arange(64)
    m01 = ((p[:, None] % 64) <= t[None, :]).astype(np.float32)
    rm = np.ones((128, 2048), np.float32)
    rm[:, ::64] = 0.0
    ident = np.eye(128, dtype=np.float32).astype(NPBF)
    return m01, rm, ident


_DBG = {}


def _run(nc, maps):
    return run_bass_kernel_spmd(nc, maps, core_ids=list(range(NCORES))).results


def kernel(x, c, ada_w, ada_b, norm_g, ffn_w_up, ffn_w_down, fox_w_in, fox_b_f, fox_w_out,
           hgrn_w_in, hgrn_norm_g, hgrn_w_out, hgrn_lb_logits, final_norm_g):
    f32 = lambda a: np.ascontiguousarray(np.asarray(a, dtype=np.float32))
    x, c, ada_w, ada_b, norm_g = f32(x), f32(c), f32(ada_w), f32(ada_b), f32(norm_g)
    ffn_w_up, ffn_w_down, fox_w_in, fox_b_f, fox_w_out = f32(ffn_w_up), f32(ffn_w_down), f32(fox_w_in), f32(fox_b_f), f32(fox_w_out)
    hgrn_w_in, hgrn_norm_g, hgrn_w_out = f32(hgrn_w_in), f32(hgrn_norm_g), f32(hgrn_w_out)
    hgrn_lb_logits, final_norm_g = f32(hgrn_lb_logits), f32(final_norm_g)

    mod = run_mod(c, ada_w, ada_b)
    _DBG["mod"] = mod
    modT = [fm_cols(mod[b].reshape(18, D)) for b in range(B)]
    gT = fm_cols(norm_g.reshape(6, D))
    cores = [(b, t) for b in range(B) for t in range(4)]

    nc1 = build_F({"ffns": [(0, 0)], "proj": ("fox", 0)})
    wi = fox_w_in[0]
    shared = {
        "gT": gT, "wup0": tile_wup(ffn_w_up[0, 0]), "wdn0": tile_wdn(ffn_w_down[0, 0]),
        "wq": tile_w_fm(wi[:, 0:D]), "wk": tile_w_fm(wi[:, D:2 * D]), "wg": tile_w_fm(wi[:, 3 * D:4 * D]),
        "wv": tile_w_tm(wi[:, 2 * D:3 * D]),
        "wf": np.ascontiguousarray(wi[:, 4 * D:4 * D + 16].reshape(8, 128, 16).transpose(1, 0, 2)).reshape(128, 128),
        "bf": np.ascontiguousarray(fox_b_f[0].reshape(16, 1)),
    }
    maps = []
    for (b, t) in cores:
        m = dict(shared)
        m["xT"] = np.ascontiguousarray(x[b, t * T:(t + 1) * T, :].T)
        m["modT"] = modT[b]
        maps.append(m)
    r1 = _run(nc1, maps)
    _DBG["r1"] = r1

    def cat_fm(res, name, b):
        return np.concatenate([res[b * 4 + t][name] for t in range(4)], axis=1)

    def cat_tm(res, name, b):
        return np.concatenate([res[b * 4 + t][name] for t in range(4)], axis=0)

    nc2 = build_fox()
    U, sel, mk = fox_consts()
    maps = []
    for b in range(B):
        qf = cat_fm(r1, "qT", b).reshape(FH, FD, S)
        kf = cat_fm(r1, "kT", b).reshape(FH, FD, S)
        vf = cat_tm(r1, "v", b)
        lf = cat_fm(r1, "lf", b)
        for g in range(4):
            l4 = lf[4 * g:4 * g + 4]
            v4 = np.stack([np.ascontiguousarray(vf[:, hd * FD:(hd + 1) * FD].reshape(64, 128, FD).transpose(1, 0, 2)).reshape(128, 64 * FD)
                           for hd in range(4 * g, 4 * g + 4)])
            maps.append({
                "q": np.ascontiguousarray(qf[4 * g:4 * g + 4]), "k": np.ascontiguousarray(kf[4 * g:4 * g + 4]), "v": v4,
                "lt": np.ascontiguousarray(l4.reshape(4, 64, 128).transpose(2, 0, 1)).reshape(128, 256),
                "lq": np.ascontiguousarray(l4.reshape(4, 16, 512).transpose(1, 0, 2)).reshape(16, 2048),
                "U": U, "sel": sel, "mk": mk,
            })
    r2 = _run(nc2, maps)
    _DBG["r2"] = r2
    ofull = [np.concatenate([r2[b * 4 + g]["o"].reshape(4 * FD, S) for g in range(4)], axis=0) for b in range(B)]

    nc3 = build_F({"epi": ("fox", 0), "ffns": [(0, 2), (1, 0)], "proj": ("hgrn", 1)})
    hi = hgrn_w_in[0]
    shared = {
        "gT": gT, "wo": tile_wo(fox_w_out[0]),
        "wup0": tile_wup(ffn_w_up[0, 1]), "wdn0": tile_wdn(ffn_w_down[0, 1]),
        "wup1": tile_wup(ffn_w_up[1, 0]), "wdn1": tile_wdn(ffn_w_down[1, 0]),
        "wq": tile_w_fm(hi[:, 0:D]), "wf": tile_w_fm(hi[:, D:2 * D]), "wg": tile_w_fm(hi[:, 3 * D:4 * D]),
        "wv": tile_w_tm(hi[:, 2 * D:3 * D]), "lbl": fm_cols(hgrn_lb_logits),
    }
    maps = []
    for i, (b, t) in enumerate(cores):
        m = dict(shared)
        m["xT"] = r1[i]["xo"]
        m["modT"] = modT[b]
        m["oT"] = np.ascontiguousarray(ofull[b][:, t * T:(t + 1) * T])
        m["sg"] = r1[i]["sgo"]
        maps.append(m)
    r3 = _run(nc3, maps)
    _DBG["r3"] = r3

    nc4 = build_hgrn()
    m01, rm, ident = hgrn_consts()
    maps = []
    for b in range(B):
        qf = cat_fm(r3, "qT", b).reshape(HH, 128, S)
        kf = cat_fm(r3, "kT", b).reshape(HH, 128, S)
        lf = cat_fm(r3, "lfT", b).reshape(HH, 128, S)
        vf = cat_tm(r3, "v", b)
        for g in range(4):
            v2 = np.stack([np.ascontiguousarray(vf[:, hd * 128:(hd + 1) * 128].reshape(64, 128, 128).transpose(1, 0, 2)).reshape(128, 64 * 128)
                           for hd in range(2 * g, 2 * g + 2)])
            maps.append({
                "q": np.ascontiguousarray(qf[2 * g:2 * g + 2]), "k": np.ascontiguousarray(kf[2 * g:2 * g + 2]),
                "lf": np.ascontiguousarray(lf[2 * g:2 * g + 2]), "v": v2, "m01": m01, "rm": rm, "ident": ident,
            })
    r4 = _run(nc4, maps)
    _DBG["r4"] = r4
    ofull = [np.concatenate([r4[b * 4 + g]["o"].reshape(256, S) for g in range(4)], axis=0) for b in range(B)]

    nc5 = build_F({"epi": ("hgrn", 1), "ffns": [(1, 2)], "final": True})
    shared = {
        "gT": gT, "wo": tile_wo(hgrn_w_out[0]), "hgn": fm_cols(hgrn_norm_g[0]),
        "wup0": tile_wup(ffn_w_up[1, 1]), "wdn0": tile_wdn(ffn_w_down[1, 1]),
        "fg": fm_cols(final_norm_g),
    }
    maps = []
    for i, (b, t) in enumerate(cores):
        m = dict(shared)
        m["xT"] = r3[i]["xo"]
        m["modT"] = modT[b]
        m["oT"] = np.ascontiguousarray(ofull[b][:, t * T:(t + 1) * T])
        m["sg"] = r3[i]["sgo"]
        maps.append(m)
    r5 = _run(nc5, maps)
    out = np.empty((B, S, D), np.float32)
    for i, (b, t) in enumerate(cores):
        out[b, t * T:(t + 1) * T, :] = r5[i]["xo"].T
    return out
```

```python
from contextlib import ExitStack
import numpy as np
import ml_dtypes
import concourse.bass as bass
import concourse.mybir as mybir
from concourse.bass_utils import run_bass_kernel_spmd

F32 = mybir.dt.float32
BF16 = mybir.dt.bfloat16
AF = mybir.ActivationFunctionType
ALU = mybir.AluOpType
NPBF = ml_dtypes.bfloat16

D = 1024
B = 2
S = 8192
DFF = 2816
NF = 22
EPS = 1e-6
NCORES = 8
T = 2048
FH = 16
FD = 64
HH = 8
CH = 64


class Buf:
    __slots__ = ("w", "r", "t", "key")

    def __init__(self, t=None):
        self.w = None
        self.r = []
        self.t = t
        self.key = None


class Prog:
    ENG = ["pe", "act", "dve", "pool", "sp"]

    def __init__(self, nc):
        self.nc = nc
        self.ops = {e: [] for e in self.ENG}
        self.clock = {e: {} for e in self.ENG}
        self.snaps = {}
        self.count = {}
        self.needed = set()
        self.nkey = 0
        self.final = None
        self.unit_keys = set()

    def sb(self, es, name, shape, dtype):
        self.nname = getattr(self, "nname", 0) + 1
        t = es.enter_context(self.nc.sbuf_tensor("%s_u%d" % (name, self.nname), list(shape), dtype))
        return Buf(t)

    def ps(self, es, name, shape, dtype):
        self.nname = getattr(self, "nname", 0) + 1
        t = es.enter_context(self.nc.psum_tensor("%s_u%d" % (name, self.nname), list(shape), dtype))
        return Buf(t)

    def newkey(self, buf):
        fk = getattr(self, "free_keys", None)
        if fk is None:
            self.free_keys, self.key_owner = [], {}
            fk = self.free_keys
        if fk:
            k = fk.pop()
        else:
            self.nkey += 1
            k = "d%d" % self.nkey
        buf.key = k
        self.key_owner[k] = buf
        return k

    def op(self, eng, fn, reads=(), writes=(), dma=None, pe_acc=False):
        need = {}

        def req(ev):
            if ev is None:
                return
            k, s = ev
            if s > need.get(k, 0):
                need[k] = s

        for b in reads:
            req(b.w)
        for b in writes:
            if not (pe_acc and b.w is not None and b.w[0] == "pe"):
                req(b.w)
            for r in b.r:
                req(r)
        key = dma or eng
        if fn is None:
            idx = 0
        else:
            idx = self.count.get(key, 0) + 1
            self.count[key] = idx
        ck = self.clock[eng]
        waits = []
        for k, s in need.items():
            if ck.get(k, 0) < s:
                waits.append((k, s))
        for k, s in waits:
            sn = self.snaps[(k, s)]
            for kk, ss in sn.items():
                if ck.get(kk, 0) < ss:
                    ck[kk] = ss
            if ck.get(k, 0) < s:
                ck[k] = s
            self.needed.add((k, s))
        self.ops[eng].append((fn, waits, key, idx))
        if fn is None:
            return None
        self.snaps[(key, idx)] = dict(ck)
        ev = (key, idx)
        for b in reads:
            b.r.append(ev)
        for b in writes:
            b.w = ev
            b.r = []
        return ev

    def dma(self, eng, out, in_, reads=(), writes=(), ow=None):
        wb = ow if ow is not None else writes[0]
        if wb.key is None:
            self.newkey(wb)
        def _fn(e, out=out, in_=in_):
            try:
                return e.dma_start(out=out, in_=in_)
            except Exception:
                print("DMA FAIL", out, in_)
                raise
        ev = self.op(eng, _fn, reads=reads, writes=writes, dma=wb.key)
        if ow is not None:
            ow.w = ev
        return ev

    def collective(self, kind, groups, src_ap, dst_ap, reads, wbuf):
        wbuf.key = "cc"
        self.unit_keys.add(wbuf.key)
        return self.op("pool", lambda e: e.collective_compute(kind, ALU.bypass, replica_groups=groups,
                                                              ins=[src_ap], outs=[dst_ap]),
                       reads=reads, writes=[wbuf], dma=wbuf.key)

    def finish(self, outs, eng="sp"):
        self.op(eng, None, reads=list(outs))

    def barrier(self):
        need = dict(self.count)
        for e in self.ENG:
            self.op(e, None, extra=need)
        for k, b in list(getattr(self, "key_owner", {}).items()):
            b.key = None
            self.free_keys.append(k)
        if hasattr(self, "key_owner"):
            self.key_owner.clear()

    def emit(self):
        nc = self.nc
        keys = list(self.count.keys())
        for e in self.ENG:
            if e not in keys:
                keys.append(e)
        rank = {}
        for k in keys:
            if k in self.ENG:
                idxs = sorted(s for (kk, s) in self.needed if kk == k)
                rank[k] = {s: i + 1 for i, s in enumerate(idxs)}
        with ExitStack() as es:
            sems = {k: es.enter_context(nc.semaphore("s_" + k)) for k in keys}
            block = es.enter_context(nc.Block())

            def run(eng_name):
                def body(e):
                    for fn, waits, key, idx in self.ops[eng_name]:
                        for k, s in waits:
                            v = rank[k][s] if k in rank else (s if k in self.unit_keys else 16 * s)
                            e.wait_ge(sems[k], v)
                        if fn is None:
                            continue
                        ins = fn(e)
                        if key in rank:
                            if (key, idx) in self.needed:
                                ins.then_inc(sems[key], 1)
                        elif key in self.unit_keys:
                            ins.then_inc(sems[key], 1)
                        else:
                            ins.then_inc(sems[key], 16)
                return body

            block.tensor(run("pe"))
            block.scalar(run("act"))
            block.vector(run("dve"))
            block.gpsimd(run("pool"))
            block.sync(run("sp"))


def _patch_op():
    base = Prog.op

    def op(self, eng, fn, reads=(), writes=(), dma=None, pe_acc=False, extra=None):
        if extra:
            dummy = []
            for k, s in extra.items():
                if s > 0:
                    b = Buf()
                    b.w = (k, s)
                    dummy.append(b)
            reads = list(reads) + dummy
            ev = base(self, eng, fn, reads=reads, writes=writes, dma=dma, pe_acc=pe_acc)
            return ev
        return base(self, eng, fn, reads=reads, writes=writes, dma=dma, pe_acc=pe_acc)

    Prog.op = op


_patch_op()


SQD = float(np.sqrt(D))


class Ctx:
    pass


def mcol(l, v, ch):
    return (l * 9 + v) * 8 + ch


def build_F(cfg):
    nc = bass.Bass("TRN2", target_bir_lowering=False)
    P = Prog(nc)
    c = Ctx()
    c.P, c.nc = P, nc
    dr = {}

    def din(name, shape, dt=F32):
        dr[name] = nc.dram_tensor(name, list(shape), dt, kind="ExternalInput").ap()
        return dr[name]

    def dout(name, shape, dt=F32):
        dr[name] = nc.dram_tensor(name, list(shape), dt, kind="ExternalOutput").ap()
        return dr[name]

    xT = din("xT", [D, T])
    modT = din("modT", [128, 144])
    gT = din("gT", [128, 48])
    outs = []
    with ExitStack() as es:
        X = es.enter_context(nc.sbuf_tensor("X", [128, 8, T], F32))
        c.X = X
        c.Xb = [[Buf(X) for _ in range(4)] for _ in range(8)]
        c.modt = P.sb(es, "modt", [128, 144], F32)
        c.gt = P.sb(es, "gt", [128, 48], F32)
        c.der = P.sb(es, "der", [128, 96], F32)
        c.ones = P.sb(es, "ones", [128, 128], BF16)
        c.bank = [P.ps(es, "bank%d" % i, [128, 512], F32) for i in range(8)]
        sqt = es.enter_context(nc.sbuf_tensor("sq", [128, 8, 512], BF16))
        c.sq = [Buf(sqt) for _ in range(8)]
        c.rstd = P.sb(es, "rstd", [128, 512], F32)
        c.tmp = [P.sb(es, "tmp%d" % i, [128, 512], F32) for i in range(2)]

        xv = xT.rearrange("(c p) t -> p c t", p=128)
        for kc in range(8):
            P.dma("sp", X[:, kc, :], xv[:, kc, :], writes=[c.Xb[kc][tt] for tt in range(4)])
        P.dma("sp", c.modt.t[:, :], modT, writes=[c.modt])
        P.dma("sp", c.gt.t[:, :], gT, writes=[c.gt])
        P.op("pool", lambda e: e.memset(c.ones.t[:, :], 1.0), writes=[c.ones])
        for l in range(2):
            for sub in range(3):
                base = ((l * 3 + sub) * 2) * 8
                sc0 = mcol(l, sub * 3 + 1, 0)
                g0 = (l * 3 + sub) * 8
                ga0 = mcol(l, sub * 3 + 2, 0)
                P.op("dve", lambda e, base=base, sc0=sc0, g0=g0: e.scalar_tensor_tensor(
                    c.der.t[:, base:base + 8], c.modt.t[:, sc0:sc0 + 8], 1.0, c.gt.t[:, g0:g0 + 8], ALU.add, ALU.mult),
                    reads=[c.modt, c.gt], writes=[c.der])
                P.op("dve", lambda e, base=base: e.tensor_scalar(
                    c.der.t[:, base:base + 8], c.der.t[:, base:base + 8], SQD, None, ALU.mult),
                    reads=[c.der], writes=[c.der])
                P.op("dve", lambda e, base=base, ga0=ga0, sub=sub: e.tensor_scalar(
                    c.der.t[:, base + 8:base + 16], c.modt.t[:, ga0:ga0 + 8], (1.0 if sub == 1 else 0.5), None, ALU.mult),
                    reads=[c.modt], writes=[c.der])

        def Acol(l, sub, ch):
            j = ((l * 3 + sub) * 2) * 8 + ch
            return c.der.t[:, j:j + 1]

        def Gcol(l, sub, ch):
            j = ((l * 3 + sub) * 2 + 1) * 8 + ch
            return c.der.t[:, j:j + 1]

        def Scol(l, sub, ch):
            j = mcol(l, sub * 3 + 0, ch)
            return c.modt.t[:, j:j + 1]

        def rstd_tile(src_fn, src_bufs, epsk):
            for kc in range(8):
                P.op("act", lambda e, kc=kc: e.activation(sqt[:, kc, :], src_fn(kc), AF.Square),
                     reads=[src_bufs[kc]], writes=[c.sq[kc]])
            for kc in range(8):
                P.op("pe", lambda e, kc=kc: e.matmul(c.bank[6].t[:, :], c.ones.t[:, :], sqt[:, kc, :],
                                                      start=(kc == 0), stop=(kc == 7)),
                     reads=[c.ones, c.sq[kc]], writes=[c.bank[6]], pe_acc=(kc > 0))
            P.op("dve", lambda e: e.tensor_scalar(c.rstd.t[:, :], c.bank[6].t[:, :], epsk, None, ALU.add),
                 reads=[c.bank[6]], writes=[c.rstd])
            P.op("act", lambda e: e.activation(c.rstd.t[:, :], c.rstd.t[:, :], AF.Sqrt), reads=[c.rstd], writes=[c.rstd])
            P.op("dve", lambda e: e.reciprocal(c.rstd.t[:, :], c.rstd.t[:, :]), reads=[c.rstd], writes=[c.rstd])

        def modnorm_tile(l, sub, tt, hdst, hbuf):
            t0 = tt * 512
            rstd_tile(lambda kc: X[:, kc, t0:t0 + 512], [c.Xb[kc][tt] for kc in range(8)], EPS * D)
            for kc in range(8):
                tb = c.tmp[kc % 2]
                P.op("dve", lambda e, kc=kc, tb=tb: e.tensor_tensor(tb.t[:, :], X[:, kc, t0:t0 + 512], c.rstd.t[:, :], ALU.mult),
                     reads=[c.Xb[kc][tt], c.rstd], writes=[tb])
                P.op("act", lambda e, kc=kc, tb=tb: e.activation(hdst(kc), tb.t[:, :], AF.Identity,
                                                               bias=Scol(l, sub, kc), scale=Acol(l, sub, kc)),
                     reads=[tb, c.der, c.modt], writes=[hbuf(kc)])

        def epilogue(kind, l):
            oT = din("oT", [D, T])
            sgd = din("sg", [D, T], BF16)
            wod = din("wo", [128, 8192])
            ov = oT.rearrange("(c p) t -> p c t", p=128)
            sv = sgd.rearrange("(c p) t -> p c t", p=128)
            with ExitStack() as e2:
                wo = P.sb(e2, "wo_sb", [128, 8, 1024], BF16)
                P.dma("pool", wo.t[:, :, :], wod.rearrange("p (k d) -> p k d", k=8), writes=[wo])
                ot = [P.sb(e2, "ot%d" % i, [128, 8, 512], F32) for i in range(2)]
                st = [P.sb(e2, "st%d" % i, [128, 8, 512], BF16) for i in range(2)]
                ogt = [e2.enter_context(nc.sbuf_tensor("og%d" % i, [128, 8, 512], BF16)) for i in range(2)]
                ogb = [[Buf(ogt[i]) for _ in range(8)] for i in range(2)]
                if kind == "hgrn":
                    hgd = din("hgn", [128, 8])
                    hg = P.sb(e2, "hg", [128, 8], F32)
                    P.dma("sp", hg.t[:, :], hgd, writes=[hg])
                    P.op("dve", lambda e: e.tensor_scalar(hg.t[:, :], hg.t[:, :], float(np.sqrt(128.0)), None, ALU.mult),
                         reads=[hg], writes=[hg])
                    sq1 = P.sb(e2, "sq1", [128, 512], BF16)
                    r1 = P.sb(e2, "r1", [128, 512], F32)
                    t1 = P.sb(e2, "t1", [128, 512], F32)
                for tt in range(4):
                    t0 = tt * 512
                    o_, s_, og_ = ot[tt % 2], st[tt % 2], ogt[tt % 2]
                    P.dma("sp", o_.t[:, :, :], ov[:, :, t0:t0 + 512], writes=[o_])
                    P.dma("sp", s_.t[:, :, :], sv[:, :, t0:t0 + 512], writes=[s_])
                    if kind == "fox":
                        for kc in range(8):
                            P.op("dve", lambda e, kc=kc, o_=o_, s_=s_, og_=og_: e.tensor_tensor(
                                og_[:, kc, :], o_.t[:, kc, :], s_.t[:, kc, :], ALU.mult),
                                reads=[o_, s_], writes=[ogb[tt % 2][kc]])
                    else:
                        for kc in range(8):
                            P.op("act", lambda e, kc=kc, o_=o_: e.activation(sq1.t[:, :], o_.t[:, kc, :], AF.Square),
                                 reads=[o_], writes=[sq1])
                            P.op("pe", lambda e: e.matmul(c.bank[7].t[:, :], c.ones.t[:, :], sq1.t[:, :], start=True, stop=True),
                                 reads=[c.ones, sq1], writes=[c.bank[7]])
                            P.op("dve", lambda e: e.tensor_scalar(r1.t[:, :], c.bank[7].t[:, :], EPS * 128.0, None, ALU.add),
                                 reads=[c.bank[7]], writes=[r1])
                            P.op("act", lambda e: e.activation(r1.t[:, :], r1.t[:, :], AF.Sqrt), reads=[r1], writes=[r1])
                            P.op("dve", lambda e: e.reciprocal(r1.t[:, :], r1.t[:, :]), reads=[r1], writes=[r1])
                            P.op("dve", lambda e, kc=kc, o_=o_: e.tensor_tensor(t1.t[:, :], o_.t[:, kc, :], r1.t[:, :], ALU.mult),
                                 reads=[o_, r1], writes=[t1])
                            P.op("dve", lambda e, kc=kc, s_=s_, og_=og_: e.scalar_tensor_tensor(
                                og_[:, kc, :], t1.t[:, :], hg.t[:, kc:kc + 1], s_.t[:, kc, :], ALU.mult, ALU.mult),
                                reads=[t1, hg, s_], writes=[ogb[tt % 2][kc]])
                    for dc in range(8):
                        bk = c.bank[4 + dc % 2]
                        for kc in range(8):
                            P.op("pe", lambda e, kc=kc, dc=dc, bk=bk, og_=og_: e.matmul(
                                bk.t[:, :], wo.t[:, kc, dc * 128:(dc + 1) * 128], og_[:, kc, :],
                                start=(kc == 0), stop=(kc == 7)),
                                reads=[wo, ogb[tt % 2][kc]], writes=[bk], pe_acc=(kc > 0))
                        P.op("dve", lambda e, dc=dc, bk=bk, t0=t0: e.scalar_tensor_tensor(
                            X[:, dc, t0:t0 + 512], bk.t[:, :], Gcol(l, 1, dc), X[:, dc, t0:t0 + 512], ALU.mult, ALU.add),
                            reads=[bk, c.der, c.Xb[dc][tt]], writes=[c.Xb[dc][tt]])
            P.barrier()

        def ffn(j, l, sub):
            wupd = din("wup%d" % j, [11, 128, 4096])
            wdnd = din("wdn%d" % j, [8, 128, 2816])
            with ExitStack() as e2:
                hbt = e2.enter_context(nc.sbuf_tensor("hb_%d" % j, [128, 8, 1024], BF16))
                hbb = [[Buf(hbt) for _ in range(2)] for _ in range(8)]
                actt = e2.enter_context(nc.sbuf_tensor("actb_%d" % j, [128, NF, 1024], BF16))
                actb = [[Buf(actt) for _ in range(2)] for _ in range(NF)]
                wu = [P.sb(e2, "wu%d_%d" % (j, i), [128, 2, 8, 256], BF16) for i in range(2)]
                wd = [P.sb(e2, "wd%d_%d" % (j, i), [128, NF, 128], BF16) for i in range(2)]
                sa = [P.sb(e2, "sa%d_%d" % (j, i), [128, 512], F32) for i in range(2)]
                for half in range(2):
                    for t2 in range(2):
                        tt = half * 2 + t2
                        modnorm_tile(l, sub, tt, lambda kc, t2=t2: hbt[:, kc, t2 * 512:(t2 + 1) * 512],
                                     lambda kc, t2=t2: hbb[kc][t2])
                    it = 0
                    for g in range(11):
                        w_ = wu[g % 2]
                        P.dma("pool", w_.t[:, :, :, :], wupd[g].rearrange("p (a k f) -> p a k f", a=2, k=8), writes=[w_])
                        for jf in range(2):
                            fc = 2 * g + jf
                            for t2 in range(2):
                                bA, bB = c.bank[it % 2], c.bank[2 + it % 2]
                                s_ = sa[it % 2]
                                it += 1
                                for kc in range(8):
                                    P.op("pe", lambda e, kc=kc, w_=w_, jf=jf, t2=t2, bA=bA: e.matmul(
                                        bA.t[:, :], w_.t[:, 0, kc, jf * 128:(jf + 1) * 128], hbt[:, kc, t2 * 512:(t2 + 1) * 512],
                                        start=(kc == 0), stop=(kc == 7)),
                                        reads=[w_, hbb[kc][t2]], writes=[bA], pe_acc=(kc > 0))
                                for kc in range(8):
                                    P.op("pe", lambda e, kc=kc, w_=w_, jf=jf, t2=t2, bB=bB: e.matmul(
                                        bB.t[:, :], w_.t[:, 1, kc, jf * 128:(jf + 1) * 128], hbt[:, kc, t2 * 512:(t2 + 1) * 512],
                                        start=(kc == 0), stop=(kc == 7)),
                                        reads=[w_, hbb[kc][t2]], writes=[bB], pe_acc=(kc > 0))
                                P.op("act", lambda e, s_=s_, bA=bA: e.activation(s_.t[:, :], bA.t[:, :], AF.Silu),
                                     reads=[bA], writes=[s_])
                                P.op("dve", lambda e, s_=s_, bB=bB, fc=fc, t2=t2: e.tensor_tensor(
                                    actt[:, fc, t2 * 512:(t2 + 1) * 512], bB.t[:, :], s_.t[:, :], ALU.mult),
                                    reads=[bB, s_], writes=[actb[fc][t2]])
                    for dc in range(8):
                        w_ = wd[dc % 2]
                        P.dma("pool", w_.t[:, :, :], wdnd[dc].rearrange("p (f d) -> p f d", f=NF), writes=[w_])
                        for t2 in range(2):
                            tt = half * 2 + t2
                            t0 = tt * 512
                            bk = c.bank[4 + (dc * 2 + t2) % 2]
                            for fc in range(NF):
                                P.op("pe", lambda e, fc=fc, w_=w_, t2=t2, bk=bk: e.matmul(
                                    bk.t[:, :], w_.t[:, fc, :], actt[:, fc, t2 * 512:(t2 + 1) * 512],
                                    start=(fc == 0), stop=(fc == NF - 1)),
                                    reads=[w_, actb[fc][t2]], writes=[bk], pe_acc=(fc > 0))
                            P.op("dve", lambda e, dc=dc, bk=bk, t0=t0: e.scalar_tensor_tensor(
                                X[:, dc, t0:t0 + 512], bk.t[:, :], Gcol(l, sub, dc), X[:, dc, t0:t0 + 512], ALU.mult, ALU.add),
                                reads=[bk, c.der, c.Xb[dc][tt]], writes=[c.Xb[dc][tt]])
            P.barrier()

        def proj_fm(wname, hbt, hbb, evac, n_oc=8):
            wd_ = din(wname, [n_oc, 128, 1024])
            with ExitStack() as e3:
                wp = [P.sb(e3, wname + "_sb%d" % i, [128, 8, 128], BF16) for i in range(2)]
                it = 0
                for oc in range(n_oc):
                    w_ = wp[oc % 2]
                    P.dma("pool", w_.t[:, :, :], wd_[oc].rearrange("p (k f) -> p k f", k=8), writes=[w_])
                    for tt in range(4):
                        bk = c.bank[it % 4]
                        it += 1
                        for kc in range(8):
                            P.op("pe", lambda e, kc=kc, w_=w_, tt=tt, bk=bk: e.matmul(
                                bk.t[:, :], w_.t[:, kc, :], hbt[:, kc, tt * 512:(tt + 1) * 512],
                                start=(kc == 0), stop=(kc == 7)),
                                reads=[w_, hbb[kc][tt]], writes=[bk], pe_acc=(kc > 0))
                        evac(oc, tt, bk)
                P.barrier()

        def proj_tm(wname, hbt, hbb, vout, vob, func):
            wd_ = din(wname, [2, 128, 4096])
            with ExitStack() as e3:
                wv = P.sb(e3, wname + "_sb", [128, 2, 8, 512], BF16)
                for cg in range(2):
                    P.dma("pool", wv.t[:, cg, :, :], wd_[cg].rearrange("p (k f) -> p k f", k=8), writes=[wv])
                vt = [P.sb(e3, "vt%d" % i, [128, 512], BF16) for i in range(2)]
                it = 0
                for tk in range(16):
                    for cg in range(2):
                        bk = c.bank[it % 4]
                        v_ = vt[it % 2]
                        it += 1
                        for kc in range(8):
                            P.op("pe", lambda e, kc=kc, tk=tk, cg=cg, bk=bk: e.matmul(
                                bk.t[:, :], hbt[:, kc, tk * 128:(tk + 1) * 128], wv.t[:, cg, kc, :],
                                start=(kc == 0), stop=(kc == 7)),
                                reads=[wv, hbb[kc][tk // 4]], writes=[bk], pe_acc=(kc > 0))
                        P.op("act", lambda e, bk=bk, v_=v_: e.activation(v_.t[:, :], bk.t[:, :], func),
                             reads=[bk], writes=[v_])
                        P.dma("sp", vout[tk * 128:(tk + 1) * 128, cg * 512:(cg + 1) * 512], v_.t[:, :], reads=[v_], ow=vob)
                P.barrier()

        def stage_out(e3, name, shape, dt):
            return [P.sb(e3, name + "%d" % i, shape, dt) for i in range(2)]

        def projections(kind, l):
            with ExitStack() as e2:
                hbt = e2.enter_context(nc.sbuf_tensor("hb2", [128, 8, T], BF16))
                hbb = [[Buf(hbt) for _ in range(4)] for _ in range(8)]
                for tt in range(4):
                    modnorm_tile(l, 1, tt, lambda kc, tt=tt: hbt[:, kc, tt * 512:(tt + 1) * 512],
                                 lambda kc, tt=tt: hbb[kc][tt])
                qo = dout("qT", [D, T], BF16)
                qob = Buf()
                outs.append(qob)
                sgo = dout("sgo", [D, T], BF16)
                sgob = Buf()
                outs.append(sgob)
                vo = dout("v", [T, D], BF16)
                vob = Buf()
                outs.append(vob)
                cnt = [0]

                def simple_evac(od, ob, func, scale, st, dt_eng="act"):
                    def evac(oc, tt, bk):
                        s_ = st[cnt[0] % 2]
                        cnt[0] += 1
                        P.op("act", lambda e, s_=s_, bk=bk: e.activation(s_.t[:, :], bk.t[:, :], func, scale=scale),
                             reads=[bk], writes=[s_])
                        P.dma("sp", od[oc * 128:(oc + 1) * 128, tt * 512:(tt + 1) * 512], s_.t[:, :], reads=[s_], ow=ob)
                    return evac

                stb = stage_out(e2, "stb", [128, 512], BF16)
                if kind == "fox":
                    ko = dout("kT", [D, T], BF16)
                    kob = Buf()
                    outs.append(kob)
                    lfo = dout("lf", [16, T], F32)
                    lfob = Buf()
                    outs.append(lfob)
                    proj_fm("wq", hbt, hbb, simple_evac(qo, qob, AF.Copy, float(FD ** -0.5), stb))
                    proj_fm("wk", hbt, hbb, simple_evac(ko, kob, AF.Copy, 1.0, stb))
                    proj_fm("wg", hbt, hbb, simple_evac(sgo, sgob, AF.Sigmoid, 1.0, stb))
                    proj_tm("wv", hbt, hbb, vo, vob, AF.Copy)
                    wfd = din("wf", [128, 128])
                    bfd = din("bf", [16, 1])
                    wf = P.sb(e2, "wf_sb", [128, 8, 16], BF16)
                    P.dma("pool", wf.t[:, :, :], wfd.rearrange("p (k f) -> p k f", k=8), writes=[wf])
                    nbf = P.sb(e2, "nbf", [16, 1], F32)
                    P.dma("sp", nbf.t[:, :], bfd, writes=[nbf])
                    P.op("dve", lambda e: e.tensor_scalar(nbf.t[:, :], nbf.t[:, :], -1.0, None, ALU.mult), reads=[nbf], writes=[nbf])
                    e1 = P.sb(e2, "e1", [16, 512], F32)
                    l1 = [P.sb(e2, "l1_%d" % i, [16, 512], F32) for i in range(2)]
                    for tt in range(4):
                        bk = c.bank[tt % 4]
                        for kc in range(8):
                            P.op("pe", lambda e, kc=kc, tt=tt, bk=bk: e.matmul(
                                bk.t[0:16, :], wf.t[:, kc, :], hbt[:, kc, tt * 512:(tt + 1) * 512],
                                start=(kc == 0), stop=(kc == 7)),
                                reads=[wf, hbb[kc][tt]], writes=[bk], pe_acc=(kc > 0))
                        l_ = l1[tt % 2]
                        P.op("act", lambda e, bk=bk: e.activation(e1.t[:, :], bk.t[0:16, :], AF.Exp, bias=nbf.t[:, 0:1], scale=-1.0),
                             reads=[bk, nbf], writes=[e1])
                        P.op("act", lambda e, l_=l_: e.activation(l_.t[:, :], e1.t[:, :], AF.Ln, bias=1.0, scale=1.0),
                             reads=[e1], writes=[l_])
                        P.op("dve", lambda e, l_=l_: e.tensor_scalar(l_.t[:, :], l_.t[:, :], -1.0, None, ALU.mult),
                             reads=[l_], writes=[l_])
                        P.dma("sp", lfo[:, tt * 512:(tt + 1) * 512], l_.t[:, :], reads=[l_], ow=lfob)
                else:
                    ko = dout("kT", [D, T], F32)
                    kob = Buf()
                    outs.append(kob)
                    lfo = dout("lfT", [D, T], F32)
                    lfob = Buf()
                    outs.append(lfob)
                    lbd = din("lbl", [128, 16])
                    lbl = P.sb(e2, "lbl_sb", [128, 16], F32)
                    lb = P.sb(e2, "lb", [128, 8], F32)
                    oml = P.sb(e2, "oml", [128, 8], F32)
                    P.dma("sp", lbl.t[:, :], lbd, writes=[lbl])
                    P.op("dve", lambda e: e.tensor_tensor(lb.t[:, :], lbl.t[:, 8:16], lbl.t[:, 0:8], ALU.subtract), reads=[lbl], writes=[lb])
                    P.op("act", lambda e: e.activation(lb.t[:, :], lb.t[:, :], AF.Sigmoid), reads=[lb], writes=[lb])
                    P.op("dve", lambda e: e.tensor_scalar(oml.t[:, :], lb.t[:, :], -1.0, 1.0, ALU.mult, ALU.add), reads=[lb], writes=[oml])
                    proj_fm("wq", hbt, hbb, simple_evac(qo, qob, AF.Copy, 1.0, stb))
                    proj_fm("wg", hbt, hbb, simple_evac(sgo, sgob, AF.Silu, 1.0, stb))
                    proj_tm("wv", hbt, hbb, vo, vob, AF.Silu)
                    sg1 = P.sb(e2, "sg1", [128, 512], F32)
                    ff = stage_out(e2, "ff", [128, 512], F32)
                    lff = stage_out(e2, "lff", [128, 512], F32)
                    kk = stage_out(e2, "kk", [128, 512], F32)

                    def f_evac(oc, tt, bk):
                        i = cnt[0] % 2
                        cnt[0] += 1
                        f_, l_, k_ = ff[i], lff[i], kk[i]
                        P.op("act", lambda e, bk=bk: e.activation(sg1.t[:, :], bk.t[:, :], AF.Sigmoid), reads=[bk], writes=[sg1])
                        P.op("dve", lambda e, f_=f_, oc=oc: e.tensor_scalar(f_.t[:, :], sg1.t[:, :], oml.t[:, oc:oc + 1], lb.t[:, oc:oc + 1], ALU.mult, ALU.add),
                             reads=[sg1, oml, lb], writes=[f_])
                        P.op("act", lambda e, f_=f_, l_=l_: e.activation(l_.t[:, :], f_.t[:, :], AF.Ln), reads=[f_], writes=[l_])
                        P.op("dve", lambda e, f_=f_, k_=k_: e.tensor_scalar(k_.t[:, :], f_.t[:, :], -1.0, 1.0, ALU.mult, ALU.add),
                             reads=[f_], writes=[k_])
                        P.dma("sp", lfo[oc * 128:(oc + 1) * 128, tt * 512:(tt + 1) * 512], l_.t[:, :], reads=[l_], ow=lfob)
                        P.dma("sp", ko[oc * 128:(oc + 1) * 128, tt * 512:(tt + 1) * 512], k_.t[:, :], reads=[k_], ow=kob)
                    proj_fm("wf", hbt, hbb, f_evac)
            P.barrier()

        if cfg.get("epi"):
            epilogue(cfg["epi"][0], cfg["epi"][1])
        for j, (l, sub) in enumerate(cfg["ffns"]):
            ffn(j, l, sub)
        if cfg.get("proj"):
            projections(cfg["proj"][0], cfg["proj"][1])
        xo = dout("xo", [D, T])
        xob = Buf()
        outs.append(xob)
        xov = xo.rearrange("(c p) t -> p c t", p=128)
        if cfg.get("final"):
            fgd = din("fg", [128, 8])
            fg = P.sb(es, "fg_sb", [128, 8], F32)
            P.dma("sp", fg.t[:, :], fgd, writes=[fg])
            P.op("dve", lambda e: e.tensor_scalar(fg.t[:, :], fg.t[:, :], SQD, None, ALU.mult), reads=[fg], writes=[fg])
            yo = [P.sb(es, "yo%d" % i, [128, 512], F32) for i in range(2)]
            it = 0
            for tt in range(4):
                t0 = tt * 512
                rstd_tile(lambda kc, t0=t0: X[:, kc, t0:t0 + 512], [c.Xb[kc][tt] for kc in range(8)], EPS * D)
                for kc in range(8):
                    y_ = yo[it % 2]
                    it += 1
                    P.op("dve", lambda e, kc=kc, y_=y_, t0=t0: e.scalar_tensor_tensor(
                        y_.t[:, :], X[:, kc, t0:t0 + 512], fg.t[:, kc:kc + 1], c.rstd.t[:, :], ALU.mult, ALU.mult),
                        reads=[c.Xb[kc][tt], fg, c.rstd], writes=[y_])
                    P.dma("sp", xov[:, kc, t0:t0 + 512], y_.t[:, :], reads=[y_], ow=xob)
        else:
            for kc in range(8):
                P.dma("sp", xov[:, kc, :], X[:, kc, :], reads=[c.Xb[kc][tt] for tt in range(4)], ow=xob)
        P.finish(outs)
        P.emit()
    return nc


MC = 2304


def build_mod():
    nc = bass.Bass("TRN2", target_bir_lowering=False)
    P = Prog(nc)
    cT = nc.dram_tensor("cT", [128, 16], F32, kind="ExternalInput").ap()
    w = nc.dram_tensor("w", [128, 8 * MC], F32, kind="ExternalInput").ap()
    bias = nc.dram_tensor("bias", [2, MC], F32, kind="ExternalInput").ap()
    mo = nc.dram_tensor("mo", [2, MC], F32, kind="ExternalOutput").ap()
    with ExitStack() as es:
        ct = P.sb(es, "ct", [128, 8, 2], F32)
        wt = [P.sb(es, "wt%d" % i, [128, 8, 384], F32) for i in range(6)]
        bt = P.sb(es, "bt", [2, MC], F32)
        ot = P.sb(es, "ot", [2, MC], F32)
        banks = [P.ps(es, "bk%d" % i, [128, 512], F32) for i in range(2)]
        P.dma("sp", ct.t[:, :, :], cT.rearrange("p (k b) -> p k b", k=8), writes=[ct])
        P.dma("sp", bt.t[:, :], bias, writes=[bt])
        wv = w.rearrange("p (k n) -> p k n", k=8)
        for i in range(6):
            P.dma("sp" if i % 2 == 0 else "pool", wt[i].t[:, :, :], wv[:, :, i * 384:(i + 1) * 384], writes=[wt[i]])
        P.op("act", lambda e: e.activation(ct.t[:, :, :], ct.t[:, :, :], AF.Silu), reads=[ct], writes=[ct])
        for i in range(6):
            bk = banks[i % 2]
            for kc in range(8):
                P.op("pe", lambda e, kc=kc, i=i, bk=bk: e.matmul(bk.t[0:2, 0:384], ct.t[:, kc, :], wt[i].t[:, kc, :],
                                                                 start=(kc == 0), stop=(kc == 7)),
                     reads=[ct, wt[i]], writes=[bk], pe_acc=(kc > 0))
            P.op("dve", lambda e, i=i, bk=bk: e.tensor_tensor(ot.t[:, i * 384:(i + 1) * 384], bk.t[0:2, 0:384],
                                                             bt.t[:, i * 384:(i + 1) * 384], ALU.add),
                 reads=[bk, bt], writes=[ot])
        ob = Buf()
        P.dma("sp", mo, ot.t[:, :], reads=[ot], ow=ob)
        P.finish([ob])
        P.emit()
    return nc


def run_mod(c, ada_w, ada_b):
    nc = build_mod()
    cT = np.ascontiguousarray(c.T.reshape(8, 128, B).transpose(1, 0, 2)).reshape(128, 16)
    wall = np.concatenate([ada_w[0], ada_w[1]], axis=1)
    ball = np.concatenate([ada_b[0], ada_b[1]], axis=0)
    maps = []
    for j in range(NCORES):
        wj = wall[:, j * MC:(j + 1) * MC].reshape(8, 128, MC).transpose(1, 0, 2)
        maps.append({"cT": cT, "w": np.ascontiguousarray(wj).reshape(128, 8 * MC),
                     "bias": np.ascontiguousarray(np.broadcast_to(ball[j * MC:(j + 1) * MC], (2, MC)))})
    res = run_bass_kernel_spmd(nc, maps, core_ids=list(range(NCORES)))
    mod = np.concatenate([r["mo"] for r in res.results], axis=1)
    return mod.reshape(B, 2, 9, D)


def fm_cols(v):
    lead = int(np.prod(v.shape[:-1])) if v.ndim > 1 else 1
    a = v.reshape(lead, 8, 128).transpose(2, 0, 1)
    return np.ascontiguousarray(a).reshape(128, lead * 8)


def tile_w_fm(w):
    n = w.shape[1] // 128
    a = w.reshape(8, 128, n, 128).transpose(2, 1, 0, 3)
    return np.ascontiguousarray(a).reshape(n, 128, 1024)


def tile_w_tm(w):
    a = w.reshape(8, 128, 2, 512).transpose(2, 1, 0, 3)
    return np.ascontiguousarray(a).reshape(2, 128, 4096)


def tile_wup(w):
    a = w.reshape(8, 128, 2, 11, 256).transpose(3, 1, 2, 0, 4)
    return np.ascontiguousarray(a).reshape(11, 128, 4096)


def tile_wdn(w):
    a = w.reshape(NF, 128, 8, 128).transpose(2, 1, 0, 3)
    return np.ascontiguousarray(a).reshape(8, 128, NF * 128)


def tile_wo(w):
    a = w.reshape(8, 128, D).transpose(1, 0, 2)
    return np.ascontiguousarray(a).reshape(128, 8 * D)


NEG = -30000.0


def build_fox():
    nc = bass.Bass("TRN2", target_bir_lowering=False)
    P = Prog(nc)
    qd = nc.dram_tensor("q", [4, 64, S], BF16, kind="ExternalInput").ap()
    kd = nc.dram_tensor("k", [4, 64, S], BF16, kind="ExternalInput").ap()
    vd = nc.dram_tensor("v", [4, 128, 64 * 64], BF16, kind="ExternalInput").ap()
    ltd = nc.dram_tensor("lt", [128, 256], F32, kind="ExternalInput").ap()
    lqd = nc.dram_tensor("lq", [16, 2048], F32, kind="ExternalInput").ap()
    Ud = nc.dram_tensor("U", [128, 128], F32, kind="ExternalInput").ap()
    seld = nc.dram_tensor("sel", [128, 128], F32, kind="ExternalInput").ap()
    mkd = nc.dram_tensor("mk", [128, 128], F32, kind="ExternalInput").ap()
    od = nc.dram_tensor("o", [4, 64, S], F32, kind="ExternalOutput").ap()
    shi = nc.dram_tensor("shi", [4, S], BF16).ap()
    slo = nc.dram_tensor("slo", [4, S], BF16).ap()
    with ExitStack() as es:
        bank = [P.ps(es, "bank%d" % i, [128, 512], F32) for i in range(8)]
        U = P.sb(es, "U_sb", [128, 128], F32)
        sel = P.sb(es, "sel_sb", [128, 128], F32)
        mk = P.sb(es, "mk_sb", [128, 128], F32)
        onesf = P.sb(es, "onesf", [128, 128], F32)
        lt = P.sb(es, "lt_sb", [128, 256], F32)
        lq = P.sb(es, "lq_sb", [16, 2048], F32)
        within = P.sb(es, "within", [128, 256], F32)
        tot = P.sb(es, "tot", [128, 256], F32)
        inc = P.sb(es, "inc", [128, 256], F32)
        GT = P.sb(es, "GT", [128, 256], F32)
        gend = P.sb(es, "gend", [128, 256], F32)
        negB = P.sb(es, "negB", [128, 4 * 16 * 64], F32)
        cl = P.sb(es, "cl", [16, 2048], F32)
        Aa = P.sb(es, "Aa", [16, 2048], F32)
        ahi = P.sb(es, "ahi", [16, 2048], BF16)
        ahf = P.sb(es, "ahf", [16, 2048], F32)
        alo = P.sb(es, "alo", [16, 2048], BF16)
        qa = [P.sb(es, "qa%d" % i, [66, S], BF16) for i in range(2)]
        ka = [P.sb(es, "ka%d" % i, [66, S], BF16) for i in range(2)]
        va = [P.sb(es, "va%d" % i, [128, 64, 65], BF16) for i in range(2)]
        pt = [P.sb(es, "pt%d" % i, [128, 512], BF16) for i in range(3)]
        drow = P.sb(es, "drow", [65, 512], F32)
        rec = P.sb(es, "rec", [64, 512], F32)
        oo = [P.sb(es, "oo%d" % i, [64, 512], F32) for i in range(2)]
        ob = Buf()

        for t_, d_ in ((U, Ud), (sel, seld), (mk, mkd), (lt, ltd), (lq, lqd)):
            P.dma("sp", t_.t[:, :], d_, writes=[t_])
        P.op("pool", lambda e: e.memset(onesf.t[:, :], 1.0), writes=[onesf])
        P.op("pe", lambda e: e.matmul(bank[6].t[:, 0:256], U.t[:, :], lt.t[:, :], start=True, stop=True), reads=[U, lt], writes=[bank[6]])
        P.op("pe", lambda e: e.matmul(bank[7].t[:, 0:256], onesf.t[:, :], lt.t[:, :], start=True, stop=True), reads=[onesf, lt], writes=[bank[7]])
        P.op("dve", lambda e: e.tensor_copy(within.t[:, :], bank[6].t[:, 0:256]), reads=[bank[6]], writes=[within])
        P.op("dve", lambda e: e.tensor_copy(tot.t[:, :], bank[7].t[:, 0:256]), reads=[bank[7]], writes=[tot])
        for h in range(4):
            P.op("dve", lambda e, h=h: e.tensor_tensor_scan(inc.t[:, h * 64:(h + 1) * 64], onesf.t[:, 0:64], tot.t[:, h * 64:(h + 1) * 64],
                                                            0.0, ALU.mult, ALU.add), reads=[onesf, tot], writes=[inc])
        P.op("dve", lambda e: e.tensor_tensor(GT.t[:, :], within.t[:, :], inc.t[:, :], ALU.add), reads=[within, inc], writes=[GT])
        P.op("dve", lambda e: e.tensor_tensor(GT.t[:, :], GT.t[:, :], tot.t[:, :], ALU.subtract), reads=[GT, tot], writes=[GT])
        P.op("pe", lambda e: e.matmul(bank[6].t[:, 0:256], sel.t[:, :], GT.t[:, :], start=True, stop=True), reads=[sel, GT], writes=[bank[6]])
        P.op("dve", lambda e: e.tensor_copy(gend.t[:, :], bank[6].t[:, 0:256]), reads=[bank[6]], writes=[gend])
        for h in range(4):
            for Q in range(16):
                j0 = (h * 16 + Q) * 64
                gc = h * 64 + 4 * Q + 3
                P.op("dve", lambda e, h=h, j0=j0, gc=gc: e.tensor_scalar(
                    negB.t[:, j0:j0 + 64], GT.t[:, h * 64:(h + 1) * 64], -1.0, gend.t[:, gc:gc + 1], ALU.mult, ALU.add),
                    reads=[GT, gend], writes=[negB])
        ones16 = P.sb(es, "ones16", [16, 512], F32)
        P.op("pool", lambda e: e.memset(ones16.t[:, :], 1.0), writes=[ones16])
        for h in range(4):
            P.op("dve", lambda e, h=h: e.tensor_tensor_scan(cl.t[:, h * 512:(h + 1) * 512], ones16.t[:, :], lq.t[:, h * 512:(h + 1) * 512],
                                                            0.0, ALU.mult, ALU.add), reads=[lq, ones16], writes=[cl])
        for h in range(4):
            P.op("dve", lambda e, h=h: e.tensor_scalar(Aa.t[:, h * 512:(h + 1) * 512], cl.t[:, h * 512:(h + 1) * 512],
                                                       cl.t[:, h * 512 + 511:h * 512 + 512], None, ALU.subtract),
                 reads=[cl], writes=[Aa])
        P.op("dve", lambda e: e.tensor_copy(ahi.t[:, :], Aa.t[:, :]), reads=[Aa], writes=[ahi])
        P.op("dve", lambda e: e.tensor_copy(ahf.t[:, :], ahi.t[:, :]), reads=[ahi], writes=[ahf])
        P.op("dve", lambda e: e.tensor_tensor(alo.t[:, :], Aa.t[:, :], ahf.t[:, :], ALU.subtract), reads=[Aa, ahf], writes=[alo])
        shb, slb = Buf(), Buf()
        P.dma("sp", shi.rearrange("h (q m) -> q h m", q=16), ahi.t[:, :].rearrange("q (h m) -> q h m", h=4), reads=[ahi], writes=[shb])
        P.dma("sp", slo.rearrange("h (q m) -> q h m", q=16), alo.t[:, :].rearrange("q (h m) -> q h m", h=4), reads=[alo], writes=[slb])

        for i in range(2):
            P.op("pool", lambda e, i=i: e.memset(ka[i].t[64:66, :], 1.0), writes=[ka[i]])
            P.op("pool", lambda e, i=i: e.memset(va[i].t[:, :, 64:65], 1.0), writes=[va[i]])

        def load_head(h):
            q_, k_, v_ = qa[h % 2], ka[h % 2], va[h % 2]
            P.dma("sp", q_.t[0:64, :], qd[h], writes=[q_])
            P.dma("sp", q_.t[64:65, :], shi[h:h + 1, :], reads=[shb], writes=[q_])
            P.dma("sp", q_.t[65:66, :], slo[h:h + 1, :], reads=[slb], writes=[q_])
            P.dma("pool", k_.t[0:64, :], kd[h], writes=[k_])
            P.dma("pool", v_.t[:, :, 0:64], vd[h].rearrange("p (t d) -> p t d", d=64), writes=[v_])

        load_head(0)

        def do_head(h, q_, k_, v_, nit):
            items = [(Q, kt) for Q in range(16) for kt in range(4 * Q + 4)]

            def emit_S(idx, it_no):
                Q, kt = items[idx]
                d = kt - 4 * Q
                c0 = 128 * d if d >= 0 else 0
                bk = bank[it_no % 3]
                p_ = pt[it_no % 3]
                P.op("pe", lambda e: e.matmul(bk.t[:, c0:512], k_.t[0:66, kt * 128:(kt + 1) * 128],
                                              q_.t[0:66, Q * 512 + c0:(Q + 1) * 512], start=True, stop=True),
                     reads=[k_, q_], writes=[bk])
                if d >= 0:
                    P.op("dve", lambda e: e.tensor_tensor(bk.t[:, c0:c0 + 128], bk.t[:, c0:c0 + 128], mk.t[:, :], ALU.add),
                         reads=[bk, mk], writes=[bk])
                jb = (h * 16 + Q) * 64 + kt
                P.op("act", lambda e: e.activation(p_.t[:, c0:512], bk.t[:, c0:512], AF.Exp, bias=negB.t[:, jb:jb + 1], scale=1.0),
                     reads=[bk, negB], writes=[p_])

            def emit_PV(idx, it_no):
                Q, kt = items[idx]
                d = kt - 4 * Q
                c0 = 128 * d if d >= 0 else 0
                p_ = pt[it_no % 3]
                ob_ = bank[3 + Q % 2]
                last = (kt == 4 * Q + 3)
                P.op("pe", lambda e: e.matmul(ob_.t[0:65, c0:512], v_.t[:, kt, :], p_.t[:, c0:512], start=(kt == 0), stop=last),
                     reads=[v_, p_], writes=[ob_], pe_acc=(kt > 0))
                if last:
                    o_ = oo[Q % 2]
                    P.op("act", lambda e: e.activation(drow.t[64:65, :], ob_.t[64:65, :], AF.Copy), reads=[ob_], writes=[drow])
                    P.op("pe", lambda e: e.matmul(bank[5].t[0:64, :], onesf.t[64:65, 0:64], drow.t[64:65, :], start=True, stop=True),
                         reads=[onesf, drow], writes=[bank[5]])
                    P.op("dve", lambda e: e.reciprocal(rec.t[:, :], bank[5].t[0:64, :]), reads=[bank[5]], writes=[rec])
                    P.op("dve", lambda e: e.tensor_tensor(o_.t[:, :], ob_.t[0:64, :], rec.t[:, :], ALU.mult), reads=[ob_, rec], writes=[o_])
                    P.dma("sp", od[h][:, Q * 512:(Q + 1) * 512], o_.t[:, :], reads=[o_], ow=ob)

            n = len(items)
            emit_S(0, nit)
            for idx in range(n):
                if idx + 1 < n:
                    emit_S(idx + 1, nit + idx + 1)
                emit_PV(idx, nit + idx)
            return nit + n

        nit = 0
        for h in range(4):
            if h + 1 < 4:
                load_head(h + 1)
            nit = do_head(h, qa[h % 2], ka[h % 2], va[h % 2], nit)
        P.finish([ob])
        P.emit()
    return nc


def fox_consts():
    k = np.arange(128)
    U = (k[:, None] <= k[None, :]).astype(np.float32)
    sel = np.zeros((128, 128), np.float32)
    sel[127, :] = 1.0
    mk = np.where(k[None, :] >= k[:, None], 0.0, NEG).astype(np.float32)
    return U, sel, mk


def build_hgrn():
    nc = bass.Bass("TRN2", target_bir_lowering=False)
    P = Prog(nc)
    qd = nc.dram_tensor("q", [2, 128, S], BF16, kind="ExternalInput").ap()
    kd = nc.dram_tensor("k", [2, 128, S], F32, kind="ExternalInput").ap()
    lfd = nc.dram_tensor("lf", [2, 128, S], F32, kind="ExternalInput").ap()
    vd = nc.dram_tensor("v", [2, 128, 64 * 128], BF16, kind="ExternalInput").ap()
    m01d = nc.dram_tensor("m01", [128, 64], F32, kind="ExternalInput").ap()
    rmd = nc.dram_tensor("rm", [128, 2048], F32, kind="ExternalInput").ap()
    idd = nc.dram_tensor("ident", [128, 128], BF16, kind="ExternalInput").ap()
    od = nc.dram_tensor("o", [2, 128, S], F32, kind="ExternalOutput").ap()
    NB = 2048
    with ExitStack() as es:
        bankA = [P.ps(es, "bankA%d" % i, [128, 512], F32) for i in range(2)]
        bankO = [P.ps(es, "bankO%d" % i, [128, 512], F32) for i in range(2)]
        bankU = [P.ps(es, "bankU%d" % i, [128, 512], F32) for i in range(2)]
        bankT = P.ps(es, "bankT", [128, 1024], BF16)
        m01 = P.sb(es, "m01_sb", [128, 64], F32)
        rm = P.sb(es, "rm_sb", [128, NB], F32)
        ident = P.sb(es, "ident_sb", [128, 128], BF16)
        P.dma("sp", m01.t[:, :], m01d, writes=[m01])
        P.dma("sp", rm.t[:, :], rmd, writes=[rm])
        P.dma("sp", ident.t[:, :], idd, writes=[ident])
        ob = Buf()
        hs = []
        for h in range(2):
            o = Ctx()
            o.qb = P.sb(es, "qb%d" % h, [128, NB], BF16)
            o.kb = P.sb(es, "kb%d" % h, [128, NB], F32)
            o.lf = P.sb(es, "lf%d" % h, [128, NB], F32)
            o.G = P.sb(es, "G%d" % h, [128, NB], F32)
            o.tmp = P.sb(es, "tmp%d" % h, [128, NB], F32)
            o.tmp2 = P.sb(es, "tmp2%d" % h, [128, NB], F32)
            o.qd = P.sb(es, "qd%d" % h, [128, NB], BF16)
            o.kdd = P.sb(es, "kdd%d" % h, [128, NB], BF16)
            o.kend = P.sb(es, "kend%d" % h, [128, NB], BF16)
            o.kT = P.sb(es, "kT%d" % h, [128, 16, 128], BF16)
            o.vb = P.sb(es, "vb%d" % h, [128, 16, 128], BF16)
            o.egl = P.sb(es, "egl%d" % h, [128, 32], F32)
            o.S32 = P.sb(es, "S32_%d" % h, [128, 128], F32)
            o.Sbf = [P.sb(es, "Sbf%d_%d" % (h, i), [128, 128], BF16) for i in range(2)]
            o.am = [P.sb(es, "am%d_%d" % (h, i), [128, 64], BF16) for i in range(2)]
            o.osb = [P.sb(es, "osb%d_%d" % (h, i), [128, 512], F32) for i in range(2)]
            P.op("pool", lambda e, o=o: e.memset(o.S32.t[:, :], 0.0), writes=[o.S32])
            P.op("pool", lambda e, o=o: e.memset(o.Sbf[0].t[:, :], 0.0), writes=[o.Sbf[0]])
            o.si = 0
            hs.append(o)

        def prep(h, blk):
            o = hs[h]
            t0 = blk * NB
            P.dma("sp", o.qb.t[:, :], qd[h][:, t0:t0 + NB], writes=[o.qb])
            P.dma("sp", o.kb.t[:, :], kd[h][:, t0:t0 + NB], writes=[o.kb])
            P.dma("sp", o.lf.t[:, :], lfd[h][:, t0:t0 + NB], writes=[o.lf])
            P.dma("pool", o.vb.t[:, :, :], vd[h][:, blk * 2048:(blk + 1) * 2048].rearrange("p (t d) -> p t d", d=128), writes=[o.vb])
            P.op("dve", lambda e: e.tensor_tensor_scan(o.G.t[:, :], rm.t[:, :], o.lf.t[:, :], 0.0, ALU.mult, ALU.add),
                 reads=[rm, o.lf], writes=[o.G])
            P.op("act", lambda e: e.activation(o.tmp.t[:, :], o.G.t[:, :], AF.Exp), reads=[o.G], writes=[o.tmp])
            P.op("dve", lambda e: e.tensor_tensor(o.qd.t[:, :], o.qb.t[:, :], o.tmp.t[:, :], ALU.mult), reads=[o.qb, o.tmp], writes=[o.qd])
            P.op("act", lambda e: e.activation(o.tmp2.t[:, :], o.G.t[:, :], AF.Exp, scale=-1.0), reads=[o.G], writes=[o.tmp2])
            P.op("dve", lambda e: e.tensor_tensor(o.tmp2.t[:, :], o.kb.t[:, :], o.tmp2.t[:, :], ALU.mult), reads=[o.kb, o.tmp2], writes=[o.tmp2])
            P.op("dve", lambda e: e.tensor_copy(o.kdd.t[:, :], o.tmp2.t[:, :]), reads=[o.tmp2], writes=[o.kdd])
            G3 = o.G.t[:, :].rearrange("p (c s) -> p c s", s=64)
            P.op("act", lambda e: e.activation(o.egl.t[:, :], G3[:, :, 63], AF.Exp), reads=[o.G], writes=[o.egl])
            for cc in range(32):
                P.op("dve", lambda e, cc=cc: e.tensor_scalar(o.kend.t[:, cc * 64:(cc + 1) * 64], o.tmp2.t[:, cc * 64:(cc + 1) * 64],
                                                             o.egl.t[:, cc:cc + 1], None, ALU.mult),
                     reads=[o.tmp2, o.egl], writes=[o.kend])
            for grp in range(2):
                for j in range(8):
                    tk = grp * 8 + j
                    P.op("pe", lambda e, tk=tk, j=j: e.transpose(bankT.t[:, j * 128:(j + 1) * 128], o.kend.t[:, tk * 128:(tk + 1) * 128], ident.t[:, :]),
                         reads=[o.kend, ident], writes=[bankT], pe_acc=(j > 0))
                P.op("act", lambda e, grp=grp: e.activation(o.kT.t[:, grp * 8:(grp + 1) * 8, :],
                                                           bankT.t[:, :].rearrange("p (t k) -> p t k", k=128), AF.Copy),
                     reads=[bankT], writes=[o.kT])

        nA = [0]

        def chunk(h, blk, cc):
            o = hs[h]
            tk, half = cc // 2, cc % 2
            pb = 64 * half
            gc = blk * 32 + cc
            cs = slice(cc * 64, (cc + 1) * 64)
            bA = bankA[nA[0] % 2]
            am = o.am[nA[0] % 2]
            nA[0] += 1
            bO = bankO[h]
            bU = bankU[h]
            oc0 = (gc % 8) * 64
            Sb = o.Sbf[o.si % 2]
            Sn = o.Sbf[(o.si + 1) % 2]
            o.si += 1
            P.op("pe", lambda e: e.matmul(bA.t[pb:pb + 64, 0:64], o.kdd.t[:, cs], o.qd.t[:, cs], start=True, stop=True),
                 reads=[o.kdd, o.qd], writes=[bA])
            P.op("dve", lambda e: e.tensor_tensor(am.t[pb:pb + 64, :], bA.t[pb:pb + 64, 0:64], m01.t[pb:pb + 64, :], ALU.mult),
                 reads=[bA, m01], writes=[am])
            P.op("pe", lambda e: e.matmul(bO.t[:, oc0:oc0 + 64], Sb.t[:, :], o.qd.t[:, cs], start=True, stop=False),
                 reads=[Sb, o.qd], writes=[bO], pe_acc=(gc % 8 != 0))
            P.op("pe", lambda e: e.matmul(bO.t[:, oc0:oc0 + 64], o.vb.t[pb:pb + 64, tk, :], am.t[pb:pb + 64, :], start=False, stop=True),
                 reads=[o.vb, am], writes=[bO], pe_acc=True)
            P.op("pe", lambda e: e.matmul(bU.t[:, 0:128], o.kT.t[pb:pb + 64, tk, :], o.vb.t[pb:pb + 64, tk, :], start=True, stop=True),
                 reads=[o.kT, o.vb], writes=[bU])
            P.op("dve", lambda e: e.scalar_tensor_tensor(o.S32.t[:, :], o.S32.t[:, :], o.egl.t[:, cc:cc + 1], bU.t[:, 0:128], ALU.mult, ALU.add),
                 reads=[o.S32, o.egl, bU], writes=[o.S32])
            P.op("act", lambda e: e.activation(Sn.t[:, :], o.S32.t[:, :], AF.Copy), reads=[o.S32], writes=[Sn])
            if gc % 8 == 7:
                os_ = o.osb[(gc // 8) % 2]
                P.op("act", lambda e: e.activation(os_.t[:, :], bO.t[:, :], AF.Copy), reads=[bO], writes=[os_])
                tok0 = (gc - 7) * 64
                P.dma("sp", od[h][:, tok0:tok0 + 512], os_.t[:, :], reads=[os_], ow=ob)

        for blk in range(4):
            for h in range(2):
                prep(h, blk)
            for cc in range(32):
                for h in range(2):
                    chunk(h, blk, cc)
        P.finish([ob])
        P.emit()
    return nc


def hgrn_consts():
    p = np.arange(128)
    t = np.arange(64)
    m01 = ((p[:, None] % 64) <= t[None, :]).astype(np.float32)
    rm = np.ones((128, 2048), np.float32)
    rm[:, ::64] = 0.0
    ident = np.eye(128, dtype=np.float32).astype(NPBF)
    return m01, rm, ident


def build_mega():
    nc = bass.Bass("TRN2", target_bir_lowering=False)
    P = Prog(nc)
    c = Ctx()
    c.P, c.nc = P, nc
    dr = {}

    def din(name, shape, dt=F32):
        dr[name] = nc.dram_tensor(name, list(shape), dt, kind="ExternalInput").ap()
        return dr[name]

    def dout(name, shape, dt=F32):
        dr[name] = nc.dram_tensor(name, list(shape), dt, kind="ExternalOutput").ap()
        return dr[name]

    xT = din("xT", [D, T])
    cTd = din("cT", [128, 8])
    modbd = din("modb", [128, 144])
    modwd = din("modw", [18, 128, 8192])
    gT = din("gT", [128, 48])
    outs = []
    pid = nc.partition_id()
    g4 = pid % 4
    G4 = [[0, 1, 2, 3], [4, 5, 6, 7]]
    idram = lambda name, shape, dt: nc.dram_tensor(name, list(shape), dt)
    with ExitStack() as es:
        X = es.enter_context(nc.sbuf_tensor("X", [128, 8, T], F32))
        c.X = X
        c.Xb = [[Buf(X) for _ in range(4)] for _ in range(8)]
        c.modt = P.sb(es, "modt", [128, 144], F32)
        c.gt = P.sb(es, "gt", [128, 48], F32)
        c.der = P.sb(es, "der", [128, 96], F32)
        c.ones = P.sb(es, "ones", [128, 128], BF16)
        c.bank = [P.ps(es, "bank%d" % i, [128, 512], F32) for i in range(7)]
        bankT = P.ps(es, "bankT", [128, 1024], BF16)
        sqt = es.enter_context(nc.sbuf_tensor("sq", [128, 8, 512], BF16))
        c.sq = [Buf(sqt) for _ in range(8)]
        c.rstd = P.sb(es, "rstd", [128, 512], F32)
        c.tmp = [P.sb(es, "tmp%d" % i, [128, 512], F32) for i in range(2)]

        xv = xT.rearrange("(c p) t -> p c t", p=128)
        for kc in range(8):
            P.dma("sp", X[:, kc, :], xv[:, kc, :], writes=[c.Xb[kc][tt] for tt in range(4)])
        P.dma("sp", c.gt.t[:, :], gT, writes=[c.gt])
        P.op("pool", lambda e: e.memset(c.ones.t[:, :], 1.0), writes=[c.ones])
        with ExitStack() as e0:
            ct = P.sb(e0, "ct", [128, 8], F32)
            ctb = P.sb(e0, "ctb", [128, 8], BF16)
            mb = P.sb(e0, "mb", [128, 144], F32)
            mw = [P.sb(e0, "mw%d" % i, [128, 8, 1024], BF16) for i in range(2)]
            P.dma("sp", ct.t[:, :], cTd, writes=[ct])
            P.dma("sp", mb.t[:, :], modbd, writes=[mb])
            P.op("act", lambda e: e.activation(ctb.t[:, :], ct.t[:, :], AF.Silu), reads=[ct], writes=[ctb])
            bm = c.bank[5]
            for v in range(18):
                w_ = mw[v % 2]
                P.dma("pool", w_.t[:, :, :], modwd[v].rearrange("p (k n) -> p k n", k=8), writes=[w_])
                for ch in range(8):
                    col = v * 8 + ch
                    for kc in range(8):
                        P.op("pe", lambda e, w_=w_, ch=ch, kc=kc, col=col: e.matmul(
                            bm.t[:, col:col + 1], w_.t[:, kc, ch * 128:(ch + 1) * 128], ctb.t[:, kc:kc + 1],
                            start=(kc == 0), stop=(kc == 7)),
                            reads=[w_, ctb], writes=[bm], pe_acc=not (v == 0 and ch == 0 and kc == 0))
            P.op("dve", lambda e: e.tensor_tensor(c.modt.t[:, :], bm.t[:, 0:144], mb.t[:, :], ALU.add),
                 reads=[bm, mb], writes=[c.modt])
        P.barrier()
        for l in range(2):
            for sub in range(3):
                base = ((l * 3 + sub) * 2) * 8
                sc0 = mcol(l, sub * 3 + 1, 0)
                g0 = (l * 3 + sub) * 8
                ga0 = mcol(l, sub * 3 + 2, 0)
                P.op("dve", lambda e, base=base, sc0=sc0, g0=g0: e.scalar_tensor_tensor(
                    c.der.t[:, base:base + 8], c.modt.t[:, sc0:sc0 + 8], 1.0, c.gt.t[:, g0:g0 + 8], ALU.add, ALU.mult),
                    reads=[c.modt, c.gt], writes=[c.der])
                P.op("dve", lambda e, base=base: e.tensor_scalar(
                    c.der.t[:, base:base + 8], c.der.t[:, base:base + 8], SQD, None, ALU.mult),
                    reads=[c.der], writes=[c.der])
                P.op("dve", lambda e, base=base, ga0=ga0, sub=sub: e.tensor_scalar(
                    c.der.t[:, base + 8:base + 16], c.modt.t[:, ga0:ga0 + 8], (1.0 if sub == 1 else 0.5), None, ALU.mult),
                    reads=[c.modt], writes=[c.der])

        def Acol(l, sub, ch):
            j = ((l * 3 + sub) * 2) * 8 + ch
            return c.der.t[:, j:j + 1]

        def Gcol(l, sub, ch):
            j = ((l * 3 + sub) * 2 + 1) * 8 + ch
            return c.der.t[:, j:j + 1]

        def Scol(l, sub, ch):
            j = mcol(l, sub * 3 + 0, ch)
            return c.modt.t[:, j:j + 1]

        def rstd_tile(src_fn, src_bufs, epsk):
            for kc in range(8):
                P.op("act", lambda e, kc=kc: e.activation(sqt[:, kc, :], src_fn(kc), AF.Square),
                     reads=[src_bufs[kc]], writes=[c.sq[kc]])
            for kc in range(8):
                P.op("pe", lambda e, kc=kc: e.matmul(c.bank[6].t[:, :], c.ones.t[:, :], sqt[:, kc, :],
                                                      start=(kc == 0), stop=(kc == 7)),
                     reads=[c.ones, c.sq[kc]], writes=[c.bank[6]], pe_acc=(kc > 0))
            P.op("dve", lambda e: e.tensor_scalar(c.rstd.t[:, :], c.bank[6].t[:, :], epsk, None, ALU.add),
                 reads=[c.bank[6]], writes=[c.rstd])
            P.op("act", lambda e: e.activation(c.rstd.t[:, :], c.rstd.t[:, :], AF.Sqrt), reads=[c.rstd], writes=[c.rstd])
            P.op("dve", lambda e: e.reciprocal(c.rstd.t[:, :], c.rstd.t[:, :]), reads=[c.rstd], writes=[c.rstd])

        def modnorm_tile(l, sub, tt, hdst, hbuf):
            t0 = tt * 512
            rstd_tile(lambda kc: X[:, kc, t0:t0 + 512], [c.Xb[kc][tt] for kc in range(8)], EPS * D)
            for kc in range(8):
                tb = c.tmp[kc % 2]
                P.op("dve", lambda e, kc=kc, tb=tb: e.tensor_tensor(tb.t[:, :], X[:, kc, t0:t0 + 512], c.rstd.t[:, :], ALU.mult),
                     reads=[c.Xb[kc][tt], c.rstd], writes=[tb])
                P.op("act", lambda e, kc=kc, tb=tb: e.activation(hdst(kc), tb.t[:, :], AF.Identity,
                                                               bias=Scol(l, sub, kc), scale=Acol(l, sub, kc)),
                     reads=[tb, c.der, c.modt], writes=[hbuf(kc)])

        def epilogue(kind, l, oG, oGb, sgd, sgb):
            wod = din(kind + "_wo", [128, 8192])
            oS, oSb = dsel(kind + "_oS", [1, D, T], BF16, oG.ap()[bass.ds(g4, 1), :, :], oGb)
            sv = sgd.ap().rearrange("(c p) t -> p c t", p=128)
            with ExitStack() as e2:
                wo = P.sb(e2, kind + "wo_sb", [128, 8, 1024], BF16)
                P.dma("pool", wo.t[:, :, :], wod.rearrange("p (k d) -> p k d", k=8), writes=[wo])
                ot = [P.sb(e2, kind + "ot%d" % i, [128, 8, 512], BF16) for i in range(2)]
                st = [P.sb(e2, kind + "st%d" % i, [128, 8, 512], BF16) for i in range(2)]
                ogt = [e2.enter_context(nc.sbuf_tensor(kind + "og%d" % i, [128, 8, 512], BF16)) for i in range(2)]
                ogb = [[Buf(ogt[i]) for _ in range(8)] for i in range(2)]
                if kind == "hgrn":
                    hgd = din("hgn", [128, 8])
                    hg = P.sb(e2, "hg", [128, 8], F32)
                    P.dma("sp", hg.t[:, :], hgd, writes=[hg])
                    P.op("dve", lambda e: e.tensor_scalar(hg.t[:, :], hg.t[:, :], float(np.sqrt(128.0)), None, ALU.mult),
                         reads=[hg], writes=[hg])
                    sq1 = P.sb(e2, "sq1", [128, 512], BF16)
                    r1 = P.sb(e2, "r1", [128, 512], F32)
                    t1 = P.sb(e2, "t1", [128, 512], F32)
                for tt in range(4):
                    t0 = tt * 512
                    o_, s_, og_ = ot[tt % 2], st[tt % 2], ogt[tt % 2]
                    P.dma("sp", o_.t[:, :, :], oS.ap()[0].rearrange("(c p) s -> p c s", p=128)[:, :, t0:t0 + 512], reads=[oSb], writes=[o_])
                    P.dma("sp", s_.t[:, :, :], sv[:, :, t0:t0 + 512], reads=[sgb], writes=[s_])
                    if kind == "fox":
                        for kc in range(8):
                            P.op("dve", lambda e, kc=kc, o_=o_, s_=s_, og_=og_: e.tensor_tensor(
                                og_[:, kc, :], o_.t[:, kc, :], s_.t[:, kc, :], ALU.mult),
                                reads=[o_, s_], writes=[ogb[tt % 2][kc]])
                    else:
                        for kc in range(8):
                            P.op("act", lambda e, kc=kc, o_=o_: e.activation(sq1.t[:, :], o_.t[:, kc, :], AF.Square),
                                 reads=[o_], writes=[sq1])
                            P.op("pe", lambda e: e.matmul(c.bank[5].t[:, :], c.ones.t[:, :], sq1.t[:, :], start=True, stop=True),
                                 reads=[c.ones, sq1], writes=[c.bank[5]])
                            P.op("dve", lambda e: e.tensor_scalar(r1.t[:, :], c.bank[5].t[:, :], EPS * 128.0, None, ALU.add),
                                 reads=[c.bank[5]], writes=[r1])
                            P.op("act", lambda e: e.activation(r1.t[:, :], r1.t[:, :], AF.Sqrt), reads=[r1], writes=[r1])
                            P.op("dve", lambda e: e.reciprocal(r1.t[:, :], r1.t[:, :]), reads=[r1], writes=[r1])
                            P.op("dve", lambda e, kc=kc, o_=o_: e.tensor_tensor(t1.t[:, :], o_.t[:, kc, :], r1.t[:, :], ALU.mult),
                                 reads=[o_, r1], writes=[t1])
                            P.op("dve", lambda e, kc=kc, s_=s_, og_=og_: e.scalar_tensor_tensor(
                                og_[:, kc, :], t1.t[:, :], hg.t[:, kc:kc + 1], s_.t[:, kc, :], ALU.mult, ALU.mult),
                                reads=[t1, hg, s_], writes=[ogb[tt % 2][kc]])
                    for dc in range(8):
                        bk = c.bank[4 + dc % 2]
                        for kc in range(8):
                            P.op("pe", lambda e, kc=kc, dc=dc, bk=bk, og_=og_: e.matmul(
                                bk.t[:, :], wo.t[:, kc, dc * 128:(dc + 1) * 128], og_[:, kc, :],
                                start=(kc == 0), stop=(kc == 7)),
                                reads=[wo, ogb[tt % 2][kc]], writes=[bk], pe_acc=(kc > 0))
                        P.op("dve", lambda e, dc=dc, bk=bk, t0=t0: e.scalar_tensor_tensor(
                            X[:, dc, t0:t0 + 512], bk.t[:, :], Gcol(l, 1, dc), X[:, dc, t0:t0 + 512], ALU.mult, ALU.add),
                            reads=[bk, c.der, c.Xb[dc][tt]], writes=[c.Xb[dc][tt]])
            P.barrier()

        def ffn(j, l, sub):
            wupd = din("wup%d" % j, [11, 128, 4096])
            wdnd = din("wdn%d" % j, [8, 128, 2816])
            with ExitStack() as e2:
                hbt = e2.enter_context(nc.sbuf_tensor("hb_%d" % j, [128, 8, 1024], BF16))
                hbb = [[Buf(hbt) for _ in range(2)] for _ in range(8)]
                actt = e2.enter_context(nc.sbuf_tensor("actb_%d" % j, [128, NF, 1024], BF16))
                actb = [[Buf(actt) for _ in range(2)] for _ in range(NF)]
                wu = [P.sb(e2, "wu%d_%d" % (j, i), [128, 2, 8, 256], BF16) for i in range(2)]
                wd = [P.sb(e2, "wd%d_%d" % (j, i), [128, NF, 128], BF16) for i in range(2)]
                sa = [P.sb(e2, "sa%d_%d" % (j, i), [128, 512], F32) for i in range(2)]
                for half in range(2):
                    for t2 in range(2):
                        tt = half * 2 + t2
                        modnorm_tile(l, sub, tt, lambda kc, t2=t2: hbt[:, kc, t2 * 512:(t2 + 1) * 512],
                                     lambda kc, t2=t2: hbb[kc][t2])
                    it = 0
                    for g in range(11):
                        w_ = wu[g % 2]
                        P.dma("pool", w_.t[:, :, :, :], wupd[g].rearrange("p (a k f) -> p a k f", a=2, k=8), writes=[w_])
                        for jf in range(2):
                            fc = 2 * g + jf
                            for t2 in range(2):
                                bA, bB = c.bank[it % 2], c.bank[2 + it % 2]
                                s_ = sa[it % 2]
                                it += 1
                                for kc in range(8):
                                    P.op("pe", lambda e, kc=kc, w_=w_, jf=jf, t2=t2, bA=bA: e.matmul(
                                        bA.t[:, :], w_.t[:, 0, kc, jf * 128:(jf + 1) * 128], hbt[:, kc, t2 * 512:(t2 + 1) * 512],
                                        start=(kc == 0), stop=(kc == 7)),
                                        reads=[w_, hbb[kc][t2]], writes=[bA], pe_acc=(kc > 0))
                                for kc in range(8):
                                    P.op("pe", lambda e, kc=kc, w_=w_, jf=jf, t2=t2, bB=bB: e.matmul(
                                        bB.t[:, :], w_.t[:, 1, kc, jf * 128:(jf + 1) * 128], hbt[:, kc, t2 * 512:(t2 + 1) * 512],
                                        start=(kc == 0), stop=(kc == 7)),
                                        reads=[w_, hbb[kc][t2]], writes=[bB], pe_acc=(kc > 0))
                                P.op("act", lambda e, s_=s_, bA=bA: e.activation(s_.t[:, :], bA.t[:, :], AF.Silu),
                                     reads=[bA], writes=[s_])
                                P.op("dve", lambda e, s_=s_, bB=bB, fc=fc, t2=t2: e.tensor_tensor(
                                    actt[:, fc, t2 * 512:(t2 + 1) * 512], bB.t[:, :], s_.t[:, :], ALU.mult),
                                    reads=[bB, s_], writes=[actb[fc][t2]])
                    for dc in range(8):
                        w_ = wd[dc % 2]
                        P.dma("pool", w_.t[:, :, :], wdnd[dc].rearrange("p (f d) -> p f d", f=NF), writes=[w_])
                        for t2 in range(2):
                            tt = half * 2 + t2
                            t0 = tt * 512
                            bk = c.bank[4 + (dc * 2 + t2) % 2]
                            for fc in range(NF):
                                P.op("pe", lambda e, fc=fc, w_=w_, t2=t2, bk=bk: e.matmul(
                                    bk.t[:, :], w_.t[:, fc, :], actt[:, fc, t2 * 512:(t2 + 1) * 512],
                                    start=(fc == 0), stop=(fc == NF - 1)),
                                    reads=[w_, actb[fc][t2]], writes=[bk], pe_acc=(fc > 0))
                            P.op("dve", lambda e, dc=dc, bk=bk, t0=t0: e.scalar_tensor_tensor(
                                X[:, dc, t0:t0 + 512], bk.t[:, :], Gcol(l, sub, dc), X[:, dc, t0:t0 + 512], ALU.mult, ALU.add),
                                reads=[bk, c.der, c.Xb[dc][tt]], writes=[c.Xb[dc][tt]])
            P.barrier()

        def proj_fm(wname, hbt, hbb, evac, n_oc=8):
            wd_ = din(wname, [n_oc, 128, 1024])
            with ExitStack() as e3:
                wp = [P.sb(e3, wname + "_sb%d" % i, [128, 8, 128], BF16) for i in range(2)]
                it = 0
                for oc in range(n_oc):
                    w_ = wp[oc % 2]
                    P.dma("pool", w_.t[:, :, :], wd_[oc].rearrange("p (k f) -> p k f", k=8), writes=[w_])
                    for tt in range(4):
                        bk = c.bank[it % 4]
                        it += 1
                        for kc in range(8):
                            P.op("pe", lambda e, kc=kc, w_=w_, tt=tt, bk=bk: e.matmul(
                                bk.t[:, :], w_.t[:, kc, :], hbt[:, kc, tt * 512:(tt + 1) * 512],
                                start=(kc == 0), stop=(kc == 7)),
                                reads=[w_, hbb[kc][tt]], writes=[bk], pe_acc=(kc > 0))
                        evac(oc, tt, bk)
                P.barrier()

        def proj_tm(wname, hbt, hbb, vout, vob, func):
            wd_ = din(wname, [2, 128, 4096])
            with ExitStack() as e3:
                wv = P.sb(e3, wname + "_sb", [128, 2, 8, 512], BF16)
                for cg in range(2):
                    P.dma("pool", wv.t[:, cg, :, :], wd_[cg].rearrange("p (k f) -> p k f", k=8), writes=[wv])
                vt = [P.sb(e3, wname + "vt%d" % i, [128, 512], BF16) for i in range(2)]
                it = 0
                for tk in range(16):
                    for cg in range(2):
                        bk = c.bank[it % 4]
                        v_ = vt[it % 2]
                        it += 1
                        for kc in range(8):
                            P.op("pe", lambda e, kc=kc, tk=tk, cg=cg, bk=bk: e.matmul(
                                bk.t[:, :], hbt[:, kc, tk * 128:(tk + 1) * 128], wv.t[:, cg, kc, :],
                                start=(kc == 0), stop=(kc == 7)),
                                reads=[wv, hbb[kc][tk // 4]], writes=[bk], pe_acc=(kc > 0))
                        P.op("act", lambda e, bk=bk, v_=v_: e.activation(v_.t[:, :], bk.t[:, :], func),
                             reads=[bk], writes=[v_])
                        P.dma("sp", vout[tk * 128:(tk + 1) * 128, cg * 512:(cg + 1) * 512], v_.t[:, :], reads=[v_], ow=vob)
                P.barrier()

        def stage_out(e3, name, shape, dt):
            return [P.sb(e3, name + "%d" % i, shape, dt) for i in range(2)]

        def projections(kind, l):
            R = Ctx()
            with ExitStack() as e2:
                hbt = e2.enter_context(nc.sbuf_tensor(kind + "hb2", [128, 8, T], BF16))
                hbb = [[Buf(hbt) for _ in range(4)] for _ in range(8)]
                for tt in range(4):
                    modnorm_tile(l, 1, tt, lambda kc, tt=tt: hbt[:, kc, tt * 512:(tt + 1) * 512],
                                 lambda kc, tt=tt: hbb[kc][tt])
                kdt = BF16 if kind == "fox" else F32
                R.q, R.qb = idram(kind + "_q", [D, T], BF16), Buf()
                R.k, R.kb = idram(kind + "_k", [D, T], kdt), Buf()
                R.sg, R.sgb = idram(kind + "_sg", [D, T], BF16), Buf()
                R.v, R.vb = idram(kind + "_v", [T, D], BF16), Buf()
                qo, ko, sgo, vo = R.q.ap(), R.k.ap(), R.sg.ap(), R.v.ap()
                qob, kob, sgob, vob = R.qb, R.kb, R.sgb, R.vb
                cnt = [0]

                def simple_evac(od, ob, func, scale, st):
                    def evac(oc, tt, bk):
                        s_ = st[cnt[0] % 2]
                        cnt[0] += 1
                        P.op("act", lambda e, s_=s_, bk=bk: e.activation(s_.t[:, :], bk.t[:, :], func, scale=scale),
                             reads=[bk], writes=[s_])
                        P.dma("sp", od[oc * 128:(oc + 1) * 128, tt * 512:(tt + 1) * 512], s_.t[:, :], reads=[s_], ow=ob)
                    return evac

                stb = stage_out(e2, kind + "stb", [128, 512], BF16)
                if kind == "fox":
                    R.lf, R.lfb = idram("fox_lf", [128, 256], F32), Buf()
                    proj_fm("fox_wq", hbt, hbb, simple_evac(qo, qob, AF.Copy, float(FD ** -0.5), stb))
                    proj_fm("fox_wk", hbt, hbb, simple_evac(ko, kob, AF.Copy, 1.0, stb))
                    proj_fm("fox_wg", hbt, hbb, simple_evac(sgo, sgob, AF.Sigmoid, 1.0, stb))
                    proj_tm("fox_wv", hbt, hbb, vo, vob, AF.Copy)
                    wfd = din("fox_wf", [128, 128])
                    bfd = din("fox_bfb", [128, 256])
                    wf = P.sb(e2, "wf_sb", [128, 8, 16], BF16)
                    P.dma("pool", wf.t[:, :, :], wfd.rearrange("p (k f) -> p k f", k=8), writes=[wf])
                    bfb = P.sb(e2, "bfb", [128, 256], F32)
                    P.dma("sp", bfb.t[:, :], bfd, writes=[bfb])
                    z1 = P.sb(e2, "z1", [128, 256], F32)
                    bk = c.bank[0]
                    for tk in range(16):
                        for kc in range(8):
                            P.op("pe", lambda e, kc=kc, tk=tk: e.matmul(
                                bk.t[:, tk * 16:(tk + 1) * 16], hbt[:, kc, tk * 128:(tk + 1) * 128], wf.t[:, kc, :],
                                start=(kc == 0), stop=(kc == 7)),
                                reads=[wf, hbb[kc][tk // 4]], writes=[bk], pe_acc=not (tk == 0 and kc == 0))
                    P.op("dve", lambda e: e.tensor_tensor(z1.t[:, :], bk.t[:, 0:256], bfb.t[:, :], ALU.add), reads=[bk, bfb], writes=[z1])
                    P.op("act", lambda e: e.activation(z1.t[:, :], z1.t[:, :], AF.Exp, scale=-1.0), reads=[z1], writes=[z1])
                    P.op("act", lambda e: e.activation(z1.t[:, :], z1.t[:, :], AF.Ln, bias=1.0, scale=1.0), reads=[z1], writes=[z1])
                    P.op("dve", lambda e: e.tensor_scalar(z1.t[:, :], z1.t[:, :], -1.0, None, ALU.mult), reads=[z1], writes=[z1])
                    P.dma("sp", R.lf.ap(), z1.t[:, :], reads=[z1], ow=R.lfb)
                else:
                    R.lf, R.lfb = idram("hgrn_lf", [D, T], F32), Buf()
                    lfo, lfob = R.lf.ap(), R.lfb
                    lbd = din("lbl", [128, 16])
                    lbl = P.sb(e2, "lbl_sb", [128, 16], F32)
                    lb = P.sb(e2, "lb", [128, 8], F32)
                    oml = P.sb(e2, "oml", [128, 8], F32)
                    P.dma("sp", lbl.t[:, :], lbd, writes=[lbl])
                    P.op("dve", lambda e: e.tensor_tensor(lb.t[:, :], lbl.t[:, 8:16], lbl.t[:, 0:8], ALU.subtract), reads=[lbl], writes=[lb])
                    P.op("act", lambda e: e.activation(lb.t[:, :], lb.t[:, :], AF.Sigmoid), reads=[lb], writes=[lb])
                    P.op("dve", lambda e: e.tensor_scalar(oml.t[:, :], lb.t[:, :], -1.0, 1.0, ALU.mult, ALU.add), reads=[lb], writes=[oml])
                    proj_fm("hgrn_wq", hbt, hbb, simple_evac(qo, qob, AF.Copy, 1.0, stb))
                    proj_fm("hgrn_wg", hbt, hbb, simple_evac(sgo, sgob, AF.Silu, 1.0, stb))
                    proj_tm("hgrn_wv", hbt, hbb, vo, vob, AF.Silu)
                    sg1 = P.sb(e2, "sg1", [128, 512], F32)
                    ff = stage_out(e2, "ff", [128, 512], F32)
                    lff = stage_out(e2, "lff", [128, 512], F32)
                    kk = stage_out(e2, "kk", [128, 512], F32)

                    def f_evac(oc, tt, bk):
                        i = cnt[0] % 2
                        cnt[0] += 1
                        f_, l_, k_ = ff[i], lff[i], kk[i]
                        P.op("act", lambda e, bk=bk: e.activation(sg1.t[:, :], bk.t[:, :], AF.Sigmoid), reads=[bk], writes=[sg1])
                        P.op("dve", lambda e, f_=f_, oc=oc: e.tensor_scalar(f_.t[:, :], sg1.t[:, :], oml.t[:, oc:oc + 1], lb.t[:, oc:oc + 1], ALU.mult, ALU.add),
                             reads=[sg1, oml, lb], writes=[f_])
                        P.op("act", lambda e, f_=f_, l_=l_: e.activation(l_.t[:, :], f_.t[:, :], AF.Ln), reads=[f_], writes=[l_])
                        P.dma("sp", lfo[oc * 128:(oc + 1) * 128, tt * 512:(tt + 1) * 512], l_.t[:, :], reads=[l_], ow=lfob)
                    proj_fm("hgrn_wf", hbt, hbb, f_evac)
            P.barrier()
            return R

        def gather(name, src, srcb, nch, rows, cols, dt):
            dst = idram(name, [nch, 4 * rows, cols], dt)
            db = Buf()
            sv = src.ap() if len(src.shape) == 2 else None
            for j in range(nch):
                sa = src.ap()[j * rows:(j + 1) * rows, :] if sv is not None else src.ap()[j]
                P.collective("AllGather", G4, sa.opt(), dst.ap()[j].opt(), [srcb], db)
            return dst, db

        def dsel(name, shape, dt, src_dyn, srcb):
            dst = idram(name, shape, dt)
            db = Buf()
            P.dma("sp", dst.ap(), src_dyn, reads=[srcb], writes=[db])
            return dst, db

        def fox_phase(R):
            qG, qGb = gather("fox_qG", R.q, R.qb, 4, 256, T, BF16)
            kG, kGb = gather("fox_kG", R.k, R.kb, 4, 256, T, BF16)
            vG, vGb = gather("fox_vG", R.v, R.vb, 4, 512, D, BF16)
            lG, lGb = gather("fox_lG", R.lf, R.lfb, 1, 128, 256, F32)
            qS, qSb = dsel("fox_qS", [1, D, T], BF16, qG.ap()[bass.ds(g4, 1), :, :], qGb)
            kS, kSb = dsel("fox_kS", [1, D, T], BF16, kG.ap()[bass.ds(g4, 1), :, :], kGb)
            vS, vSb = idram("fox_vS", [S, 256], BF16), Buf()
            for j in range(4):
                P.dma("sp", vS.ap().rearrange("(r j i) c -> j r i c", r=4, j=4)[j],
                      vG.ap()[j].rearrange("(r i) c -> r i c", r=4)[:, :, bass.ds(g4 * 256, 256)], reads=[vGb], writes=[vSb])
            lS, lSb = dsel("fox_lS", [512, 16, 4], F32, lG.ap()[0].rearrange("r (k h) -> r k h", h=16)[:, :, bass.ds(g4 * 4, 4)], lGb)
            o_loc, olb = idram("fox_o", [4, 256, T], BF16), Buf()
            shi, slo = idram("shi", [4, S], BF16), idram("slo", [4, S], BF16)
            shb, slb = Buf(), Buf()
            Ud, seld, mkd, idfd = din("U", [128, 128]), din("sel", [128, 128]), din("mk", [128, 128]), din("identf", [128, 128])
            bank = c.bank
            with ExitStack() as e2:
                U = P.sb(e2, "U_sb", [128, 128], F32)
                sel = P.sb(e2, "sel_sb", [128, 128], F32)
                mk = P.sb(e2, "mk_sb", [128, 128], F32)
                idf = P.sb(e2, "idf_sb", [128, 128], F32)
                onesf = P.sb(e2, "onesf", [128, 128], F32)
                negB = P.sb(e2, "negB", [128, 4 * 16 * 64], F32)
                for t_, d_ in ((U, Ud), (sel, seld), (mk, mkd), (idf, idfd)):
                    P.dma("sp", t_.t[:, :], d_, writes=[t_])
                P.op("pool", lambda e: e.memset(onesf.t[:, :], 1.0), writes=[onesf])
                with ExitStack() as e3:
                    lsel = P.sb(e3, "lsel", [128, 4, 16, 4], F32)
                    lt = P.sb(e3, "lt_sb", [128, 256], F32)
                    within = P.sb(e3, "within", [128, 256], F32)
                    tot = P.sb(e3, "tot", [128, 256], F32)
                    inc = P.sb(e3, "inc", [128, 256], F32)
                    GT = P.sb(e3, "GT", [128, 256], F32)
                    gend = P.sb(e3, "gend", [128, 256], F32)
                    Aa = P.sb(e3, "Aa", [128, 256], F32)
                    AT = P.sb(e3, "AT", [64, 512], F32)
                    ahi = P.sb(e3, "ahi", [64, 512], BF16)
                    ahf = P.sb(e3, "ahf", [64, 512], F32)
                    alo = P.sb(e3, "alo", [64, 512], BF16)
                    for t in range(4):
                        P.dma("sp", lsel.t[:, t, :, :], lS.ap()[t * 128:(t + 1) * 128, :, :], reads=[lSb], writes=[lsel])
                    for hl in range(4):
                        P.op("dve", lambda e, hl=hl: e.tensor_copy(
                            lt.t[:, hl * 64:(hl + 1) * 64].rearrange("p (t k) -> p t k", t=4), lsel.t[:, :, :, hl]),
                            reads=[lsel], writes=[lt])
                    P.op("pe", lambda e: e.matmul(bank[6].t[:, 0:256], U.t[:, :], lt.t[:, :], start=True, stop=True), reads=[U, lt], writes=[bank[6]])
                    P.op("pe", lambda e: e.matmul(bank[5].t[:, 0:256], onesf.t[:, :], lt.t[:, :], start=True, stop=True), reads=[onesf, lt], writes=[bank[5]])
                    P.op("dve", lambda e: e.tensor_copy(within.t[:, :], bank[6].t[:, 0:256]), reads=[bank[6]], writes=[within])
                    P.op("dve", lambda e: e.tensor_copy(tot.t[:, :], bank[5].t[:, 0:256]), reads=[bank[5]], writes=[tot])
                    for h in range(4):
                        P.op("dve", lambda e, h=h: e.tensor_tensor_scan(inc.t[:, h * 64:(h + 1) * 64], onesf.t[:, 0:64], tot.t[:, h * 64:(h + 1) * 64],
                                                                        0.0, ALU.mult, ALU.add), reads=[onesf, tot], writes=[inc])
                    P.op("dve", lambda e: e.tensor_tensor(GT.t[:, :], within.t[:, :], inc.t[:, :], ALU.add), reads=[within, inc], writes=[GT])
                    P.op("dve", lambda e: e.tensor_tensor(GT.t[:, :], GT.t[:, :], tot.t[:, :], ALU.subtract), reads=[GT, tot], writes=[GT])
                    P.op("pe", lambda e: e.matmul(bank[6].t[:, 0:256], sel.t[:, :], GT.t[:, :], start=True, stop=True), reads=[sel, GT], writes=[bank[6]])
                    P.op("dve", lambda e: e.tensor_copy(gend.t[:, :], bank[6].t[:, 0:256]), reads=[bank[6]], writes=[gend])
                    for h in range(4):
                        for Q in range(16):
                            j0 = (h * 16 + Q) * 64
                            gc = h * 64 + 4 * Q + 3
                            P.op("dve", lambda e, h=h, j0=j0, gc=gc: e.tensor_scalar(
                                negB.t[:, j0:j0 + 64], GT.t[:, h * 64:(h + 1) * 64], -1.0, gend.t[:, gc:gc + 1], ALU.mult, ALU.add),
                                reads=[GT, gend], writes=[negB])
                            a0 = h * 64 + 4 * Q
                            P.op("dve", lambda e, a0=a0, gc=gc: e.tensor_scalar(
                                Aa.t[:, a0:a0 + 4], GT.t[:, a0:a0 + 4], gend.t[:, gc:gc + 1], None, ALU.subtract),
                                reads=[GT, gend], writes=[Aa])
                    for h in range(4):
                        P.op("pe", lambda e, h=h: e.matmul(bank[5].t[0:64, h * 128:(h + 1) * 128], Aa.t[:, h * 64:(h + 1) * 64], idf.t[:, :],
                                                           start=True, stop=True), reads=[Aa, idf], writes=[bank[5]], pe_acc=(h > 0))
                    P.op("dve", lambda e: e.tensor_copy(AT.t[:, :], bank[5].t[0:64, :]), reads=[bank[5]], writes=[AT])
                    P.op("dve", lambda e: e.tensor_copy(ahi.t[:, :], AT.t[:, :]), reads=[AT], writes=[ahi])
                    P.op("dve", lambda e: e.tensor_copy(ahf.t[:, :], ahi.t[:, :]), reads=[ahi], writes=[ahf])
                    P.op("dve", lambda e: e.tensor_tensor(alo.t[:, :], AT.t[:, :], ahf.t[:, :], ALU.subtract), reads=[AT, ahf], writes=[alo])
                    P.dma("sp", shi.ap().rearrange("h (k p) -> k h p", p=128), ahi.t[:, :].rearrange("k (h p) -> k h p", h=4), reads=[ahi], writes=[shb])
                    P.dma("sp", slo.ap().rearrange("h (k p) -> k h p", p=128), alo.t[:, :].rearrange("k (h p) -> k h p", h=4), reads=[alo], writes=[slb])
                P.barrier()
                qa = [P.sb(e2, "qa%d" % i, [66, S], BF16) for i in range(2)]
                ka = [P.sb(e2, "ka%d" % i, [66, S], BF16) for i in range(2)]
                va = [P.sb(e2, "va%d" % i, [128, 64, 65], BF16) for i in range(2)]
                pt = [P.sb(e2, "pt%d" % i, [128, 512], BF16) for i in range(3)]
                drow = P.sb(e2, "drow", [65, 512], F32)
                rec = P.sb(e2, "rec", [64, 512], F32)
                oo = [P.sb(e2, "oo%d" % i, [64, 512], BF16) for i in range(2)]
                for i in range(2):
                    P.op("pool", lambda e, i=i: e.memset(ka[i].t[64:66, :], 1.0), writes=[ka[i]])
                    P.op("pool", lambda e, i=i: e.memset(va[i].t[:, :, 64:65], 1.0), writes=[va[i]])
                vGv = vS.ap().rearrange("(k p) d -> p k d", p=128)

                def load_head(h):
                    q_, k_, v_ = qa[h % 2], ka[h % 2], va[h % 2]
                    for t in range(4):
                        P.dma("sp", q_.t[0:64, t * T:(t + 1) * T], qS.ap()[0, t * 256 + h * 64:t * 256 + (h + 1) * 64, :], reads=[qSb], writes=[q_])
                        P.dma("pool", k_.t[0:64, t * T:(t + 1) * T], kS.ap()[0, t * 256 + h * 64:t * 256 + (h + 1) * 64, :], reads=[kSb], writes=[k_])
                    P.dma("sp", q_.t[64:65, :], shi.ap()[h:h + 1, :], reads=[shb], writes=[q_])
                    P.dma("sp", q_.t[65:66, :], slo.ap()[h:h + 1, :], reads=[slb], writes=[q_])
                    P.dma("pool", v_.t[:, :, 0:64], vGv[:, :, h * 64:(h + 1) * 64], reads=[vSb], writes=[v_])

                load_head(0)

                def do_head(h, q_, k_, v_, nit):
                    items = [(Q, kt) for Q in range(16) for kt in range(4 * Q + 4)]

                    def emit_S(idx, it_no):
                        Q, kt = items[idx]
                        d = kt - 4 * Q
                        c0 = 128 * d if d >= 0 else 0
                        bk = bank[it_no % 3]
                        p_ = pt[it_no % 3]
                        P.op("pe", lambda e: e.matmul(bk.t[:, c0:512], k_.t[0:66, kt * 128:(kt + 1) * 128],
                                                      q_.t[0:66, Q * 512 + c0:(Q + 1) * 512], start=True, stop=True),
                             reads=[k_, q_], writes=[bk])
                        if d >= 0:
                            P.op("dve", lambda e: e.tensor_tensor(bk.t[:, c0:c0 + 128], bk.t[:, c0:c0 + 128], mk.t[:, :], ALU.add),
                                 reads=[bk, mk], writes=[bk])
                        jb = (h * 16 + Q) * 64 + kt
                        P.op("act", lambda e: e.activation(p_.t[:, c0:512], bk.t[:, c0:512], AF.Exp, bias=negB.t[:, jb:jb + 1], scale=1.0),
                             reads=[bk, negB], writes=[p_])
                        P.op("pe", lambda e: e.matmul(bank[6].t[:, 0:256], k_.t[0:66, 0:128], q_.t[0:66, 0:256], start=True, stop=True),
                             reads=[k_, q_], writes=[junk], pe_acc=True)

                    def emit_PV(idx, it_no):
                        Q, kt = items[idx]
                        d = kt - 4 * Q
                        c0 = 128 * d if d >= 0 else 0
                        p_ = pt[it_no % 3]
                        ob_ = bank[3 + Q % 2]
                        last = (kt == 4 * Q + 3)
                        P.op("pe", lambda e: e.matmul(ob_.t[0:65, c0:512], v_.t[:, kt, :], p_.t[:, c0:512], start=(kt == 0), stop=last),
                             reads=[v_, p_], writes=[ob_], pe_acc=(kt > 0))
                        if last:
                            o_ = oo[Q % 2]
                            P.op("act", lambda e: e.activation(drow.t[64:65, :], ob_.t[64:65, :], AF.Copy), reads=[ob_], writes=[drow])
                            P.op("pe", lambda e: e.matmul(bank[5].t[0:64, :], onesf.t[64:65, 0:64], drow.t[64:65, :], start=True, stop=True),
                                 reads=[onesf, drow], writes=[bank[5]])
                            P.op("dve", lambda e: e.reciprocal(rec.t[:, :], bank[5].t[0:64, :]), reads=[bank[5]], writes=[rec])
                            P.op("dve", lambda e: e.tensor_tensor(o_.t[:, :], ob_.t[0:64, :], rec.t[:, :], ALU.mult), reads=[ob_, rec], writes=[o_])
                            P.dma("sp", o_loc.ap()[Q // 4][h * 64:(h + 1) * 64, (Q % 4) * 512:(Q % 4 + 1) * 512], o_.t[:, :], reads=[o_], ow=olb)

                    n = len(items)
                    emit_S(0, nit)
                    for idx in range(n):
                        if idx + 1 < n:
                            emit_S(idx + 1, nit + idx + 1)
                        emit_PV(idx, nit + idx)
                    return nit + n

                nit = 0
                junk = Buf()
                for h in range(4):
                    if h + 1 < 4:
                        load_head(h + 1)
                    nit = do_head(h, qa[h % 2], ka[h % 2], va[h % 2], nit)
            P.barrier()
            return gather("fox_oG", o_loc, olb, 4, 256, T, BF16)

        def hgrn_phase(R):
            qG, qGb = gather("hg_qG", R.q, R.qb, 4, 256, T, BF16)
            lG, lGb = gather("hg_lG", R.lf, R.lfb, 8, 128, T, F32)
            vG, vGb = gather("hg_vG", R.v, R.vb, 4, 512, D, BF16)
            qS, qSb = dsel("hg_qS", [1, D, T], BF16, qG.ap()[bass.ds(g4, 1), :, :], qGb)
            lS, lSb = dsel("hg_lS", [2, 512, T], F32, lG.ap()[bass.ds(g4 * 2, 2), :, :], lGb)
            vS, vSb = idram("hg_vS", [S, 256], BF16), Buf()
            for j in range(4):
                P.dma("sp", vS.ap().rearrange("(r j i) c -> j r i c", r=4, j=4)[j],
                      vG.ap()[j].rearrange("(r i) c -> r i c", r=4)[:, :, bass.ds(g4 * 256, 256)], reads=[vGb], writes=[vSb])
            o_loc, olb = idram("hg_o", [4, 256, T], BF16), Buf()
            m01d, rmd, idd = din("m01", [128, 64]), din("rm", [128, 2048]), din("ident", [128, 128], BF16)
            NB = 2048
            bankA, bankO, bankU = c.bank[0:2], c.bank[2:4], c.bank[4:6]
            with ExitStack() as e2:
                m01 = P.sb(e2, "m01_sb", [128, 64], F32)
                rm = P.sb(e2, "rm_sb", [128, NB], F32)
                ident = P.sb(e2, "ident_sb", [128, 128], BF16)
                P.dma("sp", m01.t[:, :], m01d, writes=[m01])
                P.dma("sp", rm.t[:, :], rmd, writes=[rm])
                P.dma("sp", ident.t[:, :], idd, writes=[ident])
                sh = Ctx()
                sh.qb = P.sb(e2, "hqb", [128, NB], BF16)
                sh.kb = P.sb(e2, "hkb", [128, NB], F32)
                sh.lf = P.sb(e2, "hlf", [128, NB], F32)
                sh.G = P.sb(e2, "hG", [128, NB], F32)
                sh.tmp = P.sb(e2, "htmp", [128, NB], F32)
                sh.tmp2 = P.sb(e2, "htmp2", [128, NB], F32)
                sh.kend = P.sb(e2, "hkend", [128, NB], BF16)
                hs = []
                for h in range(2):
                    o = Ctx()
                    o.qd = P.sb(e2, "hqd%d" % h, [128, NB], BF16)
                    o.kdd = P.sb(e2, "hkdd%d" % h, [128, NB], BF16)
                    o.kT = P.sb(e2, "hkT%d" % h, [128, 16, 128], BF16)
                    o.vb = P.sb(e2, "hvb%d" % h, [128, 16, 128], BF16)
                    o.egl = P.sb(e2, "hegl%d" % h, [128, 32], F32)
                    o.S32 = P.sb(e2, "hS32_%d" % h, [128, 128], F32)
                    o.Sbf = [P.sb(e2, "hSbf%d_%d" % (h, i), [128, 128], BF16) for i in range(2)]
                    o.am = [P.sb(e2, "ham%d_%d" % (h, i), [128, 64], BF16) for i in range(2)]
                    o.osb = [P.sb(e2, "hosb%d_%d" % (h, i), [128, 512], BF16) for i in range(2)]
                    P.op("pool", lambda e, o=o: e.memset(o.S32.t[:, :], 0.0), writes=[o.S32])
                    P.op("pool", lambda e, o=o: e.memset(o.Sbf[0].t[:, :], 0.0), writes=[o.Sbf[0]])
                    o.si = 0
                    hs.append(o)
                vGv = vS.ap().rearrange("(t p) d -> p t d", p=128)

                def prep(h, blk):
                    o = hs[h]
                    P.dma("sp", sh.qb.t[:, :], qS.ap()[0, blk * 256 + h * 128:blk * 256 + (h + 1) * 128, :], reads=[qSb], writes=[sh.qb])
                    P.dma("sp", sh.lf.t[:, :], lS.ap()[h, blk * 128:(blk + 1) * 128, :], reads=[lSb], writes=[sh.lf])
                    P.dma("pool", o.vb.t[:, :, :], vGv[:, blk * 16:(blk + 1) * 16, h * 128:(h + 1) * 128], reads=[vSb], writes=[o.vb])
                    P.op("act", lambda e: e.activation(sh.kb.t[:, :], sh.lf.t[:, :], AF.Exp), reads=[sh.lf], writes=[sh.kb])
                    P.op("dve", lambda e: e.tensor_scalar(sh.kb.t[:, :], sh.kb.t[:, :], -1.0, 1.0, ALU.mult, ALU.add), reads=[sh.kb], writes=[sh.kb])
                    P.op("dve", lambda e: e.tensor_tensor_scan(sh.G.t[:, :], rm.t[:, :], sh.lf.t[:, :], 0.0, ALU.mult, ALU.add),
                         reads=[rm, sh.lf], writes=[sh.G])
                    P.op("act", lambda e: e.activation(sh.tmp.t[:, :], sh.G.t[:, :], AF.Exp), reads=[sh.G], writes=[sh.tmp])
                    P.op("dve", lambda e: e.tensor_tensor(o.qd.t[:, :], sh.qb.t[:, :], sh.tmp.t[:, :], ALU.mult), reads=[sh.qb, sh.tmp], writes=[o.qd])
                    P.op("act", lambda e: e.activation(sh.tmp2.t[:, :], sh.G.t[:, :], AF.Exp, scale=-1.0), reads=[sh.G], writes=[sh.tmp2])
                    P.op("dve", lambda e: e.tensor_tensor(sh.tmp2.t[:, :], sh.kb.t[:, :], sh.tmp2.t[:, :], ALU.mult), reads=[sh.kb, sh.tmp2], writes=[sh.tmp2])
                    P.op("dve", lambda e: e.tensor_copy(o.kdd.t[:, :], sh.tmp2.t[:, :]), reads=[sh.tmp2], writes=[o.kdd])
                    G3 = sh.G.t[:, :].rearrange("p (c s) -> p c s", s=64)
                    P.op("act", lambda e: e.activation(o.egl.t[:, :], G3[:, :, 63], AF.Exp), reads=[sh.G], writes=[o.egl])
                    for cc in range(32):
                        P.op("dve", lambda e, cc=cc: e.tensor_scalar(sh.kend.t[:, cc * 64:(cc + 1) * 64], sh.tmp2.t[:, cc * 64:(cc + 1) * 64],
                                                                     o.egl.t[:, cc:cc + 1], None, ALU.mult),
                             reads=[sh.tmp2, o.egl], writes=[sh.kend])
                    for grp in range(2):
                        for j in range(8):
                            tk = grp * 8 + j
                            P.op("pe", lambda e, tk=tk, j=j: e.transpose(bankT.t[:, j * 128:(j + 1) * 128], sh.kend.t[:, tk * 128:(tk + 1) * 128], ident.t[:, :]),
                                 reads=[sh.kend, ident], writes=[bankT], pe_acc=(j > 0))
                        P.op("act", lambda e, grp=grp: e.activation(o.kT.t[:, grp * 8:(grp + 1) * 8, :],
                                                                   bankT.t[:, :].rearrange("p (t k) -> p t k", k=128), AF.Copy),
                             reads=[bankT], writes=[o.kT])

                nA = [0]

                def chunk(h, blk, cc):
                    o = hs[h]
                    tk, half = cc // 2, cc % 2
                    pb = 64 * half
                    gc = blk * 32 + cc
                    cs = slice(cc * 64, (cc + 1) * 64)
                    bA = bankA[nA[0] % 2]
                    am = o.am[nA[0] % 2]
                    nA[0] += 1
                    bO = bankO[h]
                    bU = bankU[h]
                    oc0 = (gc % 8) * 64
                    Sb = o.Sbf[o.si % 2]
                    Sn = o.Sbf[(o.si + 1) % 2]
                    o.si += 1
                    P.op("pe", lambda e: e.matmul(bA.t[pb:pb + 64, 0:64], o.kdd.t[:, cs], o.qd.t[:, cs], start=True, stop=True),
                         reads=[o.kdd, o.qd], writes=[bA])
                    P.op("dve", lambda e: e.tensor_tensor(am.t[pb:pb + 64, :], bA.t[pb:pb + 64, 0:64], m01.t[pb:pb + 64, :], ALU.mult),
                         reads=[bA, m01], writes=[am])
                    P.op("pe", lambda e: e.matmul(bO.t[:, oc0:oc0 + 64], Sb.t[:, :], o.qd.t[:, cs], start=True, stop=False),
                         reads=[Sb, o.qd], writes=[bO], pe_acc=(gc % 8 != 0))
                    P.op("pe", lambda e: e.matmul(bO.t[:, oc0:oc0 + 64], o.vb.t[pb:pb + 64, tk, :], am.t[pb:pb + 64, :], start=False, stop=True),
                         reads=[o.vb, am], writes=[bO], pe_acc=True)
                    P.op("pe", lambda e: e.matmul(bU.t[:, 0:128], o.kT.t[pb:pb + 64, tk, :], o.vb.t[pb:pb + 64, tk, :], start=True, stop=True),
                         reads=[o.kT, o.vb], writes=[bU])
                    P.op("dve", lambda e: e.scalar_tensor_tensor(o.S32.t[:, :], o.S32.t[:, :], o.egl.t[:, cc:cc + 1], bU.t[:, 0:128], ALU.mult, ALU.add),
                         reads=[o.S32, o.egl, bU], writes=[o.S32])
                    P.op("act", lambda e: e.activation(Sn.t[:, :], o.S32.t[:, :], AF.Copy), reads=[o.S32], writes=[Sn])
                    if gc % 8 == 7:
                        os_ = o.osb[(gc // 8) % 2]
                        P.op("act", lambda e: e.activation(os_.t[:, :], bO.t[:, :], AF.Copy), reads=[bO], writes=[os_])
                        tok0 = (gc - 7) * 64
                        P.dma("sp", o_loc.ap()[tok0 // T][h * 128:(h + 1) * 128, tok0 % T:tok0 % T + 512], os_.t[:, :], reads=[os_], ow=olb)

                oG = idram("hg_oG", [4, 4 * 256, T], BF16)
                oGb = Buf()
                for blk in range(4):
                    for h in range(2):
                        prep(h, blk)
                    for cc in range(32):
                        for h in range(2):
                            chunk(h, blk, cc)
                    P.collective("AllGather", G4, o_loc.ap()[blk].opt(), oG.ap()[blk].opt(), [olb], oGb)
            P.barrier()
            return oG, oGb

        ffn(0, 0, 0)
        R1 = projections("fox", 0)
        oG1, oG1b = fox_phase(R1)
        epilogue("fox", 0, oG1, oG1b, R1.sg, R1.sgb)
        ffn(1, 0, 2)
        ffn(2, 1, 0)
        R2 = projections("hgrn", 1)
        oG2, oG2b = hgrn_phase(R2)
        epilogue("hgrn", 1, oG2, oG2b, R2.sg, R2.sgb)
        ffn(3, 1, 2)
        xo = dout("xo", [D, T])
        xob = Buf()
        outs.append(xob)
        xov = xo.rearrange("(c p) t -> p c t", p=128)
        fgd = din("fg", [128, 8])
        fg = P.sb(es, "fg_sb", [128, 8], F32)
        P.dma("sp", fg.t[:, :], fgd, writes=[fg])
        P.op("dve", lambda e: e.tensor_scalar(fg.t[:, :], fg.t[:, :], SQD, None, ALU.mult), reads=[fg], writes=[fg])
        yo = [P.sb(es, "yo%d" % i, [128, 512], F32) for i in range(2)]
        it = 0
        for tt in range(4):
            t0 = tt * 512
            rstd_tile(lambda kc, t0=t0: X[:, kc, t0:t0 + 512], [c.Xb[kc][tt] for kc in range(8)], EPS * D)
            for kc in range(8):
                y_ = yo[it % 2]
                it += 1
                P.op("dve", lambda e, kc=kc, y_=y_, t0=t0: e.scalar_tensor_tensor(
                    y_.t[:, :], X[:, kc, t0:t0 + 512], fg.t[:, kc:kc + 1], c.rstd.t[:, :], ALU.mult, ALU.mult),
                    reads=[c.Xb[kc][tt], fg, c.rstd], writes=[y_])
                P.dma("sp", xov[:, kc, t0:t0 + 512], y_.t[:, :], reads=[y_], ow=xob)
        P.finish(outs)
        P.emit()
    return nc


_DBG = {}


def _run(nc, maps):
    return run_bass_kernel_spmd(nc, maps, core_ids=list(range(NCORES))).results


def kernel_unfused(x, c, ada_w, ada_b, norm_g, ffn_w_up, ffn_w_down, fox_w_in, fox_b_f, fox_w_out,
           hgrn_w_in, hgrn_norm_g, hgrn_w_out, hgrn_lb_logits, final_norm_g):
    f32 = lambda a: np.ascontiguousarray(np.asarray(a, dtype=np.float32))
    x, c, ada_w, ada_b, norm_g = f32(x), f32(c), f32(ada_w), f32(ada_b), f32(norm_g)
    ffn_w_up, ffn_w_down, fox_w_in, fox_b_f, fox_w_out = f32(ffn_w_up), f32(ffn_w_down), f32(fox_w_in), f32(fox_b_f), f32(fox_w_out)
    hgrn_w_in, hgrn_norm_g, hgrn_w_out = f32(hgrn_w_in), f32(hgrn_norm_g), f32(hgrn_w_out)
    hgrn_lb_logits, final_norm_g = f32(hgrn_lb_logits), f32(final_norm_g)

    mod = run_mod(c, ada_w, ada_b)
    _DBG["mod"] = mod
    modT = [fm_cols(mod[b].reshape(18, D)) for b in range(B)]
    gT = fm_cols(norm_g.reshape(6, D))
    cores = [(b, t) for b in range(B) for t in range(4)]

    nc1 = build_F({"ffns": [(0, 0)], "proj": ("fox", 0)})
    wi = fox_w_in[0]
    shared = {
        "gT": gT, "wup0": tile_wup(ffn_w_up[0, 0]), "wdn0": tile_wdn(ffn_w_down[0, 0]),
        "wq": tile_w_fm(wi[:, 0:D]), "wk": tile_w_fm(wi[:, D:2 * D]), "wg": tile_w_fm(wi[:, 3 * D:4 * D]),
        "wv": tile_w_tm(wi[:, 2 * D:3 * D]),
        "wf": np.ascontiguousarray(wi[:, 4 * D:4 * D + 16].reshape(8, 128, 16).transpose(1, 0, 2)).reshape(128, 128),
        "bf": np.ascontiguousarray(fox_b_f[0].reshape(16, 1)),
    }
    maps = []
    for (b, t) in cores:
        m = dict(shared)
        m["xT"] = np.ascontiguousarray(x[b, t * T:(t + 1) * T, :].T)
        m["modT"] = modT[b]
        maps.append(m)
    r1 = _run(nc1, maps)
    _DBG["r1"] = r1

    def cat_fm(res, name, b):
        return np.concatenate([res[b * 4 + t][name] for t in range(4)], axis=1)

    def cat_tm(res, name, b):
        return np.concatenate([res[b * 4 + t][name] for t in range(4)], axis=0)

    nc2 = build_fox()
    U, sel, mk = fox_consts()
    maps = []
    for b in range(B):
        qf = cat_fm(r1, "qT", b).reshape(FH, FD, S)
        kf = cat_fm(r1, "kT", b).reshape(FH, FD, S)
        vf = cat_tm(r1, "v", b)
        lf = cat_fm(r1, "lf", b)
        for g in range(4):
            l4 = lf[4 * g:4 * g + 4]
            v4 = np.stack([np.ascontiguousarray(vf[:, hd * FD:(hd + 1) * FD].reshape(64, 128, FD).transpose(1, 0, 2)).reshape(128, 64 * FD)
                           for hd in range(4 * g, 4 * g + 4)])
            maps.append({
                "q": np.ascontiguousarray(qf[4 * g:4 * g + 4]), "k": np.ascontiguousarray(kf[4 * g:4 * g + 4]), "v": v4,
                "lt": np.ascontiguousarray(l4.reshape(4, 64, 128).transpose(2, 0, 1)).reshape(128, 256),
                "lq": np.ascontiguousarray(l4.reshape(4, 16, 512).transpose(1, 0, 2)).reshape(16, 2048),
                "U": U, "sel": sel, "mk": mk,
            })
    r2 = _run(nc2, maps)
    _DBG["r2"] = r2
    ofull = [np.concatenate([r2[b * 4 + g]["o"].reshape(4 * FD, S) for g in range(4)], axis=0) for b in range(B)]

    nc3 = build_F({"epi": ("fox", 0), "ffns": [(0, 2), (1, 0)], "proj": ("hgrn", 1)})
    hi = hgrn_w_in[0]
    shared = {
        "gT": gT, "wo": tile_wo(fox_w_out[0]),
        "wup0": tile_wup(ffn_w_up[0, 1]), "wdn0": tile_wdn(ffn_w_down[0, 1]),
        "wup1": tile_wup(ffn_w_up[1, 0]), "wdn1": tile_wdn(ffn_w_down[1, 0]),
        "wq": tile_w_fm(hi[:, 0:D]), "wf": tile_w_fm(hi[:, D:2 * D]), "wg": tile_w_fm(hi[:, 3 * D:4 * D]),
        "wv": tile_w_tm(hi[:, 2 * D:3 * D]), "lbl": fm_cols(hgrn_lb_logits),
    }
    maps = []
    for i, (b, t) in enumerate(cores):
        m = dict(shared)
        m["xT"] = r1[i]["xo"]
        m["modT"] = modT[b]
        m["oT"] = np.ascontiguousarray(ofull[b][:, t * T:(t + 1) * T])
        m["sg"] = r1[i]["sgo"]
        maps.append(m)
    r3 = _run(nc3, maps)
    _DBG["r3"] = r3

    nc4 = build_hgrn()
    m01, rm, ident = hgrn_consts()
    maps = []
    for b in range(B):
        qf = cat_fm(r3, "qT", b).reshape(HH, 128, S)
        kf = cat_fm(r3, "kT", b).reshape(HH, 128, S)
        lf = cat_fm(r3, "lfT", b).reshape(HH, 128, S)
        vf = cat_tm(r3, "v", b)
        for g in range(4):
            v2 = np.stack([np.ascontiguousarray(vf[:, hd * 128:(hd + 1) * 128].reshape(64, 128, 128).transpose(1, 0, 2)).reshape(128, 64 * 128)
                           for hd in range(2 * g, 2 * g + 2)])
            maps.append({
                "q": np.ascontiguousarray(qf[2 * g:2 * g + 2]), "k": np.ascontiguousarray(kf[2 * g:2 * g + 2]),
                "lf": np.ascontiguousarray(lf[2 * g:2 * g + 2]), "v": v2, "m01": m01, "rm": rm, "ident": ident,
            })
    r4 = _run(nc4, maps)
    _DBG["r4"] = r4
    ofull = [np.concatenate([r4[b * 4 + g]["o"].reshape(256, S) for g in range(4)], axis=0) for b in range(B)]

    nc5 = build_F({"epi": ("hgrn", 1), "ffns": [(1, 2)], "final": True})
    shared = {
        "gT": gT, "wo": tile_wo(hgrn_w_out[0]), "hgn": fm_cols(hgrn_norm_g[0]),
        "wup0": tile_wup(ffn_w_up[1, 1]), "wdn0": tile_wdn(ffn_w_down[1, 1]),
        "fg": fm_cols(final_norm_g),
    }
    maps = []
    for i, (b, t) in enumerate(cores):
        m = dict(shared)
        m["xT"] = r3[i]["xo"]
        m["modT"] = modT[b]
        m["oT"] = np.ascontiguousarray(ofull[b][:, t * T:(t + 1) * T])
        m["sg"] = r3[i]["sgo"]
        maps.append(m)
    r5 = _run(nc5, maps)
    out = np.empty((B, S, D), np.float32)
    for i, (b, t) in enumerate(cores):
        out[b, t * T:(t + 1) * T, :] = r5[i]["xo"].T
    return out


def kernel(x, c, ada_w, ada_b, norm_g, ffn_w_up, ffn_w_down, fox_w_in, fox_b_f, fox_w_out,
           hgrn_w_in, hgrn_norm_g, hgrn_w_out, hgrn_lb_logits, final_norm_g):
    f32 = lambda a: np.ascontiguousarray(np.asarray(a, dtype=np.float32))
    x, c, ada_w, ada_b, norm_g = f32(x), f32(c), f32(ada_w), f32(ada_b), f32(norm_g)
    ffn_w_up, ffn_w_down, fox_w_in, fox_b_f, fox_w_out = f32(ffn_w_up), f32(ffn_w_down), f32(fox_w_in), f32(fox_b_f), f32(fox_w_out)
    hgrn_w_in, hgrn_norm_g, hgrn_w_out = f32(hgrn_w_in), f32(hgrn_norm_g), f32(hgrn_w_out)
    hgrn_lb_logits, final_norm_g = f32(hgrn_lb_logits), f32(final_norm_g)
    nc = build_mega()
    wi, hi = fox_w_in[0], hgrn_w_in[0]
    U, sel, mk = fox_consts()
    m01, rm, ident = hgrn_consts()
    shared = {
        "modb": fm_cols(ada_b.reshape(18, D)),
        "modw": np.ascontiguousarray(ada_w.reshape(2, 8, 128, 9, D).transpose(0, 3, 2, 1, 4)).reshape(18, 128, 8 * D),
        "gT": fm_cols(norm_g.reshape(6, D)),
        "wup0": tile_wup(ffn_w_up[0, 0]), "wdn0": tile_wdn(ffn_w_down[0, 0]),
        "wup1": tile_wup(ffn_w_up[0, 1]), "wdn1": tile_wdn(ffn_w_down[0, 1]),
        "wup2": tile_wup(ffn_w_up[1, 0]), "wdn2": tile_wdn(ffn_w_down[1, 0]),
        "wup3": tile_wup(ffn_w_up[1, 1]), "wdn3": tile_wdn(ffn_w_down[1, 1]),
        "fox_wq": tile_w_fm(wi[:, 0:D]), "fox_wk": tile_w_fm(wi[:, D:2 * D]), "fox_wg": tile_w_fm(wi[:, 3 * D:4 * D]),
        "fox_wv": tile_w_tm(wi[:, 2 * D:3 * D]),
        "fox_wf": np.ascontiguousarray(wi[:, 4 * D:4 * D + 16].reshape(8, 128, 16).transpose(1, 0, 2)).reshape(128, 128),
        "fox_bfb": np.ascontiguousarray(np.broadcast_to(np.tile(fox_b_f[0], 16), (128, 256))),
        "U": U, "sel": sel, "mk": mk, "identf": np.eye(128, dtype=np.float32),
        "fox_wo": tile_wo(fox_w_out[0]),
        "hgrn_wq": tile_w_fm(hi[:, 0:D]), "hgrn_wf": tile_w_fm(hi[:, D:2 * D]), "hgrn_wg": tile_w_fm(hi[:, 3 * D:4 * D]),
        "hgrn_wv": tile_w_tm(hi[:, 2 * D:3 * D]), "lbl": fm_cols(hgrn_lb_logits),
        "m01": m01, "rm": rm, "ident": ident,
        "hgrn_wo": tile_wo(hgrn_w_out[0]), "hgn": fm_cols(hgrn_norm_g[0]),
        "fg": fm_cols(final_norm_g),
    }
    maps = []
    cores = [(b, t) for b in range(B) for t in range(4)]
    for (b, t) in cores:
        m = dict(shared)
        m["xT"] = np.ascontiguousarray(x[b, t * T:(t + 1) * T, :].T)
        m["cT"] = fm_cols(c[b])
        maps.append(m)
    res = _run(nc, maps)
    out = np.empty((B, S, D), np.float32)
    for i, (b, t) in enumerate(cores):
        out[b, t * T:(t + 1) * T, :] = res[i]["xo"].T
    return out
```

```python
from contextlib import ExitStack
import numpy as np
import ml_dtypes
import concourse.bass as bass
import concourse.mybir as mybir
from concourse.bass_utils import run_bass_kernel_spmd

F32 = mybir.dt.float32
BF16 = mybir.dt.bfloat16
AF = mybir.ActivationFunctionType
ALU = mybir.AluOpType
NPBF = ml_dtypes.bfloat16

D = 1024
B = 2
S = 8192
DFF = 2816
NF = 22
EPS = 1e-6
NCORES = 8
T = 2048
FH = 16
FD = 64
HH = 8
CH = 64


class Buf:
    __slots__ = ("w", "r", "t", "key")

    def __init__(self, t=None):
        self.w = None
        self.r = []
        self.t = t
        self.key = None


class Prog:
    ENG = ["pe", "act", "dve", "pool", "sp"]

    def __init__(self, nc):
        self.nc = nc
        self.ops = {e: [] for e in self.ENG}
        self.clock = {e: {} for e in self.ENG}
        self.snaps = {}
        self.count = {}
        self.needed = set()
        self.nkey = 0
        self.final = None
        self.unit_keys = set()

    def sb(self, es, name, shape, dtype):
        self.nname = getattr(self, "nname", 0) + 1
        t = es.enter_context(self.nc.sbuf_tensor("%s_u%d" % (name, self.nname), list(shape), dtype))
        return Buf(t)

    def ps(self, es, name, shape, dtype):
        self.nname = getattr(self, "nname", 0) + 1
        t = es.enter_context(self.nc.psum_tensor("%s_u%d" % (name, self.nname), list(shape), dtype))
        return Buf(t)

    def newkey(self, buf):
        fk = getattr(self, "free_keys", None)
        if fk is None:
            self.free_keys, self.key_owner = [], {}
            fk = self.free_keys
        if fk:
            k = fk.pop()
        else:
            self.nkey += 1
            k = "d%d" % self.nkey
        buf.key = k
        self.key_owner[k] = buf
        return k

    def op(self, eng, fn, reads=(), writes=(), dma=None, pe_acc=False):
        need = {}

        def req(ev):
            if ev is None:
                return
            k, s = ev
            if s > need.get(k, 0):
                need[k] = s

        for b in reads:
            req(b.w)
        for b in writes:
            if not (pe_acc and b.w is not None and b.w[0] == "pe"):
                req(b.w)
            for r in b.r:
                req(r)
        key = dma or eng
        if fn is None:
            idx = 0
        else:
            idx = self.count.get(key, 0) + 1
            self.count[key] = idx
        ck = self.clock[eng]
        waits = []
        for k, s in need.items():
            if ck.get(k, 0) < s:
                waits.append((k, s))
        for k, s in waits:
            sn = self.snaps[(k, s)]
            for kk, ss in sn.items():
                if ck.get(kk, 0) < ss:
                    ck[kk] = ss
            if ck.get(k, 0) < s:
                ck[k] = s
            self.needed.add((k, s))
        self.ops[eng].append((fn, waits, key, idx))
        if fn is None:
            return None
        self.snaps[(key, idx)] = dict(ck)
        ev = (key, idx)
        for b in reads:
            b.r.append(ev)
        for b in writes:
            b.w = ev
            b.r = []
        return ev

    def dma(self, eng, out, in_, reads=(), writes=(), ow=None):
        wb = ow if ow is not None else writes[0]
        if wb.key is None:
            self.newkey(wb)
        def _fn(e, out=out, in_=in_):
            try:
                return e.dma_start(out=out, in_=in_)
            except Exception:
                print("DMA FAIL", out, in_)
                raise
        ev = self.op(eng, _fn, reads=reads, writes=writes, dma=wb.key)
        if ow is not None:
            ow.w = ev
        return ev

    def collective(self, kind, groups, src_ap, dst_ap, reads, wbuf):
        wbuf.key = "cc"
        self.unit_keys.add(wbuf.key)
        return self.op("pool", lambda e: e.collective_compute(kind, ALU.bypass, replica_groups=groups,
                                                              ins=[src_ap], outs=[dst_ap]),
                       reads=reads, writes=[wbuf], dma=wbuf.key)

    def finish(self, outs, eng="sp"):
        self.op(eng, None, reads=list(outs))

    def barrier(self):
        need = dict(self.count)
        for e in self.ENG:
            self.op(e, None, extra=need)
        for k, b in list(getattr(self, "key_owner", {}).items()):
            b.key = None
            self.free_keys.append(k)
        if hasattr(self, "key_owner"):
            self.key_owner.clear()

    def emit(self):
        nc = self.nc
        keys = list(self.count.keys())
        for e in self.ENG:
            if e not in keys:
                keys.append(e)
        rank = {}
        for k in keys:
            if k in self.ENG:
                idxs = sorted(s for (kk, s) in self.needed if kk == k)
                rank[k] = {s: i + 1 for i, s in enumerate(idxs)}
        with ExitStack() as es:
            sems = {k: es.enter_context(nc.semaphore("s_" + k)) for k in keys}
            block = es.enter_context(nc.Block())

            def run(eng_name):
                def body(e):
                    for fn, waits, key, idx in self.ops[eng_name]:
                        for k, s in waits:
                            v = rank[k][s] if k in rank else (s if k in self.unit_keys else 16 * s)
                            e.wait_ge(sems[k], v)
                        if fn is None:
                            continue
                        ins = fn(e)
                        if key in rank:
                            if (key, idx) in self.needed:
                                ins.then_inc(sems[key], 1)
                        elif key in self.unit_keys:
                            ins.then_inc(sems[key], 1)
                        else:
                            ins.then_inc(sems[key], 16)
                return body

            block.tensor(run("pe"))
            block.scalar(run("act"))
            block.vector(run("dve"))
            block.gpsimd(run("pool"))
            block.sync(run("sp"))


def _patch_op():
    base = Prog.op

    def op(self, eng, fn, reads=(), writes=(), dma=None, pe_acc=False, extra=None):
        if extra:
            dummy = []
            for k, s in extra.items():
                if s > 0:
                    b = Buf()
                    b.w = (k, s)
                    dummy.append(b)
            reads = list(reads) + dummy
            ev = base(self, eng, fn, reads=reads, writes=writes, dma=dma, pe_acc=pe_acc)
            return ev
        return base(self, eng, fn, reads=reads, writes=writes, dma=dma, pe_acc=pe_acc)

    Prog.op = op


_patch_op()


SQD = float(np.sqrt(D))


class Ctx:
    pass


def mcol(l, v, ch):
    return (l * 9 + v) * 8 + ch


def build_F(cfg):
    nc = bass.Bass("TRN2", target_bir_lowering=False)
    P = Prog(nc)
    c = Ctx()
    c.P, c.nc = P, nc
    dr = {}

    def din(name, shape, dt=F32):
        dr[name] = nc.dram_tensor(name, list(shape), dt, kind="ExternalInput").ap()
        return dr[name]

    def dout(name, shape, dt=F32):
        dr[name] = nc.dram_tensor(name, list(shape), dt, kind="ExternalOutput").ap()
        return dr[name]

    xT = din("xT", [D, T])
    modT = din("modT", [128, 144])
    gT = din("gT", [128, 48])
    outs = []
    with ExitStack() as es:
        X = es.enter_context(nc.sbuf_tensor("X", [128, 8, T], F32))
        c.X = X
        c.Xb = [[Buf(X) for _ in range(4)] for _ in range(8)]
        c.modt = P.sb(es, "modt", [128, 144], F32)
        c.gt = P.sb(es, "gt", [128, 48], F32)
        c.der = P.sb(es, "der", [128, 96], F32)
        c.ones = P.sb(es, "ones", [128, 128], BF16)
        c.bank = [P.ps(es, "bank%d" % i, [128, 512], F32) for i in range(8)]
        sqt = es.enter_context(nc.sbuf_tensor("sq", [128, 8, 512], BF16))
        c.sq = [Buf(sqt) for _ in range(8)]
        c.rstd = P.sb(es, "rstd", [128, 512], F32)
        c.tmp = [P.sb(es, "tmp%d" % i, [128, 512], F32) for i in range(2)]

        xv = xT.rearrange("(c p) t -> p c t", p=128)
        for kc in range(8):
            P.dma("sp", X[:, kc, :], xv[:, kc, :], writes=[c.Xb[kc][tt] for tt in range(4)])
        P.dma("sp", c.modt.t[:, :], modT, writes=[c.modt])
        P.dma("sp", c.gt.t[:, :], gT, writes=[c.gt])
        P.op("pool", lambda e: e.memset(c.ones.t[:, :], 1.0), writes=[c.ones])
        for l in range(2):
            for sub in range(3):
                base = ((l * 3 + sub) * 2) * 8
                sc0 = mcol(l, sub * 3 + 1, 0)
                g0 = (l * 3 + sub) * 8
                ga0 = mcol(l, sub * 3 + 2, 0)
                P.op("dve", lambda e, base=base, sc0=sc0, g0=g0: e.scalar_tensor_tensor(
                    c.der.t[:, base:base + 8], c.modt.t[:, sc0:sc0 + 8], 1.0, c.gt.t[:, g0:g0 + 8], ALU.add, ALU.mult),
                    reads=[c.modt, c.gt], writes=[c.der])
                P.op("dve", lambda e, base=base: e.tensor_scalar(
                    c.der.t[:, base:base + 8], c.der.t[:, base:base + 8], SQD, None, ALU.mult),
                    reads=[c.der], writes=[c.der])
                P.op("dve", lambda e, base=base, ga0=ga0, sub=sub: e.tensor_scalar(
                    c.der.t[:, base + 8:base + 16], c.modt.t[:, ga0:ga0 + 8], (1.0 if sub == 1 else 0.5), None, ALU.mult),
                    reads=[c.modt], writes=[c.der])

        def Acol(l, sub, ch):
            j = ((l * 3 + sub) * 2) * 8 + ch
            return c.der.t[:, j:j + 1]

        def Gcol(l, sub, ch):
            j = ((l * 3 + sub) * 2 + 1) * 8 + ch
            return c.der.t[:, j:j + 1]

        def Scol(l, sub, ch):
            j = mcol(l, sub * 3 + 0, ch)
            return c.modt.t[:, j:j + 1]

        def rstd_tile(src_fn, src_bufs, epsk):
            for kc in range(8):
                P.op("act", lambda e, kc=kc: e.activation(sqt[:, kc, :], src_fn(kc), AF.Square),
                     reads=[src_bufs[kc]], writes=[c.sq[kc]])
            for kc in range(8):
                P.op("pe", lambda e, kc=kc: e.matmul(c.bank[6].t[:, :], c.ones.t[:, :], sqt[:, kc, :],
                                                      start=(kc == 0), stop=(kc == 7)),
                     reads=[c.ones, c.sq[kc]], writes=[c.bank[6]], pe_acc=(kc > 0))
            P.op("dve", lambda e: e.tensor_scalar(c.rstd.t[:, :], c.bank[6].t[:, :], epsk, None, ALU.add),
                 reads=[c.bank[6]], writes=[c.rstd])
            P.op("act", lambda e: e.activation(c.rstd.t[:, :], c.rstd.t[:, :], AF.Sqrt), reads=[c.rstd], writes=[c.rstd])
            P.op("dve", lambda e: e.reciprocal(c.rstd.t[:, :], c.rstd.t[:, :]), reads=[c.rstd], writes=[c.rstd])

        def modnorm_tile(l, sub, tt, hdst, hbuf):
            t0 = tt * 512
            rstd_tile(lambda kc: X[:, kc, t0:t0 + 512], [c.Xb[kc][tt] for kc in range(8)], EPS * D)
            for kc in range(8):
                tb = c.tmp[kc % 2]
                P.op("dve", lambda e, kc=kc, tb=tb: e.tensor_tensor(tb.t[:, :], X[:, kc, t0:t0 + 512], c.rstd.t[:, :], ALU.mult),
                     reads=[c.Xb[kc][tt], c.rstd], writes=[tb])
                P.op("act", lambda e, kc=kc, tb=tb: e.activation(hdst(kc), tb.t[:, :], AF.Identity,
                                                               bias=Scol(l, sub, kc), scale=Acol(l, sub, kc)),
                     reads=[tb, c.der, c.modt], writes=[hbuf(kc)])

        def epilogue(kind, l):
            oT = din("oT", [D, T])
            sgd = din("sg", [D, T], BF16)
            wod = din("wo", [128, 8192])
            ov = oT.rearrange("(c p) t -> p c t", p=128)
            sv = sgd.rearrange("(c p) t -> p c t", p=128)
            with ExitStack() as e2:
                wo = P.sb(e2, "wo_sb", [128, 8, 1024], BF16)
                P.dma("pool", wo.t[:, :, :], wod.rearrange("p (k d) -> p k d", k=8), writes=[wo])
                ot = [P.sb(e2, "ot%d" % i, [128, 8, 512], F32) for i in range(2)]
                st = [P.sb(e2, "st%d" % i, [128, 8, 512], BF16) for i in range(2)]
                ogt = [e2.enter_context(nc.sbuf_tensor("og%d" % i, [128, 8, 512], BF16)) for i in range(2)]
                ogb = [[Buf(ogt[i]) for _ in range(8)] for i in range(2)]
                if kind == "hgrn":
                    hgd = din("hgn", [128, 8])
                    hg = P.sb(e2, "hg", [128, 8], F32)
                    P.dma("sp", hg.t[:, :], hgd, writes=[hg])
                    P.op("dve", lambda e: e.tensor_scalar(hg.t[:, :], hg.t[:, :], float(np.sqrt(128.0)), None, ALU.mult),
                         reads=[hg], writes=[hg])
                    sq1 = P.sb(e2, "sq1", [128, 512], BF16)
                    r1 = P.sb(e2, "r1", [128, 512], F32)
                    t1 = P.sb(e2, "t1", [128, 512], F32)
                for tt in range(4):
                    t0 = tt * 512
                    o_, s_, og_ = ot[tt % 2], st[tt % 2], ogt[tt % 2]
                    P.dma("sp", o_.t[:, :, :], ov[:, :, t0:t0 + 512], writes=[o_])
                    P.dma("sp", s_.t[:, :, :], sv[:, :, t0:t0 + 512], writes=[s_])
                    if kind == "fox":
                        for kc in range(8):
                            P.op("dve", lambda e, kc=kc, o_=o_, s_=s_, og_=og_: e.tensor_tensor(
                                og_[:, kc, :], o_.t[:, kc, :], s_.t[:, kc, :], ALU.mult),
                                reads=[o_, s_], writes=[ogb[tt % 2][kc]])
                    else:
                        for kc in range(8):
                            P.op("act", lambda e, kc=kc, o_=o_: e.activation(sq1.t[:, :], o_.t[:, kc, :], AF.Square),
                                 reads=[o_], writes=[sq1])
                            P.op("pe", lambda e: e.matmul(c.bank[7].t[:, :], c.ones.t[:, :], sq1.t[:, :], start=True, stop=True),
                                 reads=[c.ones, sq1], writes=[c.bank[7]])
                            P.op("dve", lambda e: e.tensor_scalar(r1.t[:, :], c.bank[7].t[:, :], EPS * 128.0, None, ALU.add),
                                 reads=[c.bank[7]], writes=[r1])
                            P.op("act", lambda e: e.activation(r1.t[:, :], r1.t[:, :], AF.Sqrt), reads=[r1], writes=[r1])
                            P.op("dve", lambda e: e.reciprocal(r1.t[:, :], r1.t[:, :]), reads=[r1], writes=[r1])
                            P.op("dve", lambda e, kc=kc, o_=o_: e.tensor_tensor(t1.t[:, :], o_.t[:, kc, :], r1.t[:, :], ALU.mult),
                                 reads=[o_, r1], writes=[t1])
                            P.op("dve", lambda e, kc=kc, s_=s_, og_=og_: e.scalar_tensor_tensor(
                                og_[:, kc, :], t1.t[:, :], hg.t[:, kc:kc + 1], s_.t[:, kc, :], ALU.mult, ALU.mult),
                                reads=[t1, hg, s_], writes=[ogb[tt % 2][kc]])
                    for dc in range(8):
                        bk = c.bank[4 + dc % 2]
                        for kc in range(8):
                            P.op("pe", lambda e, kc=kc, dc=dc, bk=bk, og_=og_: e.matmul(
                                bk.t[:, :], wo.t[:, kc, dc * 128:(dc + 1) * 128], og_[:, kc, :],
                                start=(kc == 0), stop=(kc == 7)),
                                reads=[wo, ogb[tt % 2][kc]], writes=[bk], pe_acc=(kc > 0))
                        P.op("dve", lambda e, dc=dc, bk=bk, t0=t0: e.scalar_tensor_tensor(
                            X[:, dc, t0:t0 + 512], bk.t[:, :], Gcol(l, 1, dc), X[:, dc, t0:t0 + 512], ALU.mult, ALU.add),
                            reads=[bk, c.der, c.Xb[dc][tt]], writes=[c.Xb[dc][tt]])
            P.barrier()

        def ffn(j, l, sub):
            wupd = din("wup%d" % j, [11, 128, 4096])
            wdnd = din("wdn%d" % j, [8, 128, 2816])
            with ExitStack() as e2:
                hbt = e2.enter_context(nc.sbuf_tensor("hb_%d" % j, [128, 8, 1024], BF16))
                hbb = [[Buf(hbt) for _ in range(2)] for _ in range(8)]
                actt = e2.enter_context(nc.sbuf_tensor("actb_%d" % j, [128, NF, 1024], BF16))
                actb = [[Buf(actt) for _ in range(2)] for _ in range(NF)]
                wu = [P.sb(e2, "wu%d_%d" % (j, i), [128, 2, 8, 256], BF16) for i in range(2)]
                wd = [P.sb(e2, "wd%d_%d" % (j, i), [128, NF, 128], BF16) for i in range(2)]
                sa = [P.sb(e2, "sa%d_%d" % (j, i), [128, 512], F32) for i in range(2)]
                for half in range(2):
                    for t2 in range(2):
                        tt = half * 2 + t2
                        modnorm_tile(l, sub, tt, lambda kc, t2=t2: hbt[:, kc, t2 * 512:(t2 + 1) * 512],
                                     lambda kc, t2=t2: hbb[kc][t2])
                    it = 0
                    for g in range(11):
                        w_ = wu[g % 2]
                        P.dma("pool", w_.t[:, :, :, :], wupd[g].rearrange("p (a k f) -> p a k f", a=2, k=8), writes=[w_])
                        for jf in range(2):
                            fc = 2 * g + jf
                            for t2 in range(2):
                                bA, bB = c.bank[it % 2], c.bank[2 + it % 2]
                                s_ = sa[it % 2]
                                it += 1
                                for kc in range(8):
                                    P.op("pe", lambda e, kc=kc, w_=w_, jf=jf, t2=t2, bA=bA: e.matmul(
                                        bA.t[:, :], w_.t[:, 0, kc, jf * 128:(jf + 1) * 128], hbt[:, kc, t2 * 512:(t2 + 1) * 512],
                                        start=(kc == 0), stop=(kc == 7)),
                                        reads=[w_, hbb[kc][t2]], writes=[bA], pe_acc=(kc > 0))
                                for kc in range(8):
                                    P.op("pe", lambda e, kc=kc, w_=w_, jf=jf, t2=t2, bB=bB: e.matmul(
                                        bB.t[:, :], w_.t[:, 1, kc, jf * 128:(jf + 1) * 128], hbt[:, kc, t2 * 512:(t2 + 1) * 512],
                                        start=(kc == 0), stop=(kc == 7)),
                                        reads=[w_, hbb[kc][t2]], writes=[bB], pe_acc=(kc > 0))
                                P.op("act", lambda e, s_=s_, bA=bA: e.activation(s_.t[:, :], bA.t[:, :], AF.Silu),
                                     reads=[bA], writes=[s_])
                                P.op("dve", lambda e, s_=s_, bB=bB, fc=fc, t2=t2: e.tensor_tensor(
                                    actt[:, fc, t2 * 512:(t2 + 1) * 512], bB.t[:, :], s_.t[:, :], ALU.mult),
                                    reads=[bB, s_], writes=[actb[fc][t2]])
                    for dc in range(8):
                        w_ = wd[dc % 2]
                        P.dma("pool", w_.t[:, :, :], wdnd[dc].rearrange("p (f d) -> p f d", f=NF), writes=[w_])
                        for t2 in range(2):
                            tt = half * 2 + t2
                            t0 = tt * 512
                            bk = c.bank[4 + (dc * 2 + t2) % 2]
                            for fc in range(NF):
                                P.op("pe", lambda e, fc=fc, w_=w_, t2=t2, bk=bk: e.matmul(
                                    bk.t[:, :], w_.t[:, fc, :], actt[:, fc, t2 * 512:(t2 + 1) * 512],
                                    start=(fc == 0), stop=(fc == NF - 1)),
                                    reads=[w_, actb[fc][t2]], writes=[bk], pe_acc=(fc > 0))
                            P.op("dve", lambda e, dc=dc, bk=bk, t0=t0: e.scalar_tensor_tensor(
                                X[:, dc, t0:t0 + 512], bk.t[:, :], Gcol(l, sub, dc), X[:, dc, t0:t0 + 512], ALU.mult, ALU.add),
                                reads=[bk, c.der, c.Xb[dc][tt]], writes=[c.Xb[dc][tt]])
            P.barrier()

        def proj_fm(wname, hbt, hbb, evac, n_oc=8):
            wd_ = din(wname, [n_oc, 128, 1024])
            with ExitStack() as e3:
                wp = [P.sb(e3, wname + "_sb%d" % i, [128, 8, 128], BF16) for i in range(2)]
                it = 0
                for oc in range(n_oc):
                    w_ = wp[oc % 2]
                    P.dma("pool", w_.t[:, :, :], wd_[oc].rearrange("p (k f) -> p k f", k=8), writes=[w_])
                    for tt in range(4):
                        bk = c.bank[it % 4]
                        it += 1
                        for kc in range(8):
                            P.op("pe", lambda e, kc=kc, w_=w_, tt=tt, bk=bk: e.matmul(
                                bk.t[:, :], w_.t[:, kc, :], hbt[:, kc, tt * 512:(tt + 1) * 512],
                                start=(kc == 0), stop=(kc == 7)),
                                reads=[w_, hbb[kc][tt]], writes=[bk], pe_acc=(kc > 0))
                        evac(oc, tt, bk)
                P.barrier()

        def proj_tm(wname, hbt, hbb, vout, vob, func):
            wd_ = din(wname, [2, 128, 4096])
            with ExitStack() as e3:
                wv = P.sb(e3, wname + "_sb", [128, 2, 8, 512], BF16)
                for cg in range(2):
                    P.dma("pool", wv.t[:, cg, :, :], wd_[cg].rearrange("p (k f) -> p k f", k=8), writes=[wv])
                vt = [P.sb(e3, "vt%d" % i, [128, 512], BF16) for i in range(2)]
                it = 0
                for tk in range(16):
                    for cg in range(2):
                        bk = c.bank[it % 4]
                        v_ = vt[it % 2]
                        it += 1
                        for kc in range(8):
                            P.op("pe", lambda e, kc=kc, tk=tk, cg=cg, bk=bk: e.matmul(
                                bk.t[:, :], hbt[:, kc, tk * 128:(tk + 1) * 128], wv.t[:, cg, kc, :],
                                start=(kc == 0), stop=(kc == 7)),
                                reads=[wv, hbb[kc][tk // 4]], writes=[bk], pe_acc=(kc > 0))
                        P.op("act", lambda e, bk=bk, v_=v_: e.activation(v_.t[:, :], bk.t[:, :], func),
                             reads=[bk], writes=[v_])
                        P.dma("sp", vout[tk * 128:(tk + 1) * 128, cg * 512:(cg + 1) * 512], v_.t[:, :], reads=[v_], ow=vob)
                P.barrier()

        def stage_out(e3, name, shape, dt):
            return [P.sb(e3, name + "%d" % i, shape, dt) for i in range(2)]

        def projections(kind, l):
            with ExitStack() as e2:
                hbt = e2.enter_context(nc.sbuf_tensor("hb2", [128, 8, T], BF16))
                hbb = [[Buf(hbt) for _ in range(4)] for _ in range(8)]
                for tt in range(4):
                    modnorm_tile(l, 1, tt, lambda kc, tt=tt: hbt[:, kc, tt * 512:(tt + 1) * 512],
                                 lambda kc, tt=tt: hbb[kc][tt])
                qo = dout("qT", [D, T], BF16)
                qob = Buf()
                outs.append(qob)
                sgo = dout("sgo", [D, T], BF16)
                sgob = Buf()
                outs.append(sgob)
                vo = dout("v", [T, D], BF16)
                vob = Buf()
                outs.append(vob)
                cnt = [0]

                def simple_evac(od, ob, func, scale, st, dt_eng="act"):
                    def evac(oc, tt, bk):
                        s_ = st[cnt[0] % 2]
                        cnt[0] += 1
                        P.op("act", lambda e, s_=s_, bk=bk: e.activation(s_.t[:, :], bk.t[:, :], func, scale=scale),
                             reads=[bk], writes=[s_])
                        P.dma("sp", od[oc * 128:(oc + 1) * 128, tt * 512:(tt + 1) * 512], s_.t[:, :], reads=[s_], ow=ob)
                    return evac

                stb = stage_out(e2, "stb", [128, 512], BF16)
                if kind == "fox":
                    ko = dout("kT", [D, T], BF16)
                    kob = Buf()
                    outs.append(kob)
                    lfo = dout("lf", [16, T], F32)
                    lfob = Buf()
                    outs.append(lfob)
                    proj_fm("wq", hbt, hbb, simple_evac(qo, qob, AF.Copy, float(FD ** -0.5), stb))
                    proj_fm("wk", hbt, hbb, simple_evac(ko, kob, AF.Copy, 1.0, stb))
                    proj_fm("wg", hbt, hbb, simple_evac(sgo, sgob, AF.Sigmoid, 1.0, stb))
                    proj_tm("wv", hbt, hbb, vo, vob, AF.Copy)
                    wfd = din("wf", [128, 128])
                    bfd = din("bf", [16, 1])
                    wf = P.sb(e2, "wf_sb", [128, 8, 16], BF16)
                    P.dma("pool", wf.t[:, :, :], wfd.rearrange("p (k f) -> p k f", k=8), writes=[wf])
                    nbf = P.sb(e2, "nbf", [16, 1], F32)
                    P.dma("sp", nbf.t[:, :], bfd, writes=[nbf])
                    P.op("dve", lambda e: e.tensor_scalar(nbf.t[:, :], nbf.t[:, :], -1.0, None, ALU.mult), reads=[nbf], writes=[nbf])
                    e1 = P.sb(e2, "e1", [16, 512], F32)
                    l1 = [P.sb(e2, "l1_%d" % i, [16, 512], F32) for i in range(2)]
                    for tt in range(4):
                        bk = c.bank[tt % 4]
                        for kc in range(8):
                            P.op("pe", lambda e, kc=kc, tt=tt, bk=bk: e.matmul(
                                bk.t[0:16, :], wf.t[:, kc, :], hbt[:, kc, tt * 512:(tt + 1) * 512],
                                start=(kc == 0), stop=(kc == 7)),
                                reads=[wf, hbb[kc][tt]], writes=[bk], pe_acc=(kc > 0))
                        l_ = l1[tt % 2]
                        P.op("act", lambda e, bk=bk: e.activation(e1.t[:, :], bk.t[0:16, :], AF.Exp, bias=nbf.t[:, 0:1], scale=-1.0),
                             reads=[bk, nbf], writes=[e1])
                        P.op("act", lambda e, l_=l_: e.activation(l_.t[:, :], e1.t[:, :], AF.Ln, bias=1.0, scale=1.0),
                             reads=[e1], writes=[l_])
                        P.op("dve", lambda e, l_=l_: e.tensor_scalar(l_.t[:, :], l_.t[:, :], -1.0, None, ALU.mult),
                             reads=[l_], writes=[l_])
                        P.dma("sp", lfo[:, tt * 512:(tt + 1) * 512], l_.t[:, :], reads=[l_], ow=lfob)
                else:
                    ko = dout("kT", [D, T], F32)
                    kob = Buf()
                    outs.append(kob)
                    lfo = dout("lfT", [D, T], F32)
                    lfob = Buf()
                    outs.append(lfob)
                    lbd = din("lbl", [128, 16])
                    lbl = P.sb(e2, "lbl_sb", [128, 16], F32)
                    lb = P.sb(e2, "lb", [128, 8], F32)
                    oml = P.sb(e2, "oml", [128, 8], F32)
                    P.dma("sp", lbl.t[:, :], lbd, writes=[lbl])
                    P.op("dve", lambda e: e.tensor_tensor(lb.t[:, :], lbl.t[:, 8:16], lbl.t[:, 0:8], ALU.subtract), reads=[lbl], writes=[lb])
                    P.op("act", lambda e: e.activation(lb.t[:, :], lb.t[:, :], AF.Sigmoid), reads=[lb], writes=[lb])
                    P.op("dve", lambda e: e.tensor_scalar(oml.t[:, :], lb.t[:, :], -1.0, 1.0, ALU.mult, ALU.add), reads=[lb], writes=[oml])
                    proj_fm("wq", hbt, hbb, simple_evac(qo, qob, AF.Copy, 1.0, stb))
                    proj_fm("wg", hbt, hbb, simple_evac(sgo, sgob, AF.Silu, 1.0, stb))
                    proj_tm("wv", hbt, hbb, vo, vob, AF.Silu)
                    sg1 = P.sb(e2, "sg1", [128, 512], F32)
                    ff = stage_out(e2, "ff", [128, 512], F32)
                    lff = stage_out(e2, "lff", [128, 512], F32)
                    kk = stage_out(e2, "kk", [128, 512], F32)

                    def f_evac(oc, tt, bk):
                        i = cnt[0] % 2
                        cnt[0] += 1
                        f_, l_, k_ = ff[i], lff[i], kk[i]
                        P.op("act", lambda e, bk=bk: e.activation(sg1.t[:, :], bk.t[:, :], AF.Sigmoid), reads=[bk], writes=[sg1])
                        P.op("dve", lambda e, f_=f_, oc=oc: e.tensor_scalar(f_.t[:, :], sg1.t[:, :], oml.t[:, oc:oc + 1], lb.t[:, oc:oc + 1], ALU.mult, ALU.add),
                             reads=[sg1, oml, lb], writes=[f_])
                        P.op("act", lambda e, f_=f_, l_=l_: e.activation(l_.t[:, :], f_.t[:, :], AF.Ln), reads=[f_], writes=[l_])
                        P.op("dve", lambda e, f_=f_, k_=k_: e.tensor_scalar(k_.t[:, :], f_.t[:, :], -1.0, 1.0, ALU.mult, ALU.add),
                             reads=[f_], writes=[k_])
                        P.dma("sp", lfo[oc * 128:(oc + 1) * 128, tt * 512:(tt + 1) * 512], l_.t[:, :], reads=[l_], ow=lfob)
                        P.dma("sp", ko[oc * 128:(oc + 1) * 128, tt * 512:(tt + 1) * 512], k_.t[:, :], reads=[k_], ow=kob)
                    proj_fm("wf", hbt, hbb, f_evac)
            P.barrier()

        if cfg.get("epi"):
            epilogue(cfg["epi"][0], cfg["epi"][1])
        for j, (l, sub) in enumerate(cfg["ffns"]):
            ffn(j, l, sub)
        if cfg.get("proj"):
            projections(cfg["proj"][0], cfg["proj"][1])
        xo = dout("xo", [D, T])
        xob = Buf()
        outs.append(xob)
        xov = xo.rearrange("(c p) t -> p c t", p=128)
        if cfg.get("final"):
            fgd = din("fg", [128, 8])
            fg = P.sb(es, "fg_sb", [128, 8], F32)
            P.dma("sp", fg.t[:, :], fgd, writes=[fg])
            P.op("dve", lambda e: e.tensor_scalar(fg.t[:, :], fg.t[:, :], SQD, None, ALU.mult), reads=[fg], writes=[fg])
            yo = [P.sb(es, "yo%d" % i, [128, 512], F32) for i in range(2)]
            it = 0
            for tt in range(4):
                t0 = tt * 512
                rstd_tile(lambda kc, t0=t0: X[:, kc, t0:t0 + 512], [c.Xb[kc][tt] for kc in range(8)], EPS * D)
                for kc in range(8):
                    y_ = yo[it % 2]
                    it += 1
                    P.op("dve", lambda e, kc=kc, y_=y_, t0=t0: e.scalar_tensor_tensor(
                        y_.t[:, :], X[:, kc, t0:t0 + 512], fg.t[:, kc:kc + 1], c.rstd.t[:, :], ALU.mult, ALU.mult),
                        reads=[c.Xb[kc][tt], fg, c.rstd], writes=[y_])
                    P.dma("sp", xov[:, kc, t0:t0 + 512], y_.t[:, :], reads=[y_], ow=xob)
        else:
            for kc in range(8):
                P.dma("sp", xov[:, kc, :], X[:, kc, :], reads=[c.Xb[kc][tt] for tt in range(4)], ow=xob)
        P.finish(outs)
        P.emit()
    return nc


MC = 2304


def build_mod():
    nc = bass.Bass("TRN2", target_bir_lowering=False)
    P = Prog(nc)
    cT = nc.dram_tensor("cT", [128, 16], F32, kind="ExternalInput").ap()
    w = nc.dram_tensor("w", [128, 8 * MC], F32, kind="ExternalInput").ap()
    bias = nc.dram_tensor("bias", [2, MC], F32, kind="ExternalInput").ap()
    mo = nc.dram_tensor("mo", [2, MC], F32, kind="ExternalOutput").ap()
    with ExitStack() as es:
        ct = P.sb(es, "ct", [128, 8, 2], F32)
        wt = [P.sb(es, "wt%d" % i, [128, 8, 384], F32) for i in range(6)]
        bt = P.sb(es, "bt", [2, MC], F32)
        ot = P.sb(es, "ot", [2, MC], F32)
        banks = [P.ps(es, "bk%d" % i, [128, 512], F32) for i in range(2)]
        P.dma("sp", ct.t[:, :, :], cT.rearrange("p (k b) -> p k b", k=8), writes=[ct])
        P.dma("sp", bt.t[:, :], bias, writes=[bt])
        wv = w.rearrange("p (k n) -> p k n", k=8)
        for i in range(6):
            P.dma("sp" if i % 2 == 0 else "pool", wt[i].t[:, :, :], wv[:, :, i * 384:(i + 1) * 384], writes=[wt[i]])
        P.op("act", lambda e: e.activation(ct.t[:, :, :], ct.t[:, :, :], AF.Silu), reads=[ct], writes=[ct])
        for i in range(6):
            bk = banks[i % 2]
            for kc in range(8):
                P.op("pe", lambda e, kc=kc, i=i, bk=bk: e.matmul(bk.t[0:2, 0:384], ct.t[:, kc, :], wt[i].t[:, kc, :],
                                                                 start=(kc == 0), stop=(kc == 7)),
                     reads=[ct, wt[i]], writes=[bk], pe_acc=(kc > 0))
            P.op("dve", lambda e, i=i, bk=bk: e.tensor_tensor(ot.t[:, i * 384:(i + 1) * 384], bk.t[0:2, 0:384],
                                                             bt.t[:, i * 384:(i + 1) * 384], ALU.add),
                 reads=[bk, bt], writes=[ot])
        ob = Buf()
        P.dma("sp", mo, ot.t[:, :], reads=[ot], ow=ob)
        P.finish([ob])
        P.emit()
    return nc


def run_mod(c, ada_w, ada_b):
    nc = build_mod()
    cT = np.ascontiguousarray(c.T.reshape(8, 128, B).transpose(1, 0, 2)).reshape(128, 16)
    wall = np.concatenate([ada_w[0], ada_w[1]], axis=1)
    ball = np.concatenate([ada_b[0], ada_b[1]], axis=0)
    maps = []
    for j in range(NCORES):
        wj = wall[:, j * MC:(j + 1) * MC].reshape(8, 128, MC).transpose(1, 0, 2)
        maps.append({"cT": cT, "w": np.ascontiguousarray(wj).reshape(128, 8 * MC),
                     "bias": np.ascontiguousarray(np.broadcast_to(ball[j * MC:(j + 1) * MC], (2, MC)))})
    res = run_bass_kernel_spmd(nc, maps, core_ids=list(range(NCORES)))
    mod = np.concatenate([r["mo"] for r in res.results], axis=1)
    return mod.reshape(B, 2, 9, D)


def fm_cols(v):
    lead = int(np.prod(v.shape[:-1])) if v.ndim > 1 else 1
    a = v.reshape(lead, 8, 128).transpose(2, 0, 1)
    return np.ascontiguousarray(a).reshape(128, lead * 8)


def tile_w_fm(w):
    n = w.shape[1] // 128
    a = w.reshape(8, 128, n, 128).transpose(2, 1, 0, 3)
    return np.ascontiguousarray(a).reshape(n, 128, 1024)


def tile_w_tm(w):
    a = w.reshape(8, 128, 2, 512).transpose(2, 1, 0, 3)
    return np.ascontiguousarray(a).reshape(2, 128, 4096)


def tile_wup(w):
    a = w.reshape(8, 128, 2, 11, 256).transpose(3, 1, 2, 0, 4)
    return np.ascontiguousarray(a).reshape(11, 128, 4096)


def tile_wdn(w):
    a = w.reshape(NF, 128, 8, 128).transpose(2, 1, 0, 3)
    return np.ascontiguousarray(a).reshape(8, 128, NF * 128)


def tile_wo(w):
    a = w.reshape(8, 128, D).transpose(1, 0, 2)
    return np.ascontiguousarray(a).reshape(128, 8 * D)


NEG = -30000.0


def build_fox():
    nc = bass.Bass("TRN2", target_bir_lowering=False)
    P = Prog(nc)
    qd = nc.dram_tensor("q", [4, 64, S], BF16, kind="ExternalInput").ap()
    kd = nc.dram_tensor("k", [4, 64, S], BF16, kind="ExternalInput").ap()
    vd = nc.dram_tensor("v", [4, 128, 64 * 64], BF16, kind="ExternalInput").ap()
    ltd = nc.dram_tensor("lt", [128, 256], F32, kind="ExternalInput").ap()
    lqd = nc.dram_tensor("lq", [16, 2048], F32, kind="ExternalInput").ap()
    Ud = nc.dram_tensor("U", [128, 128], F32, kind="ExternalInput").ap()
    seld = nc.dram_tensor("sel", [128, 128], F32, kind="ExternalInput").ap()
    mkd = nc.dram_tensor("mk", [128, 128], F32, kind="ExternalInput").ap()
    od = nc.dram_tensor("o", [4, 64, S], F32, kind="ExternalOutput").ap()
    shi = nc.dram_tensor("shi", [4, S], BF16).ap()
    slo = nc.dram_tensor("slo", [4, S], BF16).ap()
    with ExitStack() as es:
        bank = [P.ps(es, "bank%d" % i, [128, 512], F32) for i in range(8)]
        U = P.sb(es, "U_sb", [128, 128], F32)
        sel = P.sb(es, "sel_sb", [128, 128], F32)
        mk = P.sb(es, "mk_sb", [128, 128], F32)
        onesf = P.sb(es, "onesf", [128, 128], F32)
        lt = P.sb(es, "lt_sb", [128, 256], F32)
        lq = P.sb(es, "lq_sb", [16, 2048], F32)
        within = P.sb(es, "within", [128, 256], F32)
        tot = P.sb(es, "tot", [128, 256], F32)
        inc = P.sb(es, "inc", [128, 256], F32)
        GT = P.sb(es, "GT", [128, 256], F32)
        gend = P.sb(es, "gend", [128, 256], F32)
        negB = P.sb(es, "negB", [128, 4 * 16 * 64], F32)
        cl = P.sb(es, "cl", [16, 2048], F32)
        Aa = P.sb(es, "Aa", [16, 2048], F32)
        ahi = P.sb(es, "ahi", [16, 2048], BF16)
        ahf = P.sb(es, "ahf", [16, 2048], F32)
        alo = P.sb(es, "alo", [16, 2048], BF16)
        qa = [P.sb(es, "qa%d" % i, [66, S], BF16) for i in range(2)]
        ka = [P.sb(es, "ka%d" % i, [66, S], BF16) for i in range(2)]
        va = [P.sb(es, "va%d" % i, [128, 64, 65], BF16) for i in range(2)]
        pt = [P.sb(es, "pt%d" % i, [128, 512], BF16) for i in range(3)]
        drow = P.sb(es, "drow", [65, 512], F32)
        rec = P.sb(es, "rec", [64, 512], F32)
        oo = [P.sb(es, "oo%d" % i, [64, 512], F32) for i in range(2)]
        ob = Buf()

        for t_, d_ in ((U, Ud), (sel, seld), (mk, mkd), (lt, ltd), (lq, lqd)):
            P.dma("sp", t_.t[:, :], d_, writes=[t_])
        P.op("pool", lambda e: e.memset(onesf.t[:, :], 1.0), writes=[onesf])
        P.op("pe", lambda e: e.matmul(bank[6].t[:, 0:256], U.t[:, :], lt.t[:, :], start=True, stop=True), reads=[U, lt], writes=[bank[6]])
        P.op("pe", lambda e: e.matmul(bank[7].t[:, 0:256], onesf.t[:, :], lt.t[:, :], start=True, stop=True), reads=[onesf, lt], writes=[bank[7]])
        P.op("dve", lambda e: e.tensor_copy(within.t[:, :], bank[6].t[:, 0:256]), reads=[bank[6]], writes=[within])
        P.op("dve", lambda e: e.tensor_copy(tot.t[:, :], bank[7].t[:, 0:256]), reads=[bank[7]], writes=[tot])
        for h in range(4):
            P.op("dve", lambda e, h=h: e.tensor_tensor_scan(inc.t[:, h * 64:(h + 1) * 64], onesf.t[:, 0:64], tot.t[:, h * 64:(h + 1) * 64],
                                                            0.0, ALU.mult, ALU.add), reads=[onesf, tot], writes=[inc])
        P.op("dve", lambda e: e.tensor_tensor(GT.t[:, :], within.t[:, :], inc.t[:, :], ALU.add), reads=[within, inc], writes=[GT])
        P.op("dve", lambda e: e.tensor_tensor(GT.t[:, :], GT.t[:, :], tot.t[:, :], ALU.subtract), reads=[GT, tot], writes=[GT])
        P.op("pe", lambda e: e.matmul(bank[6].t[:, 0:256], sel.t[:, :], GT.t[:, :], start=True, stop=True), reads=[sel, GT], writes=[bank[6]])
        P.op("dve", lambda e: e.tensor_copy(gend.t[:, :], bank[6].t[:, 0:256]), reads=[bank[6]], writes=[gend])
        for h in range(4):
            for Q in range(16):
                j0 = (h * 16 + Q) * 64
                gc = h * 64 + 4 * Q + 3
                P.op("dve", lambda e, h=h, j0=j0, gc=gc: e.tensor_scalar(
                    negB.t[:, j0:j0 + 64], GT.t[:, h * 64:(h + 1) * 64], -1.0, gend.t[:, gc:gc + 1], ALU.mult, ALU.add),
                    reads=[GT, gend], writes=[negB])
        ones16 = P.sb(es, "ones16", [16, 512], F32)
        P.op("pool", lambda e: e.memset(ones16.t[:, :], 1.0), writes=[ones16])
        for h in range(4):
            P.op("dve", lambda e, h=h: e.tensor_tensor_scan(cl.t[:, h * 512:(h + 1) * 512], ones16.t[:, :], lq.t[:, h * 512:(h + 1) * 512],
                                                            0.0, ALU.mult, ALU.add), reads=[lq, ones16], writes=[cl])
        for h in range(4):
            P.op("dve", lambda e, h=h: e.tensor_scalar(Aa.t[:, h * 512:(h + 1) * 512], cl.t[:, h * 512:(h + 1) * 512],
                                                       cl.t[:, h * 512 + 511:h * 512 + 512], None, ALU.subtract),
                 reads=[cl], writes=[Aa])
        P.op("dve", lambda e: e.tensor_copy(ahi.t[:, :], Aa.t[:, :]), reads=[Aa], writes=[ahi])
        P.op("dve", lambda e: e.tensor_copy(ahf.t[:, :], ahi.t[:, :]), reads=[ahi], writes=[ahf])
        P.op("dve", lambda e: e.tensor_tensor(alo.t[:, :], Aa.t[:, :], ahf.t[:, :], ALU.subtract), reads=[Aa, ahf], writes=[alo])
        shb, slb = Buf(), Buf()
        P.dma("sp", shi.rearrange("h (q m) -> q h m", q=16), ahi.t[:, :].rearrange("q (h m) -> q h m", h=4), reads=[ahi], writes=[shb])
        P.dma("sp", slo.rearrange("h (q m) -> q h m", q=16), alo.t[:, :].rearrange("q (h m) -> q h m", h=4), reads=[alo], writes=[slb])

        for i in range(2):
            P.op("pool", lambda e, i=i: e.memset(ka[i].t[64:66, :], 1.0), writes=[ka[i]])
            P.op("pool", lambda e, i=i: e.memset(va[i].t[:, :, 64:65], 1.0), writes=[va[i]])

        def load_head(h):
            q_, k_, v_ = qa[h % 2], ka[h % 2], va[h % 2]
            P.dma("sp", q_.t[0:64, :], qd[h], writes=[q_])
            P.dma("sp", q_.t[64:65, :], shi[h:h + 1, :], reads=[shb], writes=[q_])
            P.dma("sp", q_.t[65:66, :], slo[h:h + 1, :], reads=[slb], writes=[q_])
            P.dma("pool", k_.t[0:64, :], kd[h], writes=[k_])
            P.dma("pool", v_.t[:, :, 0:64], vd[h].rearrange("p (t d) -> p t d", d=64), writes=[v_])

        load_head(0)

        def do_head(h, q_, k_, v_, nit):
            items = [(Q, kt) for Q in range(16) for kt in range(4 * Q + 4)]

            def emit_S(idx, it_no):
                Q, kt = items[idx]
                d = kt - 4 * Q
                c0 = 128 * d if d >= 0 else 0
                bk = bank[it_no % 3]
                p_ = pt[it_no % 3]
                P.op("pe", lambda e: e.matmul(bk.t[:, c0:512], k_.t[0:66, kt * 128:(kt + 1) * 128],
                                              q_.t[0:66, Q * 512 + c0:(Q + 1) * 512], start=True, stop=True),
                     reads=[k_, q_], writes=[bk])
                if d >= 0:
                    P.op("dve", lambda e: e.tensor_tensor(bk.t[:, c0:c0 + 128], bk.t[:, c0:c0 + 128], mk.t[:, :], ALU.add),
                         reads=[bk, mk], writes=[bk])
                jb = (h * 16 + Q) * 64 + kt
                P.op("act", lambda e: e.activation(p_.t[:, c0:512], bk.t[:, c0:512], AF.Exp, bias=negB.t[:, jb:jb + 1], scale=1.0),
                     reads=[bk, negB], writes=[p_])

            def emit_PV(idx, it_no):
                Q, kt = items[idx]
                d = kt - 4 * Q
                c0 = 128 * d if d >= 0 else 0
                p_ = pt[it_no % 3]
                ob_ = bank[3 + Q % 2]
                last = (kt == 4 * Q + 3)
                P.op("pe", lambda e: e.matmul(ob_.t[0:65, c0:512], v_.t[:, kt, :], p_.t[:, c0:512], start=(kt == 0), stop=last),
                     reads=[v_, p_], writes=[ob_], pe_acc=(kt > 0))
                if last:
                    o_ = oo[Q % 2]
                    P.op("act", lambda e: e.activation(drow.t[64:65, :], ob_.t[64:65, :], AF.Copy), reads=[ob_], writes=[drow])
                    P.op("pe", lambda e: e.matmul(bank[5].t[0:64, :], onesf.t[64:65, 0:64], drow.t[64:65, :], start=True, stop=True),
                         reads=[onesf, drow], writes=[bank[5]])
                    P.op("dve", lambda e: e.reciprocal(rec.t[:, :], bank[5].t[0:64, :]), reads=[bank[5]], writes=[rec])
                    P.op("dve", lambda e: e.tensor_tensor(o_.t[:, :], ob_.t[0:64, :], rec.t[:, :], ALU.mult), reads=[ob_, rec], writes=[o_])
                    P.dma("sp", od[h][:, Q * 512:(Q + 1) * 512], o_.t[:, :], reads=[o_], ow=ob)

            n = len(items)
            emit_S(0, nit)
            for idx in range(n):
                if idx + 1 < n:
                    emit_S(idx + 1, nit + idx + 1)
                emit_PV(idx, nit + idx)
            return nit + n

        nit = 0
        for h in range(4):
            if h + 1 < 4:
                load_head(h + 1)
            nit = do_head(h, qa[h % 2], ka[h % 2], va[h % 2], nit)
        P.finish([ob])
        P.emit()
    return nc


def fox_consts():
    k = np.arange(128)
    U = (k[:, None] <= k[None, :]).astype(np.float32)
    sel = np.zeros((128, 128), np.float32)
    sel[127, :] = 1.0
    mk = np.where(k[None, :] >= k[:, None], 0.0, NEG).astype(np.float32)
    return U, sel, mk


def build_hgrn():
    nc = bass.Bass("TRN2", target_bir_lowering=False)
    P = Prog(nc)
    qd = nc.dram_tensor("q", [2, 128, S], BF16, kind="ExternalInput").ap()
    kd = nc.dram_tensor("k", [2, 128, S], F32, kind="ExternalInput").ap()
    lfd = nc.dram_tensor("lf", [2, 128, S], F32, kind="ExternalInput").ap()
    vd = nc.dram_tensor("v", [2, 128, 64 * 128], BF16, kind="ExternalInput").ap()
    m01d = nc.dram_tensor("m01", [128, 64], F32, kind="ExternalInput").ap()
    rmd = nc.dram_tensor("rm", [128, 2048], F32, kind="ExternalInput").ap()
    idd = nc.dram_tensor("ident", [128, 128], BF16, kind="ExternalInput").ap()
    od = nc.dram_tensor("o", [2, 128, S], F32, kind="ExternalOutput").ap()
    NB = 2048
    with ExitStack() as es:
        bankA = [P.ps(es, "bankA%d" % i, [128, 512], F32) for i in range(2)]
        bankO = [P.ps(es, "bankO%d" % i, [128, 512], F32) for i in range(2)]
        bankU = [P.ps(es, "bankU%d" % i, [128, 512], F32) for i in range(2)]
        bankT = P.ps(es, "bankT", [128, 1024], BF16)
        m01 = P.sb(es, "m01_sb", [128, 64], F32)
        rm = P.sb(es, "rm_sb", [128, NB], F32)
        ident = P.sb(es, "ident_sb", [128, 128], BF16)
        P.dma("sp", m01.t[:, :], m01d, writes=[m01])
        P.dma("sp", rm.t[:, :], rmd, writes=[rm])
        P.dma("sp", ident.t[:, :], idd, writes=[ident])
        ob = Buf()
        hs = []
        for h in range(2):
            o = Ctx()
            o.qb = P.sb(es, "qb%d" % h, [128, NB], BF16)
            o.kb = P.sb(es, "kb%d" % h, [128, NB], F32)
            o.lf = P.sb(es, "lf%d" % h, [128, NB], F32)
            o.G = P.sb(es, "G%d" % h, [128, NB], F32)
            o.tmp = P.sb(es, "tmp%d" % h, [128, NB], F32)
            o.tmp2 = P.sb(es, "tmp2%d" % h, [128, NB], F32)
            o.qd = P.sb(es, "qd%d" % h, [128, NB], BF16)
            o.kdd = P.sb(es, "kdd%d" % h, [128, NB], BF16)
            o.kend = P.sb(es, "kend%d" % h, [128, NB], BF16)
            o.kT = P.sb(es, "kT%d" % h, [128, 16, 128], BF16)
            o.vb = P.sb(es, "vb%d" % h, [128, 16, 128], BF16)
            o.egl = P.sb(es, "egl%d" % h, [128, 32], F32)
            o.S32 = P.sb(es, "S32_%d" % h, [128, 128], F32)
            o.Sbf = [P.sb(es, "Sbf%d_%d" % (h, i), [128, 128], BF16) for i in range(2)]
            o.am = [P.sb(es, "am%d_%d" % (h, i), [128, 64], BF16) for i in range(2)]
            o.osb = [P.sb(es, "osb%d_%d" % (h, i), [128, 512], F32) for i in range(2)]
            P.op("pool", lambda e, o=o: e.memset(o.S32.t[:, :], 0.0), writes=[o.S32])
            P.op("pool", lambda e, o=o: e.memset(o.Sbf[0].t[:, :], 0.0), writes=[o.Sbf[0]])
            o.si = 0
            hs.append(o)

        def prep(h, blk):
            o = hs[h]
            t0 = blk * NB
            P.dma("sp", o.qb.t[:, :], qd[h][:, t0:t0 + NB], writes=[o.qb])
            P.dma("sp", o.kb.t[:, :], kd[h][:, t0:t0 + NB], writes=[o.kb])
            P.dma("sp", o.lf.t[:, :], lfd[h][:, t0:t0 + NB], writes=[o.lf])
            P.dma("pool", o.vb.t[:, :, :], vd[h][:, blk * 2048:(blk + 1) * 2048].rearrange("p (t d) -> p t d", d=128), writes=[o.vb])
            P.op("dve", lambda e: e.tensor_tensor_scan(o.G.t[:, :], rm.t[:, :], o.lf.t[:, :], 0.0, ALU.mult, ALU.add),
                 reads=[rm, o.lf], writes=[o.G])
            P.op("act", lambda e: e.activation(o.tmp.t[:, :], o.G.t[:, :], AF.Exp), reads=[o.G], writes=[o.tmp])
            P.op("dve", lambda e: e.tensor_tensor(o.qd.t[:, :], o.qb.t[:, :], o.tmp.t[:, :], ALU.mult), reads=[o.qb, o.tmp], writes=[o.qd])
            P.op("act", lambda e: e.activation(o.tmp2.t[:, :], o.G.t[:, :], AF.Exp, scale=-1.0), reads=[o.G], writes=[o.tmp2])
            P.op("dve", lambda e: e.tensor_tensor(o.tmp2.t[:, :], o.kb.t[:, :], o.tmp2.t[:, :], ALU.mult), reads=[o.kb, o.tmp2], writes=[o.tmp2])
            P.op("dve", lambda e: e.tensor_copy(o.kdd.t[:, :], o.tmp2.t[:, :]), reads=[o.tmp2], writes=[o.kdd])
            G3 = o.G.t[:, :].rearrange("p (c s) -> p c s", s=64)
            P.op("act", lambda e: e.activation(o.egl.t[:, :], G3[:, :, 63], AF.Exp), reads=[o.G], writes=[o.egl])
            for cc in range(32):
                P.op("dve", lambda e, cc=cc: e.tensor_scalar(o.kend.t[:, cc * 64:(cc + 1) * 64], o.tmp2.t[:, cc * 64:(cc + 1) * 64],
                                                             o.egl.t[:, cc:cc + 1], None, ALU.mult),
                     reads=[o.tmp2, o.egl], writes=[o.kend])
            for grp in range(2):
                for j in range(8):
                    tk = grp * 8 + j
                    P.op("pe", lambda e, tk=tk, j=j: e.transpose(bankT.t[:, j * 128:(j + 1) * 128], o.kend.t[:, tk * 128:(tk + 1) * 128], ident.t[:, :]),
                         reads=[o.kend, ident], writes=[bankT], pe_acc=(j > 0))
                P.op("act", lambda e, grp=grp: e.activation(o.kT.t[:, grp * 8:(grp + 1) * 8, :],
                                                           bankT.t[:, :].rearrange("p (t k) -> p t k", k=128), AF.Copy),
                     reads=[bankT], writes=[o.kT])

        nA = [0]

        def chunk(h, blk, cc):
            o = hs[h]
            tk, half = cc // 2, cc % 2
            pb = 64 * half
            gc = blk * 32 + cc
            cs = slice(cc * 64, (cc + 1) * 64)
            bA = bankA[nA[0] % 2]
            am = o.am[nA[0] % 2]
            nA[0] += 1
            bO = bankO[h]
            bU = bankU[h]
            oc0 = (gc % 8) * 64
            Sb = o.Sbf[o.si % 2]
            Sn = o.Sbf[(o.si + 1) % 2]
            o.si += 1
            P.op("pe", lambda e: e.matmul(bA.t[pb:pb + 64, 0:64], o.kdd.t[:, cs], o.qd.t[:, cs], start=True, stop=True),
                 reads=[o.kdd, o.qd], writes=[bA])
            P.op("dve", lambda e: e.tensor_tensor(am.t[pb:pb + 64, :], bA.t[pb:pb + 64, 0:64], m01.t[pb:pb + 64, :], ALU.mult),
                 reads=[bA, m01], writes=[am])
            P.op("pe", lambda e: e.matmul(bO.t[:, oc0:oc0 + 64], Sb.t[:, :], o.qd.t[:, cs], start=True, stop=False),
                 reads=[Sb, o.qd], writes=[bO], pe_acc=(gc % 8 != 0))
            P.op("pe", lambda e: e.matmul(bO.t[:, oc0:oc0 + 64], o.vb.t[pb:pb + 64, tk, :], am.t[pb:pb + 64, :], start=False, stop=True),
                 reads=[o.vb, am], writes=[bO], pe_acc=True)
            P.op("pe", lambda e: e.matmul(bU.t[:, 0:128], o.kT.t[pb:pb + 64, tk, :], o.vb.t[pb:pb + 64, tk, :], start=True, stop=True),
                 reads=[o.kT, o.vb], writes=[bU])
            P.op("dve", lambda e: e.scalar_tensor_tensor(o.S32.t[:, :], o.S32.t[:, :], o.egl.t[:, cc:cc + 1], bU.t[:, 0:128], ALU.mult, ALU.add),
                 reads=[o.S32, o.egl, bU], writes=[o.S32])
            P.op("act", lambda e: e.activation(Sn.t[:, :], o.S32.t[:, :], AF.Copy), reads=[o.S32], writes=[Sn])
            if gc % 8 == 7:
                os_ = o.osb[(gc // 8) % 2]
                P.op("act", lambda e: e.activation(os_.t[:, :], bO.t[:, :], AF.Copy), reads=[bO], writes=[os_])
                tok0 = (gc - 7) * 64
                P.dma("sp", od[h][:, tok0:tok0 + 512], os_.t[:, :], reads=[os_], ow=ob)

        for blk in range(4):
            for h in range(2):
                prep(h, blk)
            for cc in range(32):
                for h in range(2):
                    chunk(h, blk, cc)
        P.finish([ob])
        P.emit()
    return nc


def hgrn_consts():
    p = np.arange(128)
    t = np.arange(64)
    m01 = ((p[:, None] % 64) <= t[None, :]).astype(np.float32)
    rm = np.ones((128, 2048), np.float32)
    rm[:, ::64] = 0.0
    ident = np.eye(128, dtype=np.float32).astype(NPBF)
    return m01, rm, ident


def build_mega():
    nc = bass.Bass("TRN2", target_bir_lowering=False)
    P = Prog(nc)
    c = Ctx()
    c.P, c.nc = P, nc
    dr = {}

    def din(name, shape, dt=F32):
        dr[name] = nc.dram_tensor(name, list(shape), dt, kind="ExternalInput").ap()
        return dr[name]

    def dout(name, shape, dt=F32):
        dr[name] = nc.dram_tensor(name, list(shape), dt, kind="ExternalOutput").ap()
        return dr[name]

    xT = din("xT", [D, T])
    cTd = din("cT", [128, 8])
    modbd = din("modb", [128, 144])
    modwd = din("modw", [18, 128, 8192])
    gT = din("gT", [128, 48])
    outs = []
    pid = nc.partition_id()
    g4 = pid % 4
    G4 = [[0, 1, 2, 3], [4, 5, 6, 7]]
    idram = lambda name, shape, dt: nc.dram_tensor(name, list(shape), dt)
    with ExitStack() as es:
        X = es.enter_context(nc.sbuf_tensor("X", [128, 8, T], F32))
        c.X = X
        c.Xb = [[Buf(X) for _ in range(4)] for _ in range(8)]
        c.modt = P.sb(es, "modt", [128, 144], F32)
        c.gt = P.sb(es, "gt", [128, 48], F32)
        c.der = P.sb(es, "der", [128, 96], F32)
        c.ones = P.sb(es, "ones", [128, 128], BF16)
        c.bank = [P.ps(es, "bank%d" % i, [128, 512], F32) for i in range(7)]
        bankT = P.ps(es, "bankT", [128, 1024], BF16)
        sqt = es.enter_context(nc.sbuf_tensor("sq", [128, 8, 512], BF16))
        c.sq = [Buf(sqt) for _ in range(8)]
        c.rstd = P.sb(es, "rstd", [128, 512], F32)
        c.tmp = [P.sb(es, "tmp%d" % i, [128, 512], F32) for i in range(2)]

        xv = xT.rearrange("(c p) t -> p c t", p=128)
        for kc in range(8):
            P.dma("sp", X[:, kc, :], xv[:, kc, :], writes=[c.Xb[kc][tt] for tt in range(4)])
        P.dma("sp", c.gt.t[:, :], gT, writes=[c.gt])
        P.op("pool", lambda e: e.memset(c.ones.t[:, :], 1.0), writes=[c.ones])
        with ExitStack() as e0:
            ct = P.sb(e0, "ct", [128, 8], F32)
            ctb = P.sb(e0, "ctb", [128, 8], BF16)
            mb = P.sb(e0, "mb", [128, 144], F32)
            mw = [P.sb(e0, "mw%d" % i, [128, 8, 1024], BF16) for i in range(2)]
            P.dma("sp", ct.t[:, :], cTd, writes=[ct])
            P.dma("sp", mb.t[:, :], modbd, writes=[mb])
            P.op("act", lambda e: e.activation(ctb.t[:, :], ct.t[:, :], AF.Silu), reads=[ct], writes=[ctb])
            bm = c.bank[5]
            for v in range(18):
                w_ = mw[v % 2]
                P.dma("pool", w_.t[:, :, :], modwd[v].rearrange("p (k n) -> p k n", k=8), writes=[w_])
                for ch in range(8):
                    col = v * 8 + ch
                    for kc in range(8):
                        P.op("pe", lambda e, w_=w_, ch=ch, kc=kc, col=col: e.matmul(
                            bm.t[:, col:col + 1], w_.t[:, kc, ch * 128:(ch + 1) * 128], ctb.t[:, kc:kc + 1],
                            start=(kc == 0), stop=(kc == 7)),
                            reads=[w_, ctb], writes=[bm], pe_acc=not (v == 0 and ch == 0 and kc == 0))
            P.op("dve", lambda e: e.tensor_tensor(c.modt.t[:, :], bm.t[:, 0:144], mb.t[:, :], ALU.add),
                 reads=[bm, mb], writes=[c.modt])
        P.barrier()
        for l in range(2):
            for sub in range(3):
                base = ((l * 3 + sub) * 2) * 8
                sc0 = mcol(l, sub * 3 + 1, 0)
                g0 = (l * 3 + sub) * 8
                ga0 = mcol(l, sub * 3 + 2, 0)
                P.op("dve", lambda e, base=base, sc0=sc0, g0=g0: e.scalar_tensor_tensor(
                    c.der.t[:, base:base + 8], c.modt.t[:, sc0:sc0 + 8], 1.0, c.gt.t[:, g0:g0 + 8], ALU.add, ALU.mult),
                    reads=[c.modt, c.gt], writes=[c.der])
                P.op("dve", lambda e, base=base: e.tensor_scalar(
                    c.der.t[:, base:base + 8], c.der.t[:, base:base + 8], SQD, None, ALU.mult),
                    reads=[c.der], writes=[c.der])
                P.op("dve", lambda e, base=base, ga0=ga0, sub=sub: e.tensor_scalar(
                    c.der.t[:, base + 8:base + 16], c.modt.t[:, ga0:ga0 + 8], (1.0 if sub == 1 else 0.5), None, ALU.mult),
                    reads=[c.modt], writes=[c.der])

        def Acol(l, sub, ch):
            j = ((l * 3 + sub) * 2) * 8 + ch
            return c.der.t[:, j:j + 1]

        def Gcol(l, sub, ch):
            j = ((l * 3 + sub) * 2 + 1) * 8 + ch
            return c.der.t[:, j:j + 1]

        def Scol(l, sub, ch):
            j = mcol(l, sub * 3 + 0, ch)
            return c.modt.t[:, j:j + 1]

        def rstd_tile(src_fn, src_bufs, epsk):
            for kc in range(8):
                P.op("act", lambda e, kc=kc: e.activation(sqt[:, kc, :], src_fn(kc), AF.Square),
                     reads=[src_bufs[kc]], writes=[c.sq[kc]])
            for kc in range(8):
                P.op("pe", lambda e, kc=kc: e.matmul(c.bank[6].t[:, :], c.ones.t[:, :], sqt[:, kc, :],
                                                      start=(kc == 0), stop=(kc == 7)),
                     reads=[c.ones, c.sq[kc]], writes=[c.bank[6]], pe_acc=(kc > 0))
            P.op("dve", lambda e: e.tensor_scalar(c.rstd.t[:, :], c.bank[6].t[:, :], epsk, None, ALU.add),
                 reads=[c.bank[6]], writes=[c.rstd])
            P.op("act", lambda e: e.activation(c.rstd.t[:, :], c.rstd.t[:, :], AF.Sqrt), reads=[c.rstd], writes=[c.rstd])
            P.op("dve", lambda e: e.reciprocal(c.rstd.t[:, :], c.rstd.t[:, :]), reads=[c.rstd], writes=[c.rstd])

        def modnorm_tile(l, sub, tt, hdst, hbuf):
            t0 = tt * 512
            rstd_tile(lambda kc: X[:, kc, t0:t0 + 512], [c.Xb[kc][tt] for kc in range(8)], EPS * D)
            for kc in range(8):
                tb = c.tmp[kc % 2]
                P.op("dve", lambda e, kc=kc, tb=tb: e.tensor_tensor(tb.t[:, :], X[:, kc, t0:t0 + 512], c.rstd.t[:, :], ALU.mult),
                     reads=[c.Xb[kc][tt], c.rstd], writes=[tb])
                P.op("act", lambda e, kc=kc, tb=tb: e.activation(hdst(kc), tb.t[:, :], AF.Identity,
                                                               bias=Scol(l, sub, kc), scale=Acol(l, sub, kc)),
                     reads=[tb, c.der, c.modt], writes=[hbuf(kc)])

        def epilogue(kind, l, oG, oGb, sgd, sgb):
            wod = din(kind + "_wo", [128, 8192])
            oS, oSb = dsel(kind + "_oS", [1, D, T], BF16, oG.ap()[bass.ds(g4, 1), :, :], oGb)
            sv = sgd.ap().rearrange("(c p) t -> p c t", p=128)
            with ExitStack() as e2:
                wo = P.sb(e2, kind + "wo_sb", [128, 8, 1024], BF16)
                P.dma("pool", wo.t[:, :, :], wod.rearrange("p (k d) -> p k d", k=8), writes=[wo])
                ot = [P.sb(e2, kind + "ot%d" % i, [128, 8, 512], BF16) for i in range(2)]
                st = [P.sb(e2, kind + "st%d" % i, [128, 8, 512], BF16) for i in range(2)]
                ogt = [e2.enter_context(nc.sbuf_tensor(kind + "og%d" % i, [128, 8, 512], BF16)) for i in range(2)]
                ogb = [[Buf(ogt[i]) for _ in range(8)] for i in range(2)]
                if kind == "hgrn":
                    hgd = din("hgn", [128, 8])
                    hg = P.sb(e2, "hg", [128, 8], F32)
                    P.dma("sp", hg.t[:, :], hgd, writes=[hg])
                    P.op("dve", lambda e: e.tensor_scalar(hg.t[:, :], hg.t[:, :], float(np.sqrt(128.0)), None, ALU.mult),
                         reads=[hg], writes=[hg])
                    sq1 = P.sb(e2, "sq1", [128, 512], BF16)
                    r1 = P.sb(e2, "r1", [128, 512], F32)
                    t1 = P.sb(e2, "t1", [128, 512], F32)
                for tt in range(4):
                    t0 = tt * 512
                    o_, s_, og_ = ot[tt % 2], st[tt % 2], ogt[tt % 2]
                    P.dma("sp", o_.t[:, :, :], oS.ap()[0].rearrange("(c p) s -> p c s", p=128)[:, :, t0:t0 + 512], reads=[oSb], writes=[o_])
                    P.dma("sp", s_.t[:, :, :], sv[:, :, t0:t0 + 512], reads=[sgb], writes=[s_])
                    if kind == "fox":
                        for kc in range(8):
                            P.op("dve", lambda e, kc=kc, o_=o_, s_=s_, og_=og_: e.tensor_tensor(
                                og_[:, kc, :], o_.t[:, kc, :], s_.t[:, kc, :], ALU.mult),
                                reads=[o_, s_], writes=[ogb[tt % 2][kc]])
                    else:
                        for kc in range(8):
                            P.op("act", lambda e, kc=kc, o_=o_: e.activation(sq1.t[:, :], o_.t[:, kc, :], AF.Square),
                                 reads=[o_], writes=[sq1])
                            P.op("pe", lambda e: e.matmul(c.bank[5].t[:, :], c.ones.t[:, :], sq1.t[:, :], start=True, stop=True),
                                 reads=[c.ones, sq1], writes=[c.bank[5]])
                            P.op("dve", lambda e: e.tensor_scalar(r1.t[:, :], c.bank[5].t[:, :], EPS * 128.0, None, ALU.add),
                                 reads=[c.bank[5]], writes=[r1])
                            P.op("act", lambda e: e.activation(r1.t[:, :], r1.t[:, :], AF.Sqrt), reads=[r1], writes=[r1])
                            P.op("dve", lambda e: e.reciprocal(r1.t[:, :], r1.t[:, :]), reads=[r1], writes=[r1])
                            P.op("dve", lambda e, kc=kc, o_=o_: e.tensor_tensor(t1.t[:, :], o_.t[:, kc, :], r1.t[:, :], ALU.mult),
                                 reads=[o_, r1], writes=[t1])
                            P.op("dve", lambda e, kc=kc, s_=s_, og_=og_: e.scalar_tensor_tensor(
                                og_[:, kc, :], t1.t[:, :], hg.t[:, kc:kc + 1], s_.t[:, kc, :], ALU.mult, ALU.mult),
                                reads=[t1, hg, s_], writes=[ogb[tt % 2][kc]])
                    for dc in range(8):
                        bk = c.bank[4 + dc % 2]
                        for kc in range(8):
                            P.op("pe", lambda e, kc=kc, dc=dc, bk=bk, og_=og_: e.matmul(
                                bk.t[:, :], wo.t[:, kc, dc * 128:(dc + 1) * 128], og_[:, kc, :],
                                start=(kc == 0), stop=(kc == 7)),
                                reads=[wo, ogb[tt % 2][kc]], writes=[bk], pe_acc=(kc > 0))
                        P.op("dve", lambda e, dc=dc, bk=bk, t0=t0: e.scalar_tensor_tensor(
                            X[:, dc, t0:t0 + 512], bk.t[:, :], Gcol(l, 1, dc), X[:, dc, t0:t0 + 512], ALU.mult, ALU.add),
                            reads=[bk, c.der, c.Xb[dc][tt]], writes=[c.Xb[dc][tt]])
            P.barrier()

        def ffn(j, l, sub):
            wupd = din("wup%d" % j, [11, 128, 4096])
            wdnd = din("wdn%d" % j, [8, 128, 2816])
            with ExitStack() as e2:
                hbt = e2.enter_context(nc.sbuf_tensor("hb_%d" % j, [128, 8, 1024], BF16))
                hbb = [[Buf(hbt) for _ in range(2)] for _ in range(8)]
                actt = e2.enter_context(nc.sbuf_tensor("actb_%d" % j, [128, NF, 1024], BF16))
                actb = [[Buf(actt) for _ in range(2)] for _ in range(NF)]
                wu = [P.sb(e2, "wu%d_%d" % (j, i), [128, 2, 8, 256], BF16) for i in range(2)]
                wd = [P.sb(e2, "wd%d_%d" % (j, i), [128, NF, 128], BF16) for i in range(2)]
                sa = [P.sb(e2, "sa%d_%d" % (j, i), [128, 512], F32) for i in range(2)]
                for half in range(2):
                    for t2 in range(2):
                        tt = half * 2 + t2
                        modnorm_tile(l, sub, tt, lambda kc, t2=t2: hbt[:, kc, t2 * 512:(t2 + 1) * 512],
                                     lambda kc, t2=t2: hbb[kc][t2])
                    it = 0
                    for g in range(11):
                        w_ = wu[g % 2]
                        P.dma("pool", w_.t[:, :, :, :], wupd[g].rearrange("p (a k f) -> p a k f", a=2, k=8), writes=[w_])
                        for jf in range(2):
                            fc = 2 * g + jf
                            for t2 in range(2):
                                bA, bB = c.bank[it % 2], c.bank[2 + it % 2]
                                s_ = sa[it % 2]
                                it += 1
                                for kc in range(8):
                                    P.op("pe", lambda e, kc=kc, w_=w_, jf=jf, t2=t2, bA=bA: e.matmul(
                                        bA.t[:, :], w_.t[:, 0, kc, jf * 128:(jf + 1) * 128], hbt[:, kc, t2 * 512:(t2 + 1) * 512],
                                        start=(kc == 0), stop=(kc == 7)),
                                        reads=[w_, hbb[kc][t2]], writes=[bA], pe_acc=(kc > 0))
                                for kc in range(8):
                                    P.op("pe", lambda e, kc=kc, w_=w_, jf=jf, t2=t2, bB=bB: e.matmul(
                                        bB.t[:, :], w_.t[:, 1, kc, jf * 128:(jf + 1) * 128], hbt[:, kc, t2 * 512:(t2 + 1) * 512],
                                        start=(kc == 0), stop=(kc == 7)),
                                        reads=[w_, hbb[kc][t2]], writes=[bB], pe_acc=(kc > 0))
                                P.op("act", lambda e, s_=s_, bA=bA: e.activation(s_.t[:, :], bA.t[:, :], AF.Silu),
                                     reads=[bA], writes=[s_])
                                P.op("dve", lambda e, s_=s_, bB=bB, fc=fc, t2=t2: e.tensor_tensor(
                                    actt[:, fc, t2 * 512:(t2 + 1) * 512], bB.t[:, :], s_.t[:, :], ALU.mult),
                                    reads=[bB, s_], writes=[actb[fc][t2]])
                    for dc in range(8):
                        w_ = wd[dc % 2]
                        P.dma("pool", w_.t[:, :, :], wdnd[dc].rearrange("p (f d) -> p f d", f=NF), writes=[w_])
                        for t2 in range(2):
                            tt = half * 2 + t2
                            t0 = tt * 512
                            bk = c.bank[4 + (dc * 2 + t2) % 2]
                            for fc in range(NF):
                                P.op("pe", lambda e, fc=fc, w_=w_, t2=t2, bk=bk: e.matmul(
                                    bk.t[:, :], w_.t[:, fc, :], actt[:, fc, t2 * 512:(t2 + 1) * 512],
                                    start=(fc == 0), stop=(fc == NF - 1)),
                                    reads=[w_, actb[fc][t2]], writes=[bk], pe_acc=(fc > 0))
                            P.op("dve", lambda e, dc=dc, bk=bk, t0=t0: e.scalar_tensor_tensor(
                                X[:, dc, t0:t0 + 512], bk.t[:, :], Gcol(l, sub, dc), X[:, dc, t0:t0 + 512], ALU.mult, ALU.add),
                                reads=[bk, c.der, c.Xb[dc][tt]], writes=[c.Xb[dc][tt]])
            P.barrier()

        def proj_fm(wname, hbt, hbb, evac, n_oc=8):
            wd_ = din(wname, [n_oc, 128, 1024])
            with ExitStack() as e3:
                wp = [P.sb(e3, wname + "_sb%d" % i, [128, 8, 128], BF16) for i in range(2)]
                it = 0
                for oc in range(n_oc):
                    w_ = wp[oc % 2]
                    P.dma("pool", w_.t[:, :, :], wd_[oc].rearrange("p (k f) -> p k f", k=8), writes=[w_])
                    for tt in range(4):
                        bk = c.bank[it % 4]
                        it += 1
                        for kc in range(8):
                            P.op("pe", lambda e, kc=kc, w_=w_, tt=tt, bk=bk: e.matmul(
                                bk.t[:, :], w_.t[:, kc, :], hbt[:, kc, tt * 512:(tt + 1) * 512],
                                start=(kc == 0), stop=(kc == 7)),
                                reads=[w_, hbb[kc][tt]], writes=[bk], pe_acc=(kc > 0))
                        evac(oc, tt, bk)
                P.barrier()

        def proj_tm(wname, hbt, hbb, vout, vob, func):
            wd_ = din(wname, [2, 128, 4096])
            with ExitStack() as e3:
                wv = P.sb(e3, wname + "_sb", [128, 2, 8, 512], BF16)
                for cg in range(2):
                    P.dma("pool", wv.t[:, cg, :, :], wd_[cg].rearrange("p (k f) -> p k f", k=8), writes=[wv])
                vt = [P.sb(e3, wname + "vt%d" % i, [128, 512], BF16) for i in range(2)]
                it = 0
                for tk in range(16):
                    for cg in range(2):
                        bk = c.bank[it % 4]
                        v_ = vt[it % 2]
                        it += 1
                        for kc in range(8):
                            P.op("pe", lambda e, kc=kc, tk=tk, cg=cg, bk=bk: e.matmul(
                                bk.t[:, :], hbt[:, kc, tk * 128:(tk + 1) * 128], wv.t[:, cg, kc, :],
                                start=(kc == 0), stop=(kc == 7)),
                                reads=[wv, hbb[kc][tk // 4]], writes=[bk], pe_acc=(kc > 0))
                        P.op("act", lambda e, bk=bk, v_=v_: e.activation(v_.t[:, :], bk.t[:, :], func),
                             reads=[bk], writes=[v_])
                        P.dma("sp", vout[tk * 128:(tk + 1) * 128, cg * 512:(cg + 1) * 512], v_.t[:, :], reads=[v_], ow=vob)
                P.barrier()

        def stage_out(e3, name, shape, dt):
            return [P.sb(e3, name + "%d" % i, shape, dt) for i in range(2)]

        def projections(kind, l):
            R = Ctx()
            with ExitStack() as e2:
                hbt = e2.enter_context(nc.sbuf_tensor(kind + "hb2", [128, 8, T], BF16))
                hbb = [[Buf(hbt) for _ in range(4)] for _ in range(8)]
                for tt in range(4):
                    modnorm_tile(l, 1, tt, lambda kc, tt=tt: hbt[:, kc, tt * 512:(tt + 1) * 512],
                                 lambda kc, tt=tt: hbb[kc][tt])
                kdt = BF16 if kind == "fox" else F32
                R.q, R.qb = idram(kind + "_q", [D, T], BF16), Buf()
                R.k, R.kb = idram(kind + "_k", [D, T], kdt), Buf()
                R.sg, R.sgb = idram(kind + "_sg", [D, T], BF16), Buf()
                R.v, R.vb = idram(kind + "_v", [T, D], BF16), Buf()
                qo, ko, sgo, vo = R.q.ap(), R.k.ap(), R.sg.ap(), R.v.ap()
                qob, kob, sgob, vob = R.qb, R.kb, R.sgb, R.vb
                cnt = [0]

                def simple_evac(od, ob, func, scale, st):
                    def evac(oc, tt, bk):
                        s_ = st[cnt[0] % 2]
                        cnt[0] += 1
                        P.op("act", lambda e, s_=s_, bk=bk: e.activation(s_.t[:, :], bk.t[:, :], func, scale=scale),
                             reads=[bk], writes=[s_])
                        P.dma("sp", od[oc * 128:(oc + 1) * 128, tt * 512:(tt + 1) * 512], s_.t[:, :], reads=[s_], ow=ob)
                    return evac

                stb = stage_out(e2, kind + "stb", [128, 512], BF16)
                if kind == "fox":
                    R.lf, R.lfb = idram("fox_lf", [128, 256], F32), Buf()
                    proj_fm("fox_wq", hbt, hbb, simple_evac(qo, qob, AF.Copy, float(FD ** -0.5), stb))
                    proj_fm("fox_wk", hbt, hbb, simple_evac(ko, kob, AF.Copy, 1.0, stb))
                    proj_fm("fox_wg", hbt, hbb, simple_evac(sgo, sgob, AF.Sigmoid, 1.0, stb))
                    proj_tm("fox_wv", hbt, hbb, vo, vob, AF.Copy)
                    wfd = din("fox_wf", [128, 128])
                    bfd = din("fox_bfb", [128, 256])
                    wf = P.sb(e2, "wf_sb", [128, 8, 16], BF16)
                    P.dma("pool", wf.t[:, :, :], wfd.rearrange("p (k f) -> p k f", k=8), writes=[wf])
                    bfb = P.sb(e2, "bfb", [128, 256], F32)
                    P.dma("sp", bfb.t[:, :], bfd, writes=[bfb])
                    z1 = P.sb(e2, "z1", [128, 256], F32)
                    bk = c.bank[0]
                    for tk in range(16):
                        for kc in range(8):
                            P.op("pe", lambda e, kc=kc, tk=tk: e.matmul(
                                bk.t[:, tk * 16:(tk + 1) * 16], hbt[:, kc, tk * 128:(tk + 1) * 128], wf.t[:, kc, :],
                                start=(kc == 0), stop=(kc == 7)),
                                reads=[wf, hbb[kc][tk // 4]], writes=[bk], pe_acc=not (tk == 0 and kc == 0))
                    P.op("dve", lambda e: e.tensor_tensor(z1.t[:, :], bk.t[:, 0:256], bfb.t[:, :], ALU.add), reads=[bk, bfb], writes=[z1])
                    P.op("act", lambda e: e.activation(z1.t[:, :], z1.t[:, :], AF.Exp, scale=-1.0), reads=[z1], writes=[z1])
                    P.op("act", lambda e: e.activation(z1.t[:, :], z1.t[:, :], AF.Ln, bias=1.0, scale=1.0), reads=[z1], writes=[z1])
                    P.op("dve", lambda e: e.tensor_scalar(z1.t[:, :], z1.t[:, :], -1.0, None, ALU.mult), reads=[z1], writes=[z1])
                    P.dma("sp", R.lf.ap(), z1.t[:, :], reads=[z1], ow=R.lfb)
                else:
                    R.lf, R.lfb = idram("hgrn_lf", [D, T], F32), Buf()
                    lfo, lfob = R.lf.ap(), R.lfb
                    lbd = din("lbl", [128, 16])
                    lbl = P.sb(e2, "lbl_sb", [128, 16], F32)
                    lb = P.sb(e2, "lb", [128, 8], F32)
                    oml = P.sb(e2, "oml", [128, 8], F32)
                    P.dma("sp", lbl.t[:, :], lbd, writes=[lbl])
                    P.op("dve", lambda e: e.tensor_tensor(lb.t[:, :], lbl.t[:, 8:16], lbl.t[:, 0:8], ALU.subtract), reads=[lbl], writes=[lb])
                    P.op("act", lambda e: e.activation(lb.t[:, :], lb.t[:, :], AF.Sigmoid), reads=[lb], writes=[lb])
                    P.op("dve", lambda e: e.tensor_scalar(oml.t[:, :], lb.t[:, :], -1.0, 1.0, ALU.mult, ALU.add), reads=[lb], writes=[oml])
                    proj_fm("hgrn_wq", hbt, hbb, simple_evac(qo, qob, AF.Copy, 1.0, stb))
                    proj_fm("hgrn_wg", hbt, hbb, simple_evac(sgo, sgob, AF.Silu, 1.0, stb))
                    proj_tm("hgrn_wv", hbt, hbb, vo, vob, AF.Silu)
                    sg1 = P.sb(e2, "sg1", [128, 512], F32)
                    ff = stage_out(e2, "ff", [128, 512], F32)
                    lff = stage_out(e2, "lff", [128, 512], F32)
                    kk = stage_out(e2, "kk", [128, 512], F32)

                    def f_evac(oc, tt, bk):
                        i = cnt[0] % 2
                        cnt[0] += 1
                        f_, l_, k_ = ff[i], lff[i], kk[i]
                        P.op("act", lambda e, bk=bk: e.activation(sg1.t[:, :], bk.t[:, :], AF.Sigmoid), reads=[bk], writes=[sg1])
                        P.op("dve", lambda e, f_=f_, oc=oc: e.tensor_scalar(f_.t[:, :], sg1.t[:, :], oml.t[:, oc:oc + 1], lb.t[:, oc:oc + 1], ALU.mult, ALU.add),
                             reads=[sg1, oml, lb], writes=[f_])
                        P.op("act", lambda e, f_=f_, l_=l_: e.activation(l_.t[:, :], f_.t[:, :], AF.Ln), reads=[f_], writes=[l_])
                        P.dma("sp", lfo[oc * 128:(oc + 1) * 128, tt * 512:(tt + 1) * 512], l_.t[:, :], reads=[l_], ow=lfob)
                    proj_fm("hgrn_wf", hbt, hbb, f_evac)
            P.barrier()
            return R

        def gather(name, src, srcb, nch, rows, cols, dt):
            dst = idram(name, [nch, 4 * rows, cols], dt)
            db = Buf()
            sv = src.ap() if len(src.shape) == 2 else None
            for j in range(nch):
                sa = src.ap()[j * rows:(j + 1) * rows, :] if sv is not None else src.ap()[j]
                P.collective("AllGather", G4, sa.opt(), dst.ap()[j].opt(), [srcb], db)
            return dst, db

        def dsel(name, shape, dt, src_dyn, srcb):
            dst = idram(name, shape, dt)
            db = Buf()
            P.dma("sp", dst.ap(), src_dyn, reads=[srcb], writes=[db])
            return dst, db

        def fox_phase(R):
            qG, qGb = gather("fox_qG", R.q, R.qb, 4, 256, T, BF16)
            kG, kGb = gather("fox_kG", R.k, R.kb, 4, 256, T, BF16)
            vG, vGb = gather("fox_vG", R.v, R.vb, 4, 512, D, BF16)
            lG, lGb = gather("fox_lG", R.lf, R.lfb, 1, 128, 256, F32)
            qS, qSb = dsel("fox_qS", [1, D, T], BF16, qG.ap()[bass.ds(g4, 1), :, :], qGb)
            kS, kSb = dsel("fox_kS", [1, D, T], BF16, kG.ap()[bass.ds(g4, 1), :, :], kGb)
            vS, vSb = idram("fox_vS", [S, 256], BF16), Buf()
            for j in range(4):
                P.dma("sp", vS.ap().rearrange("(r j i) c -> j r i c", r=4, j=4)[j],
                      vG.ap()[j].rearrange("(r i) c -> r i c", r=4)[:, :, bass.ds(g4 * 256, 256)], reads=[vGb], writes=[vSb])
            lS, lSb = dsel("fox_lS", [512, 16, 4], F32, lG.ap()[0].rearrange("r (k h) -> r k h", h=16)[:, :, bass.ds(g4 * 4, 4)], lGb)
            o_loc, olb = idram("fox_o", [4, 256, T], BF16), Buf()
            shi, slo = idram("shi", [4, S], BF16), idram("slo", [4, S], BF16)
            shb, slb = Buf(), Buf()
            Ud, seld, mkd, idfd = din("U", [128, 128]), din("sel", [128, 128]), din("mk", [128, 128]), din("identf", [128, 128])
            bank = c.bank
            with ExitStack() as e2:
                U = P.sb(e2, "U_sb", [128, 128], F32)
                sel = P.sb(e2, "sel_sb", [128, 128], F32)
                mk = P.sb(e2, "mk_sb", [128, 128], F32)
                idf = P.sb(e2, "idf_sb", [128, 128], F32)
                onesf = P.sb(e2, "onesf", [128, 128], F32)
                negB = P.sb(e2, "negB", [128, 4 * 16 * 64], F32)
                for t_, d_ in ((U, Ud), (sel, seld), (mk, mkd), (idf, idfd)):
                    P.dma("sp", t_.t[:, :], d_, writes=[t_])
                P.op("pool", lambda e: e.memset(onesf.t[:, :], 1.0), writes=[onesf])
                with ExitStack() as e3:
                    lsel = P.sb(e3, "lsel", [128, 4, 16, 4], F32)
                    lt = P.sb(e3, "lt_sb", [128, 256], F32)
                    within = P.sb(e3, "within", [128, 256], F32)
                    tot = P.sb(e3, "tot", [128, 256], F32)
                    inc = P.sb(e3, "inc", [128, 256], F32)
                    GT = P.sb(e3, "GT", [128, 256], F32)
                    gend = P.sb(e3, "gend", [128, 256], F32)
                    Aa = P.sb(e3, "Aa", [128, 256], F32)
                    AT = P.sb(e3, "AT", [64, 512], F32)
                    ahi = P.sb(e3, "ahi", [64, 512], BF16)
                    ahf = P.sb(e3, "ahf", [64, 512], F32)
                    alo = P.sb(e3, "alo", [64, 512], BF16)
                    for t in range(4):
                        P.dma("sp", lsel.t[:, t, :, :], lS.ap()[t * 128:(t + 1) * 128, :, :], reads=[lSb], writes=[lsel])
                    for hl in range(4):
                        P.op("dve", lambda e, hl=hl: e.tensor_copy(
                            lt.t[:, hl * 64:(hl + 1) * 64].rearrange("p (t k) -> p t k", t=4), lsel.t[:, :, :, hl]),
                            reads=[lsel], writes=[lt])
                    P.op("pe", lambda e: e.matmul(bank[6].t[:, 0:256], U.t[:, :], lt.t[:, :], start=True, stop=True), reads=[U, lt], writes=[bank[6]])
                    P.op("pe", lambda e: e.matmul(bank[5].t[:, 0:256], onesf.t[:, :], lt.t[:, :], start=True, stop=True), reads=[onesf, lt], writes=[bank[5]])
                    P.op("dve", lambda e: e.tensor_copy(within.t[:, :], bank[6].t[:, 0:256]), reads=[bank[6]], writes=[within])
                    P.op("dve", lambda e: e.tensor_copy(tot.t[:, :], bank[5].t[:, 0:256]), reads=[bank[5]], writes=[tot])
                    for h in range(4):
                        P.op("dve", lambda e, h=h: e.tensor_tensor_scan(inc.t[:, h * 64:(h + 1) * 64], onesf.t[:, 0:64], tot.t[:, h * 64:(h + 1) * 64],
                                                                        0.0, ALU.mult, ALU.add), reads=[onesf, tot], writes=[inc])
                    P.op("dve", lambda e: e.tensor_tensor(GT.t[:, :], within.t[:, :], inc.t[:, :], ALU.add), reads=[within, inc], writes=[GT])
                    P.op("dve", lambda e: e.tensor_tensor(GT.t[:, :], GT.t[:, :], tot.t[:, :], ALU.subtract), reads=[GT, tot], writes=[GT])
                    P.op("pe", lambda e: e.matmul(bank[6].t[:, 0:256], sel.t[:, :], GT.t[:, :], start=True, stop=True), reads=[sel, GT], writes=[bank[6]])
                    P.op("dve", lambda e: e.tensor_copy(gend.t[:, :], bank[6].t[:, 0:256]), reads=[bank[6]], writes=[gend])
                    for h in range(4):
                        for Q in range(16):
                            j0 = (h * 16 + Q) * 64
                            gc = h * 64 + 4 * Q + 3
                            P.op("dve", lambda e, h=h, j0=j0, gc=gc: e.tensor_scalar(
                                negB.t[:, j0:j0 + 64], GT.t[:, h * 64:(h + 1) * 64], -1.0, gend.t[:, gc:gc + 1], ALU.mult, ALU.add),
                                reads=[GT, gend], writes=[negB])
                            a0 = h * 64 + 4 * Q
                            P.op("dve", lambda e, a0=a0, gc=gc: e.tensor_scalar(
                                Aa.t[:, a0:a0 + 4], GT.t[:, a0:a0 + 4], gend.t[:, gc:gc + 1], None, ALU.subtract),
                                reads=[GT, gend], writes=[Aa])
                    for h in range(4):
                        P.op("pe", lambda e, h=h: e.matmul(bank[5].t[0:64, h * 128:(h + 1) * 128], Aa.t[:, h * 64:(h + 1) * 64], idf.t[:, :],
                                                           start=True, stop=True), reads=[Aa, idf], writes=[bank[5]], pe_acc=(h > 0))
                    P.op("dve", lambda e: e.tensor_copy(AT.t[:, :], bank[5].t[0:64, :]), reads=[bank[5]], writes=[AT])
                    P.op("dve", lambda e: e.tensor_copy(ahi.t[:, :], AT.t[:, :]), reads=[AT], writes=[ahi])
                    P.op("dve", lambda e: e.tensor_copy(ahf.t[:, :], ahi.t[:, :]), reads=[ahi], writes=[ahf])
                    P.op("dve", lambda e: e.tensor_tensor(alo.t[:, :], AT.t[:, :], ahf.t[:, :], ALU.subtract), reads=[AT, ahf], writes=[alo])
                    P.dma("sp", shi.ap().rearrange("h (k p) -> k h p", p=128), ahi.t[:, :].rearrange("k (h p) -> k h p", h=4), reads=[ahi], writes=[shb])
                    P.dma("sp", slo.ap().rearrange("h (k p) -> k h p", p=128), alo.t[:, :].rearrange("k (h p) -> k h p", h=4), reads=[alo], writes=[slb])
                P.barrier()
                qa = [P.sb(e2, "qa%d" % i, [128, S], BF16) for i in range(2)]
                ka = [P.sb(e2, "ka%d" % i, [128, S], BF16) for i in range(2)]
                va = [P.sb(e2, "va%d" % i, [128, 64 * 65 + 64], BF16) for i in range(2)]
                pt = [P.sb(e2, "pt%d" % i, [128, 512], BF16) for i in range(3)]
                drow = P.sb(e2, "drow", [65, 512], F32)
                rec = P.sb(e2, "rec", [64, 512], F32)
                oo = [P.sb(e2, "oo%d" % i, [64, 512], BF16) for i in range(2)]
                vv = lambda v_: v_.t[:, 0:64 * 65].rearrange("p (t d) -> p t d", d=65)
                for i in range(2):
                    P.op("pool", lambda e, i=i: e.memset(ka[i].t[64:128, :], 0.0), writes=[ka[i]])
                    P.op("pool", lambda e, i=i: e.memset(qa[i].t[64:128, :], 0.0), writes=[qa[i]])
                    P.op("pool", lambda e, i=i: e.memset(ka[i].t[64:66, :], 1.0), writes=[ka[i]])
                    P.op("pool", lambda e, i=i: e.memset(va[i].t[:, :], 0.0), writes=[va[i]])
                    P.op("pool", lambda e, i=i: e.memset(vv(va[i])[:, :, 64:65], 1.0), writes=[va[i]])
                vGv = vS.ap().rearrange("(k p) d -> p k d", p=128)

                def load_head(h):
                    q_, k_, v_ = qa[h % 2], ka[h % 2], va[h % 2]
                    for t in range(4):
                        P.dma("sp", q_.t[0:64, t * T:(t + 1) * T], qS.ap()[0, t * 256 + h * 64:t * 256 + (h + 1) * 64, :], reads=[qSb], writes=[q_])
                        P.dma("pool", k_.t[0:64, t * T:(t + 1) * T], kS.ap()[0, t * 256 + h * 64:t * 256 + (h + 1) * 64, :], reads=[kSb], writes=[k_])
                    P.dma("sp", q_.t[64:65, :], shi.ap()[h:h + 1, :], reads=[shb], writes=[q_])
                    P.dma("sp", q_.t[65:66, :], slo.ap()[h:h + 1, :], reads=[slb], writes=[q_])
                    P.dma("pool", vv(v_)[:, :, 0:64], vGv[:, :, h * 64:(h + 1) * 64], reads=[vSb], writes=[v_])

                load_head(0)

                def do_head(h, q_, k_, v_, nit):
                    items = [(Q, kt) for Q in range(16) for kt in range(4 * Q + 4)]

                    def emit_S(idx, it_no):
                        Q, kt = items[idx]
                        d = kt - 4 * Q
                        c0 = 128 * d if d >= 0 else 0
                        bk = bank[it_no % 3]
                        p_ = pt[it_no % 3]
                        P.op("pe", lambda e: e.matmul(bk.t[:, c0:512], k_.t[0:128, kt * 128:(kt + 1) * 128],
                                                      q_.t[0:128, Q * 512 + c0:(Q + 1) * 512], start=True, stop=True),
                             reads=[k_, q_], writes=[bk])
                        if d >= 0:
                            P.op("dve", lambda e: e.tensor_tensor(bk.t[:, c0:c0 + 128], bk.t[:, c0:c0 + 128], mk.t[:, :], ALU.add),
                                 reads=[bk, mk], writes=[bk])
                        jb = (h * 16 + Q) * 64 + kt
                        P.op("act", lambda e: e.activation(p_.t[:, c0:512], bk.t[:, c0:512], AF.Exp, bias=negB.t[:, jb:jb + 1], scale=1.0),
                             reads=[bk, negB], writes=[p_])

                    def emit_PV(idx, it_no):
                        Q, kt = items[idx]
                        d = kt - 4 * Q
                        c0 = 128 * d if d >= 0 else 0
                        p_ = pt[it_no % 3]
                        ob_ = bank[3 + Q % 2]
                        last = (kt == 4 * Q + 3)
                        P.op("pe", lambda e: e.matmul(ob_.t[0:128, c0:512], v_.t[:, kt * 65:kt * 65 + 128], p_.t[:, c0:512], start=(kt == 0), stop=last),
                             reads=[v_, p_], writes=[ob_], pe_acc=(kt > 0))
                        if last:
                            o_ = oo[Q % 2]
                            P.op("act", lambda e: e.activation(drow.t[64:65, :], ob_.t[64:65, :], AF.Copy), reads=[ob_], writes=[drow])
                            P.op("pe", lambda e: e.matmul(bank[5].t[0:64, :], onesf.t[64:65, 0:64], drow.t[64:65, :], start=True, stop=True),
                                 reads=[onesf, drow], writes=[bank[5]])
                            P.op("dve", lambda e: e.reciprocal(rec.t[:, :], bank[5].t[0:64, :]), reads=[bank[5]], writes=[rec])
                            P.op("dve", lambda e: e.tensor_tensor(o_.t[:, :], ob_.t[0:64, :], rec.t[:, :], ALU.mult), reads=[ob_, rec], writes=[o_])
                            P.dma("sp", o_loc.ap()[Q // 4][h * 64:(h + 1) * 64, (Q % 4) * 512:(Q % 4 + 1) * 512], o_.t[:, :], reads=[o_], ow=olb)

                    n = len(items)
                    emit_S(0, nit)
                    for idx in range(n):
                        if idx + 1 < n:
                            emit_S(idx + 1, nit + idx + 1)
                        emit_PV(idx, nit + idx)
                    return nit + n

                nit = 0
                junk = Buf()
                for h in range(4):
                    if h + 1 < 4:
                        load_head(h + 1)
                    nit = do_head(h, qa[h % 2], ka[h % 2], va[h % 2], nit)
            P.barrier()
            return gather("fox_oG", o_loc, olb, 4, 256, T, BF16)

        def hgrn_phase(R):
            qG, qGb = gather("hg_qG", R.q, R.qb, 4, 256, T, BF16)
            lG, lGb = gather("hg_lG", R.lf, R.lfb, 8, 128, T, F32)
            vG, vGb = gather("hg_vG", R.v, R.vb, 4, 512, D, BF16)
            qS, qSb = dsel("hg_qS", [1, D, T], BF16, qG.ap()[bass.ds(g4, 1), :, :], qGb)
            lS, lSb = dsel("hg_lS", [2, 512, T], F32, lG.ap()[bass.ds(g4 * 2, 2), :, :], lGb)
            vS, vSb = idram("hg_vS", [S, 256], BF16), Buf()
            for j in range(4):
                P.dma("sp", vS.ap().rearrange("(r j i) c -> j r i c", r=4, j=4)[j],
                      vG.ap()[j].rearrange("(r i) c -> r i c", r=4)[:, :, bass.ds(g4 * 256, 256)], reads=[vGb], writes=[vSb])
            o_loc, olb = idram("hg_o", [4, 256, T], BF16), Buf()
            m01d, rmd, idd = din("m01", [128, 64]), din("rm", [128, 2048]), din("ident", [128, 128], BF16)
            NB = 2048
            bankA, bankO, bankU = c.bank[0:2], c.bank[2:4], c.bank[4:6]
            with ExitStack() as e2:
                m01 = P.sb(e2, "m01_sb", [128, 64], F32)
                rm = P.sb(e2, "rm_sb", [128, NB], F32)
                ident = P.sb(e2, "ident_sb", [128, 128], BF16)
                P.dma("sp", m01.t[:, :], m01d, writes=[m01])
                P.dma("sp", rm.t[:, :], rmd, writes=[rm])
                P.dma("sp", ident.t[:, :], idd, writes=[ident])
                sh = Ctx()
                sh.qb = P.sb(e2, "hqb", [128, NB], BF16)
                sh.kb = P.sb(e2, "hkb", [128, NB], F32)
                sh.lf = P.sb(e2, "hlf", [128, NB], F32)
                sh.G = P.sb(e2, "hG", [128, NB], F32)
                sh.tmp = P.sb(e2, "htmp", [128, NB], F32)
                sh.tmp2 = P.sb(e2, "htmp2", [128, NB], F32)
                sh.kend = P.sb(e2, "hkend", [128, NB], BF16)
                hs = []
                for h in range(2):
                    o = Ctx()
                    o.qd = P.sb(e2, "hqd%d" % h, [128, NB], BF16)
                    o.kdd = P.sb(e2, "hkdd%d" % h, [128, NB], BF16)
                    o.kT = P.sb(e2, "hkT%d" % h, [128, 16, 128], BF16)
                    o.vb = P.sb(e2, "hvb%d" % h, [128, 16, 128], BF16)
                    o.egl = P.sb(e2, "hegl%d" % h, [128, 32], F32)
                    o.S32 = P.sb(e2, "hS32_%d" % h, [128, 128], F32)
                    o.Sbf = [P.sb(e2, "hSbf%d_%d" % (h, i), [128, 128], BF16) for i in range(2)]
                    o.am = [P.sb(e2, "ham%d_%d" % (h, i), [128, 64], BF16) for i in range(2)]
                    o.osb = [P.sb(e2, "hosb%d_%d" % (h, i), [128, 512], BF16) for i in range(2)]
                    P.op("pool", lambda e, o=o: e.memset(o.S32.t[:, :], 0.0), writes=[o.S32])
                    P.op("pool", lambda e, o=o: e.memset(o.Sbf[0].t[:, :], 0.0), writes=[o.Sbf[0]])
                    o.si = 0
                    hs.append(o)
                vGv = vS.ap().rearrange("(t p) d -> p t d", p=128)

                def prep(h, blk):
                    o = hs[h]
                    P.dma("sp", sh.qb.t[:, :], qS.ap()[0, blk * 256 + h * 128:blk * 256 + (h + 1) * 128, :], reads=[qSb], writes=[sh.qb])
                    P.dma("sp", sh.lf.t[:, :], lS.ap()[h, blk * 128:(blk + 1) * 128, :], reads=[lSb], writes=[sh.lf])
                    P.dma("pool", o.vb.t[:, :, :], vGv[:, blk * 16:(blk + 1) * 16, h * 128:(h + 1) * 128], reads=[vSb], writes=[o.vb])
                    P.op("act", lambda e: e.activation(sh.kb.t[:, :], sh.lf.t[:, :], AF.Exp), reads=[sh.lf], writes=[sh.kb])
                    P.op("dve", lambda e: e.tensor_scalar(sh.kb.t[:, :], sh.kb.t[:, :], -1.0, 1.0, ALU.mult, ALU.add), reads=[sh.kb], writes=[sh.kb])
                    P.op("dve", lambda e: e.tensor_tensor_scan(sh.G.t[:, :], rm.t[:, :], sh.lf.t[:, :], 0.0, ALU.mult, ALU.add),
                         reads=[rm, sh.lf], writes=[sh.G])
                    P.op("act", lambda e: e.activation(sh.tmp.t[:, :], sh.G.t[:, :], AF.Exp), reads=[sh.G], writes=[sh.tmp])
                    P.op("dve", lambda e: e.tensor_tensor(o.qd.t[:, :], sh.qb.t[:, :], sh.tmp.t[:, :], ALU.mult), reads=[sh.qb, sh.tmp], writes=[o.qd])
                    P.op("act", lambda e: e.activation(sh.tmp2.t[:, :], sh.G.t[:, :], AF.Exp, scale=-1.0), reads=[sh.G], writes=[sh.tmp2])
                    P.op("dve", lambda e: e.tensor_tensor(sh.tmp2.t[:, :], sh.kb.t[:, :], sh.tmp2.t[:, :], ALU.mult), reads=[sh.kb, sh.tmp2], writes=[sh.tmp2])
                    P.op("dve", lambda e: e.tensor_copy(o.kdd.t[:, :], sh.tmp2.t[:, :]), reads=[sh.tmp2], writes=[o.kdd])
                    G3 = sh.G.t[:, :].rearrange("p (c s) -> p c s", s=64)
                    P.op("act", lambda e: e.activation(o.egl.t[:, :], G3[:, :, 63], AF.Exp), reads=[sh.G], writes=[o.egl])
                    for cc in range(32):
                        P.op("dve", lambda e, cc=cc: e.tensor_scalar(sh.kend.t[:, cc * 64:(cc + 1) * 64], sh.tmp2.t[:, cc * 64:(cc + 1) * 64],
                                                                     o.egl.t[:, cc:cc + 1], None, ALU.mult),
                             reads=[sh.tmp2, o.egl], writes=[sh.kend])
                    for grp in range(2):
                        for j in range(8):
                            tk = grp * 8 + j
                            P.op("pe", lambda e, tk=tk, j=j: e.transpose(bankT.t[:, j * 128:(j + 1) * 128], sh.kend.t[:, tk * 128:(tk + 1) * 128], ident.t[:, :]),
                                 reads=[sh.kend, ident], writes=[bankT], pe_acc=(j > 0))
                        P.op("act", lambda e, grp=grp: e.activation(o.kT.t[:, grp * 8:(grp + 1) * 8, :],
                                                                   bankT.t[:, :].rearrange("p (t k) -> p t k", k=128), AF.Copy),
                             reads=[bankT], writes=[o.kT])

                nA = [0]

                def chunk(h, blk, cc):
                    o = hs[h]
                    tk, half = cc // 2, cc % 2
                    pb = 64 * half
                    gc = blk * 32 + cc
                    cs = slice(cc * 64, (cc + 1) * 64)
                    bA = bankA[nA[0] % 2]
                    am = o.am[nA[0] % 2]
                    nA[0] += 1
                    bO = bankO[h]
                    bU = bankU[h]
                    oc0 = (gc % 8) * 64
                    Sb = o.Sbf[o.si % 2]
                    Sn = o.Sbf[(o.si + 1) % 2]
                    o.si += 1
                    P.op("pe", lambda e: e.matmul(bA.t[pb:pb + 64, 0:64], o.kdd.t[:, cs], o.qd.t[:, cs], start=True, stop=True),
                         reads=[o.kdd, o.qd], writes=[bA])
                    P.op("dve", lambda e: e.tensor_tensor(am.t[pb:pb + 64, :], bA.t[pb:pb + 64, 0:64], m01.t[pb:pb + 64, :], ALU.mult),
                         reads=[bA, m01], writes=[am])
                    P.op("pe", lambda e: e.matmul(bO.t[:, oc0:oc0 + 64], Sb.t[:, :], o.qd.t[:, cs], start=True, stop=False),
                         reads=[Sb, o.qd], writes=[bO], pe_acc=(gc % 8 != 0))
                    P.op("pe", lambda e: e.matmul(bO.t[:, oc0:oc0 + 64], o.vb.t[pb:pb + 64, tk, :], am.t[pb:pb + 64, :], start=False, stop=True),
                         reads=[o.vb, am], writes=[bO], pe_acc=True)
                    P.op("pe", lambda e: e.matmul(bU.t[:, 0:128], o.kT.t[pb:pb + 64, tk, :], o.vb.t[pb:pb + 64, tk, :], start=True, stop=True),
                         reads=[o.kT, o.vb], writes=[bU])
                    P.op("dve", lambda e: e.scalar_tensor_tensor(o.S32.t[:, :], o.S32.t[:, :], o.egl.t[:, cc:cc + 1], bU.t[:, 0:128], ALU.mult, ALU.add),
                         reads=[o.S32, o.egl, bU], writes=[o.S32])
                    P.op("act", lambda e: e.activation(Sn.t[:, :], o.S32.t[:, :], AF.Copy), reads=[o.S32], writes=[Sn])
                    if gc % 8 == 7:
                        os_ = o.osb[(gc // 8) % 2]
                        P.op("act", lambda e: e.activation(os_.t[:, :], bO.t[:, :], AF.Copy), reads=[bO], writes=[os_])
                        tok0 = (gc - 7) * 64
                        P.dma("sp", o_loc.ap()[tok0 // T][h * 128:(h + 1) * 128, tok0 % T:tok0 % T + 512], os_.t[:, :], reads=[os_], ow=olb)

                oG = idram("hg_oG", [4, 4 * 256, T], BF16)
                oGb = Buf()
                for blk in range(4):
                    for h in range(2):
                        prep(h, blk)
                    for cc in range(32):
                        for h in range(2):
                            chunk(h, blk, cc)
                    P.collective("AllGather", G4, o_loc.ap()[blk].opt(), oG.ap()[blk].opt(), [olb], oGb)
            P.barrier()
            return oG, oGb

        ffn(0, 0, 0)
        R1 = projections("fox", 0)
        oG1, oG1b = fox_phase(R1)
        epilogue("fox", 0, oG1, oG1b, R1.sg, R1.sgb)
        ffn(1, 0, 2)
        ffn(2, 1, 0)
        R2 = projections("hgrn", 1)
        oG2, oG2b = hgrn_phase(R2)
        epilogue("hgrn", 1, oG2, oG2b, R2.sg, R2.sgb)
        ffn(3, 1, 2)
        xo = dout("xo", [D, T])
        xob = Buf()
        outs.append(xob)
        xov = xo.rearrange("(c p) t -> p c t", p=128)
        fgd = din("fg", [128, 8])
        fg = P.sb(es, "fg_sb", [128, 8], F32)
        P.dma("sp", fg.t[:, :], fgd, writes=[fg])
        P.op("dve", lambda e: e.tensor_scalar(fg.t[:, :], fg.t[:, :], SQD, None, ALU.mult), reads=[fg], writes=[fg])
        yo = [P.sb(es, "yo%d" % i, [128, 512], F32) for i in range(2)]
        it = 0
        for tt in range(4):
            t0 = tt * 512
            rstd_tile(lambda kc, t0=t0: X[:, kc, t0:t0 + 512], [c.Xb[kc][tt] for kc in range(8)], EPS * D)
            for kc in range(8):
                y_ = yo[it % 2]
                it += 1
                P.op("dve", lambda e, kc=kc, y_=y_, t0=t0: e.scalar_tensor_tensor(
                    y_.t[:, :], X[:, kc, t0:t0 + 512], fg.t[:, kc:kc + 1], c.rstd.t[:, :], ALU.mult, ALU.mult),
                    reads=[c.Xb[kc][tt], fg, c.rstd], writes=[y_])
                P.dma("sp", xov[:, kc, t0:t0 + 512], y_.t[:, :], reads=[y_], ow=xob)
        P.finish(outs)
        P.emit()
    return nc


_DBG = {}


def _run(nc, maps):
    return run_bass_kernel_spmd(nc, maps, core_ids=list(range(NCORES))).results


def kernel_unfused(x, c, ada_w, ada_b, norm_g, ffn_w_up, ffn_w_down, fox_w_in, fox_b_f, fox_w_out,
           hgrn_w_in, hgrn_norm_g, hgrn_w_out, hgrn_lb_logits, final_norm_g):
    f32 = lambda a: np.ascontiguousarray(np.asarray(a, dtype=np.float32))
    x, c, ada_w, ada_b, norm_g = f32(x), f32(c), f32(ada_w), f32(ada_b), f32(norm_g)
    ffn_w_up, ffn_w_down, fox_w_in, fox_b_f, fox_w_out = f32(ffn_w_up), f32(ffn_w_down), f32(fox_w_in), f32(fox_b_f), f32(fox_w_out)
    hgrn_w_in, hgrn_norm_g, hgrn_w_out = f32(hgrn_w_in), f32(hgrn_norm_g), f32(hgrn_w_out)
    hgrn_lb_logits, final_norm_g = f32(hgrn_lb_logits), f32(final_norm_g)

    mod = run_mod(c, ada_w, ada_b)
    _DBG["mod"] = mod
    modT = [fm_cols(mod[b].reshape(18, D)) for b in range(B)]
    gT = fm_cols(norm_g.reshape(6, D))
    cores = [(b, t) for b in range(B) for t in range(4)]

    nc1 = build_F({"ffns": [(0, 0)], "proj": ("fox", 0)})
    wi = fox_w_in[0]
    shared = {
        "gT": gT, "wup0": tile_wup(ffn_w_up[0, 0]), "wdn0": tile_wdn(ffn_w_down[0, 0]),
        "wq": tile_w_fm(wi[:, 0:D]), "wk": tile_w_fm(wi[:, D:2 * D]), "wg": tile_w_fm(wi[:, 3 * D:4 * D]),
        "wv": tile_w_tm(wi[:, 2 * D:3 * D]),
        "wf": np.ascontiguousarray(wi[:, 4 * D:4 * D + 16].reshape(8, 128, 16).transpose(1, 0, 2)).reshape(128, 128),
        "bf": np.ascontiguousarray(fox_b_f[0].reshape(16, 1)),
    }
    maps = []
    for (b, t) in cores:
        m = dict(shared)
        m["xT"] = np.ascontiguousarray(x[b, t * T:(t + 1) * T, :].T)
        m["modT"] = modT[b]
        maps.append(m)
    r1 = _run(nc1, maps)
    _DBG["r1"] = r1

    def cat_fm(res, name, b):
        return np.concatenate([res[b * 4 + t][name] for t in range(4)], axis=1)

    def cat_tm(res, name, b):
        return np.concatenate([res[b * 4 + t][name] for t in range(4)], axis=0)

    nc2 = build_fox()
    U, sel, mk = fox_consts()
    maps = []
    for b in range(B):
        qf = cat_fm(r1, "qT", b).reshape(FH, FD, S)
        kf = cat_fm(r1, "kT", b).reshape(FH, FD, S)
        vf = cat_tm(r1, "v", b)
        lf = cat_fm(r1, "lf", b)
        for g in range(4):
            l4 = lf[4 * g:4 * g + 4]
            v4 = np.stack([np.ascontiguousarray(vf[:, hd * FD:(hd + 1) * FD].reshape(64, 128, FD).transpose(1, 0, 2)).reshape(128, 64 * FD)
                           for hd in range(4 * g, 4 * g + 4)])
            maps.append({
                "q": np.ascontiguousarray(qf[4 * g:4 * g + 4]), "k": np.ascontiguousarray(kf[4 * g:4 * g + 4]), "v": v4,
                "lt": np.ascontiguousarray(l4.reshape(4, 64, 128).transpose(2, 0, 1)).reshape(128, 256),
                "lq": np.ascontiguousarray(l4.reshape(4, 16, 512).transpose(1, 0, 2)).reshape(16, 2048),
                "U": U, "sel": sel, "mk": mk,
            })
    r2 = _run(nc2, maps)
    _DBG["r2"] = r2
    ofull = [np.concatenate([r2[b * 4 + g]["o"].reshape(4 * FD, S) for g in range(4)], axis=0) for b in range(B)]

    nc3 = build_F({"epi": ("fox", 0), "ffns": [(0, 2), (1, 0)], "proj": ("hgrn", 1)})
    hi = hgrn_w_in[0]
    shared = {
        "gT": gT, "wo": tile_wo(fox_w_out[0]),
        "wup0": tile_wup(ffn_w_up[0, 1]), "wdn0": tile_wdn(ffn_w_down[0, 1]),
        "wup1": tile_wup(ffn_w_up[1, 0]), "wdn1": tile_wdn(ffn_w_down[1, 0]),
        "wq": tile_w_fm(hi[:, 0:D]), "wf": tile_w_fm(hi[:, D:2 * D]), "wg": tile_w_fm(hi[:, 3 * D:4 * D]),
        "wv": tile_w_tm(hi[:, 2 * D:3 * D]), "lbl": fm_cols(hgrn_lb_logits),
    }
    maps = []
    for i, (b, t) in enumerate(cores):
        m = dict(shared)
        m["xT"] = r1[i]["xo"]
        m["modT"] = modT[b]
        m["oT"] = np.ascontiguousarray(ofull[b][:, t * T:(t + 1) * T])
        m["sg"] = r1[i]["sgo"]
        maps.append(m)
    r3 = _run(nc3, maps)
    _DBG["r3"] = r3

    nc4 = build_hgrn()
    m01, rm, ident = hgrn_consts()
    maps = []
    for b in range(B):
        qf = cat_fm(r3, "qT", b).reshape(HH, 128, S)
        kf = cat_fm(r3, "kT", b).reshape(HH, 128, S)
        lf = cat_fm(r3, "lfT", b).reshape(HH, 128, S)
        vf = cat_tm(r3, "v", b)
        for g in range(4):
            v2 = np.stack([np.ascontiguousarray(vf[:, hd * 128:(hd + 1) * 128].reshape(64, 128, 128).transpose(1, 0, 2)).reshape(128, 64 * 128)
                           for hd in range(2 * g, 2 * g + 2)])
            maps.append({
                "q": np.ascontiguousarray(qf[2 * g:2 * g + 2]), "k": np.ascontiguousarray(kf[2 * g:2 * g + 2]),
                "lf": np.ascontiguousarray(lf[2 * g:2 * g + 2]), "v": v2, "m01": m01, "rm": rm, "ident": ident,
            })
    r4 = _run(nc4, maps)
    _DBG["r4"] = r4
    ofull = [np.concatenate([r4[b * 4 + g]["o"].reshape(256, S) for g in range(4)], axis=0) for b in range(B)]

    nc5 = build_F({"epi": ("hgrn", 1), "ffns": [(1, 2)], "final": True})
    shared = {
        "gT": gT, "wo": tile_wo(hgrn_w_out[0]), "hgn": fm_cols(hgrn_norm_g[0]),
        "wup0": tile_wup(ffn_w_up[1, 1]), "wdn0": tile_wdn(ffn_w_down[1, 1]),
        "fg": fm_cols(final_norm_g),
    }
    maps = []
    for i, (b, t) in enumerate(cores):
        m = dict(shared)
        m["xT"] = r3[i]["xo"]
        m["modT"] = modT[b]
        m["oT"] = np.ascontiguousarray(ofull[b][:, t * T:(t + 1) * T])
        m["sg"] = r3[i]["sgo"]
        maps.append(m)
    r5 = _run(nc5, maps)
    out = np.empty((B, S, D), np.float32)
    for i, (b, t) in enumerate(cores):
        out[b, t * T:(t + 1) * T, :] = r5[i]["xo"].T
    return out


def kernel(x, c, ada_w, ada_b, norm_g, ffn_w_up, ffn_w_down, fox_w_in, fox_b_f, fox_w_out,
           hgrn_w_in, hgrn_norm_g, hgrn_w_out, hgrn_lb_logits, final_norm_g):
    f32 = lambda a: np.ascontiguousarray(np.asarray(a, dtype=np.float32))
    x, c, ada_w, ada_b, norm_g = f32(x), f32(c), f32(ada_w), f32(ada_b), f32(norm_g)
    ffn_w_up, ffn_w_down, fox_w_in, fox_b_f, fox_w_out = f32(ffn_w_up), f32(ffn_w_down), f32(fox_w_in), f32(fox_b_f), f32(fox_w_out)
    hgrn_w_in, hgrn_norm_g, hgrn_w_out = f32(hgrn_w_in), f32(hgrn_norm_g), f32(hgrn_w_out)
    hgrn_lb_logits, final_norm_g = f32(hgrn_lb_logits), f32(final_norm_g)
    nc = build_mega()
    wi, hi = fox_w_in[0], hgrn_w_in[0]
    U, sel, mk = fox_consts()
    m01, rm, ident = hgrn_consts()
    shared = {
        "modb": fm_cols(ada_b.reshape(18, D)),
        "modw": np.ascontiguousarray(ada_w.reshape(2, 8, 128, 9, D).transpose(0, 3, 2, 1, 4)).reshape(18, 128, 8 * D),
        "gT": fm_cols(norm_g.reshape(6, D)),
        "wup0": tile_wup(ffn_w_up[0, 0]), "wdn0": tile_wdn(ffn_w_down[0, 0]),
        "wup1": tile_wup(ffn_w_up[0, 1]), "wdn1": tile_wdn(ffn_w_down[0, 1]),
        "wup2": tile_wup(ffn_w_up[1, 0]), "wdn2": tile_wdn(ffn_w_down[1, 0]),
        "wup3": tile_wup(ffn_w_up[1, 1]), "wdn3": tile_wdn(ffn_w_down[1, 1]),
        "fox_wq": tile_w_fm(wi[:, 0:D]), "fox_wk": tile_w_fm(wi[:, D:2 * D]), "fox_wg": tile_w_fm(wi[:, 3 * D:4 * D]),
        "fox_wv": tile_w_tm(wi[:, 2 * D:3 * D]),
        "fox_wf": np.ascontiguousarray(wi[:, 4 * D:4 * D + 16].reshape(8, 128, 16).transpose(1, 0, 2)).reshape(128, 128),
        "fox_bfb": np.ascontiguousarray(np.broadcast_to(np.tile(fox_b_f[0], 16), (128, 256))),
        "U": U, "sel": sel, "mk": mk, "identf": np.eye(128, dtype=np.float32),
        "fox_wo": tile_wo(fox_w_out[0]),
        "hgrn_wq": tile_w_fm(hi[:, 0:D]), "hgrn_wf": tile_w_fm(hi[:, D:2 * D]), "hgrn_wg": tile_w_fm(hi[:, 3 * D:4 * D]),
        "hgrn_wv": tile_w_tm(hi[:, 2 * D:3 * D]), "lbl": fm_cols(hgrn_lb_logits),
        "m01": m01, "rm": rm, "ident": ident,
        "hgrn_wo": tile_wo(hgrn_w_out[0]), "hgn": fm_cols(hgrn_norm_g[0]),
        "fg": fm_cols(final_norm_g),
    }
    maps = []
    cores = [(b, t) for b in range(B) for t in range(4)]
    for (b, t) in cores:
        m = dict(shared)
        m["xT"] = np.ascontiguousarray(x[b, t * T:(t + 1) * T, :].T)
        m["cT"] = fm_cols(c[b])
        maps.append(m)
    res = _run(nc, maps)
    out = np.empty((B, S, D), np.float32)
    for i, (b, t) in enumerate(cores):
        out[b, t * T:(t + 1) * T, :] = res[i]["xo"].T
    return out
```

```python
from contextlib import ExitStack
import numpy as np
import ml_dtypes
import concourse.bass as bass
import concourse.mybir as mybir
from concourse.bass_utils import run_bass_kernel_spmd

F32 = mybir.dt.float32
BF16 = mybir.dt.bfloat16
AF = mybir.ActivationFunctionType
ALU = mybir.AluOpType
NPBF = ml_dtypes.bfloat16

D = 1024
B = 2
S = 8192
DFF = 2816
NF = 22
EPS = 1e-6
NCORES = 8
T = 2048
FH = 16
FD = 64
HH = 8
CH = 64


class Buf:
    __slots__ = ("w", "r", "t", "key")

    def __init__(self, t=None):
        self.w = None
        self.r = []
        self.t = t
        self.key = None


class Prog:
    ENG = ["pe", "act", "dve", "pool", "sp"]

    def __init__(self, nc):
        self.nc = nc
        self.ops = {e: [] for e in self.ENG}
        self.clock = {e: {} for e in self.ENG}
        self.snaps = {}
        self.count = {}
        self.needed = set()
        self.nkey = 0
        self.final = None
        self.unit_keys = set()

    def sb(self, es, name, shape, dtype):
        self.nname = getattr(self, "nname", 0) + 1
        t = es.enter_context(self.nc.sbuf_tensor("%s_u%d" % (name, self.nname), list(shape), dtype))
        return Buf(t)

    def ps(self, es, name, shape, dtype):
        self.nname = getattr(self, "nname", 0) + 1
        t = es.enter_context(self.nc.psum_tensor("%s_u%d" % (name, self.nname), list(shape), dtype))
        return Buf(t)

    def newkey(self, buf):
        fk = getattr(self, "free_keys", None)
        if fk is None:
            self.free_keys, self.key_owner = [], {}
            fk = self.free_keys
        if fk:
            k = fk.pop()
        else:
            self.nkey += 1
            k = "d%d" % self.nkey
        buf.key = k
        self.key_owner[k] = buf
        return k

    def op(self, eng, fn, reads=(), writes=(), dma=None, pe_acc=False):
        need = {}

        def req(ev):
            if ev is None:
                return
            k, s = ev
            if s > need.get(k, 0):
                need[k] = s

        for b in reads:
            req(b.w)
        for b in writes:
            if not (pe_acc and b.w is not None and b.w[0] == "pe"):
                req(b.w)
            for r in b.r:
                req(r)
        key = dma or eng
        if fn is None:
            idx = 0
        else:
            idx = self.count.get(key, 0) + 1
            self.count[key] = idx
        ck = self.clock[eng]
        waits = []
        for k, s in need.items():
            if ck.get(k, 0) < s:
                waits.append((k, s))
        for k, s in waits:
            sn = self.snaps[(k, s)]
            for kk, ss in sn.items():
                if ck.get(kk, 0) < ss:
                    ck[kk] = ss
            if ck.get(k, 0) < s:
                ck[k] = s
            self.needed.add((k, s))
        self.ops[eng].append((fn, waits, key, idx))
        if fn is None:
            return None
        self.snaps[(key, idx)] = dict(ck)
        ev = (key, idx)
        for b in reads:
            b.r.append(ev)
        for b in writes:
            b.w = ev
            b.r = []
        return ev

    def dma(self, eng, out, in_, reads=(), writes=(), ow=None):
        wb = ow if ow is not None else writes[0]
        if wb.key is None:
            self.newkey(wb)
        def _fn(e, out=out, in_=in_):
            try:
                return e.dma_start(out=out, in_=in_)
            except Exception:
                print("DMA FAIL", out, in_)
                raise
        ev = self.op(eng, _fn, reads=reads, writes=writes, dma=wb.key)
        if ow is not None:
            ow.w = ev
        return ev

    def collective(self, kind, groups, src_ap, dst_ap, reads, wbuf):
        wbuf.key = "cc"
        self.unit_keys.add(wbuf.key)
        return self.op("pool", lambda e: e.collective_compute(kind, ALU.bypass, replica_groups=groups,
                                                              ins=[src_ap], outs=[dst_ap]),
                       reads=reads, writes=[wbuf], dma=wbuf.key)

    def finish(self, outs, eng="sp"):
        self.op(eng, None, reads=list(outs))

    def barrier(self):
        need = dict(self.count)
        for e in self.ENG:
            self.op(e, None, extra=need)
        for k, b in list(getattr(self, "key_owner", {}).items()):
            b.key = None
            self.free_keys.append(k)
        if hasattr(self, "key_owner"):
            self.key_owner.clear()

    def emit(self):
        nc = self.nc
        keys = list(self.count.keys())
        for e in self.ENG:
            if e not in keys:
                keys.append(e)
        rank = {}
        for k in keys:
            if k in self.ENG:
                idxs = sorted(s for (kk, s) in self.needed if kk == k)
                rank[k] = {s: i + 1 for i, s in enumerate(idxs)}
        with ExitStack() as es:
            sems = {k: es.enter_context(nc.semaphore("s_" + k)) for k in keys}
            block = es.enter_context(nc.Block())

            def run(eng_name):
                def body(e):
                    for fn, waits, key, idx in self.ops[eng_name]:
                        for k, s in waits:
                            v = rank[k][s] if k in rank else (s if k in self.unit_keys else 16 * s)
                            e.wait_ge(sems[k], v)
                        if fn is None:
                            continue
                        ins = fn(e)
                        if key in rank:
                            if (key, idx) in self.needed:
                                ins.then_inc(sems[key], 1)
                        elif key in self.unit_keys:
                            ins.then_inc(sems[key], 1)
                        else:
                            ins.then_inc(sems[key], 16)
                return body

            block.tensor(run("pe"))
            block.scalar(run("act"))
            block.vector(run("dve"))
            block.gpsimd(run("pool"))
            block.sync(run("sp"))


def _patch_op():
    base = Prog.op

    def op(self, eng, fn, reads=(), writes=(), dma=None, pe_acc=False, extra=None):
        if extra:
            dummy = []
            for k, s in extra.items():
                if s > 0:
                    b = Buf()
                    b.w = (k, s)
                    dummy.append(b)
            reads = list(reads) + dummy
            ev = base(self, eng, fn, reads=reads, writes=writes, dma=dma, pe_acc=pe_acc)
            return ev
        return base(self, eng, fn, reads=reads, writes=writes, dma=dma, pe_acc=pe_acc)

    Prog.op = op


_patch_op()


SQD = float(np.sqrt(D))


class Ctx:
    pass


def mcol(l, v, ch):
    return (l * 9 + v) * 8 + ch


def build_F(cfg):
    nc = bass.Bass("TRN2", target_bir_lowering=False)
    P = Prog(nc)
    c = Ctx()
    c.P, c.nc = P, nc
    dr = {}

    def din(name, shape, dt=F32):
        dr[name] = nc.dram_tensor(name, list(shape), dt, kind="ExternalInput").ap()
        return dr[name]

    def dout(name, shape, dt=F32):
        dr[name] = nc.dram_tensor(name, list(shape), dt, kind="ExternalOutput").ap()
        return dr[name]

    xT = din("xT", [D, T])
    modT = din("modT", [128, 144])
    gT = din("gT", [128, 48])
    outs = []
    with ExitStack() as es:
        X = es.enter_context(nc.sbuf_tensor("X", [128, 8, T], F32))
        c.X = X
        c.Xb = [[Buf(X) for _ in range(4)] for _ in range(8)]
        c.modt = P.sb(es, "modt", [128, 144], F32)
        c.gt = P.sb(es, "gt", [128, 48], F32)
        c.der = P.sb(es, "der", [128, 96], F32)
        c.ones = P.sb(es, "ones", [128, 128], BF16)
        c.bank = [P.ps(es, "bank%d" % i, [128, 512], F32) for i in range(8)]
        sqt = es.enter_context(nc.sbuf_tensor("sq", [128, 8, 512], BF16))
        c.sq = [Buf(sqt) for _ in range(8)]
        c.rstd = P.sb(es, "rstd", [128, 512], F32)
        c.tmp = [P.sb(es, "tmp%d" % i, [128, 512], F32) for i in range(2)]

        xv = xT.rearrange("(c p) t -> p c t", p=128)
        for kc in range(8):
            P.dma("sp", X[:, kc, :], xv[:, kc, :], writes=[c.Xb[kc][tt] for tt in range(4)])
        P.dma("sp", c.modt.t[:, :], modT, writes=[c.modt])
        P.dma("sp", c.gt.t[:, :], gT, writes=[c.gt])
        P.op("pool", lambda e: e.memset(c.ones.t[:, :], 1.0), writes=[c.ones])
        for l in range(2):
            for sub in range(3):
                base = ((l * 3 + sub) * 2) * 8
                sc0 = mcol(l, sub * 3 + 1, 0)
                g0 = (l * 3 + sub) * 8
                ga0 = mcol(l, sub * 3 + 2, 0)
                P.op("dve", lambda e, base=base, sc0=sc0, g0=g0: e.scalar_tensor_tensor(
                    c.der.t[:, base:base + 8], c.modt.t[:, sc0:sc0 + 8], 1.0, c.gt.t[:, g0:g0 + 8], ALU.add, ALU.mult),
                    reads=[c.modt, c.gt], writes=[c.der])
                P.op("dve", lambda e, base=base: e.tensor_scalar(
                    c.der.t[:, base:base + 8], c.der.t[:, base:base + 8], SQD, None, ALU.mult),
                    reads=[c.der], writes=[c.der])
                P.op("dve", lambda e, base=base, ga0=ga0, sub=sub: e.tensor_scalar(
                    c.der.t[:, base + 8:base + 16], c.modt.t[:, ga0:ga0 + 8], (1.0 if sub == 1 else 0.5), None, ALU.mult),
                    reads=[c.modt], writes=[c.der])

        def Acol(l, sub, ch):
            j = ((l * 3 + sub) * 2) * 8 + ch
            return c.der.t[:, j:j + 1]

        def Gcol(l, sub, ch):
            j = ((l * 3 + sub) * 2 + 1) * 8 + ch
            return c.der.t[:, j:j + 1]

        def Scol(l, sub, ch):
            j = mcol(l, sub * 3 + 0, ch)
            return c.modt.t[:, j:j + 1]

        def rstd_tile(src_fn, src_bufs, epsk):
            for kc in range(8):
                P.op("act", lambda e, kc=kc: e.activation(sqt[:, kc, :], src_fn(kc), AF.Square),
                     reads=[src_bufs[kc]], writes=[c.sq[kc]])
            for kc in range(8):
                P.op("pe", lambda e, kc=kc: e.matmul(c.bank[6].t[:, :], c.ones.t[:, :], sqt[:, kc, :],
                                                      start=(kc == 0), stop=(kc == 7)),
                     reads=[c.ones, c.sq[kc]], writes=[c.bank[6]], pe_acc=(kc > 0))
            P.op("dve", lambda e: e.tensor_scalar(c.rstd.t[:, :], c.bank[6].t[:, :], epsk, None, ALU.add),
                 reads=[c.bank[6]], writes=[c.rstd])
            P.op("act", lambda e: e.activation(c.rstd.t[:, :], c.rstd.t[:, :], AF.Sqrt), reads=[c.rstd], writes=[c.rstd])
            P.op("dve", lambda e: e.reciprocal(c.rstd.t[:, :], c.rstd.t[:, :]), reads=[c.rstd], writes=[c.rstd])

        def modnorm_tile(l, sub, tt, hdst, hbuf):
            t0 = tt * 512
            rstd_tile(lambda kc: X[:, kc, t0:t0 + 512], [c.Xb[kc][tt] for kc in range(8)], EPS * D)
            for kc in range(8):
                tb = c.tmp[kc % 2]
                P.op("dve", lambda e, kc=kc, tb=tb: e.tensor_tensor(tb.t[:, :], X[:, kc, t0:t0 + 512], c.rstd.t[:, :], ALU.mult),
                     reads=[c.Xb[kc][tt], c.rstd], writes=[tb])
                P.op("act", lambda e, kc=kc, tb=tb: e.activation(hdst(kc), tb.t[:, :], AF.Identity,
                                                               bias=Scol(l, sub, kc), scale=Acol(l, sub, kc)),
                     reads=[tb, c.der, c.modt], writes=[hbuf(kc)])

        def epilogue(kind, l):
            oT = din("oT", [D, T])
            sgd = din("sg", [D, T], BF16)
            wod = din("wo", [128, 8192])
            ov = oT.rearrange("(c p) t -> p c t", p=128)
            sv = sgd.rearrange("(c p) t -> p c t", p=128)
            with ExitStack() as e2:
                wo = P.sb(e2, "wo_sb", [128, 8, 1024], BF16)
                P.dma("pool", wo.t[:, :, :], wod.rearrange("p (k d) -> p k d", k=8), writes=[wo])
                ot = [P.sb(e2, "ot%d" % i, [128, 8, 512], F32) for i in range(2)]
                st = [P.sb(e2, "st%d" % i, [128, 8, 512], BF16) for i in range(2)]
                ogt = [e2.enter_context(nc.sbuf_tensor("og%d" % i, [128, 8, 512], BF16)) for i in range(2)]
                ogb = [[Buf(ogt[i]) for _ in range(8)] for i in range(2)]
                if kind == "hgrn":
                    hgd = din("hgn", [128, 8])
                    hg = P.sb(e2, "hg", [128, 8], F32)
                    P.dma("sp", hg.t[:, :], hgd, writes=[hg])
                    P.op("dve", lambda e: e.tensor_scalar(hg.t[:, :], hg.t[:, :], float(np.sqrt(128.0)), None, ALU.mult),
                         reads=[hg], writes=[hg])
                    sq1 = P.sb(e2, "sq1", [128, 512], BF16)
                    r1 = P.sb(e2, "r1", [128, 512], F32)
                    t1 = P.sb(e2, "t1", [128, 512], F32)
                for tt in range(4):
                    t0 = tt * 512
                    o_, s_, og_ = ot[tt % 2], st[tt % 2], ogt[tt % 2]
                    P.dma("sp", o_.t[:, :, :], ov[:, :, t0:t0 + 512], writes=[o_])
                    P.dma("sp", s_.t[:, :, :], sv[:, :, t0:t0 + 512], writes=[s_])
                    if kind == "fox":
                        for kc in range(8):
                            P.op("dve", lambda e, kc=kc, o_=o_, s_=s_, og_=og_: e.tensor_tensor(
                                og_[:, kc, :], o_.t[:, kc, :], s_.t[:, kc, :], ALU.mult),
                                reads=[o_, s_], writes=[ogb[tt % 2][kc]])
                    else:
                        for kc in range(8):
                            P.op("act", lambda e, kc=kc, o_=o_: e.activation(sq1.t[:, :], o_.t[:, kc, :], AF.Square),
                                 reads=[o_], writes=[sq1])
                            P.op("pe", lambda e: e.matmul(c.bank[7].t[:, :], c.ones.t[:, :], sq1.t[:, :], start=True, stop=True),
                                 reads=[c.ones, sq1], writes=[c.bank[7]])
                            P.op("dve", lambda e: e.tensor_scalar(r1.t[:, :], c.bank[7].t[:, :], EPS * 128.0, None, ALU.add),
                                 reads=[c.bank[7]], writes=[r1])
                            P.op("act", lambda e: e.activation(r1.t[:, :], r1.t[:, :], AF.Sqrt), reads=[r1], writes=[r1])
                            P.op("dve", lambda e: e.reciprocal(r1.t[:, :], r1.t[:, :]), reads=[r1], writes=[r1])
                            P.op("dve", lambda e, kc=kc, o_=o_: e.tensor_tensor(t1.t[:, :], o_.t[:, kc, :], r1.t[:, :], ALU.mult),
                                 reads=[o_, r1], writes=[t1])
                            P.op("dve", lambda e, kc=kc, s_=s_, og_=og_: e.scalar_tensor_tensor(
                                og_[:, kc, :], t1.t[:, :], hg.t[:, kc:kc + 1], s_.t[:, kc, :], ALU.mult, ALU.mult),
                                reads=[t1, hg, s_], writes=[ogb[tt % 2][kc]])
                    for dc in range(8):
                        bk = c.bank[4 + dc % 2]
                        for kc in range(8):
                            P.op("pe", lambda e, kc=kc, dc=dc, bk=bk, og_=og_: e.matmul(
                                bk.t[:, :], wo.t[:, kc, dc * 128:(dc + 1) * 128], og_[:, kc, :],
                                start=(kc == 0), stop=(kc == 7)),
                                reads=[wo, ogb[tt % 2][kc]], writes=[bk], pe_acc=(kc > 0))
                        P.op("dve", lambda e, dc=dc, bk=bk, t0=t0: e.scalar_tensor_tensor(
                            X[:, dc, t0:t0 + 512], bk.t[:, :], Gcol(l, 1, dc), X[:, dc, t0:t0 + 512], ALU.mult, ALU.add),
                            reads=[bk, c.der, c.Xb[dc][tt]], writes=[c.Xb[dc][tt]])
            P.barrier()

        def ffn(j, l, sub):
            wupd = din("wup%d" % j, [11, 128, 4096])
            wdnd = din("wdn%d" % j, [8, 128, 2816])
            with ExitStack() as e2:
                hbt = e2.enter_context(nc.sbuf_tensor("hb_%d" % j, [128, 8, 1024], BF16))
                hbb = [[Buf(hbt) for _ in range(2)] for _ in range(8)]
                actt = e2.enter_context(nc.sbuf_tensor("actb_%d" % j, [128, NF, 1024], BF16))
                actb = [[Buf(actt) for _ in range(2)] for _ in range(NF)]
                wu = [P.sb(e2, "wu%d_%d" % (j, i), [128, 2, 8, 256], BF16) for i in range(2)]
                wd = [P.sb(e2, "wd%d_%d" % (j, i), [128, NF, 128], BF16) for i in range(2)]
                sa = [P.sb(e2, "sa%d_%d" % (j, i), [128, 512], F32) for i in range(2)]
                for half in range(2):
                    for t2 in range(2):
                        tt = half * 2 + t2
                        modnorm_tile(l, sub, tt, lambda kc, t2=t2: hbt[:, kc, t2 * 512:(t2 + 1) * 512],
                                     lambda kc, t2=t2: hbb[kc][t2])
                    it = 0
                    for g in range(11):
                        w_ = wu[g % 2]
                        P.dma("pool", w_.t[:, :, :, :], wupd[g].rearrange("p (a k f) -> p a k f", a=2, k=8), writes=[w_])
                        for jf in range(2):
                            fc = 2 * g + jf
                            for t2 in range(2):
                                bA, bB = c.bank[it % 2], c.bank[2 + it % 2]
                                s_ = sa[it % 2]
                                it += 1
                                for kc in range(8):
                                    P.op("pe", lambda e, kc=kc, w_=w_, jf=jf, t2=t2, bA=bA: e.matmul(
                                        bA.t[:, :], w_.t[:, 0, kc, jf * 128:(jf + 1) * 128], hbt[:, kc, t2 * 512:(t2 + 1) * 512],
                                        start=(kc == 0), stop=(kc == 7)),
                                        reads=[w_, hbb[kc][t2]], writes=[bA], pe_acc=(kc > 0))
                                for kc in range(8):
                                    P.op("pe", lambda e, kc=kc, w_=w_, jf=jf, t2=t2, bB=bB: e.matmul(
                                        bB.t[:, :], w_.t[:, 1, kc, jf * 128:(jf + 1) * 128], hbt[:, kc, t2 * 512:(t2 + 1) * 512],
                                        start=(kc == 0), stop=(kc == 7)),
                                        reads=[w_, hbb[kc][t2]], writes=[bB], pe_acc=(kc > 0))
                                P.op("act", lambda e, s_=s_, bA=bA: e.activation(s_.t[:, :], bA.t[:, :], AF.Silu),
                                     reads=[bA], writes=[s_])
                                P.op("dve", lambda e, s_=s_, bB=bB, fc=fc, t2=t2: e.tensor_tensor(
                                    actt[:, fc, t2 * 512:(t2 + 1) * 512], bB.t[:, :], s_.t[:, :], ALU.mult),
                                    reads=[bB, s_], writes=[actb[fc][t2]])
                    for dc in range(8):
                        w_ = wd[dc % 2]
                        P.dma("pool", w_.t[:, :, :], wdnd[dc].rearrange("p (f d) -> p f d", f=NF), writes=[w_])
                        for t2 in range(2):
                            tt = half * 2 + t2
                            t0 = tt * 512
                            bk = c.bank[4 + (dc * 2 + t2) % 2]
                            for fc in range(NF):
                                P.op("pe", lambda e, fc=fc, w_=w_, t2=t2, bk=bk: e.matmul(
                                    bk.t[:, :], w_.t[:, fc, :], actt[:, fc, t2 * 512:(t2 + 1) * 512],
                                    start=(fc == 0), stop=(fc == NF - 1)),
                                    reads=[w_, actb[fc][t2]], writes=[bk], pe_acc=(fc > 0))
                            P.op("dve", lambda e, dc=dc, bk=bk, t0=t0: e.scalar_tensor_tensor(
                                X[:, dc, t0:t0 + 512], bk.t[:, :], Gcol(l, sub, dc), X[:, dc, t0:t0 + 512], ALU.mult, ALU.add),
                                reads=[bk, c.der, c.Xb[dc][tt]], writes=[c.Xb[dc][tt]])
            P.barrier()

        def proj_fm(wname, hbt, hbb, evac, n_oc=8):
            wd_ = din(wname, [n_oc, 128, 1024])
            with ExitStack() as e3:
                wp = [P.sb(e3, wname + "_sb%d" % i, [128, 8, 128], BF16) for i in range(2)]
                it = 0
                for oc in range(n_oc):
                    w_ = wp[oc % 2]
                    P.dma("pool", w_.t[:, :, :], wd_[oc].rearrange("p (k f) -> p k f", k=8), writes=[w_])
                    for tt in range(4):
                        bk = c.bank[it % 4]
                        it += 1
                        for kc in range(8):
                            P.op("pe", lambda e, kc=kc, w_=w_, tt=tt, bk=bk: e.matmul(
                                bk.t[:, :], w_.t[:, kc, :], hbt[:, kc, tt * 512:(tt + 1) * 512],
                                start=(kc == 0), stop=(kc == 7)),
                                reads=[w_, hbb[kc][tt]], writes=[bk], pe_acc=(kc > 0))
                        evac(oc, tt, bk)
                P.barrier()

        def proj_tm(wname, hbt, hbb, vout, vob, func):
            wd_ = din(wname, [2, 128, 4096])
            with ExitStack() as e3:
                wv = P.sb(e3, wname + "_sb", [128, 2, 8, 512], BF16)
                for cg in range(2):
                    P.dma("pool", wv.t[:, cg, :, :], wd_[cg].rearrange("p (k f) -> p k f", k=8), writes=[wv])
                vt = [P.sb(e3, "vt%d" % i, [128, 512], BF16) for i in range(2)]
                it = 0
                for tk in range(16):
                    for cg in range(2):
                        bk = c.bank[it % 4]
                        v_ = vt[it % 2]
                        it += 1
                        for kc in range(8):
                            P.op("pe", lambda e, kc=kc, tk=tk, cg=cg, bk=bk: e.matmul(
                                bk.t[:, :], hbt[:, kc, tk * 128:(tk + 1) * 128], wv.t[:, cg, kc, :],
                                start=(kc == 0), stop=(kc == 7)),
                                reads=[wv, hbb[kc][tk // 4]], writes=[bk], pe_acc=(kc > 0))
                        P.op("act", lambda e, bk=bk, v_=v_: e.activation(v_.t[:, :], bk.t[:, :], func),
                             reads=[bk], writes=[v_])
                        P.dma("sp", vout[tk * 128:(tk + 1) * 128, cg * 512:(cg + 1) * 512], v_.t[:, :], reads=[v_], ow=vob)
                P.barrier()

        def stage_out(e3, name, shape, dt):
            return [P.sb(e3, name + "%d" % i, shape, dt) for i in range(2)]

        def projections(kind, l):
            with ExitStack() as e2:
                hbt = e2.enter_context(nc.sbuf_tensor("hb2", [128, 8, T], BF16))
                hbb = [[Buf(hbt) for _ in range(4)] for _ in range(8)]
                for tt in range(4):
                    modnorm_tile(l, 1, tt, lambda kc, tt=tt: hbt[:, kc, tt * 512:(tt + 1) * 512],
                                 lambda kc, tt=tt: hbb[kc][tt])
                qo = dout("qT", [D, T], BF16)
                qob = Buf()
                outs.append(qob)
                sgo = dout("sgo", [D, T], BF16)
                sgob = Buf()
                outs.append(sgob)
                vo = dout("v", [T, D], BF16)
                vob = Buf()
                outs.append(vob)
                cnt = [0]

                def simple_evac(od, ob, func, scale, st, dt_eng="act"):
                    def evac(oc, tt, bk):
                        s_ = st[cnt[0] % 2]
                        cnt[0] += 1
                        P.op("act", lambda e, s_=s_, bk=bk: e.activation(s_.t[:, :], bk.t[:, :], func, scale=scale),
                             reads=[bk], writes=[s_])
                        P.dma("sp", od[oc * 128:(oc + 1) * 128, tt * 512:(tt + 1) * 512], s_.t[:, :], reads=[s_], ow=ob)
                    return evac

                stb = stage_out(e2, "stb", [128, 512], BF16)
                if kind == "fox":
                    ko = dout("kT", [D, T], BF16)
                    kob = Buf()
                    outs.append(kob)
                    lfo = dout("lf", [16, T], F32)
                    lfob = Buf()
                    outs.append(lfob)
                    proj_fm("wq", hbt, hbb, simple_evac(qo, qob, AF.Copy, float(FD ** -0.5), stb))
                    proj_fm("wk", hbt, hbb, simple_evac(ko, kob, AF.Copy, 1.0, stb))
                    proj_fm("wg", hbt, hbb, simple_evac(sgo, sgob, AF.Sigmoid, 1.0, stb))
                    proj_tm("wv", hbt, hbb, vo, vob, AF.Copy)
                    wfd = din("wf", [128, 128])
                    bfd = din("bf", [16, 1])
                    wf = P.sb(e2, "wf_sb", [128, 8, 16], BF16)
                    P.dma("pool", wf.t[:, :, :], wfd.rearrange("p (k f) -> p k f", k=8), writes=[wf])
                    nbf = P.sb(e2, "nbf", [16, 1], F32)
                    P.dma("sp", nbf.t[:, :], bfd, writes=[nbf])
                    P.op("dve", lambda e: e.tensor_scalar(nbf.t[:, :], nbf.t[:, :], -1.0, None, ALU.mult), reads=[nbf], writes=[nbf])
                    e1 = P.sb(e2, "e1", [16, 512], F32)
                    l1 = [P.sb(e2, "l1_%d" % i, [16, 512], F32) for i in range(2)]
                    for tt in range(4):
                        bk = c.bank[tt % 4]
                        for kc in range(8):
                            P.op("pe", lambda e, kc=kc, tt=tt, bk=bk: e.matmul(
                                bk.t[0:16, :], wf.t[:, kc, :], hbt[:, kc, tt * 512:(tt + 1) * 512],
                                start=(kc == 0), stop=(kc == 7)),
                                reads=[wf, hbb[kc][tt]], writes=[bk], pe_acc=(kc > 0))
                        l_ = l1[tt % 2]
                        P.op("act", lambda e, bk=bk: e.activation(e1.t[:, :], bk.t[0:16, :], AF.Exp, bias=nbf.t[:, 0:1], scale=-1.0),
                             reads=[bk, nbf], writes=[e1])
                        P.op("act", lambda e, l_=l_: e.activation(l_.t[:, :], e1.t[:, :], AF.Ln, bias=1.0, scale=1.0),
                             reads=[e1], writes=[l_])
                        P.op("dve", lambda e, l_=l_: e.tensor_scalar(l_.t[:, :], l_.t[:, :], -1.0, None, ALU.mult),
                             reads=[l_], writes=[l_])
                        P.dma("sp", lfo[:, tt * 512:(tt + 1) * 512], l_.t[:, :], reads=[l_], ow=lfob)
                else:
                    ko = dout("kT", [D, T], F32)
                    kob = Buf()
                    outs.append(kob)
                    lfo = dout("lfT", [D, T], F32)
                    lfob = Buf()
                    outs.append(lfob)
                    lbd = din("lbl", [128, 16])
                    lbl = P.sb(e2, "lbl_sb", [128, 16], F32)
                    lb = P.sb(e2, "lb", [128, 8], F32)
                    oml = P.sb(e2, "oml", [128, 8], F32)
                    P.dma("sp", lbl.t[:, :], lbd, writes=[lbl])
                    P.op("dve", lambda e: e.tensor_tensor(lb.t[:, :], lbl.t[:, 8:16], lbl.t[:, 0:8], ALU.subtract), reads=[lbl], writes=[lb])
                    P.op("act", lambda e: e.activation(lb.t[:, :], lb.t[:, :], AF.Sigmoid), reads=[lb], writes=[lb])
                    P.op("dve", lambda e: e.tensor_scalar(oml.t[:, :], lb.t[:, :], -1.0, 1.0, ALU.mult, ALU.add), reads=[lb], writes=[oml])
                    proj_fm("wq", hbt, hbb, simple_evac(qo, qob, AF.Copy, 1.0, stb))
                    proj_fm("wg", hbt, hbb, simple_evac(sgo, sgob, AF.Silu, 1.0, stb))
                    proj_tm("wv", hbt, hbb, vo, vob, AF.Silu)
                    sg1 = P.sb(e2, "sg1", [128, 512], F32)
                    ff = stage_out(e2, "ff", [128, 512], F32)
                    lff = stage_out(e2, "lff", [128, 512], F32)
                    kk = stage_out(e2, "kk", [128, 512], F32)

                    def f_evac(oc, tt, bk):
                        i = cnt[0] % 2
                        cnt[0] += 1
                        f_, l_, k_ = ff[i], lff[i], kk[i]
                        P.op("act", lambda e, bk=bk: e.activation(sg1.t[:, :], bk.t[:, :], AF.Sigmoid), reads=[bk], writes=[sg1])
                        P.op("dve", lambda e, f_=f_, oc=oc: e.tensor_scalar(f_.t[:, :], sg1.t[:, :], oml.t[:, oc:oc + 1], lb.t[:, oc:oc + 1], ALU.mult, ALU.add),
                             reads=[sg1, oml, lb], writes=[f_])
                        P.op("act", lambda e, f_=f_, l_=l_: e.activation(l_.t[:, :], f_.t[:, :], AF.Ln), reads=[f_], writes=[l_])
                        P.op("dve", lambda e, f_=f_, k_=k_: e.tensor_scalar(k_.t[:, :], f_.t[:, :], -1.0, 1.0, ALU.mult, ALU.add),
                             reads=[f_], writes=[k_])
                        P.dma("sp", lfo[oc * 128:(oc + 1) * 128, tt * 512:(tt + 1) * 512], l_.t[:, :], reads=[l_], ow=lfob)
                        P.dma("sp", ko[oc * 128:(oc + 1) * 128, tt * 512:(tt + 1) * 512], k_.t[:, :], reads=[k_], ow=kob)
                    proj_fm("wf", hbt, hbb, f_evac)
            P.barrier()

        if cfg.get("epi"):
            epilogue(cfg["epi"][0], cfg["epi"][1])
        for j, (l, sub) in enumerate(cfg["ffns"]):
            ffn(j, l, sub)
        if cfg.get("proj"):
            projections(cfg["proj"][0], cfg["proj"][1])
        xo = dout("xo", [D, T])
        xob = Buf()
        outs.append(xob)
        xov = xo.rearrange("(c p) t -> p c t", p=128)
        if cfg.get("final"):
            fgd = din("fg", [128, 8])
            fg = P.sb(es, "fg_sb", [128, 8], F32)
            P.dma("sp", fg.t[:, :], fgd, writes=[fg])
            P.op("dve", lambda e: e.tensor_scalar(fg.t[:, :], fg.t[:, :], SQD, None, ALU.mult), reads=[fg], writes=[fg])
            yo = [P.sb(es, "yo%d" % i, [128, 512], F32) for i in range(2)]
            it = 0
            for tt in range(4):
                t0 = tt * 512
                rstd_tile(lambda kc, t0=t0: X[:, kc, t0:t0 + 512], [c.Xb[kc][tt] for kc in range(8)], EPS * D)
                for kc in range(8):
                    y_ = yo[it % 2]
                    it += 1
                    P.op("dve", lambda e, kc=kc, y_=y_, t0=t0: e.scalar_tensor_tensor(
                        y_.t[:, :], X[:, kc, t0:t0 + 512], fg.t[:, kc:kc + 1], c.rstd.t[:, :], ALU.mult, ALU.mult),
                        reads=[c.Xb[kc][tt], fg, c.rstd], writes=[y_])
                    P.dma("sp", xov[:, kc, t0:t0 + 512], y_.t[:, :], reads=[y_], ow=xob)
        else:
            for kc in range(8):
                P.dma("sp", xov[:, kc, :], X[:, kc, :], reads=[c.Xb[kc][tt] for tt in range(4)], ow=xob)
        P.finish(outs)
        P.emit()
    return nc


MC = 2304


def build_mod():
    nc = bass.Bass("TRN2", target_bir_lowering=False)
    P = Prog(nc)
    cT = nc.dram_tensor("cT", [128, 16], F32, kind="ExternalInput").ap()
    w = nc.dram_tensor("w", [128, 8 * MC], F32, kind="ExternalInput").ap()
    bias = nc.dram_tensor("bias", [2, MC], F32, kind="ExternalInput").ap()
    mo = nc.dram_tensor("mo", [2, MC], F32, kind="ExternalOutput").ap()
    with ExitStack() as es:
        ct = P.sb(es, "ct", [128, 8, 2], F32)
        wt = [P.sb(es, "wt%d" % i, [128, 8, 384], F32) for i in range(6)]
        bt = P.sb(es, "bt", [2, MC], F32)
        ot = P.sb(es, "ot", [2, MC], F32)
        banks = [P.ps(es, "bk%d" % i, [128, 512], F32) for i in range(2)]
        P.dma("sp", ct.t[:, :, :], cT.rearrange("p (k b) -> p k b", k=8), writes=[ct])
        P.dma("sp", bt.t[:, :], bias, writes=[bt])
        wv = w.rearrange("p (k n) -> p k n", k=8)
        for i in range(6):
            P.dma("sp" if i % 2 == 0 else "pool", wt[i].t[:, :, :], wv[:, :, i * 384:(i + 1) * 384], writes=[wt[i]])
        P.op("act", lambda e: e.activation(ct.t[:, :, :], ct.t[:, :, :], AF.Silu), reads=[ct], writes=[ct])
        for i in range(6):
            bk = banks[i % 2]
            for kc in range(8):
                P.op("pe", lambda e, kc=kc, i=i, bk=bk: e.matmul(bk.t[0:2, 0:384], ct.t[:, kc, :], wt[i].t[:, kc, :],
                                                                 start=(kc == 0), stop=(kc == 7)),
                     reads=[ct, wt[i]], writes=[bk], pe_acc=(kc > 0))
            P.op("dve", lambda e, i=i, bk=bk: e.tensor_tensor(ot.t[:, i * 384:(i + 1) * 384], bk.t[0:2, 0:384],
                                                             bt.t[:, i * 384:(i + 1) * 384], ALU.add),
                 reads=[bk, bt], writes=[ot])
        ob = Buf()
        P.dma("sp", mo, ot.t[:, :], reads=[ot], ow=ob)
        P.finish([ob])
        P.emit()
    return nc


def run_mod(c, ada_w, ada_b):
    nc = build_mod()
    cT = np.ascontiguousarray(c.T.reshape(8, 128, B).transpose(1, 0, 2)).reshape(128, 16)
    wall = np.concatenate([ada_w[0], ada_w[1]], axis=1)
    ball = np.concatenate([ada_b[0], ada_b[1]], axis=0)
    maps = []
    for j in range(NCORES):
        wj = wall[:, j * MC:(j + 1) * MC].reshape(8, 128, MC).transpose(1, 0, 2)
        maps.append({"cT": cT, "w": np.ascontiguousarray(wj).reshape(128, 8 * MC),
                     "bias": np.ascontiguousarray(np.broadcast_to(ball[j * MC:(j + 1) * MC], (2, MC)))})
    res = run_bass_kernel_spmd(nc, maps, core_ids=list(range(NCORES)))
    mod = np.concatenate([r["mo"] for r in res.results], axis=1)
    return mod.reshape(B, 2, 9, D)


def fm_cols(v):
    lead = int(np.prod(v.shape[:-1])) if v.ndim > 1 else 1
    a = v.reshape(lead, 8, 128).transpose(2, 0, 1)
    return np.ascontiguousarray(a).reshape(128, lead * 8)


def tile_w_fm(w):
    n = w.shape[1] // 128
    a = w.reshape(8, 128, n, 128).transpose(2, 1, 0, 3)
    return np.ascontiguousarray(a).reshape(n, 128, 1024)


def tile_w_tm(w):
    a = w.reshape(8, 128, 2, 512).transpose(2, 1, 0, 3)
    return np.ascontiguousarray(a).reshape(2, 128, 4096)


def tile_wup(w):
    a = w.reshape(8, 128, 2, 11, 256).transpose(3, 1, 2, 0, 4)
    return np.ascontiguousarray(a).reshape(11, 128, 4096)


def tile_wdn(w):
    a = w.reshape(NF, 128, 8, 128).transpose(2, 1, 0, 3)
    return np.ascontiguousarray(a).reshape(8, 128, NF * 128)


def tile_wo(w):
    a = w.reshape(8, 128, D).transpose(1, 0, 2)
    return np.ascontiguousarray(a).reshape(128, 8 * D)


NEG = -30000.0


def build_fox():
    nc = bass.Bass("TRN2", target_bir_lowering=False)
    P = Prog(nc)
    qd = nc.dram_tensor("q", [4, 64, S], BF16, kind="ExternalInput").ap()
    kd = nc.dram_tensor("k", [4, 64, S], BF16, kind="ExternalInput").ap()
    vd = nc.dram_tensor("v", [4, 128, 64 * 64], BF16, kind="ExternalInput").ap()
    ltd = nc.dram_tensor("lt", [128, 256], F32, kind="ExternalInput").ap()
    lqd = nc.dram_tensor("lq", [16, 2048], F32, kind="ExternalInput").ap()
    Ud = nc.dram_tensor("U", [128, 128], F32, kind="ExternalInput").ap()
    seld = nc.dram_tensor("sel", [128, 128], F32, kind="ExternalInput").ap()
    mkd = nc.dram_tensor("mk", [128, 128], F32, kind="ExternalInput").ap()
    od = nc.dram_tensor("o", [4, 64, S], F32, kind="ExternalOutput").ap()
    shi = nc.dram_tensor("shi", [4, S], BF16).ap()
    slo = nc.dram_tensor("slo", [4, S], BF16).ap()
    with ExitStack() as es:
        bank = [P.ps(es, "bank%d" % i, [128, 512], F32) for i in range(8)]
        U = P.sb(es, "U_sb", [128, 128], F32)
        sel = P.sb(es, "sel_sb", [128, 128], F32)
        mk = P.sb(es, "mk_sb", [128, 128], F32)
        onesf = P.sb(es, "onesf", [128, 128], F32)
        lt = P.sb(es, "lt_sb", [128, 256], F32)
        lq = P.sb(es, "lq_sb", [16, 2048], F32)
        within = P.sb(es, "within", [128, 256], F32)
        tot = P.sb(es, "tot", [128, 256], F32)
        inc = P.sb(es, "inc", [128, 256], F32)
        GT = P.sb(es, "GT", [128, 256], F32)
        gend = P.sb(es, "gend", [128, 256], F32)
        negB = P.sb(es, "negB", [128, 4 * 16 * 64], F32)
        cl = P.sb(es, "cl", [16, 2048], F32)
        Aa = P.sb(es, "Aa", [16, 2048], F32)
        ahi = P.sb(es, "ahi", [16, 2048], BF16)
        ahf = P.sb(es, "ahf", [16, 2048], F32)
        alo = P.sb(es, "alo", [16, 2048], BF16)
        qa = [P.sb(es, "qa%d" % i, [66, S], BF16) for i in range(2)]
        ka = [P.sb(es, "ka%d" % i, [66, S], BF16) for i in range(2)]
        va = [P.sb(es, "va%d" % i, [128, 64, 65], BF16) for i in range(2)]
        pt = [P.sb(es, "pt%d" % i, [128, 512], BF16) for i in range(3)]
        drow = P.sb(es, "drow", [65, 512], F32)
        rec = P.sb(es, "rec", [64, 512], F32)
        oo = [P.sb(es, "oo%d" % i, [64, 512], F32) for i in range(2)]
        ob = Buf()

        for t_, d_ in ((U, Ud), (sel, seld), (mk, mkd), (lt, ltd), (lq, lqd)):
            P.dma("sp", t_.t[:, :], d_, writes=[t_])
        P.op("pool", lambda e: e.memset(onesf.t[:, :], 1.0), writes=[onesf])
        P.op("pe", lambda e: e.matmul(bank[6].t[:, 0:256], U.t[:, :], lt.t[:, :], start=True, stop=True), reads=[U, lt], writes=[bank[6]])
        P.op("pe", lambda e: e.matmul(bank[7].t[:, 0:256], onesf.t[:, :], lt.t[:, :], start=True, stop=True), reads=[onesf, lt], writes=[bank[7]])
        P.op("dve", lambda e: e.tensor_copy(within.t[:, :], bank[6].t[:, 0:256]), reads=[bank[6]], writes=[within])
        P.op("dve", lambda e: e.tensor_copy(tot.t[:, :], bank[7].t[:, 0:256]), reads=[bank[7]], writes=[tot])
        for h in range(4):
            P.op("dve", lambda e, h=h: e.tensor_tensor_scan(inc.t[:, h * 64:(h + 1) * 64], onesf.t[:, 0:64], tot.t[:, h * 64:(h + 1) * 64],
                                                            0.0, ALU.mult, ALU.add), reads=[onesf, tot], writes=[inc])
        P.op("dve", lambda e: e.tensor_tensor(GT.t[:, :], within.t[:, :], inc.t[:, :], ALU.add), reads=[within, inc], writes=[GT])
        P.op("dve", lambda e: e.tensor_tensor(GT.t[:, :], GT.t[:, :], tot.t[:, :], ALU.subtract), reads=[GT, tot], writes=[GT])
        P.op("pe", lambda e: e.matmul(bank[6].t[:, 0:256], sel.t[:, :], GT.t[:, :], start=True, stop=True), reads=[sel, GT], writes=[bank[6]])
        P.op("dve", lambda e: e.tensor_copy(gend.t[:, :], bank[6].t[:, 0:256]), reads=[bank[6]], writes=[gend])
        for h in range(4):
            for Q in range(16):
                j0 = (h * 16 + Q) * 64
                gc = h * 64 + 4 * Q + 3
                P.op("dve", lambda e, h=h, j0=j0, gc=gc: e.tensor_scalar(
                    negB.t[:, j0:j0 + 64], GT.t[:, h * 64:(h + 1) * 64], -1.0, gend.t[:, gc:gc + 1], ALU.mult, ALU.add),
                    reads=[GT, gend], writes=[negB])
        ones16 = P.sb(es, "ones16", [16, 512], F32)
        P.op("pool", lambda e: e.memset(ones16.t[:, :], 1.0), writes=[ones16])
        for h in range(4):
            P.op("dve", lambda e, h=h: e.tensor_tensor_scan(cl.t[:, h * 512:(h + 1) * 512], ones16.t[:, :], lq.t[:, h * 512:(h + 1) * 512],
                                                            0.0, ALU.mult, ALU.add), reads=[lq, ones16], writes=[cl])
        for h in range(4):
            P.op("dve", lambda e, h=h: e.tensor_scalar(Aa.t[:, h * 512:(h + 1) * 512], cl.t[:, h * 512:(h + 1) * 512],
                                                       cl.t[:, h * 512 + 511:h * 512 + 512], None, ALU.subtract),
                 reads=[cl], writes=[Aa])
        P.op("dve", lambda e: e.tensor_copy(ahi.t[:, :], Aa.t[:, :]), reads=[Aa], writes=[ahi])
        P.op("dve", lambda e: e.tensor_copy(ahf.t[:, :], ahi.t[:, :]), reads=[ahi], writes=[ahf])
        P.op("dve", lambda e: e.tensor_tensor(alo.t[:, :], Aa.t[:, :], ahf.t[:, :], ALU.subtract), reads=[Aa, ahf], writes=[alo])
        shb, slb = Buf(), Buf()
        P.dma("sp", shi.rearrange("h (q m) -> q h m", q=16), ahi.t[:, :].rearrange("q (h m) -> q h m", h=4), reads=[ahi], writes=[shb])
        P.dma("sp", slo.rearrange("h (q m) -> q h m", q=16), alo.t[:, :].rearrange("q (h m) -> q h m", h=4), reads=[alo], writes=[slb])

        for i in range(2):
            P.op("pool", lambda e, i=i: e.memset(ka[i].t[64:66, :], 1.0), writes=[ka[i]])
            P.op("pool", lambda e, i=i: e.memset(va[i].t[:, :, 64:65], 1.0), writes=[va[i]])

        def load_head(h):
            q_, k_, v_ = qa[h % 2], ka[h % 2], va[h % 2]
            P.dma("sp", q_.t[0:64, :], qd[h], writes=[q_])
            P.dma("sp", q_.t[64:65, :], shi[h:h + 1, :], reads=[shb], writes=[q_])
            P.dma("sp", q_.t[65:66, :], slo[h:h + 1, :], reads=[slb], writes=[q_])
            P.dma("pool", k_.t[0:64, :], kd[h], writes=[k_])
            P.dma("pool", v_.t[:, :, 0:64], vd[h].rearrange("p (t d) -> p t d", d=64), writes=[v_])

        load_head(0)

        def do_head(h, q_, k_, v_, nit):
            items = [(Q, kt) for Q in range(16) for kt in range(4 * Q + 4)]

            def emit_S(idx, it_no):
                Q, kt = items[idx]
                d = kt - 4 * Q
                c0 = 128 * d if d >= 0 else 0
                bk = bank[it_no % 3]
                p_ = pt[it_no % 3]
                P.op("pe", lambda e: e.matmul(bk.t[:, c0:512], k_.t[0:66, kt * 128:(kt + 1) * 128],
                                              q_.t[0:66, Q * 512 + c0:(Q + 1) * 512], start=True, stop=True),
                     reads=[k_, q_], writes=[bk])
                if d >= 0:
                    P.op("dve", lambda e: e.tensor_tensor(bk.t[:, c0:c0 + 128], bk.t[:, c0:c0 + 128], mk.t[:, :], ALU.add),
                         reads=[bk, mk], writes=[bk])
                jb = (h * 16 + Q) * 64 + kt
                P.op("act", lambda e: e.activation(p_.t[:, c0:512], bk.t[:, c0:512], AF.Exp, bias=negB.t[:, jb:jb + 1], scale=1.0),
                     reads=[bk, negB], writes=[p_])

            def emit_PV(idx, it_no):
                Q, kt = items[idx]
                d = kt - 4 * Q
                c0 = 128 * d if d >= 0 else 0
                p_ = pt[it_no % 3]
                ob_ = bank[3 + Q % 2]
                last = (kt == 4 * Q + 3)
                P.op("pe", lambda e: e.matmul(ob_.t[0:65, c0:512], v_.t[:, kt, :], p_.t[:, c0:512], start=(kt == 0), stop=last),
                     reads=[v_, p_], writes=[ob_], pe_acc=(kt > 0))
                if last:
                    o_ = oo[Q % 2]
                    P.op("act", lambda e: e.activation(drow.t[64:65, :], ob_.t[64:65, :], AF.Copy), reads=[ob_], writes=[drow])
                    P.op("pe", lambda e: e.matmul(bank[5].t[0:64, :], onesf.t[64:65, 0:64], drow.t[64:65, :], start=True, stop=True),
                         reads=[onesf, drow], writes=[bank[5]])
                    P.op("dve", lambda e: e.reciprocal(rec.t[:, :], bank[5].t[0:64, :]), reads=[bank[5]], writes=[rec])
                    P.op("dve", lambda e: e.tensor_tensor(o_.t[:, :], ob_.t[0:64, :], rec.t[:, :], ALU.mult), reads=[ob_, rec], writes=[o_])
                    P.dma("sp", od[h][:, Q * 512:(Q + 1) * 512], o_.t[:, :], reads=[o_], ow=ob)

            n = len(items)
            emit_S(0, nit)
            for idx in range(n):
                if idx + 1 < n:
                    emit_S(idx + 1, nit + idx + 1)
                emit_PV(idx, nit + idx)
            return nit + n

        nit = 0
        for h in range(4):
            if h + 1 < 4:
                load_head(h + 1)
            nit = do_head(h, qa[h % 2], ka[h % 2], va[h % 2], nit)
        P.finish([ob])
        P.emit()
    return nc


def fox_consts():
    k = np.arange(128)
    U = (k[:, None] <= k[None, :]).astype(np.float32)
    sel = np.zeros((128, 128), np.float32)
    sel[127, :] = 1.0
    mk = np.where(k[None, :] >= k[:, None], 0.0, NEG).astype(np.float32)
    return U, sel, mk


def build_hgrn():
    nc = bass.Bass("TRN2", target_bir_lowering=False)
    P = Prog(nc)
    qd = nc.dram_tensor("q", [2, 128, S], BF16, kind="ExternalInput").ap()
    kd = nc.dram_tensor("k", [2, 128, S], F32, kind="ExternalInput").ap()
    lfd = nc.dram_tensor("lf", [2, 128, S], F32, kind="ExternalInput").ap()
    vd = nc.dram_tensor("v", [2, 128, 64 * 128], BF16, kind="ExternalInput").ap()
    m01d = nc.dram_tensor("m01", [128, 64], F32, kind="ExternalInput").ap()
    rmd = nc.dram_tensor("rm", [128, 2048], F32, kind="ExternalInput").ap()
    idd = nc.dram_tensor("ident", [128, 128], BF16, kind="ExternalInput").ap()
    od = nc.dram_tensor("o", [2, 128, S], F32, kind="ExternalOutput").ap()
    NB = 2048
    with ExitStack() as es:
        bankA = [P.ps(es, "bankA%d" % i, [128, 512], F32) for i in range(2)]
        bankO = [P.ps(es, "bankO%d" % i, [128, 512], F32) for i in range(2)]
        bankU = [P.ps(es, "bankU%d" % i, [128, 512], F32) for i in range(2)]
        bankT = P.ps(es, "bankT", [128, 1024], BF16)
        m01 = P.sb(es, "m01_sb", [128, 64], F32)
        rm = P.sb(es, "rm_sb", [128, NB], F32)
        ident = P.sb(es, "ident_sb", [128, 128], BF16)
        P.dma("sp", m01.t[:, :], m01d, writes=[m01])
        P.dma("sp", rm.t[:, :], rmd, writes=[rm])
        P.dma("sp", ident.t[:, :], idd, writes=[ident])
        ob = Buf()
        hs = []
        for h in range(2):
            o = Ctx()
            o.qb = P.sb(es, "qb%d" % h, [128, NB], BF16)
            o.kb = P.sb(es, "kb%d" % h, [128, NB], F32)
            o.lf = P.sb(es, "lf%d" % h, [128, NB], F32)
            o.G = P.sb(es, "G%d" % h, [128, NB], F32)
            o.tmp = P.sb(es, "tmp%d" % h, [128, NB], F32)
            o.tmp2 = P.sb(es, "tmp2%d" % h, [128, NB], F32)
            o.qd = P.sb(es, "qd%d" % h, [128, NB], BF16)
            o.kdd = P.sb(es, "kdd%d" % h, [128, NB], BF16)
            o.kend = P.sb(es, "kend%d" % h, [128, NB], BF16)
            o.kT = P.sb(es, "kT%d" % h, [128, 16, 128], BF16)
            o.vb = P.sb(es, "vb%d" % h, [128, 16, 128], BF16)
            o.egl = P.sb(es, "egl%d" % h, [128, 32], F32)
            o.S32 = P.sb(es, "S32_%d" % h, [128, 128], F32)
            o.Sbf = [P.sb(es, "Sbf%d_%d" % (h, i), [128, 128], BF16) for i in range(2)]
            o.am = [P.sb(es, "am%d_%d" % (h, i), [128, 64], BF16) for i in range(2)]
            o.osb = [P.sb(es, "osb%d_%d" % (h, i), [128, 512], F32) for i in range(2)]
            P.op("pool", lambda e, o=o: e.memset(o.S32.t[:, :], 0.0), writes=[o.S32])
            P.op("pool", lambda e, o=o: e.memset(o.Sbf[0].t[:, :], 0.0), writes=[o.Sbf[0]])
            o.si = 0
            hs.append(o)

        def prep(h, blk):
            o = hs[h]
            t0 = blk * NB
            P.dma("sp", o.qb.t[:, :], qd[h][:, t0:t0 + NB], writes=[o.qb])
            P.dma("sp", o.kb.t[:, :], kd[h][:, t0:t0 + NB], writes=[o.kb])
            P.dma("sp", o.lf.t[:, :], lfd[h][:, t0:t0 + NB], writes=[o.lf])
            P.dma("pool", o.vb.t[:, :, :], vd[h][:, blk * 2048:(blk + 1) * 2048].rearrange("p (t d) -> p t d", d=128), writes=[o.vb])
            P.op("dve", lambda e: e.tensor_tensor_scan(o.G.t[:, :], rm.t[:, :], o.lf.t[:, :], 0.0, ALU.mult, ALU.add),
                 reads=[rm, o.lf], writes=[o.G])
            P.op("act", lambda e: e.activation(o.tmp.t[:, :], o.G.t[:, :], AF.Exp), reads=[o.G], writes=[o.tmp])
            P.op("dve", lambda e: e.tensor_tensor(o.qd.t[:, :], o.qb.t[:, :], o.tmp.t[:, :], ALU.mult), reads=[o.qb, o.tmp], writes=[o.qd])
            P.op("act", lambda e: e.activation(o.tmp2.t[:, :], o.G.t[:, :], AF.Exp, scale=-1.0), reads=[o.G], writes=[o.tmp2])
            P.op("dve", lambda e: e.tensor_tensor(o.tmp2.t[:, :], o.kb.t[:, :], o.tmp2.t[:, :], ALU.mult), reads=[o.kb, o.tmp2], writes=[o.tmp2])
            P.op("dve", lambda e: e.tensor_copy(o.kdd.t[:, :], o.tmp2.t[:, :]), reads=[o.tmp2], writes=[o.kdd])
            G3 = o.G.t[:, :].rearrange("p (c s) -> p c s", s=64)
            P.op("act", lambda e: e.activation(o.egl.t[:, :], G3[:, :, 63], AF.Exp), reads=[o.G], writes=[o.egl])
            for cc in range(32):
                P.op("dve", lambda e, cc=cc: e.tensor_scalar(o.kend.t[:, cc * 64:(cc + 1) * 64], o.tmp2.t[:, cc * 64:(cc + 1) * 64],
                                                             o.egl.t[:, cc:cc + 1], None, ALU.mult),
                     reads=[o.tmp2, o.egl], writes=[o.kend])
            for grp in range(2):
                for j in range(8):
                    tk = grp * 8 + j
                    P.op("pe", lambda e, tk=tk, j=j: e.transpose(bankT.t[:, j * 128:(j + 1) * 128], o.kend.t[:, tk * 128:(tk + 1) * 128], ident.t[:, :]),
                         reads=[o.kend, ident], writes=[bankT], pe_acc=(j > 0))
                P.op("act", lambda e, grp=grp: e.activation(o.kT.t[:, grp * 8:(grp + 1) * 8, :],
                                                           bankT.t[:, :].rearrange("p (t k) -> p t k", k=128), AF.Copy),
                     reads=[bankT], writes=[o.kT])

        nA = [0]

        def chunk(h, blk, cc):
            o = hs[h]
            tk, half = cc // 2, cc % 2
            pb = 64 * half
            gc = blk * 32 + cc
            cs = slice(cc * 64, (cc + 1) * 64)
            bA = bankA[nA[0] % 2]
            am = o.am[nA[0] % 2]
            nA[0] += 1
            bO = bankO[h]
            bU = bankU[h]
            oc0 = (gc % 8) * 64
            Sb = o.Sbf[o.si % 2]
            Sn = o.Sbf[(o.si + 1) % 2]
            o.si += 1
            P.op("pe", lambda e: e.matmul(bA.t[pb:pb + 64, 0:64], o.kdd.t[:, cs], o.qd.t[:, cs], start=True, stop=True),
                 reads=[o.kdd, o.qd], writes=[bA])
            P.op("dve", lambda e: e.tensor_tensor(am.t[pb:pb + 64, :], bA.t[pb:pb + 64, 0:64], m01.t[pb:pb + 64, :], ALU.mult),
                 reads=[bA, m01], writes=[am])
            P.op("pe", lambda e: e.matmul(bO.t[:, oc0:oc0 + 64], Sb.t[:, :], o.qd.t[:, cs], start=True, stop=False),
                 reads=[Sb, o.qd], writes=[bO], pe_acc=(gc % 8 != 0))
            P.op("pe", lambda e: e.matmul(bO.t[:, oc0:oc0 + 64], o.vb.t[pb:pb + 64, tk, :], am.t[pb:pb + 64, :], start=False, stop=True),
                 reads=[o.vb, am], writes=[bO], pe_acc=True)
            P.op("pe", lambda e: e.matmul(bU.t[:, 0:128], o.kT.t[pb:pb + 64, tk, :], o.vb.t[pb:pb + 64, tk, :], start=True, stop=True),
                 reads=[o.kT, o.vb], writes=[bU])
            P.op("dve", lambda e: e.scalar_tensor_tensor(o.S32.t[:, :], o.S32.t[:, :], o.egl.t[:, cc:cc + 1], bU.t[:, 0:128], ALU.mult, ALU.add),
                 reads=[o.S32, o.egl, bU], writes=[o.S32])
            P.op("act", lambda e: e.activation(Sn.t[:, :], o.S32.t[:, :], AF.Copy), reads=[o.S32], writes=[Sn])
            if gc % 8 == 7:
                os_ = o.osb[(gc // 8) % 2]
                P.op("act", lambda e: e.activation(os_.t[:, :], bO.t[:, :], AF.Copy), reads=[bO], writes=[os_])
                tok0 = (gc - 7) * 64
                P.dma("sp", od[h][:, tok0:tok0 + 512], os_.t[:, :], reads=[os_], ow=ob)

        for blk in range(4):
            for h in range(2):
                prep(h, blk)
            for cc in range(32):
                for h in range(2):
                    chunk(h, blk, cc)
        P.finish([ob])
        P.emit()
    return nc


def hgrn_consts():
    p = np.arange(128)
    t = np.arange(64)
    m01 = ((p[:, None] % 64) <= t[None, :]).astype(np.float32)
    rm = np.ones((128, 2048), np.float32)
    rm[:, ::64] = 0.0
    ident = np.eye(128, dtype=np.float32).astype(NPBF)
    return m01, rm, ident


def build_mega():
    nc = bass.Bass("TRN2", target_bir_lowering=False)
    P = Prog(nc)
    c = Ctx()
    c.P, c.nc = P, nc
    dr = {}

    def din(name, shape, dt=F32):
        dr[name] = nc.dram_tensor(name, list(shape), dt, kind="ExternalInput").ap()
        return dr[name]

    def dout(name, shape, dt=F32):
        dr[name] = nc.dram_tensor(name, list(shape), dt, kind="ExternalOutput").ap()
        return dr[name]

    xT = din("xT", [D, T])
    cTd = din("cT", [128, 8])
    modbd = din("modb", [128, 144])
    modwd = din("modw", [18, 128, 8192])
    gT = din("gT", [128, 48])
    outs = []
    pid = nc.partition_id()
    g4 = pid % 4
    G4 = [[0, 1, 2, 3], [4, 5, 6, 7]]
    idram = lambda name, shape, dt: nc.dram_tensor(name, list(shape), dt)
    with ExitStack() as es:
        X = es.enter_context(nc.sbuf_tensor("X", [128, 8, T], F32))
        c.X = X
        c.Xb = [[Buf(X) for _ in range(4)] for _ in range(8)]
        c.modt = P.sb(es, "modt", [128, 144], F32)
        c.gt = P.sb(es, "gt", [128, 48], F32)
        c.der = P.sb(es, "der", [128, 96], F32)
        c.ones = P.sb(es, "ones", [128, 128], BF16)
        c.bank = [P.ps(es, "bank%d" % i, [128, 512], F32) for i in range(7)]
        bankT = P.ps(es, "bankT", [128, 1024], BF16)
        sqt = es.enter_context(nc.sbuf_tensor("sq", [128, 8, 512], BF16))
        c.sq = [Buf(sqt) for _ in range(8)]
        c.rstd = P.sb(es, "rstd", [128, 512], F32)
        c.tmp = [P.sb(es, "tmp%d" % i, [128, 512], F32) for i in range(2)]

        xv = xT.rearrange("(c p) t -> p c t", p=128)
        for kc in range(8):
            P.dma("sp", X[:, kc, :], xv[:, kc, :], writes=[c.Xb[kc][tt] for tt in range(4)])
        P.dma("sp", c.gt.t[:, :], gT, writes=[c.gt])
        P.op("pool", lambda e: e.memset(c.ones.t[:, :], 1.0), writes=[c.ones])
        with ExitStack() as e0:
            ct = P.sb(e0, "ct", [128, 8], F32)
            ctb = P.sb(e0, "ctb", [128, 8], BF16)
            mb = P.sb(e0, "mb", [128, 144], F32)
            mw = [P.sb(e0, "mw%d" % i, [128, 8, 1024], BF16) for i in range(2)]
            P.dma("sp", ct.t[:, :], cTd, writes=[ct])
            P.dma("sp", mb.t[:, :], modbd, writes=[mb])
            P.op("act", lambda e: e.activation(ctb.t[:, :], ct.t[:, :], AF.Silu), reads=[ct], writes=[ctb])
            bm = c.bank[5]
            for v in range(18):
                w_ = mw[v % 2]
                P.dma("pool", w_.t[:, :, :], modwd[v].rearrange("p (k n) -> p k n", k=8), writes=[w_])
                for ch in range(8):
                    col = v * 8 + ch
                    for kc in range(8):
                        P.op("pe", lambda e, w_=w_, ch=ch, kc=kc, col=col: e.matmul(
                            bm.t[:, col:col + 1], w_.t[:, kc, ch * 128:(ch + 1) * 128], ctb.t[:, kc:kc + 1],
                            start=(kc == 0), stop=(kc == 7)),
                            reads=[w_, ctb], writes=[bm], pe_acc=not (v == 0 and ch == 0 and kc == 0))
            P.op("dve", lambda e: e.tensor_tensor(c.modt.t[:, :], bm.t[:, 0:144], mb.t[:, :], ALU.add),
                 reads=[bm, mb], writes=[c.modt])
        P.barrier()
        for l in range(2):
            for sub in range(3):
                base = ((l * 3 + sub) * 2) * 8
                sc0 = mcol(l, sub * 3 + 1, 0)
                g0 = (l * 3 + sub) * 8
                ga0 = mcol(l, sub * 3 + 2, 0)
                P.op("dve", lambda e, base=base, sc0=sc0, g0=g0: e.scalar_tensor_tensor(
                    c.der.t[:, base:base + 8], c.modt.t[:, sc0:sc0 + 8], 1.0, c.gt.t[:, g0:g0 + 8], ALU.add, ALU.mult),
                    reads=[c.modt, c.gt], writes=[c.der])
                P.op("dve", lambda e, base=base: e.tensor_scalar(
                    c.der.t[:, base:base + 8], c.der.t[:, base:base + 8], SQD, None, ALU.mult),
                    reads=[c.der], writes=[c.der])
                P.op("dve", lambda e, base=base, ga0=ga0, sub=sub: e.tensor_scalar(
                    c.der.t[:, base + 8:base + 16], c.modt.t[:, ga0:ga0 + 8], (1.0 if sub == 1 else 0.5), None, ALU.mult),
                    reads=[c.modt], writes=[c.der])

        def Acol(l, sub, ch):
            j = ((l * 3 + sub) * 2) * 8 + ch
            return c.der.t[:, j:j + 1]

        def Gcol(l, sub, ch):
            j = ((l * 3 + sub) * 2 + 1) * 8 + ch
            return c.der.t[:, j:j + 1]

        def Scol(l, sub, ch):
            j = mcol(l, sub * 3 + 0, ch)
            return c.modt.t[:, j:j + 1]

        def rstd_tile(src_fn, src_bufs, epsk):
            for kc in range(8):
                P.op("act", lambda e, kc=kc: e.activation(sqt[:, kc, :], src_fn(kc), AF.Square),
                     reads=[src_bufs[kc]], writes=[c.sq[kc]])
            for kc in range(8):
                P.op("pe", lambda e, kc=kc: e.matmul(c.bank[6].t[:, :], c.ones.t[:, :], sqt[:, kc, :],
                                                      start=(kc == 0), stop=(kc == 7)),
                     reads=[c.ones, c.sq[kc]], writes=[c.bank[6]], pe_acc=(kc > 0))
            P.op("dve", lambda e: e.tensor_scalar(c.rstd.t[:, :], c.bank[6].t[:, :], epsk, None, ALU.add),
                 reads=[c.bank[6]], writes=[c.rstd])
            P.op("act", lambda e: e.activation(c.rstd.t[:, :], c.rstd.t[:, :], AF.Sqrt), reads=[c.rstd], writes=[c.rstd])
            P.op("dve", lambda e: e.reciprocal(c.rstd.t[:, :], c.rstd.t[:, :]), reads=[c.rstd], writes=[c.rstd])

        def modnorm_tile(l, sub, tt, hdst, hbuf):
            t0 = tt * 512
            rstd_tile(lambda kc: X[:, kc, t0:t0 + 512], [c.Xb[kc][tt] for kc in range(8)], EPS * D)
            for kc in range(8):
                tb = c.tmp[kc % 2]
                P.op("dve", lambda e, kc=kc, tb=tb: e.tensor_tensor(tb.t[:, :], X[:, kc, t0:t0 + 512], c.rstd.t[:, :], ALU.mult),
                     reads=[c.Xb[kc][tt], c.rstd], writes=[tb])
                P.op("act", lambda e, kc=kc, tb=tb: e.activation(hdst(kc), tb.t[:, :], AF.Identity,
                                                               bias=Scol(l, sub, kc), scale=Acol(l, sub, kc)),
                     reads=[tb, c.der, c.modt], writes=[hbuf(kc)])

        def epilogue(kind, l, oG, oGb, sgd, sgb):
            wod = din(kind + "_wo", [128, 8192])
            oS, oSb = dsel(kind + "_oS", [1, D, T], BF16, oG.ap()[bass.ds(g4, 1), :, :], oGb)
            sv = sgd.ap().rearrange("(c p) t -> p c t", p=128)
            with ExitStack() as e2:
                wo = P.sb(e2, kind + "wo_sb", [128, 8, 1024], BF16)
                P.dma("pool", wo.t[:, :, :], wod.rearrange("p (k d) -> p k d", k=8), writes=[wo])
                ot = [P.sb(e2, kind + "ot%d" % i, [128, 8, 512], BF16) for i in range(2)]
                st = [P.sb(e2, kind + "st%d" % i, [128, 8, 512], BF16) for i in range(2)]
                ogt = [e2.enter_context(nc.sbuf_tensor(kind + "og%d" % i, [128, 8, 512], BF16)) for i in range(2)]
                ogb = [[Buf(ogt[i]) for _ in range(8)] for i in range(2)]
                if kind == "hgrn":
                    hgd = din("hgn", [128, 8])
                    hg = P.sb(e2, "hg", [128, 8], F32)
                    P.dma("sp", hg.t[:, :], hgd, writes=[hg])
                    P.op("dve", lambda e: e.tensor_scalar(hg.t[:, :], hg.t[:, :], float(np.sqrt(128.0)), None, ALU.mult),
                         reads=[hg], writes=[hg])
                    sq1 = P.sb(e2, "sq1", [128, 512], BF16)
                    r1 = P.sb(e2, "r1", [128, 512], F32)
                    t1 = P.sb(e2, "t1", [128, 512], F32)
                for tt in range(4):
                    t0 = tt * 512
                    o_, s_, og_ = ot[tt % 2], st[tt % 2], ogt[tt % 2]
                    P.dma("sp", o_.t[:, :, :], oS.ap()[0].rearrange("(c p) s -> p c s", p=128)[:, :, t0:t0 + 512], reads=[oSb], writes=[o_])
                    P.dma("sp", s_.t[:, :, :], sv[:, :, t0:t0 + 512], reads=[sgb], writes=[s_])
                    if kind == "fox":
                        for kc in range(8):
                            P.op("dve", lambda e, kc=kc, o_=o_, s_=s_, og_=og_: e.tensor_tensor(
                                og_[:, kc, :], o_.t[:, kc, :], s_.t[:, kc, :], ALU.mult),
                                reads=[o_, s_], writes=[ogb[tt % 2][kc]])
                    else:
                        for kc in range(8):
                            P.op("act", lambda e, kc=kc, o_=o_: e.activation(sq1.t[:, :], o_.t[:, kc, :], AF.Square),
                                 reads=[o_], writes=[sq1])
                            P.op("pe", lambda e: e.matmul(c.bank[5].t[:, :], c.ones.t[:, :], sq1.t[:, :], start=True, stop=True),
                                 reads=[c.ones, sq1], writes=[c.bank[5]])
                            P.op("dve", lambda e: e.tensor_scalar(r1.t[:, :], c.bank[5].t[:, :], EPS * 128.0, None, ALU.add),
                                 reads=[c.bank[5]], writes=[r1])
                            P.op("act", lambda e: e.activation(r1.t[:, :], r1.t[:, :], AF.Sqrt), reads=[r1], writes=[r1])
                            P.op("dve", lambda e: e.reciprocal(r1.t[:, :], r1.t[:, :]), reads=[r1], writes=[r1])
                            P.op("dve", lambda e, kc=kc, o_=o_: e.tensor_tensor(t1.t[:, :], o_.t[:, kc, :], r1.t[:, :], ALU.mult),
                                 reads=[o_, r1], writes=[t1])
                            P.op("dve", lambda e, kc=kc, s_=s_, og_=og_: e.scalar_tensor_tensor(
                                og_[:, kc, :], t1.t[:, :], hg.t[:, kc:kc + 1], s_.t[:, kc, :], ALU.mult, ALU.mult),
                                reads=[t1, hg, s_], writes=[ogb[tt % 2][kc]])
                    for dc in range(8):
                        bk = c.bank[4 + dc % 2]
                        for kc in range(8):
                            P.op("pe", lambda e, kc=kc, dc=dc, bk=bk, og_=og_: e.matmul(
                                bk.t[:, :], wo.t[:, kc, dc * 128:(dc + 1) * 128], og_[:, kc, :],
                                start=(kc == 0), stop=(kc == 7)),
                                reads=[wo, ogb[tt % 2][kc]], writes=[bk], pe_acc=(kc > 0))
                        P.op("dve", lambda e, dc=dc, bk=bk, t0=t0: e.scalar_tensor_tensor(
                            X[:, dc, t0:t0 + 512], bk.t[:, :], Gcol(l, 1, dc), X[:, dc, t0:t0 + 512], ALU.mult, ALU.add),
                            reads=[bk, c.der, c.Xb[dc][tt]], writes=[c.Xb[dc][tt]])
            P.barrier()

        def ffn(j, l, sub):
            wupd = din("wup%d" % j, [11, 128, 4096])
            wdnd = din("wdn%d" % j, [8, 128, 2816])
            with ExitStack() as e2:
                hbt = e2.enter_context(nc.sbuf_tensor("hb_%d" % j, [128, 8, 1024], BF16))
                hbb = [[Buf(hbt) for _ in range(2)] for _ in range(8)]
                actt = e2.enter_context(nc.sbuf_tensor("actb_%d" % j, [128, NF, 1024], BF16))
                actb = [[Buf(actt) for _ in range(2)] for _ in range(NF)]
                wu = [P.sb(e2, "wu%d_%d" % (j, i), [128, 2, 8, 256], BF16) for i in range(2)]
                wd = [P.sb(e2, "wd%d_%d" % (j, i), [128, NF, 128], BF16) for i in range(2)]
                sa = [P.sb(e2, "sa%d_%d" % (j, i), [128, 512], F32) for i in range(2)]
                for half in range(2):
                    for t2 in range(2):
                        tt = half * 2 + t2
                        modnorm_tile(l, sub, tt, lambda kc, t2=t2: hbt[:, kc, t2 * 512:(t2 + 1) * 512],
                                     lambda kc, t2=t2: hbb[kc][t2])
                    it = 0
                    for g in range(11):
                        w_ = wu[g % 2]
                        P.dma("pool", w_.t[:, :, :, :], wupd[g].rearrange("p (a k f) -> p a k f", a=2, k=8), writes=[w_])
                        for jf in range(2):
                            fc = 2 * g + jf
                            for t2 in range(2):
                                bA, bB = c.bank[it % 2], c.bank[2 + it % 2]
                                s_ = sa[it % 2]
                                it += 1
                                for kc in range(8):
                                    P.op("pe", lambda e, kc=kc, w_=w_, jf=jf, t2=t2, bA=bA: e.matmul(
                                        bA.t[:, :], w_.t[:, 0, kc, jf * 128:(jf + 1) * 128], hbt[:, kc, t2 * 512:(t2 + 1) * 512],
                                        start=(kc == 0), stop=(kc == 7)),
                                        reads=[w_, hbb[kc][t2]], writes=[bA], pe_acc=(kc > 0))
                                for kc in range(8):
                                    P.op("pe", lambda e, kc=kc, w_=w_, jf=jf, t2=t2, bB=bB: e.matmul(
                                        bB.t[:, :], w_.t[:, 1, kc, jf * 128:(jf + 1) * 128], hbt[:, kc, t2 * 512:(t2 + 1) * 512],
                                        start=(kc == 0), stop=(kc == 7)),
                                        reads=[w_, hbb[kc][t2]], writes=[bB], pe_acc=(kc > 0))
                                P.op("act", lambda e, s_=s_, bA=bA: e.activation(s_.t[:, :], bA.t[:, :], AF.Silu),
                                     reads=[bA], writes=[s_])
                                P.op("dve", lambda e, s_=s_, bB=bB, fc=fc, t2=t2: e.tensor_tensor(
                                    actt[:, fc, t2 * 512:(t2 + 1) * 512], bB.t[:, :], s_.t[:, :], ALU.mult),
                                    reads=[bB, s_], writes=[actb[fc][t2]])
                    for dc in range(8):
                        w_ = wd[dc % 2]
                        P.dma("pool", w_.t[:, :, :], wdnd[dc].rearrange("p (f d) -> p f d", f=NF), writes=[w_])
                        for t2 in range(2):
                            tt = half * 2 + t2
                            t0 = tt * 512
                            bk = c.bank[4 + (dc * 2 + t2) % 2]
                            for fc in range(NF):
                                P.op("pe", lambda e, fc=fc, w_=w_, t2=t2, bk=bk: e.matmul(
                                    bk.t[:, :], w_.t[:, fc, :], actt[:, fc, t2 * 512:(t2 + 1) * 512],
                                    start=(fc == 0), stop=(fc == NF - 1)),
                                    reads=[w_, actb[fc][t2]], writes=[bk], pe_acc=(fc > 0))
                            P.op("dve", lambda e, dc=dc, bk=bk, t0=t0: e.scalar_tensor_tensor(
                                X[:, dc, t0:t0 + 512], bk.t[:, :], Gcol(l, sub, dc), X[:, dc, t0:t0 + 512], ALU.mult, ALU.add),
                                reads=[bk, c.der, c.Xb[dc][tt]], writes=[c.Xb[dc][tt]])
            P.barrier()

        def proj_fm(wname, hbt, hbb, evac, n_oc=8):
            wd_ = din(wname, [n_oc, 128, 1024])
            with ExitStack() as e3:
                wp = [P.sb(e3, wname + "_sb%d" % i, [128, 8, 128], BF16) for i in range(2)]
                it = 0
                for oc in range(n_oc):
                    w_ = wp[oc % 2]
                    P.dma("pool", w_.t[:, :, :], wd_[oc].rearrange("p (k f) -> p k f", k=8), writes=[w_])
                    for tt in range(4):
                        bk = c.bank[it % 4]
                        it += 1
                        for kc in range(8):
                            P.op("pe", lambda e, kc=kc, w_=w_, tt=tt, bk=bk: e.matmul(
                                bk.t[:, :], w_.t[:, kc, :], hbt[:, kc, tt * 512:(tt + 1) * 512],
                                start=(kc == 0), stop=(kc == 7)),
                                reads=[w_, hbb[kc][tt]], writes=[bk], pe_acc=(kc > 0))
                        evac(oc, tt, bk)
                P.barrier()

        def proj_tm(wname, hbt, hbb, vout, vob, func):
            wd_ = din(wname, [2, 128, 4096])
            with ExitStack() as e3:
                wv = P.sb(e3, wname + "_sb", [128, 2, 8, 512], BF16)
                for cg in range(2):
                    P.dma("pool", wv.t[:, cg, :, :], wd_[cg].rearrange("p (k f) -> p k f", k=8), writes=[wv])
                vt = [P.sb(e3, wname + "vt%d" % i, [128, 512], BF16) for i in range(2)]
                it = 0
                for tk in range(16):
                    for cg in range(2):
                        bk = c.bank[it % 4]
                        v_ = vt[it % 2]
                        it += 1
                        for kc in range(8):
                            P.op("pe", lambda e, kc=kc, tk=tk, cg=cg, bk=bk: e.matmul(
                                bk.t[:, :], hbt[:, kc, tk * 128:(tk + 1) * 128], wv.t[:, cg, kc, :],
                                start=(kc == 0), stop=(kc == 7)),
                                reads=[wv, hbb[kc][tk // 4]], writes=[bk], pe_acc=(kc > 0))
                        P.op("act", lambda e, bk=bk, v_=v_: e.activation(v_.t[:, :], bk.t[:, :], func),
                             reads=[bk], writes=[v_])
                        P.dma("sp", vout[tk * 128:(tk + 1) * 128, cg * 512:(cg + 1) * 512], v_.t[:, :], reads=[v_], ow=vob)
                P.barrier()

        def stage_out(e3, name, shape, dt):
            return [P.sb(e3, name + "%d" % i, shape, dt) for i in range(2)]

        def projections(kind, l):
            R = Ctx()
            with ExitStack() as e2:
                hbt = e2.enter_context(nc.sbuf_tensor(kind + "hb2", [128, 8, T], BF16))
                hbb = [[Buf(hbt) for _ in range(4)] for _ in range(8)]
                for tt in range(4):
                    modnorm_tile(l, 1, tt, lambda kc, tt=tt: hbt[:, kc, tt * 512:(tt + 1) * 512],
                                 lambda kc, tt=tt: hbb[kc][tt])
                kdt = BF16 if kind == "fox" else F32
                R.q, R.qb = idram(kind + "_q", [D, T], BF16), Buf()
                R.k, R.kb = idram(kind + "_k", [D, T], kdt), Buf()
                R.sg, R.sgb = idram(kind + "_sg", [D, T], BF16), Buf()
                R.v, R.vb = idram(kind + "_v", [T, D], BF16), Buf()
                qo, ko, sgo, vo = R.q.ap(), R.k.ap(), R.sg.ap(), R.v.ap()
                qob, kob, sgob, vob = R.qb, R.kb, R.sgb, R.vb
                cnt = [0]

                def simple_evac(od, ob, func, scale, st):
                    def evac(oc, tt, bk):
                        s_ = st[cnt[0] % 2]
                        cnt[0] += 1
                        P.op("act", lambda e, s_=s_, bk=bk: e.activation(s_.t[:, :], bk.t[:, :], func, scale=scale),
                             reads=[bk], writes=[s_])
                        P.dma("sp", od[oc * 128:(oc + 1) * 128, tt * 512:(tt + 1) * 512], s_.t[:, :], reads=[s_], ow=ob)
                    return evac

                stb = stage_out(e2, kind + "stb", [128, 512], BF16)
                if kind == "fox":
                    R.lf, R.lfb = idram("fox_lf", [128, 256], F32), Buf()
                    proj_fm("fox_wq", hbt, hbb, simple_evac(qo, qob, AF.Copy, float(FD ** -0.5), stb))
                    proj_fm("fox_wk", hbt, hbb, simple_evac(ko, kob, AF.Copy, 1.0, stb))
                    proj_fm("fox_wg", hbt, hbb, simple_evac(sgo, sgob, AF.Sigmoid, 1.0, stb))
                    proj_tm("fox_wv", hbt, hbb, vo, vob, AF.Copy)
                    wfd = din("fox_wf", [128, 128])
                    bfd = din("fox_bfb", [128, 256])
                    wf = P.sb(e2, "wf_sb", [128, 8, 16], BF16)
                    P.dma("pool", wf.t[:, :, :], wfd.rearrange("p (k f) -> p k f", k=8), writes=[wf])
                    bfb = P.sb(e2, "bfb", [128, 256], F32)
                    P.dma("sp", bfb.t[:, :], bfd, writes=[bfb])
                    z1 = P.sb(e2, "z1", [128, 256], F32)
                    bk = c.bank[0]
                    for tk in range(16):
                        for kc in range(8):
                            P.op("pe", lambda e, kc=kc, tk=tk: e.matmul(
                                bk.t[:, tk * 16:(tk + 1) * 16], hbt[:, kc, tk * 128:(tk + 1) * 128], wf.t[:, kc, :],
                                start=(kc == 0), stop=(kc == 7)),
                                reads=[wf, hbb[kc][tk // 4]], writes=[bk], pe_acc=not (tk == 0 and kc == 0))
                    P.op("dve", lambda e: e.tensor_tensor(z1.t[:, :], bk.t[:, 0:256], bfb.t[:, :], ALU.add), reads=[bk, bfb], writes=[z1])
                    P.op("act", lambda e: e.activation(z1.t[:, :], z1.t[:, :], AF.Exp, scale=-1.0), reads=[z1], writes=[z1])
                    P.op("act", lambda e: e.activation(z1.t[:, :], z1.t[:, :], AF.Ln, bias=1.0, scale=1.0), reads=[z1], writes=[z1])
                    P.op("dve", lambda e: e.tensor_scalar(z1.t[:, :], z1.t[:, :], -1.0, None, ALU.mult), reads=[z1], writes=[z1])
                    P.dma("sp", R.lf.ap(), z1.t[:, :], reads=[z1], ow=R.lfb)
                else:
                    R.lf, R.lfb = idram("hgrn_lf", [D, T], F32), Buf()
                    lfo, lfob = R.lf.ap(), R.lfb
                    lbd = din("lbl", [128, 16])
                    lbl = P.sb(e2, "lbl_sb", [128, 16], F32)
                    lb = P.sb(e2, "lb", [128, 8], F32)
                    oml = P.sb(e2, "oml", [128, 8], F32)
                    P.dma("sp", lbl.t[:, :], lbd, writes=[lbl])
                    P.op("dve", lambda e: e.tensor_tensor(lb.t[:, :], lbl.t[:, 8:16], lbl.t[:, 0:8], ALU.subtract), reads=[lbl], writes=[lb])
                    P.op("act", lambda e: e.activation(lb.t[:, :], lb.t[:, :], AF.Sigmoid), reads=[lb], writes=[lb])
                    P.op("dve", lambda e: e.tensor_scalar(oml.t[:, :], lb.t[:, :], -1.0, 1.0, ALU.mult, ALU.add), reads=[lb], writes=[oml])
                    proj_fm("hgrn_wq", hbt, hbb, simple_evac(qo, qob, AF.Copy, 1.0, stb))
                    proj_fm("hgrn_wg", hbt, hbb, simple_evac(sgo, sgob, AF.Silu, 1.0, stb))
                    proj_tm("hgrn_wv", hbt, hbb, vo, vob, AF.Silu)
                    sg1 = P.sb(e2, "sg1", [128, 512], F32)
                    ff = stage_out(e2, "ff", [128, 512], F32)
                    lff = stage_out(e2, "lff", [128, 512], F32)
                    kk = stage_out(e2, "kk", [128, 512], F32)

                    def f_evac(oc, tt, bk):
                        i = cnt[0] % 2
                        cnt[0] += 1
                        f_, l_, k_ = ff[i], lff[i], kk[i]
                        P.op("act", lambda e, bk=bk: e.activation(sg1.t[:, :], bk.t[:, :], AF.Sigmoid), reads=[bk], writes=[sg1])
                        P.op("dve", lambda e, f_=f_, oc=oc: e.tensor_scalar(f_.t[:, :], sg1.t[:, :], oml.t[:, oc:oc + 1], lb.t[:, oc:oc + 1], ALU.mult, ALU.add),
                             reads=[sg1, oml, lb], writes=[f_])
                        P.op("act", lambda e, f_=f_, l_=l_: e.activation(l_.t[:, :], f_.t[:, :], AF.Ln), reads=[f_], writes=[l_])
                        P.dma("sp", lfo[oc * 128:(oc + 1) * 128, tt * 512:(tt + 1) * 512], l_.t[:, :], reads=[l_], ow=lfob)
                    proj_fm("hgrn_wf", hbt, hbb, f_evac)
            P.barrier()
            return R

        def gather(name, src, srcb, nch, rows, cols, dt):
            dst = idram(name, [nch, 4 * rows, cols], dt)
            db = Buf()
            sv = src.ap() if len(src.shape) == 2 else None
            for j in range(nch):
                sa = src.ap()[j * rows:(j + 1) * rows, :] if sv is not None else src.ap()[j]
                P.collective("AllGather", G4, sa.opt(), dst.ap()[j].opt(), [srcb], db)
            return dst, db

        def dsel(name, shape, dt, src_dyn, srcb):
            dst = idram(name, shape, dt)
            db = Buf()
            P.dma("sp", dst.ap(), src_dyn, reads=[srcb], writes=[db])
            return dst, db

        def fox_phase(R):
            qG, qGb = gather("fox_qG", R.q, R.qb, 4, 256, T, BF16)
            kG, kGb = gather("fox_kG", R.k, R.kb, 4, 256, T, BF16)
            vG, vGb = gather("fox_vG", R.v, R.vb, 4, 512, D, BF16)
            lG, lGb = gather("fox_lG", R.lf, R.lfb, 1, 128, 256, F32)
            qS, qSb = dsel("fox_qS", [1, D, T], BF16, qG.ap()[bass.ds(g4, 1), :, :], qGb)
            kS, kSb = dsel("fox_kS", [1, D, T], BF16, kG.ap()[bass.ds(g4, 1), :, :], kGb)
            vS, vSb = idram("fox_vS", [S, 256], BF16), Buf()
            for j in range(4):
                P.dma("sp", vS.ap().rearrange("(r j i) c -> j r i c", r=4, j=4)[j],
                      vG.ap()[j].rearrange("(r i) c -> r i c", r=4)[:, :, bass.ds(g4 * 256, 256)], reads=[vGb], writes=[vSb])
            lS, lSb = dsel("fox_lS", [512, 16, 4], F32, lG.ap()[0].rearrange("r (k h) -> r k h", h=16)[:, :, bass.ds(g4 * 4, 4)], lGb)
            o_loc, olb = idram("fox_o", [4, 256, T], BF16), Buf()
            shi, slo = idram("shi", [4, S], BF16), idram("slo", [4, S], BF16)
            shb, slb = Buf(), Buf()
            Ud, seld, mkd, idfd = din("U", [128, 128]), din("sel", [128, 128]), din("mk", [128, 128]), din("identf", [128, 128])
            bank = c.bank
            with ExitStack() as e2:
                U = P.sb(e2, "U_sb", [128, 128], F32)
                sel = P.sb(e2, "sel_sb", [128, 128], F32)
                mk = P.sb(e2, "mk_sb", [128, 128], F32)
                idf = P.sb(e2, "idf_sb", [128, 128], F32)
                onesf = P.sb(e2, "onesf", [128, 128], F32)
                negB = P.sb(e2, "negB", [128, 4 * 16 * 64], F32)
                for t_, d_ in ((U, Ud), (sel, seld), (mk, mkd), (idf, idfd)):
                    P.dma("sp", t_.t[:, :], d_, writes=[t_])
                P.op("pool", lambda e: e.memset(onesf.t[:, :], 1.0), writes=[onesf])
                with ExitStack() as e3:
                    lsel = P.sb(e3, "lsel", [128, 4, 16, 4], F32)
                    lt = P.sb(e3, "lt_sb", [128, 256], F32)
                    within = P.sb(e3, "within", [128, 256], F32)
                    tot = P.sb(e3, "tot", [128, 256], F32)
                    inc = P.sb(e3, "inc", [128, 256], F32)
                    GT = P.sb(e3, "GT", [128, 256], F32)
                    gend = P.sb(e3, "gend", [128, 256], F32)
                    Aa = P.sb(e3, "Aa", [128, 256], F32)
                    AT = P.sb(e3, "AT", [64, 512], F32)
                    ahi = P.sb(e3, "ahi", [64, 512], BF16)
                    ahf = P.sb(e3, "ahf", [64, 512], F32)
                    alo = P.sb(e3, "alo", [64, 512], BF16)
                    for t in range(4):
                        P.dma("sp", lsel.t[:, t, :, :], lS.ap()[t * 128:(t + 1) * 128, :, :], reads=[lSb], writes=[lsel])
                    for hl in range(4):
                        P.op("dve", lambda e, hl=hl: e.tensor_copy(
                            lt.t[:, hl * 64:(hl + 1) * 64].rearrange("p (t k) -> p t k", t=4), lsel.t[:, :, :, hl]),
                            reads=[lsel], writes=[lt])
                    P.op("pe", lambda e: e.matmul(bank[6].t[:, 0:256], U.t[:, :], lt.t[:, :], start=True, stop=True), reads=[U, lt], writes=[bank[6]])
                    P.op("pe", lambda e: e.matmul(bank[5].t[:, 0:256], onesf.t[:, :], lt.t[:, :], start=True, stop=True), reads=[onesf, lt], writes=[bank[5]])
                    P.op("dve", lambda e: e.tensor_copy(within.t[:, :], bank[6].t[:, 0:256]), reads=[bank[6]], writes=[within])
                    P.op("dve", lambda e: e.tensor_copy(tot.t[:, :], bank[5].t[:, 0:256]), reads=[bank[5]], writes=[tot])
                    for h in range(4):
                        P.op("dve", lambda e, h=h: e.tensor_tensor_scan(inc.t[:, h * 64:(h + 1) * 64], onesf.t[:, 0:64], tot.t[:, h * 64:(h + 1) * 64],
                                                                        0.0, ALU.mult, ALU.add), reads=[onesf, tot], writes=[inc])
                    P.op("dve", lambda e: e.tensor_tensor(GT.t[:, :], within.t[:, :], inc.t[:, :], ALU.add), reads=[within, inc], writes=[GT])
                    P.op("dve", lambda e: e.tensor_tensor(GT.t[:, :], GT.t[:, :], tot.t[:, :], ALU.subtract), reads=[GT, tot], writes=[GT])
                    P.op("pe", lambda e: e.matmul(bank[6].t[:, 0:256], sel.t[:, :], GT.t[:, :], start=True, stop=True), reads=[sel, GT], writes=[bank[6]])
                    P.op("dve", lambda e: e.tensor_copy(gend.t[:, :], bank[6].t[:, 0:256]), reads=[bank[6]], writes=[gend])
                    for h in range(4):
                        for Q in range(16):
                            j0 = (h * 16 + Q) * 64
                            gc = h * 64 + 4 * Q + 3
                            P.op("dve", lambda e, h=h, j0=j0, gc=gc: e.tensor_scalar(
                                negB.t[:, j0:j0 + 64], GT.t[:, h * 64:(h + 1) * 64], -1.0, gend.t[:, gc:gc + 1], ALU.mult, ALU.add),
                                reads=[GT, gend], writes=[negB])
                            a0 = h * 64 + 4 * Q
                            P.op("dve", lambda e, a0=a0, gc=gc: e.tensor_scalar(
                                Aa.t[:, a0:a0 + 4], GT.t[:, a0:a0 + 4], gend.t[:, gc:gc + 1], None, ALU.subtract),
                                reads=[GT, gend], writes=[Aa])
                    for h in range(4):
                        P.op("pe", lambda e, h=h: e.matmul(bank[5].t[0:64, h * 128:(h + 1) * 128], Aa.t[:, h * 64:(h + 1) * 64], idf.t[:, :],
                                                           start=True, stop=True), reads=[Aa, idf], writes=[bank[5]], pe_acc=(h > 0))
                    P.op("dve", lambda e: e.tensor_copy(AT.t[:, :], bank[5].t[0:64, :]), reads=[bank[5]], writes=[AT])
                    P.op("dve", lambda e: e.tensor_copy(ahi.t[:, :], AT.t[:, :]), reads=[AT], writes=[ahi])
                    P.op("dve", lambda e: e.tensor_copy(ahf.t[:, :], ahi.t[:, :]), reads=[ahi], writes=[ahf])
                    P.op("dve", lambda e: e.tensor_tensor(alo.t[:, :], AT.t[:, :], ahf.t[:, :], ALU.subtract), reads=[AT, ahf], writes=[alo])
                    P.dma("sp", shi.ap().rearrange("h (k p) -> k h p", p=128), ahi.t[:, :].rearrange("k (h p) -> k h p", h=4), reads=[ahi], writes=[shb])
                    P.dma("sp", slo.ap().rearrange("h (k p) -> k h p", p=128), alo.t[:, :].rearrange("k (h p) -> k h p", h=4), reads=[alo], writes=[slb])
                P.barrier()
                qa = [P.sb(e2, "qa%d" % i, [128, S], BF16) for i in range(2)]
                ka = [P.sb(e2, "ka%d" % i, [128, S], BF16) for i in range(2)]
                va = [P.sb(e2, "va%d" % i, [128, 64 * 65 + 64], BF16) for i in range(2)]
                pt = [P.sb(e2, "pt%d" % i, [128, 512], BF16) for i in range(4)]
                sbanks = [bank[0], bank[1], bank[2], bank[6]]
                drow = P.sb(e2, "drow", [65, 512], F32)
                rec = P.sb(e2, "rec", [64, 512], F32)
                oo = [P.sb(e2, "oo%d" % i, [64, 512], BF16) for i in range(2)]
                vv = lambda v_: v_.t[:, 0:64 * 65].rearrange("p (t d) -> p t d", d=65)
                for i in range(2):
                    P.op("pool", lambda e, i=i: e.memset(ka[i].t[64:128, :], 0.0), writes=[ka[i]])
                    P.op("pool", lambda e, i=i: e.memset(qa[i].t[64:128, :], 0.0), writes=[qa[i]])
                    P.op("pool", lambda e, i=i: e.memset(ka[i].t[64:66, :], 1.0), writes=[ka[i]])
                    P.op("pool", lambda e, i=i: e.memset(va[i].t[:, :], 0.0), writes=[va[i]])
                    P.op("pool", lambda e, i=i: e.memset(vv(va[i])[:, :, 64:65], 1.0), writes=[va[i]])
                vGv = vS.ap().rearrange("(k p) d -> p k d", p=128)

                def load_head(h):
                    q_, k_, v_ = qa[h % 2], ka[h % 2], va[h % 2]
                    for t in range(4):
                        P.dma("sp", q_.t[0:64, t * T:(t + 1) * T], qS.ap()[0, t * 256 + h * 64:t * 256 + (h + 1) * 64, :], reads=[qSb], writes=[q_])
                        P.dma("pool", k_.t[0:64, t * T:(t + 1) * T], kS.ap()[0, t * 256 + h * 64:t * 256 + (h + 1) * 64, :], reads=[kSb], writes=[k_])
                    P.dma("sp", q_.t[64:65, :], shi.ap()[h:h + 1, :], reads=[shb], writes=[q_])
                    P.dma("sp", q_.t[65:66, :], slo.ap()[h:h + 1, :], reads=[slb], writes=[q_])
                    P.dma("pool", vv(v_)[:, :, 0:64], vGv[:, :, h * 64:(h + 1) * 64], reads=[vSb], writes=[v_])

                load_head(0)

                def do_head(h, q_, k_, v_, nit):
                    items = [(Q, kt) for Q in range(16) for kt in range(4 * Q + 4)]

                    def emit_S(idx, it_no):
                        Q, kt = items[idx]
                        d = kt - 4 * Q
                        c0 = 128 * d if d >= 0 else 0
                        bk = sbanks[it_no % 4]
                        p_ = pt[it_no % 4]
                        P.op("pe", lambda e: e.matmul(bk.t[:, c0:512], k_.t[0:128, kt * 128:(kt + 1) * 128],
                                                      q_.t[0:128, Q * 512 + c0:(Q + 1) * 512], start=True, stop=True),
                             reads=[k_, q_], writes=[bk])
                        if d >= 0:
                            P.op("dve", lambda e: e.tensor_tensor(bk.t[:, c0:c0 + 128], bk.t[:, c0:c0 + 128], mk.t[:, :], ALU.add),
                                 reads=[bk, mk], writes=[bk])
                        jb = (h * 16 + Q) * 64 + kt
                        P.op("act", lambda e: e.activation(p_.t[:, c0:512], bk.t[:, c0:512], AF.Exp, bias=negB.t[:, jb:jb + 1], scale=1.0),
                             reads=[bk, negB], writes=[p_])

                    def emit_PV(idx, it_no):
                        Q, kt = items[idx]
                        d = kt - 4 * Q
                        c0 = 128 * d if d >= 0 else 0
                        p_ = pt[it_no % 4]
                        ob_ = bank[3 + Q % 2]
                        last = (kt == 4 * Q + 3)
                        P.op("pe", lambda e: e.matmul(ob_.t[0:128, c0:512], v_.t[:, kt * 65:kt * 65 + 128], p_.t[:, c0:512], start=(kt == 0), stop=last),
                             reads=[v_, p_], writes=[ob_], pe_acc=(kt > 0))
                        if last:
                            o_ = oo[Q % 2]
                            P.op("act", lambda e: e.activation(drow.t[64:65, :], ob_.t[64:65, :], AF.Copy), reads=[ob_], writes=[drow])
                            P.op("pe", lambda e: e.matmul(bank[5].t[0:64, :], onesf.t[64:65, 0:64], drow.t[64:65, :], start=True, stop=True),
                                 reads=[onesf, drow], writes=[bank[5]])
                            P.op("dve", lambda e: e.reciprocal(rec.t[:, :], bank[5].t[0:64, :]), reads=[bank[5]], writes=[rec])
                            P.op("dve", lambda e: e.tensor_tensor(o_.t[:, :], ob_.t[0:64, :], rec.t[:, :], ALU.mult), reads=[ob_, rec], writes=[o_])
                            P.dma("sp", o_loc.ap()[Q // 4][h * 64:(h + 1) * 64, (Q % 4) * 512:(Q % 4 + 1) * 512], o_.t[:, :], reads=[o_], ow=olb)

                    n = len(items)
                    emit_S(0, nit)
                    emit_S(1, nit + 1)
                    for idx in range(n):
                        if idx + 2 < n:
                            emit_S(idx + 2, nit + idx + 2)
                        emit_PV(idx, nit + idx)
                    return nit + n

                nit = 0
                junk = Buf()
                for h in range(4):
                    if h + 1 < 4:
                        load_head(h + 1)
                    nit = do_head(h, qa[h % 2], ka[h % 2], va[h % 2], nit)
            P.barrier()
            return gather("fox_oG", o_loc, olb, 4, 256, T, BF16)

        def hgrn_phase(R):
            qG, qGb = gather("hg_qG", R.q, R.qb, 4, 256, T, BF16)
            lG, lGb = gather("hg_lG", R.lf, R.lfb, 8, 128, T, F32)
            vG, vGb = gather("hg_vG", R.v, R.vb, 4, 512, D, BF16)
            qS, qSb = dsel("hg_qS", [1, D, T], BF16, qG.ap()[bass.ds(g4, 1), :, :], qGb)
            lS, lSb = dsel("hg_lS", [2, 512, T], F32, lG.ap()[bass.ds(g4 * 2, 2), :, :], lGb)
            vS, vSb = idram("hg_vS", [S, 256], BF16), Buf()
            for j in range(4):
                P.dma("sp", vS.ap().rearrange("(r j i) c -> j r i c", r=4, j=4)[j],
                      vG.ap()[j].rearrange("(r i) c -> r i c", r=4)[:, :, bass.ds(g4 * 256, 256)], reads=[vGb], writes=[vSb])
            o_loc, olb = idram("hg_o", [4, 256, T], BF16), Buf()
            m01d, rmd, idd = din("m01", [128, 64]), din("rm", [128, 2048]), din("ident", [128, 128], BF16)
            NB = 2048
            bankA, bankO, bankU = c.bank[0:2], c.bank[2:4], c.bank[4:6]
            with ExitStack() as e2:
                m01 = P.sb(e2, "m01_sb", [128, 64], F32)
                rm = P.sb(e2, "rm_sb", [128, NB], F32)
                ident = P.sb(e2, "ident_sb", [128, 128], BF16)
                P.dma("sp", m01.t[:, :], m01d, writes=[m01])
                P.dma("sp", rm.t[:, :], rmd, writes=[rm])
                P.dma("sp", ident.t[:, :], idd, writes=[ident])
                sh = Ctx()
                sh.qb = P.sb(e2, "hqb", [128, NB], BF16)
                sh.kb = P.sb(e2, "hkb", [128, NB], F32)
                sh.lf = P.sb(e2, "hlf", [128, NB], F32)
                sh.G = P.sb(e2, "hG", [128, NB], F32)
                sh.tmp = P.sb(e2, "htmp", [128, NB], F32)
                sh.tmp2 = P.sb(e2, "htmp2", [128, NB], F32)
                sh.kend = P.sb(e2, "hkend", [128, NB], BF16)
                hs = []
                for h in range(2):
                    o = Ctx()
                    o.qd = P.sb(e2, "hqd%d" % h, [128, NB], BF16)
                    o.kdd = P.sb(e2, "hkdd%d" % h, [128, NB], BF16)
                    o.kT = P.sb(e2, "hkT%d" % h, [128, 16, 128], BF16)
                    o.vb = P.sb(e2, "hvb%d" % h, [128, 16, 128], BF16)
                    o.egl = P.sb(e2, "hegl%d" % h, [128, 32], F32)
                    o.S32 = P.sb(e2, "hS32_%d" % h, [128, 128], F32)
                    o.Sbf = [P.sb(e2, "hSbf%d_%d" % (h, i), [128, 128], BF16) for i in range(2)]
                    o.am = [P.sb(e2, "ham%d_%d" % (h, i), [128, 64], BF16) for i in range(2)]
                    o.osb = [P.sb(e2, "hosb%d_%d" % (h, i), [128, 512], BF16) for i in range(2)]
                    P.op("pool", lambda e, o=o: e.memset(o.S32.t[:, :], 0.0), writes=[o.S32])
                    P.op("pool", lambda e, o=o: e.memset(o.Sbf[0].t[:, :], 0.0), writes=[o.Sbf[0]])
                    o.si = 0
                    hs.append(o)
                vGv = vS.ap().rearrange("(t p) d -> p t d", p=128)

                def prep(h, blk):
                    o = hs[h]
                    P.dma("sp", sh.qb.t[:, :], qS.ap()[0, blk * 256 + h * 128:blk * 256 + (h + 1) * 128, :], reads=[qSb], writes=[sh.qb])
                    P.dma("sp", sh.lf.t[:, :], lS.ap()[h, blk * 128:(blk + 1) * 128, :], reads=[lSb], writes=[sh.lf])
                    P.dma("pool", o.vb.t[:, :, :], vGv[:, blk * 16:(blk + 1) * 16, h * 128:(h + 1) * 128], reads=[vSb], writes=[o.vb])
                    P.op("act", lambda e: e.activation(sh.kb.t[:, :], sh.lf.t[:, :], AF.Exp), reads=[sh.lf], writes=[sh.kb])
                    P.op("dve", lambda e: e.tensor_scalar(sh.kb.t[:, :], sh.kb.t[:, :], -1.0, 1.0, ALU.mult, ALU.add), reads=[sh.kb], writes=[sh.kb])
                    P.op("dve", lambda e: e.tensor_tensor_scan(sh.G.t[:, :], rm.t[:, :], sh.lf.t[:, :], 0.0, ALU.mult, ALU.add),
                         reads=[rm, sh.lf], writes=[sh.G])
                    P.op("act", lambda e: e.activation(sh.tmp.t[:, :], sh.G.t[:, :], AF.Exp), reads=[sh.G], writes=[sh.tmp])
                    P.op("dve", lambda e: e.tensor_tensor(o.qd.t[:, :], sh.qb.t[:, :], sh.tmp.t[:, :], ALU.mult), reads=[sh.qb, sh.tmp], writes=[o.qd])
                    P.op("act", lambda e: e.activation(sh.tmp2.t[:, :], sh.G.t[:, :], AF.Exp, scale=-1.0), reads=[sh.G], writes=[sh.tmp2])
                    P.op("dve", lambda e: e.tensor_tensor(sh.tmp2.t[:, :], sh.kb.t[:, :], sh.tmp2.t[:, :], ALU.mult), reads=[sh.kb, sh.tmp2], writes=[sh.tmp2])
                    P.op("dve", lambda e: e.tensor_copy(o.kdd.t[:, :], sh.tmp2.t[:, :]), reads=[sh.tmp2], writes=[o.kdd])
                    G3 = sh.G.t[:, :].rearrange("p (c s) -> p c s", s=64)
                    P.op("act", lambda e: e.activation(o.egl.t[:, :], G3[:, :, 63], AF.Exp), reads=[sh.G], writes=[o.egl])
                    for cc in range(32):
                        P.op("dve", lambda e, cc=cc: e.tensor_scalar(sh.kend.t[:, cc * 64:(cc + 1) * 64], sh.tmp2.t[:, cc * 64:(cc + 1) * 64],
                                                                     o.egl.t[:, cc:cc + 1], None, ALU.mult),
                             reads=[sh.tmp2, o.egl], writes=[sh.kend])
                    for grp in range(2):
                        for j in range(8):
                            tk = grp * 8 + j
                            P.op("pe", lambda e, tk=tk, j=j: e.transpose(bankT.t[:, j * 128:(j + 1) * 128], sh.kend.t[:, tk * 128:(tk + 1) * 128], ident.t[:, :]),
                                 reads=[sh.kend, ident], writes=[bankT], pe_acc=(j > 0))
                        P.op("act", lambda e, grp=grp: e.activation(o.kT.t[:, grp * 8:(grp + 1) * 8, :],
                                                                   bankT.t[:, :].rearrange("p (t k) -> p t k", k=128), AF.Copy),
                             reads=[bankT], writes=[o.kT])

                nA = [0]

                def chunk(h, blk, cc):
                    o = hs[h]
                    tk, half = cc // 2, cc % 2
                    pb = 64 * half
                    gc = blk * 32 + cc
                    cs = slice(cc * 64, (cc + 1) * 64)
                    bA = bankA[nA[0] % 2]
                    am = o.am[nA[0] % 2]
                    nA[0] += 1
                    bO = bankO[h]
                    bU = bankU[h]
                    oc0 = (gc % 8) * 64
                    Sb = o.Sbf[o.si % 2]
                    Sn = o.Sbf[(o.si + 1) % 2]
                    o.si += 1
                    P.op("pe", lambda e: e.matmul(bA.t[pb:pb + 64, 0:64], o.kdd.t[:, cs], o.qd.t[:, cs], start=True, stop=True),
                         reads=[o.kdd, o.qd], writes=[bA])
                    P.op("dve", lambda e: e.tensor_tensor(am.t[pb:pb + 64, :], bA.t[pb:pb + 64, 0:64], m01.t[pb:pb + 64, :], ALU.mult),
                         reads=[bA, m01], writes=[am])
                    P.op("pe", lambda e: e.matmul(bO.t[:, oc0:oc0 + 64], Sb.t[:, :], o.qd.t[:, cs], start=True, stop=False),
                         reads=[Sb, o.qd], writes=[bO], pe_acc=(gc % 8 != 0))
                    P.op("pe", lambda e: e.matmul(bO.t[:, oc0:oc0 + 64], o.vb.t[pb:pb + 64, tk, :], am.t[pb:pb + 64, :], start=False, stop=True),
                         reads=[o.vb, am], writes=[bO], pe_acc=True)
                    P.op("pe", lambda e: e.matmul(bU.t[:, 0:128], o.kT.t[pb:pb + 64, tk, :], o.vb.t[pb:pb + 64, tk, :], start=True, stop=True),
                         reads=[o.kT, o.vb], writes=[bU])
                    P.op("dve", lambda e: e.scalar_tensor_tensor(o.S32.t[:, :], o.S32.t[:, :], o.egl.t[:, cc:cc + 1], bU.t[:, 0:128], ALU.mult, ALU.add),
                         reads=[o.S32, o.egl, bU], writes=[o.S32])
                    P.op("act", lambda e: e.activation(Sn.t[:, :], o.S32.t[:, :], AF.Copy), reads=[o.S32], writes=[Sn])
                    if gc % 8 == 7:
                        os_ = o.osb[(gc // 8) % 2]
                        P.op("act", lambda e: e.activation(os_.t[:, :], bO.t[:, :], AF.Copy), reads=[bO], writes=[os_])
                        tok0 = (gc - 7) * 64
                        P.dma("sp", o_loc.ap()[tok0 // T][h * 128:(h + 1) * 128, tok0 % T:tok0 % T + 512], os_.t[:, :], reads=[os_], ow=olb)

                oG = idram("hg_oG", [4, 4 * 256, T], BF16)
                oGb = Buf()
                for blk in range(4):
                    for h in range(2):
                        prep(h, blk)
                    for cc in range(32):
                        for h in range(2):
                            chunk(h, blk, cc)
                    P.collective("AllGather", G4, o_loc.ap()[blk].opt(), oG.ap()[blk].opt(), [olb], oGb)
            P.barrier()
            return oG, oGb

        ffn(0, 0, 0)
        R1 = projections("fox", 0)
        oG1, oG1b = fox_phase(R1)
        epilogue("fox", 0, oG1, oG1b, R1.sg, R1.sgb)
        ffn(1, 0, 2)
        ffn(2, 1, 0)
        R2 = projections("hgrn", 1)
        oG2, oG2b = hgrn_phase(R2)
        epilogue("hgrn", 1, oG2, oG2b, R2.sg, R2.sgb)
        ffn(3, 1, 2)
        xo = dout("xo", [D, T])
        xob = Buf()
        outs.append(xob)
        xov = xo.rearrange("(c p) t -> p c t", p=128)
        fgd = din("fg", [128, 8])
        fg = P.sb(es, "fg_sb", [128, 8], F32)
        P.dma("sp", fg.t[:, :], fgd, writes=[fg])
        P.op("dve", lambda e: e.tensor_scalar(fg.t[:, :], fg.t[:, :], SQD, None, ALU.mult), reads=[fg], writes=[fg])
        yo = [P.sb(es, "yo%d" % i, [128, 512], F32) for i in range(2)]
        it = 0
        for tt in range(4):
            t0 = tt * 512
            rstd_tile(lambda kc, t0=t0: X[:, kc, t0:t0 + 512], [c.Xb[kc][tt] for kc in range(8)], EPS * D)
            for kc in range(8):
                y_ = yo[it % 2]
                it += 1
                P.op("dve", lambda e, kc=kc, y_=y_, t0=t0: e.scalar_tensor_tensor(
                    y_.t[:, :], X[:, kc, t0:t0 + 512], fg.t[:, kc:kc + 1], c.rstd.t[:, :], ALU.mult, ALU.mult),
                    reads=[c.Xb[kc][tt], fg, c.rstd], writes=[y_])
                P.dma("sp", xov[:, kc, t0:t0 + 512], y_.t[:, :], reads=[y_], ow=xob)
        P.finish(outs)
        P.emit()
    return nc


_DBG = {}


def _run(nc, maps):
    return run_bass_kernel_spmd(nc, maps, core_ids=list(range(NCORES))).results


def kernel_unfused(x, c, ada_w, ada_b, norm_g, ffn_w_up, ffn_w_down, fox_w_in, fox_b_f, fox_w_out,
           hgrn_w_in, hgrn_norm_g, hgrn_w_out, hgrn_lb_logits, final_norm_g):
    f32 = lambda a: np.ascontiguousarray(np.asarray(a, dtype=np.float32))
    x, c, ada_w, ada_b, norm_g = f32(x), f32(c), f32(ada_w), f32(ada_b), f32(norm_g)
    ffn_w_up, ffn_w_down, fox_w_in, fox_b_f, fox_w_out = f32(ffn_w_up), f32(ffn_w_down), f32(fox_w_in), f32(fox_b_f), f32(fox_w_out)
    hgrn_w_in, hgrn_norm_g, hgrn_w_out = f32(hgrn_w_in), f32(hgrn_norm_g), f32(hgrn_w_out)
    hgrn_lb_logits, final_norm_g = f32(hgrn_lb_logits), f32(final_norm_g)

    mod = run_mod(c, ada_w, ada_b)
    _DBG["mod"] = mod
    modT = [fm_cols(mod[b].reshape(18, D)) for b in range(B)]
    gT = fm_cols(norm_g.reshape(6, D))
    cores = [(b, t) for b in range(B) for t in range(4)]

    nc1 = build_F({"ffns": [(0, 0)], "proj": ("fox", 0)})
    wi = fox_w_in[0]
    shared = {
        "gT": gT, "wup0": tile_wup(ffn_w_up[0, 0]), "wdn0": tile_wdn(ffn_w_down[0, 0]),
        "wq": tile_w_fm(wi[:, 0:D]), "wk": tile_w_fm(wi[:, D:2 * D]), "wg": tile_w_fm(wi[:, 3 * D:4 * D]),
        "wv": tile_w_tm(wi[:, 2 * D:3 * D]),
        "wf": np.ascontiguousarray(wi[:, 4 * D:4 * D + 16].reshape(8, 128, 16).transpose(1, 0, 2)).reshape(128, 128),
        "bf": np.ascontiguousarray(fox_b_f[0].reshape(16, 1)),
    }
    maps = []
    for (b, t) in cores:
        m = dict(shared)
        m["xT"] = np.ascontiguousarray(x[b, t * T:(t + 1) * T, :].T)
        m["modT"] = modT[b]
        maps.append(m)
    r1 = _run(nc1, maps)
    _DBG["r1"] = r1

    def cat_fm(res, name, b):
        return np.concatenate([res[b * 4 + t][name] for t in range(4)], axis=1)

    def cat_tm(res, name, b):
        return np.concatenate([res[b * 4 + t][name] for t in range(4)], axis=0)

    nc2 = build_fox()
    U, sel, mk = fox_consts()
    maps = []
    for b in range(B):
        qf = cat_fm(r1, "qT", b).reshape(FH, FD, S)
        kf = cat_fm(r1, "kT", b).reshape(FH, FD, S)
        vf = cat_tm(r1, "v", b)
        lf = cat_fm(r1, "lf", b)
        for g in range(4):
            l4 = lf[4 * g:4 * g + 4]
            v4 = np.stack([np.ascontiguousarray(vf[:, hd * FD:(hd + 1) * FD].reshape(64, 128, FD).transpose(1, 0, 2)).reshape(128, 64 * FD)
                           for hd in range(4 * g, 4 * g + 4)])
            maps.append({
                "q": np.ascontiguousarray(qf[4 * g:4 * g + 4]), "k": np.ascontiguousarray(kf[4 * g:4 * g + 4]), "v": v4,
                "lt": np.ascontiguousarray(l4.reshape(4, 64, 128).transpose(2, 0, 1)).reshape(128, 256),
                "lq": np.ascontiguousarray(l4.reshape(4, 16, 512).transpose(1, 0, 2)).reshape(16, 2048),
                "U": U, "sel": sel, "mk": mk,
            })
    r2 = _run(nc2, maps)
    _DBG["r2"] = r2
    ofull = [np.concatenate([r2[b * 4 + g]["o"].reshape(4 * FD, S) for g in range(4)], axis=0) for b in range(B)]

    nc3 = build_F({"epi": ("fox", 0), "ffns": [(0, 2), (1, 0)], "proj": ("hgrn", 1)})
    hi = hgrn_w_in[0]
    shared = {
        "gT": gT, "wo": tile_wo(fox_w_out[0]),
        "wup0": tile_wup(ffn_w_up[0, 1]), "wdn0": tile_wdn(ffn_w_down[0, 1]),
        "wup1": tile_wup(ffn_w_up[1, 0]), "wdn1": tile_wdn(ffn_w_down[1, 0]),
        "wq": tile_w_fm(hi[:, 0:D]), "wf": tile_w_fm(hi[:, D:2 * D]), "wg": tile_w_fm(hi[:, 3 * D:4 * D]),
        "wv": tile_w_tm(hi[:, 2 * D:3 * D]), "lbl": fm_cols(hgrn_lb_logits),
    }
    maps = []
    for i, (b, t) in enumerate(cores):
        m = dict(shared)
        m["xT"] = r1[i]["xo"]
        m["modT"] = modT[b]
        m["oT"] = np.ascontiguousarray(ofull[b][:, t * T:(t + 1) * T])
        m["sg"] = r1[i]["sgo"]
        maps.append(m)
    r3 = _run(nc3, maps)
    _DBG["r3"] = r3

    nc4 = build_hgrn()
    m01, rm, ident = hgrn_consts()
    maps = []
    for b in range(B):
        qf = cat_fm(r3, "qT", b).reshape(HH, 128, S)
        kf = cat_fm(r3, "kT", b).reshape(HH, 128, S)
        lf = cat_fm(r3, "lfT", b).reshape(HH, 128, S)
        vf = cat_tm(r3, "v", b)
        for g in range(4):
            v2 = np.stack([np.ascontiguousarray(vf[:, hd * 128:(hd + 1) * 128].reshape(64, 128, 128).transpose(1, 0, 2)).reshape(128, 64 * 128)
                           for hd in range(2 * g, 2 * g + 2)])
            maps.append({
                "q": np.ascontiguousarray(qf[2 * g:2 * g + 2]), "k": np.ascontiguousarray(kf[2 * g:2 * g + 2]),
                "lf": np.ascontiguousarray(lf[2 * g:2 * g + 2]), "v": v2, "m01": m01, "rm": rm, "ident": ident,
            })
    r4 = _run(nc4, maps)
    _DBG["r4"] = r4
    ofull = [np.concatenate([r4[b * 4 + g]["o"].reshape(256, S) for g in range(4)], axis=0) for b in range(B)]

    nc5 = build_F({"epi": ("hgrn", 1), "ffns": [(1, 2)], "final": True})
    shared = {
        "gT": gT, "wo": tile_wo(hgrn_w_out[0]), "hgn": fm_cols(hgrn_norm_g[0]),
        "wup0": tile_wup(ffn_w_up[1, 1]), "wdn0": tile_wdn(ffn_w_down[1, 1]),
        "fg": fm_cols(final_norm_g),
    }
    maps = []
    for i, (b, t) in enumerate(cores):
        m = dict(shared)
        m["xT"] = r3[i]["xo"]
        m["modT"] = modT[b]
        m["oT"] = np.ascontiguousarray(ofull[b][:, t * T:(t + 1) * T])
        m["sg"] = r3[i]["sgo"]
        maps.append(m)
    r5 = _run(nc5, maps)
    out = np.empty((B, S, D), np.float32)
    for i, (b, t) in enumerate(cores):
        out[b, t * T:(t + 1) * T, :] = r5[i]["xo"].T
    return out


def kernel(x, c, ada_w, ada_b, norm_g, ffn_w_up, ffn_w_down, fox_w_in, fox_b_f, fox_w_out,
           hgrn_w_in, hgrn_norm_g, hgrn_w_out, hgrn_lb_logits, final_norm_g):
    f32 = lambda a: np.ascontiguousarray(np.asarray(a, dtype=np.float32))
    x, c, ada_w, ada_b, norm_g = f32(x), f32(c), f32(ada_w), f32(ada_b), f32(norm_g)
    ffn_w_up, ffn_w_down, fox_w_in, fox_b_f, fox_w_out = f32(ffn_w_up), f32(ffn_w_down), f32(fox_w_in), f32(fox_b_f), f32(fox_w_out)
    hgrn_w_in, hgrn_norm_g, hgrn_w_out = f32(hgrn_w_in), f32(hgrn_norm_g), f32(hgrn_w_out)
    hgrn_lb_logits, final_norm_g = f32(hgrn_lb_logits), f32(final_norm_g)
    nc = build_mega()
    wi, hi = fox_w_in[0], hgrn_w_in[0]
    U, sel, mk = fox_consts()
    m01, rm, ident = hgrn_consts()
    shared = {
        "modb": fm_cols(ada_b.reshape(18, D)),
        "modw": np.ascontiguousarray(ada_w.reshape(2, 8, 128, 9, D).transpose(0, 3, 2, 1, 4)).reshape(18, 128, 8 * D),
        "gT": fm_cols(norm_g.reshape(6, D)),
        "wup0": tile_wup(ffn_w_up[0, 0]), "wdn0": tile_wdn(ffn_w_down[0, 0]),
        "wup1": tile_wup(ffn_w_up[0, 1]), "wdn1": tile_wdn(ffn_w_down[0, 1]),
        "wup2": tile_wup(ffn_w_up[1, 0]), "wdn2": tile_wdn(ffn_w_down[1, 0]),
        "wup3": tile_wup(ffn_w_up[1, 1]), "wdn3": tile_wdn(ffn_w_down[1, 1]),
        "fox_wq": tile_w_fm(wi[:, 0:D]), "fox_wk": tile_w_fm(wi[:, D:2 * D]), "fox_wg": tile_w_fm(wi[:, 3 * D:4 * D]),
        "fox_wv": tile_w_tm(wi[:, 2 * D:3 * D]),
        "fox_wf": np.ascontiguousarray(wi[:, 4 * D:4 * D + 16].reshape(8, 128, 16).transpose(1, 0, 2)).reshape(128, 128),
        "fox_bfb": np.ascontiguousarray(np.broadcast_to(np.tile(fox_b_f[0], 16), (128, 256))),
        "U": U, "sel": sel, "mk": mk, "identf": np.eye(128, dtype=np.float32),
        "fox_wo": tile_wo(fox_w_out[0]),
        "hgrn_wq": tile_w_fm(hi[:, 0:D]), "hgrn_wf": tile_w_fm(hi[:, D:2 * D]), "hgrn_wg": tile_w_fm(hi[:, 3 * D:4 * D]),
        "hgrn_wv": tile_w_tm(hi[:, 2 * D:3 * D]), "lbl": fm_cols(hgrn_lb_logits),
        "m01": m01, "rm": rm, "ident": ident,
        "hgrn_wo": tile_wo(hgrn_w_out[0]), "hgn": fm_cols(hgrn_norm_g[0]),
        "fg": fm_cols(final_norm_g),
    }
    maps = []
    cores = [(b, t) for b in range(B) for t in range(4)]
    for (b, t) in cores:
        m = dict(shared)
        m["xT"] = np.ascontiguousarray(x[b, t * T:(t + 1) * T, :].T)
        m["cT"] = fm_cols(c[b])
        maps.append(m)
    res = _run(nc, maps)
    out = np.empty((B, S, D), np.float32)
    for i, (b, t) in enumerate(cores):
        out[b, t * T:(t + 1) * T, :] = res[i]["xo"].T
    return out
```

```python
from contextlib import ExitStack
import numpy as np
import ml_dtypes
import concourse.bass as bass
import concourse.mybir as mybir
from concourse.bass_utils import run_bass_kernel_spmd

F32 = mybir.dt.float32
BF16 = mybir.dt.bfloat16
AF = mybir.ActivationFunctionType
ALU = mybir.AluOpType
NPBF = ml_dtypes.bfloat16

D = 1024
B = 2
S = 8192
DFF = 2816
NF = 22
EPS = 1e-6
NCORES = 8
T = 2048
FH = 16
FD = 64
HH = 8
CH = 64


class Buf:
    __slots__ = ("w", "r", "t", "key")

    def __init__(self, t=None):
        self.w = None
        self.r = []
        self.t = t
        self.key = None


class Prog:
    ENG = ["pe", "act", "dve", "pool", "sp"]

    def __init__(self, nc):
        self.nc = nc
        self.ops = {e: [] for e in self.ENG}
        self.clock = {e: {} for e in self.ENG}
        self.snaps = {}
        self.count = {}
        self.needed = set()
        self.nkey = 0
        self.final = None
        self.unit_keys = set()

    def sb(self, es, name, shape, dtype):
        self.nname = getattr(self, "nname", 0) + 1
        t = es.enter_context(self.nc.sbuf_tensor("%s_u%d" % (name, self.nname), list(shape), dtype))
        return Buf(t)

    def ps(self, es, name, shape, dtype):
        self.nname = getattr(self, "nname", 0) + 1
        t = es.enter_context(self.nc.psum_tensor("%s_u%d" % (name, self.nname), list(shape), dtype))
        return Buf(t)

    def newkey(self, buf):
        fk = getattr(self, "free_keys", None)
        if fk is None:
            self.free_keys, self.key_owner = [], {}
            fk = self.free_keys
        if fk:
            k = fk.pop()
        else:
            self.nkey += 1
            k = "d%d" % self.nkey
        buf.key = k
        self.key_owner[k] = buf
        return k

    def op(self, eng, fn, reads=(), writes=(), dma=None, pe_acc=False):
        need = {}

        def req(ev):
            if ev is None:
                return
            k, s = ev
            if s > need.get(k, 0):
                need[k] = s

        for b in reads:
            req(b.w)
        for b in writes:
            if not (pe_acc and b.w is not None and b.w[0] == "pe"):
                req(b.w)
            for r in b.r:
                req(r)
        key = dma or eng
        if fn is None:
            idx = 0
        else:
            idx = self.count.get(key, 0) + 1
            self.count[key] = idx
        ck = self.clock[eng]
        waits = []
        for k, s in need.items():
            if ck.get(k, 0) < s:
                waits.append((k, s))
        for k, s in waits:
            sn = self.snaps[(k, s)]
            for kk, ss in sn.items():
                if ck.get(kk, 0) < ss:
                    ck[kk] = ss
            if ck.get(k, 0) < s:
                ck[k] = s
            self.needed.add((k, s))
        self.ops[eng].append((fn, waits, key, idx))
        if fn is None:
            return None
        self.snaps[(key, idx)] = dict(ck)
        ev = (key, idx)
        for b in reads:
            b.r.append(ev)
        for b in writes:
            b.w = ev
            b.r = []
        return ev

    def dma(self, eng, out, in_, reads=(), writes=(), ow=None):
        wb = ow if ow is not None else writes[0]
        if wb.key is None:
            self.newkey(wb)
        def _fn(e, out=out, in_=in_):
            try:
                return e.dma_start(out=out, in_=in_)
            except Exception:
                print("DMA FAIL", out, in_)
                raise
        ev = self.op(eng, _fn, reads=reads, writes=writes, dma=wb.key)
        if ow is not None:
            ow.w = ev
        return ev

    def collective(self, kind, groups, src_ap, dst_ap, reads, wbuf):
        wbuf.key = "cc"
        self.unit_keys.add(wbuf.key)
        return self.op("pool", lambda e: e.collective_compute(kind, ALU.bypass, replica_groups=groups,
                                                              ins=[src_ap], outs=[dst_ap]),
                       reads=reads, writes=[wbuf], dma=wbuf.key)

    def finish(self, outs, eng="sp"):
        self.op(eng, None, reads=list(outs))

    def barrier(self):
        need = dict(self.count)
        for e in self.ENG:
            self.op(e, None, extra=need)
        for k, b in list(getattr(self, "key_owner", {}).items()):
            b.key = None
            self.free_keys.append(k)
        if hasattr(self, "key_owner"):
            self.key_owner.clear()

    def emit(self):
        nc = self.nc
        keys = list(self.count.keys())
        for e in self.ENG:
            if e not in keys:
                keys.append(e)
        rank = {}
        for k in keys:
            if k in self.ENG:
                idxs = sorted(s for (kk, s) in self.needed if kk == k)
                rank[k] = {s: i + 1 for i, s in enumerate(idxs)}
        with ExitStack() as es:
            sems = {k: es.enter_context(nc.semaphore("s_" + k)) for k in keys}
            block = es.enter_context(nc.Block())

            def run(eng_name):
                def body(e):
                    for fn, waits, key, idx in self.ops[eng_name]:
                        for k, s in waits:
                            v = rank[k][s] if k in rank else (s if k in self.unit_keys else 16 * s)
                            e.wait_ge(sems[k], v)
                        if fn is None:
                            continue
                        ins = fn(e)
                        if key in rank:
                            if (key, idx) in self.needed:
                                ins.then_inc(sems[key], 1)
                        elif key in self.unit_keys:
                            ins.then_inc(sems[key], 1)
                        else:
                            ins.then_inc(sems[key], 16)
                return body

            block.tensor(run("pe"))
            block.scalar(run("act"))
            block.vector(run("dve"))
            block.gpsimd(run("pool"))
            block.sync(run("sp"))


def _patch_op():
    base = Prog.op

    def op(self, eng, fn, reads=(), writes=(), dma=None, pe_acc=False, extra=None):
        if extra:
            dummy = []
            for k, s in extra.items():
                if s > 0:
                    b = Buf()
                    b.w = (k, s)
                    dummy.append(b)
            reads = list(reads) + dummy
            ev = base(self, eng, fn, reads=reads, writes=writes, dma=dma, pe_acc=pe_acc)
            return ev
        return base(self, eng, fn, reads=reads, writes=writes, dma=dma, pe_acc=pe_acc)

    Prog.op = op


_patch_op()


SQD = float(np.sqrt(D))


class Ctx:
    pass


def mcol(l, v, ch):
    return (l * 9 + v) * 8 + ch


def build_F(cfg):
    nc = bass.Bass("TRN2", target_bir_lowering=False)
    P = Prog(nc)
    c = Ctx()
    c.P, c.nc = P, nc
    dr = {}

    def din(name, shape, dt=F32):
        dr[name] = nc.dram_tensor(name, list(shape), dt, kind="ExternalInput").ap()
        return dr[name]

    def dout(name, shape, dt=F32):
        dr[name] = nc.dram_tensor(name, list(shape), dt, kind="ExternalOutput").ap()
        return dr[name]

    xT = din("xT", [D, T])
    modT = din("modT", [128, 144])
    gT = din("gT", [128, 48])
    outs = []
    with ExitStack() as es:
        X = es.enter_context(nc.sbuf_tensor("X", [128, 8, T], F32))
        c.X = X
        c.Xb = [[Buf(X) for _ in range(4)] for _ in range(8)]
        c.modt = P.sb(es, "modt", [128, 144], F32)
        c.gt = P.sb(es, "gt", [128, 48], F32)
        c.der = P.sb(es, "der", [128, 96], F32)
        c.ones = P.sb(es, "ones", [128, 128], BF16)
        c.bank = [P.ps(es, "bank%d" % i, [128, 512], F32) for i in range(8)]
        sqt = es.enter_context(nc.sbuf_tensor("sq", [128, 8, 512], BF16))
        c.sq = [Buf(sqt) for _ in range(8)]
        c.rstd = P.sb(es, "rstd", [128, 512], F32)
        c.tmp = [P.sb(es, "tmp%d" % i, [128, 512], F32) for i in range(2)]

        xv = xT.rearrange("(c p) t -> p c t", p=128)
        for kc in range(8):
            P.dma("sp", X[:, kc, :], xv[:, kc, :], writes=[c.Xb[kc][tt] for tt in range(4)])
        P.dma("sp", c.modt.t[:, :], modT, writes=[c.modt])
        P.dma("sp", c.gt.t[:, :], gT, writes=[c.gt])
        P.op("pool", lambda e: e.memset(c.ones.t[:, :], 1.0), writes=[c.ones])
        for l in range(2):
            for sub in range(3):
                base = ((l * 3 + sub) * 2) * 8
                sc0 = mcol(l, sub * 3 + 1, 0)
                g0 = (l * 3 + sub) * 8
                ga0 = mcol(l, sub * 3 + 2, 0)
                P.op("dve", lambda e, base=base, sc0=sc0, g0=g0: e.scalar_tensor_tensor(
                    c.der.t[:, base:base + 8], c.modt.t[:, sc0:sc0 + 8], 1.0, c.gt.t[:, g0:g0 + 8], ALU.add, ALU.mult),
                    reads=[c.modt, c.gt], writes=[c.der])
                P.op("dve", lambda e, base=base: e.tensor_scalar(
                    c.der.t[:, base:base + 8], c.der.t[:, base:base + 8], SQD, None, ALU.mult),
                    reads=[c.der], writes=[c.der])
                P.op("dve", lambda e, base=base, ga0=ga0, sub=sub: e.tensor_scalar(
                    c.der.t[:, base + 8:base + 16], c.modt.t[:, ga0:ga0 + 8], (1.0 if sub == 1 else 0.5), None, ALU.mult),
                    reads=[c.modt], writes=[c.der])

        def Acol(l, sub, ch):
            j = ((l * 3 + sub) * 2) * 8 + ch
            return c.der.t[:, j:j + 1]

        def Gcol(l, sub, ch):
            j = ((l * 3 + sub) * 2 + 1) * 8 + ch
            return c.der.t[:, j:j + 1]

        def Scol(l, sub, ch):
            j = mcol(l, sub * 3 + 0, ch)
            return c.modt.t[:, j:j + 1]

        def rstd_tile(src_fn, src_bufs, epsk):
            for kc in range(8):
                P.op("act", lambda e, kc=kc: e.activation(sqt[:, kc, :], src_fn(kc), AF.Square),
                     reads=[src_bufs[kc]], writes=[c.sq[kc]])
            for kc in range(8):
                P.op("pe", lambda e, kc=kc: e.matmul(c.bank[6].t[:, :], c.ones.t[:, :], sqt[:, kc, :],
                                                      start=(kc == 0), stop=(kc == 7)),
                     reads=[c.ones, c.sq[kc]], writes=[c.bank[6]], pe_acc=(kc > 0))
            P.op("dve", lambda e: e.tensor_scalar(c.rstd.t[:, :], c.bank[6].t[:, :], epsk, None, ALU.add),
                 reads=[c.bank[6]], writes=[c.rstd])
            P.op("act", lambda e: e.activation(c.rstd.t[:, :], c.rstd.t[:, :], AF.Sqrt), reads=[c.rstd], writes=[c.rstd])
            P.op("dve", lambda e: e.reciprocal(c.rstd.t[:, :], c.rstd.t[:, :]), reads=[c.rstd], writes=[c.rstd])

        def modnorm_tile(l, sub, tt, hdst, hbuf):
            t0 = tt * 512
            rstd_tile(lambda kc: X[:, kc, t0:t0 + 512], [c.Xb[kc][tt] for kc in range(8)], EPS * D)
            for kc in range(8):
                tb = c.tmp[kc % 2]
                P.op("dve", lambda e, kc=kc, tb=tb: e.tensor_tensor(tb.t[:, :], X[:, kc, t0:t0 + 512], c.rstd.t[:, :], ALU.mult),
                     reads=[c.Xb[kc][tt], c.rstd], writes=[tb])
                P.op("act", lambda e, kc=kc, tb=tb: e.activation(hdst(kc), tb.t[:, :], AF.Identity,
                                                               bias=Scol(l, sub, kc), scale=Acol(l, sub, kc)),
                     reads=[tb, c.der, c.modt], writes=[hbuf(kc)])

        def epilogue(kind, l):
            oT = din("oT", [D, T])
            sgd = din("sg", [D, T], BF16)
            wod = din("wo", [128, 8192])
            ov = oT.rearrange("(c p) t -> p c t", p=128)
            sv = sgd.rearrange("(c p) t -> p c t", p=128)
            with ExitStack() as e2:
                wo = P.sb(e2, "wo_sb", [128, 8, 1024], BF16)
                P.dma("pool", wo.t[:, :, :], wod.rearrange("p (k d) -> p k d", k=8), writes=[wo])
                ot = [P.sb(e2, "ot%d" % i, [128, 8, 512], F32) for i in range(2)]
                st = [P.sb(e2, "st%d" % i, [128, 8, 512], BF16) for i in range(2)]
                ogt = [e2.enter_context(nc.sbuf_tensor("og%d" % i, [128, 8, 512], BF16)) for i in range(2)]
                ogb = [[Buf(ogt[i]) for _ in range(8)] for i in range(2)]
                if kind == "hgrn":
                    hgd = din("hgn", [128, 8])
                    hg = P.sb(e2, "hg", [128, 8], F32)
                    P.dma("sp", hg.t[:, :], hgd, writes=[hg])
                    P.op("dve", lambda e: e.tensor_scalar(hg.t[:, :], hg.t[:, :], float(np.sqrt(128.0)), None, ALU.mult),
                         reads=[hg], writes=[hg])
                    sq1 = P.sb(e2, "sq1", [128, 512], BF16)
                    r1 = P.sb(e2, "r1", [128, 512], F32)
                    t1 = P.sb(e2, "t1", [128, 512], F32)
                for tt in range(4):
                    t0 = tt * 512
                    o_, s_, og_ = ot[tt % 2], st[tt % 2], ogt[tt % 2]
                    P.dma("sp", o_.t[:, :, :], ov[:, :, t0:t0 + 512], writes=[o_])
                    P.dma("sp", s_.t[:, :, :], sv[:, :, t0:t0 + 512], writes=[s_])
                    if kind == "fox":
                        for kc in range(8):
                            P.op("dve", lambda e, kc=kc, o_=o_, s_=s_, og_=og_: e.tensor_tensor(
                                og_[:, kc, :], o_.t[:, kc, :], s_.t[:, kc, :], ALU.mult),
                                reads=[o_, s_], writes=[ogb[tt % 2][kc]])
                    else:
                        for kc in range(8):
                            P.op("act", lambda e, kc=kc, o_=o_: e.activation(sq1.t[:, :], o_.t[:, kc, :], AF.Square),
                                 reads=[o_], writes=[sq1])
                            P.op("pe", lambda e: e.matmul(c.bank[7].t[:, :], c.ones.t[:, :], sq1.t[:, :], start=True, stop=True),
                                 reads=[c.ones, sq1], writes=[c.bank[7]])
                            P.op("dve", lambda e: e.tensor_scalar(r1.t[:, :], c.bank[7].t[:, :], EPS * 128.0, None, ALU.add),
                                 reads=[c.bank[7]], writes=[r1])
                            P.op("act", lambda e: e.activation(r1.t[:, :], r1.t[:, :], AF.Sqrt), reads=[r1], writes=[r1])
                            P.op("dve", lambda e: e.reciprocal(r1.t[:, :], r1.t[:, :]), reads=[r1], writes=[r1])
                            P.op("dve", lambda e, kc=kc, o_=o_: e.tensor_tensor(t1.t[:, :], o_.t[:, kc, :], r1.t[:, :], ALU.mult),
                                 reads=[o_, r1], writes=[t1])
                            P.op("dve", lambda e, kc=kc, s_=s_, og_=og_: e.scalar_tensor_tensor(
                                og_[:, kc, :], t1.t[:, :], hg.t[:, kc:kc + 1], s_.t[:, kc, :], ALU.mult, ALU.mult),
                                reads=[t1, hg, s_], writes=[ogb[tt % 2][kc]])
                    for dc in range(8):
                        bk = c.bank[4 + dc % 2]
                        for kc in range(8):
                            P.op("pe", lambda e, kc=kc, dc=dc, bk=bk, og_=og_: e.matmul(
                                bk.t[:, :], wo.t[:, kc, dc * 128:(dc + 1) * 128], og_[:, kc, :],
                                start=(kc == 0), stop=(kc == 7)),
                                reads=[wo, ogb[tt % 2][kc]], writes=[bk], pe_acc=(kc > 0))
                        P.op("dve", lambda e, dc=dc, bk=bk, t0=t0: e.scalar_tensor_tensor(
                            X[:, dc, t0:t0 + 512], bk.t[:, :], Gcol(l, 1, dc), X[:, dc, t0:t0 + 512], ALU.mult, ALU.add),
                            reads=[bk, c.der, c.Xb[dc][tt]], writes=[c.Xb[dc][tt]])
            P.barrier()

        def ffn(j, l, sub):
            wupd = din("wup%d" % j, [11, 128, 4096])
            wdnd = din("wdn%d" % j, [8, 128, 2816])
            with ExitStack() as e2:
                hbt = e2.enter_context(nc.sbuf_tensor("hb_%d" % j, [128, 8, 1024], BF16))
                hbb = [[Buf(hbt) for _ in range(2)] for _ in range(8)]
                actt = e2.enter_context(nc.sbuf_tensor("actb_%d" % j, [128, NF, 1024], BF16))
                actb = [[Buf(actt) for _ in range(2)] for _ in range(NF)]
                wu = [P.sb(e2, "wu%d_%d" % (j, i), [128, 2, 8, 256], BF16) for i in range(2)]
                wd = [P.sb(e2, "wd%d_%d" % (j, i), [128, NF, 128], BF16) for i in range(2)]
                sa = [P.sb(e2, "sa%d_%d" % (j, i), [128, 512], F32) for i in range(2)]
                for half in range(2):
                    for t2 in range(2):
                        tt = half * 2 + t2
                        modnorm_tile(l, sub, tt, lambda kc, t2=t2: hbt[:, kc, t2 * 512:(t2 + 1) * 512],
                                     lambda kc, t2=t2: hbb[kc][t2])
                    it = 0
                    for g in range(11):
                        w_ = wu[g % 2]
                        P.dma("pool", w_.t[:, :, :, :], wupd[g].rearrange("p (a k f) -> p a k f", a=2, k=8), writes=[w_])
                        for jf in range(2):
                            fc = 2 * g + jf
                            for t2 in range(2):
                                bA, bB = c.bank[it % 2], c.bank[2 + it % 2]
                                s_ = sa[it % 2]
                                it += 1
                                for kc in range(8):
                                    P.op("pe", lambda e, kc=kc, w_=w_, jf=jf, t2=t2, bA=bA: e.matmul(
                                        bA.t[:, :], w_.t[:, 0, kc, jf * 128:(jf + 1) * 128], hbt[:, kc, t2 * 512:(t2 + 1) * 512],
                                        start=(kc == 0), stop=(kc == 7)),
                                        reads=[w_, hbb[kc][t2]], writes=[bA], pe_acc=(kc > 0))
                                for kc in range(8):
                                    P.op("pe", lambda e, kc=kc, w_=w_, jf=jf, t2=t2, bB=bB: e.matmul(
                                        bB.t[:, :], w_.t[:, 1, kc, jf * 128:(jf + 1) * 128], hbt[:, kc, t2 * 512:(t2 + 1) * 512],
                                        start=(kc == 0), stop=(kc == 7)),
                                        reads=[w_, hbb[kc][t2]], writes=[bB], pe_acc=(kc > 0))
                                P.op("act", lambda e, s_=s_, bA=bA: e.activation(s_.t[:, :], bA.t[:, :], AF.Silu),
                                     reads=[bA], writes=[s_])
                                P.op("dve", lambda e, s_=s_, bB=bB, fc=fc, t2=t2: e.tensor_tensor(
                                    actt[:, fc, t2 * 512:(t2 + 1) * 512], bB.t[:, :], s_.t[:, :], ALU.mult),
                                    reads=[bB, s_], writes=[actb[fc][t2]])
                    for dc in range(8):
                        w_ = wd[dc % 2]
                        P.dma("pool", w_.t[:, :, :], wdnd[dc].rearrange("p (f d) -> p f d", f=NF), writes=[w_])
                        for t2 in range(2):
                            tt = half * 2 + t2
                            t0 = tt * 512
                            bk = c.bank[4 + (dc * 2 + t2) % 2]
                            for fc in range(NF):
                                P.op("pe", lambda e, fc=fc, w_=w_, t2=t2, bk=bk: e.matmul(
                                    bk.t[:, :], w_.t[:, fc, :], actt[:, fc, t2 * 512:(t2 + 1) * 512],
                                    start=(fc == 0), stop=(fc == NF - 1)),
                                    reads=[w_, actb[fc][t2]], writes=[bk], pe_acc=(fc > 0))
                            P.op("dve", lambda e, dc=dc, bk=bk, t0=t0: e.scalar_tensor_tensor(
                                X[:, dc, t0:t0 + 512], bk.t[:, :], Gcol(l, sub, dc), X[:, dc, t0:t0 + 512], ALU.mult, ALU.add),
                                reads=[bk, c.der, c.Xb[dc][tt]], writes=[c.Xb[dc][tt]])
            P.barrier()

        def proj_fm(wname, hbt, hbb, evac, n_oc=8):
            wd_ = din(wname, [n_oc, 128, 1024])
            with ExitStack() as e3:
                wp = [P.sb(e3, wname + "_sb%d" % i, [128, 8, 128], BF16) for i in range(2)]
                it = 0
                for oc in range(n_oc):
                    w_ = wp[oc % 2]
                    P.dma("pool", w_.t[:, :, :], wd_[oc].rearrange("p (k f) -> p k f", k=8), writes=[w_])
                    for tt in range(4):
                        bk = c.bank[it % 4]
                        it += 1
                        for kc in range(8):
                            P.op("pe", lambda e, kc=kc, w_=w_, tt=tt, bk=bk: e.matmul(
                                bk.t[:, :], w_.t[:, kc, :], hbt[:, kc, tt * 512:(tt + 1) * 512],
                                start=(kc == 0), stop=(kc == 7)),
                                reads=[w_, hbb[kc][tt]], writes=[bk], pe_acc=(kc > 0))
                        evac(oc, tt, bk)
                P.barrier()

        def proj_tm(wname, hbt, hbb, vout, vob, func):
            wd_ = din(wname, [2, 128, 4096])
            with ExitStack() as e3:
                wv = P.sb(e3, wname + "_sb", [128, 2, 8, 512], BF16)
                for cg in range(2):
                    P.dma("pool", wv.t[:, cg, :, :], wd_[cg].rearrange("p (k f) -> p k f", k=8), writes=[wv])
                vt = [P.sb(e3, "vt%d" % i, [128, 512], BF16) for i in range(2)]
                it = 0
                for tk in range(16):
                    for cg in range(2):
                        bk = c.bank[it % 4]
                        v_ = vt[it % 2]
                        it += 1
                        for kc in range(8):
                            P.op("pe", lambda e, kc=kc, tk=tk, cg=cg, bk=bk: e.matmul(
                                bk.t[:, :], hbt[:, kc, tk * 128:(tk + 1) * 128], wv.t[:, cg, kc, :],
                                start=(kc == 0), stop=(kc == 7)),
                                reads=[wv, hbb[kc][tk // 4]], writes=[bk], pe_acc=(kc > 0))
                        P.op("act", lambda e, bk=bk, v_=v_: e.activation(v_.t[:, :], bk.t[:, :], func),
                             reads=[bk], writes=[v_])
                        P.dma("sp", vout[tk * 128:(tk + 1) * 128, cg * 512:(cg + 1) * 512], v_.t[:, :], reads=[v_], ow=vob)
                P.barrier()

        def stage_out(e3, name, shape, dt):
            return [P.sb(e3, name + "%d" % i, shape, dt) for i in range(2)]

        def projections(kind, l):
            with ExitStack() as e2:
                hbt = e2.enter_context(nc.sbuf_tensor("hb2", [128, 8, T], BF16))
                hbb = [[Buf(hbt) for _ in range(4)] for _ in range(8)]
                for tt in range(4):
                    modnorm_tile(l, 1, tt, lambda kc, tt=tt: hbt[:, kc, tt * 512:(tt + 1) * 512],
                                 lambda kc, tt=tt: hbb[kc][tt])
                qo = dout("qT", [D, T], BF16)
                qob = Buf()
                outs.append(qob)
                sgo = dout("sgo", [D, T], BF16)
                sgob = Buf()
                outs.append(sgob)
                vo = dout("v", [T, D], BF16)
                vob = Buf()
                outs.append(vob)
                cnt = [0]

                def simple_evac(od, ob, func, scale, st, dt_eng="act"):
                    def evac(oc, tt, bk):
                        s_ = st[cnt[0] % 2]
                        cnt[0] += 1
                        P.op("act", lambda e, s_=s_, bk=bk: e.activation(s_.t[:, :], bk.t[:, :], func, scale=scale),
                             reads=[bk], writes=[s_])
                        P.dma("sp", od[oc * 128:(oc + 1) * 128, tt * 512:(tt + 1) * 512], s_.t[:, :], reads=[s_], ow=ob)
                    return evac

                stb = stage_out(e2, "stb", [128, 512], BF16)
                if kind == "fox":
                    ko = dout("kT", [D, T], BF16)
                    kob = Buf()
                    outs.append(kob)
                    lfo = dout("lf", [16, T], F32)
                    lfob = Buf()
                    outs.append(lfob)
                    proj_fm("wq", hbt, hbb, simple_evac(qo, qob, AF.Copy, float(FD ** -0.5), stb))
                    proj_fm("wk", hbt, hbb, simple_evac(ko, kob, AF.Copy, 1.0, stb))
                    proj_fm("wg", hbt, hbb, simple_evac(sgo, sgob, AF.Sigmoid, 1.0, stb))
                    proj_tm("wv", hbt, hbb, vo, vob, AF.Copy)
                    wfd = din("wf", [128, 128])
                    bfd = din("bf", [16, 1])
                    wf = P.sb(e2, "wf_sb", [128, 8, 16], BF16)
                    P.dma("pool", wf.t[:, :, :], wfd.rearrange("p (k f) -> p k f", k=8), writes=[wf])
                    nbf = P.sb(e2, "nbf", [16, 1], F32)
                    P.dma("sp", nbf.t[:, :], bfd, writes=[nbf])
                    P.op("dve", lambda e: e.tensor_scalar(nbf.t[:, :], nbf.t[:, :], -1.0, None, ALU.mult), reads=[nbf], writes=[nbf])
                    e1 = P.sb(e2, "e1", [16, 512], F32)
                    l1 = [P.sb(e2, "l1_%d" % i, [16, 512], F32) for i in range(2)]
                    for tt in range(4):
                        bk = c.bank[tt % 4]
                        for kc in range(8):
                            P.op("pe", lambda e, kc=kc, tt=tt, bk=bk: e.matmul(
                                bk.t[0:16, :], wf.t[:, kc, :], hbt[:, kc, tt * 512:(tt + 1) * 512],
                                start=(kc == 0), stop=(kc == 7)),
                                reads=[wf, hbb[kc][tt]], writes=[bk], pe_acc=(kc > 0))
                        l_ = l1[tt % 2]
                        P.op("act", lambda e, bk=bk: e.activation(e1.t[:, :], bk.t[0:16, :], AF.Exp, bias=nbf.t[:, 0:1], scale=-1.0),
                             reads=[bk, nbf], writes=[e1])
                        P.op("act", lambda e, l_=l_: e.activation(l_.t[:, :], e1.t[:, :], AF.Ln, bias=1.0, scale=1.0),
                             reads=[e1], writes=[l_])
                        P.op("dve", lambda e, l_=l_: e.tensor_scalar(l_.t[:, :], l_.t[:, :], -1.0, None, ALU.mult),
                             reads=[l_], writes=[l_])
                        P.dma("sp", lfo[:, tt * 512:(tt + 1) * 512], l_.t[:, :], reads=[l_], ow=lfob)
                else:
                    ko = dout("kT", [D, T], F32)
                    kob = Buf()
                    outs.append(kob)
                    lfo = dout("lfT", [D, T], F32)
                    lfob = Buf()
                    outs.append(lfob)
                    lbd = din("lbl", [128, 16])
                    lbl = P.sb(e2, "lbl_sb", [128, 16], F32)
                    lb = P.sb(e2, "lb", [128, 8], F32)
                    oml = P.sb(e2, "oml", [128, 8], F32)
                    P.dma("sp", lbl.t[:, :], lbd, writes=[lbl])
                    P.op("dve", lambda e: e.tensor_tensor(lb.t[:, :], lbl.t[:, 8:16], lbl.t[:, 0:8], ALU.subtract), reads=[lbl], writes=[lb])
                    P.op("act", lambda e: e.activation(lb.t[:, :], lb.t[:, :], AF.Sigmoid), reads=[lb], writes=[lb])
                    P.op("dve", lambda e: e.tensor_scalar(oml.t[:, :], lb.t[:, :], -1.0, 1.0, ALU.mult, ALU.add), reads=[lb], writes=[oml])
                    proj_fm("wq", hbt, hbb, simple_evac(qo, qob, AF.Copy, 1.0, stb))
                    proj_fm("wg", hbt, hbb, simple_evac(sgo, sgob, AF.Silu, 1.0, stb))
                    proj_tm("wv", hbt, hbb, vo, vob, AF.Silu)
                    sg1 = P.sb(e2, "sg1", [128, 512], F32)
                    ff = stage_out(e2, "ff", [128, 512], F32)
                    lff = stage_out(e2, "lff", [128, 512], F32)
                    kk = stage_out(e2, "kk", [128, 512], F32)

                    def f_evac(oc, tt, bk):
                        i = cnt[0] % 2
                        cnt[0] += 1
                        f_, l_, k_ = ff[i], lff[i], kk[i]
                        P.op("act", lambda e, bk=bk: e.activation(sg1.t[:, :], bk.t[:, :], AF.Sigmoid), reads=[bk], writes=[sg1])
                        P.op("dve", lambda e, f_=f_, oc=oc: e.tensor_scalar(f_.t[:, :], sg1.t[:, :], oml.t[:, oc:oc + 1], lb.t[:, oc:oc + 1], ALU.mult, ALU.add),
                             reads=[sg1, oml, lb], writes=[f_])
                        P.op("act", lambda e, f_=f_, l_=l_: e.activation(l_.t[:, :], f_.t[:, :], AF.Ln), reads=[f_], writes=[l_])
                        P.op("dve", lambda e, f_=f_, k_=k_: e.tensor_scalar(k_.t[:, :], f_.t[:, :], -1.0, 1.0, ALU.mult, ALU.add),
                             reads=[f_], writes=[k_])
                        P.dma("sp", lfo[oc * 128:(oc + 1) * 128, tt * 512:(tt + 1) * 512], l_.t[:, :], reads=[l_], ow=lfob)
                        P.dma("sp", ko[oc * 128:(oc + 1) * 128, tt * 512:(tt + 1) * 512], k_.t[:, :], reads=[k_], ow=kob)
                    proj_fm("wf", hbt, hbb, f_evac)
            P.barrier()

        if cfg.get("epi"):
            epilogue(cfg["epi"][0], cfg["epi"][1])
        for j, (l, sub) in enumerate(cfg["ffns"]):
            ffn(j, l, sub)
        if cfg.get("proj"):
            projections(cfg["proj"][0], cfg["proj"][1])
        xo = dout("xo", [D, T])
        xob = Buf()
        outs.append(xob)
        xov = xo.rearrange("(c p) t -> p c t", p=128)
        if cfg.get("final"):
            fgd = din("fg", [128, 8])
            fg = P.sb(es, "fg_sb", [128, 8], F32)
            P.dma("sp", fg.t[:, :], fgd, writes=[fg])
            P.op("dve", lambda e: e.tensor_scalar(fg.t[:, :], fg.t[:, :], SQD, None, ALU.mult), reads=[fg], writes=[fg])
            yo = [P.sb(es, "yo%d" % i, [128, 512], F32) for i in range(2)]
            it = 0
            for tt in range(4):
                t0 = tt * 512
                rstd_tile(lambda kc, t0=t0: X[:, kc, t0:t0 + 512], [c.Xb[kc][tt] for kc in range(8)], EPS * D)
                for kc in range(8):
                    y_ = yo[it % 2]
                    it += 1
                    P.op("dve", lambda e, kc=kc, y_=y_, t0=t0: e.scalar_tensor_tensor(
                        y_.t[:, :], X[:, kc, t0:t0 + 512], fg.t[:, kc:kc + 1], c.rstd.t[:, :], ALU.mult, ALU.mult),
                        reads=[c.Xb[kc][tt], fg, c.rstd], writes=[y_])
                    P.dma("sp", xov[:, kc, t0:t0 + 512], y_.t[:, :], reads=[y_], ow=xob)
        else:
            for kc in range(8):
                P.dma("sp", xov[:, kc, :], X[:, kc, :], reads=[c.Xb[kc][tt] for tt in range(4)], ow=xob)
        P.finish(outs)
        P.emit()
    return nc


MC = 2304


def build_mod():
    nc = bass.Bass("TRN2", target_bir_lowering=False)
    P = Prog(nc)
    cT = nc.dram_tensor("cT", [128, 16], F32, kind="ExternalInput").ap()
    w = nc.dram_tensor("w", [128, 8 * MC], F32, kind="ExternalInput").ap()
    bias = nc.dram_tensor("bias", [2, MC], F32, kind="ExternalInput").ap()
    mo = nc.dram_tensor("mo", [2, MC], F32, kind="ExternalOutput").ap()
    with ExitStack() as es:
        ct = P.sb(es, "ct", [128, 8, 2], F32)
        wt = [P.sb(es, "wt%d" % i, [128, 8, 384], F32) for i in range(6)]
        bt = P.sb(es, "bt", [2, MC], F32)
        ot = P.sb(es, "ot", [2, MC], F32)
        banks = [P.ps(es, "bk%d" % i, [128, 512], F32) for i in range(2)]
        P.dma("sp", ct.t[:, :, :], cT.rearrange("p (k b) -> p k b", k=8), writes=[ct])
        P.dma("sp", bt.t[:, :], bias, writes=[bt])
        wv = w.rearrange("p (k n) -> p k n", k=8)
        for i in range(6):
            P.dma("sp" if i % 2 == 0 else "pool", wt[i].t[:, :, :], wv[:, :, i * 384:(i + 1) * 384], writes=[wt[i]])
        P.op("act", lambda e: e.activation(ct.t[:, :, :], ct.t[:, :, :], AF.Silu), reads=[ct], writes=[ct])
        for i in range(6):
            bk = banks[i % 2]
            for kc in range(8):
                P.op("pe", lambda e, kc=kc, i=i, bk=bk: e.matmul(bk.t[0:2, 0:384], ct.t[:, kc, :], wt[i].t[:, kc, :],
                                                                 start=(kc == 0), stop=(kc == 7)),
                     reads=[ct, wt[i]], writes=[bk], pe_acc=(kc > 0))
            P.op("dve", lambda e, i=i, bk=bk: e.tensor_tensor(ot.t[:, i * 384:(i + 1) * 384], bk.t[0:2, 0:384],
                                                             bt.t[:, i * 384:(i + 1) * 384], ALU.add),
                 reads=[bk, bt], writes=[ot])
        ob = Buf()
        P.dma("sp", mo, ot.t[:, :], reads=[ot], ow=ob)
        P.finish([ob])
        P.emit()
    return nc


def run_mod(c, ada_w, ada_b):
    nc = build_mod()
    cT = np.ascontiguousarray(c.T.reshape(8, 128, B).transpose(1, 0, 2)).reshape(128, 16)
    wall = np.concatenate([ada_w[0], ada_w[1]], axis=1)
    ball = np.concatenate([ada_b[0], ada_b[1]], axis=0)
    maps = []
    for j in range(NCORES):
        wj = wall[:, j * MC:(j + 1) * MC].reshape(8, 128, MC).transpose(1, 0, 2)
        maps.append({"cT": cT, "w": np.ascontiguousarray(wj).reshape(128, 8 * MC),
                     "bias": np.ascontiguousarray(np.broadcast_to(ball[j * MC:(j + 1) * MC], (2, MC)))})
    res = run_bass_kernel_spmd(nc, maps, core_ids=list(range(NCORES)))
    mod = np.concatenate([r["mo"] for r in res.results], axis=1)
    return mod.reshape(B, 2, 9, D)


def fm_cols(v):
    lead = int(np.prod(v.shape[:-1])) if v.ndim > 1 else 1
    a = v.reshape(lead, 8, 128).transpose(2, 0, 1)
    return np.ascontiguousarray(a).reshape(128, lead * 8)


def tile_w_fm(w):
    n = w.shape[1] // 128
    a = w.reshape(8, 128, n, 128).transpose(2, 1, 0, 3)
    return np.ascontiguousarray(a).reshape(n, 128, 1024)


def tile_w_tm(w):
    a = w.reshape(8, 128, 2, 512).transpose(2, 1, 0, 3)
    return np.ascontiguousarray(a).reshape(2, 128, 4096)


def tile_wup(w):
    a = w.reshape(8, 128, 2, 11, 256).transpose(3, 1, 2, 0, 4)
    return np.ascontiguousarray(a).reshape(11, 128, 4096)


def tile_wdn(w):
    a = w.reshape(NF, 128, 8, 128).transpose(2, 1, 0, 3)
    return np.ascontiguousarray(a).reshape(8, 128, NF * 128)


def tile_wo(w):
    a = w.reshape(8, 128, D).transpose(1, 0, 2)
    return np.ascontiguousarray(a).reshape(128, 8 * D)


NEG = -30000.0


def build_fox():
    nc = bass.Bass("TRN2", target_bir_lowering=False)
    P = Prog(nc)
    qd = nc.dram_tensor("q", [4, 64, S], BF16, kind="ExternalInput").ap()
    kd = nc.dram_tensor("k", [4, 64, S], BF16, kind="ExternalInput").ap()
    vd = nc.dram_tensor("v", [4, 128, 64 * 64], BF16, kind="ExternalInput").ap()
    ltd = nc.dram_tensor("lt", [128, 256], F32, kind="ExternalInput").ap()
    lqd = nc.dram_tensor("lq", [16, 2048], F32, kind="ExternalInput").ap()
    Ud = nc.dram_tensor("U", [128, 128], F32, kind="ExternalInput").ap()
    seld = nc.dram_tensor("sel", [128, 128], F32, kind="ExternalInput").ap()
    mkd = nc.dram_tensor("mk", [128, 128], F32, kind="ExternalInput").ap()
    od = nc.dram_tensor("o", [4, 64, S], F32, kind="ExternalOutput").ap()
    shi = nc.dram_tensor("shi", [4, S], BF16).ap()
    slo = nc.dram_tensor("slo", [4, S], BF16).ap()
    with ExitStack() as es:
        bank = [P.ps(es, "bank%d" % i, [128, 512], F32) for i in range(8)]
        U = P.sb(es, "U_sb", [128, 128], F32)
        sel = P.sb(es, "sel_sb", [128, 128], F32)
        mk = P.sb(es, "mk_sb", [128, 128], F32)
        onesf = P.sb(es, "onesf", [128, 128], F32)
        lt = P.sb(es, "lt_sb", [128, 256], F32)
        lq = P.sb(es, "lq_sb", [16, 2048], F32)
        within = P.sb(es, "within", [128, 256], F32)
        tot = P.sb(es, "tot", [128, 256], F32)
        inc = P.sb(es, "inc", [128, 256], F32)
        GT = P.sb(es, "GT", [128, 256], F32)
        gend = P.sb(es, "gend", [128, 256], F32)
        negB = P.sb(es, "negB", [128, 4 * 16 * 64], F32)
        cl = P.sb(es, "cl", [16, 2048], F32)
        Aa = P.sb(es, "Aa", [16, 2048], F32)
        ahi = P.sb(es, "ahi", [16, 2048], BF16)
        ahf = P.sb(es, "ahf", [16, 2048], F32)
        alo = P.sb(es, "alo", [16, 2048], BF16)
        qa = [P.sb(es, "qa%d" % i, [66, S], BF16) for i in range(2)]
        ka = [P.sb(es, "ka%d" % i, [66, S], BF16) for i in range(2)]
        va = [P.sb(es, "va%d" % i, [128, 64, 65], BF16) for i in range(2)]
        pt = [P.sb(es, "pt%d" % i, [128, 512], BF16) for i in range(3)]
        drow = P.sb(es, "drow", [65, 512], F32)
        rec = P.sb(es, "rec", [64, 512], F32)
        oo = [P.sb(es, "oo%d" % i, [64, 512], F32) for i in range(2)]
        ob = Buf()

        for t_, d_ in ((U, Ud), (sel, seld), (mk, mkd), (lt, ltd), (lq, lqd)):
            P.dma("sp", t_.t[:, :], d_, writes=[t_])
        P.op("pool", lambda e: e.memset(onesf.t[:, :], 1.0), writes=[onesf])
        P.op("pe", lambda e: e.matmul(bank[6].t[:, 0:256], U.t[:, :], lt.t[:, :], start=True, stop=True), reads=[U, lt], writes=[bank[6]])
        P.op("pe", lambda e: e.matmul(bank[7].t[:, 0:256], onesf.t[:, :], lt.t[:, :], start=True, stop=True), reads=[onesf, lt], writes=[bank[7]])
        P.op("dve", lambda e: e.tensor_copy(within.t[:, :], bank[6].t[:, 0:256]), reads=[bank[6]], writes=[within])
        P.op("dve", lambda e: e.tensor_copy(tot.t[:, :], bank[7].t[:, 0:256]), reads=[bank[7]], writes=[tot])
        for h in range(4):
            P.op("dve", lambda e, h=h: e.tensor_tensor_scan(inc.t[:, h * 64:(h + 1) * 64], onesf.t[:, 0:64], tot.t[:, h * 64:(h + 1) * 64],
                                                            0.0, ALU.mult, ALU.add), reads=[onesf, tot], writes=[inc])
        P.op("dve", lambda e: e.tensor_tensor(GT.t[:, :], within.t[:, :], inc.t[:, :], ALU.add), reads=[within, inc], writes=[GT])
        P.op("dve", lambda e: e.tensor_tensor(GT.t[:, :], GT.t[:, :], tot.t[:, :], ALU.subtract), reads=[GT, tot], writes=[GT])
        P.op("pe", lambda e: e.matmul(bank[6].t[:, 0:256], sel.t[:, :], GT.t[:, :], start=True, stop=True), reads=[sel, GT], writes=[bank[6]])
        P.op("dve", lambda e: e.tensor_copy(gend.t[:, :], bank[6].t[:, 0:256]), reads=[bank[6]], writes=[gend])
        for h in range(4):
            for Q in range(16):
                j0 = (h * 16 + Q) * 64
                gc = h * 64 + 4 * Q + 3
                P.op("dve", lambda e, h=h, j0=j0, gc=gc: e.tensor_scalar(
                    negB.t[:, j0:j0 + 64], GT.t[:, h * 64:(h + 1) * 64], -1.0, gend.t[:, gc:gc + 1], ALU.mult, ALU.add),
                    reads=[GT, gend], writes=[negB])
        ones16 = P.sb(es, "ones16", [16, 512], F32)
        P.op("pool", lambda e: e.memset(ones16.t[:, :], 1.0), writes=[ones16])
        for h in range(4):
            P.op("dve", lambda e, h=h: e.tensor_tensor_scan(cl.t[:, h * 512:(h + 1) * 512], ones16.t[:, :], lq.t[:, h * 512:(h + 1) * 512],
                                                            0.0, ALU.mult, ALU.add), reads=[lq, ones16], writes=[cl])
        for h in range(4):
            P.op("dve", lambda e, h=h: e.tensor_scalar(Aa.t[:, h * 512:(h + 1) * 512], cl.t[:, h * 512:(h + 1) * 512],
                                                       cl.t[:, h * 512 + 511:h * 512 + 512], None, ALU.subtract),
                 reads=[cl], writes=[Aa])
        P.op("dve", lambda e: e.tensor_copy(ahi.t[:, :], Aa.t[:, :]), reads=[Aa], writes=[ahi])
        P.op("dve", lambda e: e.tensor_copy(ahf.t[:, :], ahi.t[:, :]), reads=[ahi], writes=[ahf])
        P.op("dve", lambda e: e.tensor_tensor(alo.t[:, :], Aa.t[:, :], ahf.t[:, :], ALU.subtract), reads=[Aa, ahf], writes=[alo])
        shb, slb = Buf(), Buf()
        P.dma("sp", shi.rearrange("h (q m) -> q h m", q=16), ahi.t[:, :].rearrange("q (h m) -> q h m", h=4), reads=[ahi], writes=[shb])
        P.dma("sp", slo.rearrange("h (q m) -> q h m", q=16), alo.t[:, :].rearrange("q (h m) -> q h m", h=4), reads=[alo], writes=[slb])

        for i in range(2):
            P.op("pool", lambda e, i=i: e.memset(ka[i].t[64:66, :], 1.0), writes=[ka[i]])
            P.op("pool", lambda e, i=i: e.memset(va[i].t[:, :, 64:65], 1.0), writes=[va[i]])

        def load_head(h):
            q_, k_, v_ = qa[h % 2], ka[h % 2], va[h % 2]
            P.dma("sp", q_.t[0:64, :], qd[h], writes=[q_])
            P.dma("sp", q_.t[64:65, :], shi[h:h + 1, :], reads=[shb], writes=[q_])
            P.dma("sp", q_.t[65:66, :], slo[h:h + 1, :], reads=[slb], writes=[q_])
            P.dma("pool", k_.t[0:64, :], kd[h], writes=[k_])
            P.dma("pool", v_.t[:, :, 0:64], vd[h].rearrange("p (t d) -> p t d", d=64), writes=[v_])

        load_head(0)

        def do_head(h, q_, k_, v_, nit):
            items = [(Q, kt) for Q in range(16) for kt in range(4 * Q + 4)]

            def emit_S(idx, it_no):
                Q, kt = items[idx]
                d = kt - 4 * Q
                c0 = 128 * d if d >= 0 else 0
                bk = bank[it_no % 3]
                p_ = pt[it_no % 3]
                P.op("pe", lambda e: e.matmul(bk.t[:, c0:512], k_.t[0:66, kt * 128:(kt + 1) * 128],
                                              q_.t[0:66, Q * 512 + c0:(Q + 1) * 512], start=True, stop=True),
                     reads=[k_, q_], writes=[bk])
                if d >= 0:
                    P.op("dve", lambda e: e.tensor_tensor(bk.t[:, c0:c0 + 128], bk.t[:, c0:c0 + 128], mk.t[:, :], ALU.add),
                         reads=[bk, mk], writes=[bk])
                jb = (h * 16 + Q) * 64 + kt
                P.op("act", lambda e: e.activation(p_.t[:, c0:512], bk.t[:, c0:512], AF.Exp, bias=negB.t[:, jb:jb + 1], scale=1.0),
                     reads=[bk, negB], writes=[p_])

            def emit_PV(idx, it_no):
                Q, kt = items[idx]
                d = kt - 4 * Q
                c0 = 128 * d if d >= 0 else 0
                p_ = pt[it_no % 3]
                ob_ = bank[3 + Q % 2]
                last = (kt == 4 * Q + 3)
                P.op("pe", lambda e: e.matmul(ob_.t[0:65, c0:512], v_.t[:, kt, :], p_.t[:, c0:512], start=(kt == 0), stop=last),
                     reads=[v_, p_], writes=[ob_], pe_acc=(kt > 0))
                if last:
                    o_ = oo[Q % 2]
                    P.op("act", lambda e: e.activation(drow.t[64:65, :], ob_.t[64:65, :], AF.Copy), reads=[ob_], writes=[drow])
                    P.op("pe", lambda e: e.matmul(bank[5].t[0:64, :], onesf.t[64:65, 0:64], drow.t[64:65, :], start=True, stop=True),
                         reads=[onesf, drow], writes=[bank[5]])
                    P.op("dve", lambda e: e.reciprocal(rec.t[:, :], bank[5].t[0:64, :]), reads=[bank[5]], writes=[rec])
                    P.op("dve", lambda e: e.tensor_tensor(o_.t[:, :], ob_.t[0:64, :], rec.t[:, :], ALU.mult), reads=[ob_, rec], writes=[o_])
                    P.dma("sp", od[h][:, Q * 512:(Q + 1) * 512], o_.t[:, :], reads=[o_], ow=ob)

            n = len(items)
            emit_S(0, nit)
            for idx in range(n):
                if idx + 1 < n:
                    emit_S(idx + 1, nit + idx + 1)
                emit_PV(idx, nit + idx)
            return nit + n

        nit = 0
        for h in range(4):
            if h + 1 < 4:
                load_head(h + 1)
            nit = do_head(h, qa[h % 2], ka[h % 2], va[h % 2], nit)
        P.finish([ob])
        P.emit()
    return nc


def fox_consts():
    k = np.arange(128)
    U = (k[:, None] <= k[None, :]).astype(np.float32)
    sel = np.zeros((128, 128), np.float32)
    sel[127, :] = 1.0
    mk = np.where(k[None, :] >= k[:, None], 0.0, NEG).astype(np.float32)
    return U, sel, mk


def build_hgrn():
    nc = bass.Bass("TRN2", target_bir_lowering=False)
    P = Prog(nc)
    qd = nc.dram_tensor("q", [2, 128, S], BF16, kind="ExternalInput").ap()
    kd = nc.dram_tensor("k", [2, 128, S], F32, kind="ExternalInput").ap()
    lfd = nc.dram_tensor("lf", [2, 128, S], F32, kind="ExternalInput").ap()
    vd = nc.dram_tensor("v", [2, 128, 64 * 128], BF16, kind="ExternalInput").ap()
    m01d = nc.dram_tensor("m01", [128, 64], F32, kind="ExternalInput").ap()
    rmd = nc.dram_tensor("rm", [128, 2048], F32, kind="ExternalInput").ap()
    idd = nc.dram_tensor("ident", [128, 128], BF16, kind="ExternalInput").ap()
    od = nc.dram_tensor("o", [2, 128, S], F32, kind="ExternalOutput").ap()
    NB = 2048
    with ExitStack() as es:
        bankA = [P.ps(es, "bankA%d" % i, [128, 512], F32) for i in range(2)]
        bankO = [P.ps(es, "bankO%d" % i, [128, 512], F32) for i in range(2)]
        bankU = [P.ps(es, "bankU%d" % i, [128, 512], F32) for i in range(2)]
        bankT = P.ps(es, "bankT", [128, 1024], BF16)
        m01 = P.sb(es, "m01_sb", [128, 64], F32)
        rm = P.sb(es, "rm_sb", [128, NB], F32)
        ident = P.sb(es, "ident_sb", [128, 128], BF16)
        P.dma("sp", m01.t[:, :], m01d, writes=[m01])
        P.dma("sp", rm.t[:, :], rmd, writes=[rm])
        P.dma("sp", ident.t[:, :], idd, writes=[ident])
        ob = Buf()
        hs = []
        for h in range(2):
            o = Ctx()
            o.qb = P.sb(es, "qb%d" % h, [128, NB], BF16)
            o.kb = P.sb(es, "kb%d" % h, [128, NB], F32)
            o.lf = P.sb(es, "lf%d" % h, [128, NB], F32)
            o.G = P.sb(es, "G%d" % h, [128, NB], F32)
            o.tmp = P.sb(es, "tmp%d" % h, [128, NB], F32)
            o.tmp2 = P.sb(es, "tmp2%d" % h, [128, NB], F32)
            o.qd = P.sb(es, "qd%d" % h, [128, NB], BF16)
            o.kdd = P.sb(es, "kdd%d" % h, [128, NB], BF16)
            o.kend = P.sb(es, "kend%d" % h, [128, NB], BF16)
            o.kT = P.sb(es, "kT%d" % h, [128, 16, 128], BF16)
            o.vb = P.sb(es, "vb%d" % h, [128, 16, 128], BF16)
            o.egl = P.sb(es, "egl%d" % h, [128, 32], F32)
            o.S32 = P.sb(es, "S32_%d" % h, [128, 128], F32)
            o.Sbf = [P.sb(es, "Sbf%d_%d" % (h, i), [128, 128], BF16) for i in range(2)]
            o.am = [P.sb(es, "am%d_%d" % (h, i), [128, 64], BF16) for i in range(2)]
            o.osb = [P.sb(es, "osb%d_%d" % (h, i), [128, 512], F32) for i in range(2)]
            P.op("pool", lambda e, o=o: e.memset(o.S32.t[:, :], 0.0), writes=[o.S32])
            P.op("pool", lambda e, o=o: e.memset(o.Sbf[0].t[:, :], 0.0), writes=[o.Sbf[0]])
            o.si = 0
            hs.append(o)

        def prep(h, blk):
            o = hs[h]
            t0 = blk * NB
            P.dma("sp", o.qb.t[:, :], qd[h][:, t0:t0 + NB], writes=[o.qb])
            P.dma("sp", o.kb.t[:, :], kd[h][:, t0:t0 + NB], writes=[o.kb])
            P.dma("sp", o.lf.t[:, :], lfd[h][:, t0:t0 + NB], writes=[o.lf])
            P.dma("pool", o.vb.t[:, :, :], vd[h][:, blk * 2048:(blk + 1) * 2048].rearrange("p (t d) -> p t d", d=128), writes=[o.vb])
            P.op("dve", lambda e: e.tensor_tensor_scan(o.G.t[:, :], rm.t[:, :], o.lf.t[:, :], 0.0, ALU.mult, ALU.add),
                 reads=[rm, o.lf], writes=[o.G])
            P.op("act", lambda e: e.activation(o.tmp.t[:, :], o.G.t[:, :], AF.Exp), reads=[o.G], writes=[o.tmp])
            P.op("dve", lambda e: e.tensor_tensor(o.qd.t[:, :], o.qb.t[:, :], o.tmp.t[:, :], ALU.mult), reads=[o.qb, o.tmp], writes=[o.qd])
            P.op("act", lambda e: e.activation(o.tmp2.t[:, :], o.G.t[:, :], AF.Exp, scale=-1.0), reads=[o.G], writes=[o.tmp2])
            P.op("dve", lambda e: e.tensor_tensor(o.tmp2.t[:, :], o.kb.t[:, :], o.tmp2.t[:, :], ALU.mult), reads=[o.kb, o.tmp2], writes=[o.tmp2])
            P.op("dve", lambda e: e.tensor_copy(o.kdd.t[:, :], o.tmp2.t[:, :]), reads=[o.tmp2], writes=[o.kdd])
            G3 = o.G.t[:, :].rearrange("p (c s) -> p c s", s=64)
            P.op("act", lambda e: e.activation(o.egl.t[:, :], G3[:, :, 63], AF.Exp), reads=[o.G], writes=[o.egl])
            for cc in range(32):
                P.op("dve", lambda e, cc=cc: e.tensor_scalar(o.kend.t[:, cc * 64:(cc + 1) * 64], o.tmp2.t[:, cc * 64:(cc + 1) * 64],
                                                             o.egl.t[:, cc:cc + 1], None, ALU.mult),
                     reads=[o.tmp2, o.egl], writes=[o.kend])
            for grp in range(2):
                for j in range(8):
                    tk = grp * 8 + j
                    P.op("pe", lambda e, tk=tk, j=j: e.transpose(bankT.t[:, j * 128:(j + 1) * 128], o.kend.t[:, tk * 128:(tk + 1) * 128], ident.t[:, :]),
                         reads=[o.kend, ident], writes=[bankT], pe_acc=(j > 0))
                P.op("act", lambda e, grp=grp: e.activation(o.kT.t[:, grp * 8:(grp + 1) * 8, :],
                                                           bankT.t[:, :].rearrange("p (t k) -> p t k", k=128), AF.Copy),
                     reads=[bankT], writes=[o.kT])

        nA = [0]

        def chunk(h, blk, cc):
            o = hs[h]
            tk, half = cc // 2, cc % 2
            pb = 64 * half
            gc = blk * 32 + cc
            cs = slice(cc * 64, (cc + 1) * 64)
            bA = bankA[nA[0] % 2]
            am = o.am[nA[0] % 2]
            nA[0] += 1
            bO = bankO[h]
            bU = bankU[h]
            oc0 = (gc % 8) * 64
            Sb = o.Sbf[o.si % 2]
            Sn = o.Sbf[(o.si + 1) % 2]
            o.si += 1
            P.op("pe", lambda e: e.matmul(bA.t[pb:pb + 64, 0:64], o.kdd.t[:, cs], o.qd.t[:, cs], start=True, stop=True),
                 reads=[o.kdd, o.qd], writes=[bA])
            P.op("dve", lambda e: e.tensor_tensor(am.t[pb:pb + 64, :], bA.t[pb:pb + 64, 0:64], m01.t[pb:pb + 64, :], ALU.mult),
                 reads=[bA, m01], writes=[am])
            P.op("pe", lambda e: e.matmul(bO.t[:, oc0:oc0 + 64], Sb.t[:, :], o.qd.t[:, cs], start=True, stop=False),
                 reads=[Sb, o.qd], writes=[bO], pe_acc=(gc % 8 != 0))
            P.op("pe", lambda e: e.matmul(bO.t[:, oc0:oc0 + 64], o.vb.t[pb:pb + 64, tk, :], am.t[pb:pb + 64, :], start=False, stop=True),
                 reads=[o.vb, am], writes=[bO], pe_acc=True)
            P.op("pe", lambda e: e.matmul(bU.t[:, 0:128], o.kT.t[pb:pb + 64, tk, :], o.vb.t[pb:pb + 64, tk, :], start=True, stop=True),
                 reads=[o.kT, o.vb], writes=[bU])
            P.op("dve", lambda e: e.scalar_tensor_tensor(o.S32.t[:, :], o.S32.t[:, :], o.egl.t[:, cc:cc + 1], bU.t[:, 0:128], ALU.mult, ALU.add),
                 reads=[o.S32, o.egl, bU], writes=[o.S32])
            P.op("act", lambda e: e.activation(Sn.t[:, :], o.S32.t[:, :], AF.Copy), reads=[o.S32], writes=[Sn])
            if gc % 8 == 7:
                os_ = o.osb[(gc // 8) % 2]
                P.op("act", lambda e: e.activation(os_.t[:, :], bO.t[:, :], AF.Copy), reads=[bO], writes=[os_])
                tok0 = (gc - 7) * 64
                P.dma("sp", od[h][:, tok0:tok0 + 512], os_.t[:, :], reads=[os_], ow=ob)

        for blk in range(4):
            for h in range(2):
                prep(h, blk)
            for cc in range(32):
                for h in range(2):
                    chunk(h, blk, cc)
        P.finish([ob])
        P.emit()
    return nc


def hgrn_consts():
    p = np.arange(128)
    t = np.arange(64)
    m01 = ((p[:, None] % 64) <= t[None, :]).astype(np.float32)
    rm = np.ones((128, 2048), np.float32)
    rm[:, ::64] = 0.0
    ident = np.eye(128, dtype=np.float32).astype(NPBF)
    return m01, rm, ident


def build_mega():
    nc = bass.Bass("TRN2", target_bir_lowering=False)
    P = Prog(nc)
    c = Ctx()
    c.P, c.nc = P, nc
    dr = {}

    def din(name, shape, dt=F32):
        dr[name] = nc.dram_tensor(name, list(shape), dt, kind="ExternalInput").ap()
        return dr[name]

    def dout(name, shape, dt=F32):
        dr[name] = nc.dram_tensor(name, list(shape), dt, kind="ExternalOutput").ap()
        return dr[name]

    xT = din("xT", [D, T])
    cTd = din("cT", [128, 8])
    modbd = din("modb", [128, 144])
    modwd = din("modw", [18, 128, 8192])
    gT = din("gT", [128, 48])
    outs = []
    pid = nc.partition_id()
    g4 = pid % 4
    G4 = [[0, 1, 2, 3], [4, 5, 6, 7]]
    idram = lambda name, shape, dt: nc.dram_tensor(name, list(shape), dt)
    with ExitStack() as es:
        X = es.enter_context(nc.sbuf_tensor("X", [128, 8, T], F32))
        c.X = X
        c.Xb = [[Buf(X) for _ in range(4)] for _ in range(8)]
        c.modt = P.sb(es, "modt", [128, 144], F32)
        c.gt = P.sb(es, "gt", [128, 48], F32)
        c.der = P.sb(es, "der", [128, 96], F32)
        c.ones = P.sb(es, "ones", [128, 128], BF16)
        c.bank = [P.ps(es, "bank%d" % i, [128, 512], F32) for i in range(7)]
        bankT = P.ps(es, "bankT", [128, 1024], BF16)
        sqt = es.enter_context(nc.sbuf_tensor("sq", [128, 8, 512], BF16))
        c.sq = [Buf(sqt) for _ in range(8)]
        c.rstd = P.sb(es, "rstd", [128, 512], F32)
        c.tmp = [P.sb(es, "tmp%d" % i, [128, 512], F32) for i in range(2)]

        xv = xT.rearrange("(c p) t -> p c t", p=128)
        for kc in range(8):
            P.dma("sp", X[:, kc, :], xv[:, kc, :], writes=[c.Xb[kc][tt] for tt in range(4)])
        P.dma("sp", c.gt.t[:, :], gT, writes=[c.gt])
        P.op("pool", lambda e: e.memset(c.ones.t[:, :], 1.0), writes=[c.ones])
        with ExitStack() as e0:
            ct = P.sb(e0, "ct", [128, 8], F32)
            ctb = P.sb(e0, "ctb", [128, 8], BF16)
            mb = P.sb(e0, "mb", [128, 144], F32)
            mw = [P.sb(e0, "mw%d" % i, [128, 8, 1024], BF16) for i in range(2)]
            P.dma("sp", ct.t[:, :], cTd, writes=[ct])
            P.dma("sp", mb.t[:, :], modbd, writes=[mb])
            P.op("act", lambda e: e.activation(ctb.t[:, :], ct.t[:, :], AF.Silu), reads=[ct], writes=[ctb])
            bm = c.bank[5]
            for v in range(18):
                w_ = mw[v % 2]
                P.dma("pool", w_.t[:, :, :], modwd[v].rearrange("p (k n) -> p k n", k=8), writes=[w_])
                for ch in range(8):
                    col = v * 8 + ch
                    for kc in range(8):
                        P.op("pe", lambda e, w_=w_, ch=ch, kc=kc, col=col: e.matmul(
                            bm.t[:, col:col + 1], w_.t[:, kc, ch * 128:(ch + 1) * 128], ctb.t[:, kc:kc + 1],
                            start=(kc == 0), stop=(kc == 7)),
                            reads=[w_, ctb], writes=[bm], pe_acc=not (v == 0 and ch == 0 and kc == 0))
            P.op("dve", lambda e: e.tensor_tensor(c.modt.t[:, :], bm.t[:, 0:144], mb.t[:, :], ALU.add),
                 reads=[bm, mb], writes=[c.modt])
        P.barrier()
        for l in range(2):
            for sub in range(3):
                base = ((l * 3 + sub) * 2) * 8
                sc0 = mcol(l, sub * 3 + 1, 0)
                g0 = (l * 3 + sub) * 8
                ga0 = mcol(l, sub * 3 + 2, 0)
                P.op("dve", lambda e, base=base, sc0=sc0, g0=g0: e.scalar_tensor_tensor(
                    c.der.t[:, base:base + 8], c.modt.t[:, sc0:sc0 + 8], 1.0, c.gt.t[:, g0:g0 + 8], ALU.add, ALU.mult),
                    reads=[c.modt, c.gt], writes=[c.der])
                P.op("dve", lambda e, base=base: e.tensor_scalar(
                    c.der.t[:, base:base + 8], c.der.t[:, base:base + 8], SQD, None, ALU.mult),
                    reads=[c.der], writes=[c.der])
                P.op("dve", lambda e, base=base, ga0=ga0, sub=sub: e.tensor_scalar(
                    c.der.t[:, base + 8:base + 16], c.modt.t[:, ga0:ga0 + 8], (1.0 if sub == 1 else 0.5), None, ALU.mult),
                    reads=[c.modt], writes=[c.der])

        def Acol(l, sub, ch):
            j = ((l * 3 + sub) * 2) * 8 + ch
            return c.der.t[:, j:j + 1]

        def Gcol(l, sub, ch):
            j = ((l * 3 + sub) * 2 + 1) * 8 + ch
            return c.der.t[:, j:j + 1]

        def Scol(l, sub, ch):
            j = mcol(l, sub * 3 + 0, ch)
            return c.modt.t[:, j:j + 1]

        def rstd_tile(src_fn, src_bufs, epsk):
            for kc in range(8):
                P.op("act", lambda e, kc=kc: e.activation(sqt[:, kc, :], src_fn(kc), AF.Square),
                     reads=[src_bufs[kc]], writes=[c.sq[kc]])
            for kc in range(8):
                P.op("pe", lambda e, kc=kc: e.matmul(c.bank[6].t[:, :], c.ones.t[:, :], sqt[:, kc, :],
                                                      start=(kc == 0), stop=(kc == 7)),
                     reads=[c.ones, c.sq[kc]], writes=[c.bank[6]], pe_acc=(kc > 0))
            P.op("dve", lambda e: e.tensor_scalar(c.rstd.t[:, :], c.bank[6].t[:, :], epsk, None, ALU.add),
                 reads=[c.bank[6]], writes=[c.rstd])
            P.op("act", lambda e: e.activation(c.rstd.t[:, :], c.rstd.t[:, :], AF.Sqrt), reads=[c.rstd], writes=[c.rstd])
            P.op("dve", lambda e: e.reciprocal(c.rstd.t[:, :], c.rstd.t[:, :]), reads=[c.rstd], writes=[c.rstd])

        def modnorm_tile(l, sub, tt, hdst, hbuf):
            t0 = tt * 512
            rstd_tile(lambda kc: X[:, kc, t0:t0 + 512], [c.Xb[kc][tt] for kc in range(8)], EPS * D)
            for kc in range(8):
                tb = c.tmp[kc % 2]
                P.op("dve", lambda e, kc=kc, tb=tb: e.tensor_tensor(tb.t[:, :], X[:, kc, t0:t0 + 512], c.rstd.t[:, :], ALU.mult),
                     reads=[c.Xb[kc][tt], c.rstd], writes=[tb])
                P.op("act", lambda e, kc=kc, tb=tb: e.activation(hdst(kc), tb.t[:, :], AF.Identity,
                                                               bias=Scol(l, sub, kc), scale=Acol(l, sub, kc)),
                     reads=[tb, c.der, c.modt], writes=[hbuf(kc)])

        def epilogue(kind, l, oG, oGb, sgd, sgb):
            wod = din(kind + "_wo", [128, 8192])
            oS, oSb = dsel(kind + "_oS", [1, D, T], BF16, oG.ap()[bass.ds(g4, 1), :, :], oGb)
            sv = sgd.ap().rearrange("(c p) t -> p c t", p=128)
            with ExitStack() as e2:
                wo = P.sb(e2, kind + "wo_sb", [128, 8, 1024], BF16)
                P.dma("pool", wo.t[:, :, :], wod.rearrange("p (k d) -> p k d", k=8), writes=[wo])
                ot = [P.sb(e2, kind + "ot%d" % i, [128, 8, 512], BF16) for i in range(2)]
                st = [P.sb(e2, kind + "st%d" % i, [128, 8, 512], BF16) for i in range(2)]
                ogt = [e2.enter_context(nc.sbuf_tensor(kind + "og%d" % i, [128, 8, 512], BF16)) for i in range(2)]
                ogb = [[Buf(ogt[i]) for _ in range(8)] for i in range(2)]
                if kind == "hgrn":
                    hgd = din("hgn", [128, 8])
                    hg = P.sb(e2, "hg", [128, 8], F32)
                    P.dma("sp", hg.t[:, :], hgd, writes=[hg])
                    P.op("dve", lambda e: e.tensor_scalar(hg.t[:, :], hg.t[:, :], float(np.sqrt(128.0)), None, ALU.mult),
                         reads=[hg], writes=[hg])
                    sq1 = P.sb(e2, "sq1", [128, 512], BF16)
                    r1 = P.sb(e2, "r1", [128, 512], F32)
                    t1 = P.sb(e2, "t1", [128, 512], F32)
                for tt in range(4):
                    t0 = tt * 512
                    o_, s_, og_ = ot[tt % 2], st[tt % 2], ogt[tt % 2]
                    P.dma("sp", o_.t[:, :, :], oS.ap()[0].rearrange("(c p) s -> p c s", p=128)[:, :, t0:t0 + 512], reads=[oSb], writes=[o_])
                    P.dma("sp", s_.t[:, :, :], sv[:, :, t0:t0 + 512], reads=[sgb], writes=[s_])
                    if kind == "fox":
                        for kc in range(8):
                            P.op("dve", lambda e, kc=kc, o_=o_, s_=s_, og_=og_: e.tensor_tensor(
                                og_[:, kc, :], o_.t[:, kc, :], s_.t[:, kc, :], ALU.mult),
                                reads=[o_, s_], writes=[ogb[tt % 2][kc]])
                    else:
                        for kc in range(8):
                            P.op("act", lambda e, kc=kc, o_=o_: e.activation(sq1.t[:, :], o_.t[:, kc, :], AF.Square),
                                 reads=[o_], writes=[sq1])
                            P.op("pe", lambda e: e.matmul(c.bank[5].t[:, :], c.ones.t[:, :], sq1.t[:, :], start=True, stop=True),
                                 reads=[c.ones, sq1], writes=[c.bank[5]])
                            P.op("dve", lambda e: e.tensor_scalar(r1.t[:, :], c.bank[5].t[:, :], EPS * 128.0, None, ALU.add),
                                 reads=[c.bank[5]], writes=[r1])
                            P.op("act", lambda e: e.activation(r1.t[:, :], r1.t[:, :], AF.Sqrt), reads=[r1], writes=[r1])
                            P.op("dve", lambda e: e.reciprocal(r1.t[:, :], r1.t[:, :]), reads=[r1], writes=[r1])
                            P.op("dve", lambda e, kc=kc, o_=o_: e.tensor_tensor(t1.t[:, :], o_.t[:, kc, :], r1.t[:, :], ALU.mult),
                                 reads=[o_, r1], writes=[t1])
                            P.op("dve", lambda e, kc=kc, s_=s_, og_=og_: e.scalar_tensor_tensor(
                                og_[:, kc, :], t1.t[:, :], hg.t[:, kc:kc + 1], s_.t[:, kc, :], ALU.mult, ALU.mult),
                                reads=[t1, hg, s_], writes=[ogb[tt % 2][kc]])
                    for dc in range(8):
                        bk = c.bank[4 + dc % 2]
                        for kc in range(8):
                            P.op("pe", lambda e, kc=kc, dc=dc, bk=bk, og_=og_: e.matmul(
                                bk.t[:, :], wo.t[:, kc, dc * 128:(dc + 1) * 128], og_[:, kc, :],
                                start=(kc == 0), stop=(kc == 7)),
                                reads=[wo, ogb[tt % 2][kc]], writes=[bk], pe_acc=(kc > 0))
                        P.op("dve", lambda e, dc=dc, bk=bk, t0=t0: e.scalar_tensor_tensor(
                            X[:, dc, t0:t0 + 512], bk.t[:, :], Gcol(l, 1, dc), X[:, dc, t0:t0 + 512], ALU.mult, ALU.add),
                            reads=[bk, c.der, c.Xb[dc][tt]], writes=[c.Xb[dc][tt]])
            P.barrier()

        def ffn(j, l, sub):
            wupd = din("wup%d" % j, [11, 128, 4096])
            wdnd = din("wdn%d" % j, [8, 128, 2816])
            with ExitStack() as e2:
                hbt = e2.enter_context(nc.sbuf_tensor("hb_%d" % j, [128, 8, 1024], BF16))
                hbb = [[Buf(hbt) for _ in range(2)] for _ in range(8)]
                actt = e2.enter_context(nc.sbuf_tensor("actb_%d" % j, [128, NF, 1024], BF16))
                actb = [[Buf(actt) for _ in range(2)] for _ in range(NF)]
                wu = [P.sb(e2, "wu%d_%d" % (j, i), [128, 2, 8, 256], BF16) for i in range(2)]
                wd = [P.sb(e2, "wd%d_%d" % (j, i), [128, NF, 128], BF16) for i in range(2)]
                sa = [P.sb(e2, "sa%d_%d" % (j, i), [128, 512], F32) for i in range(2)]
                for half in range(2):
                    for t2 in range(2):
                        tt = half * 2 + t2
                        modnorm_tile(l, sub, tt, lambda kc, t2=t2: hbt[:, kc, t2 * 512:(t2 + 1) * 512],
                                     lambda kc, t2=t2: hbb[kc][t2])
                    it = 0
                    for g in range(11):
                        w_ = wu[g % 2]
                        P.dma("pool", w_.t[:, :, :, :], wupd[g].rearrange("p (a k f) -> p a k f", a=2, k=8), writes=[w_])
                        for jf in range(2):
                            fc = 2 * g + jf
                            for t2 in range(2):
                                bA, bB = c.bank[it % 2], c.bank[2 + it % 2]
                                s_ = sa[it % 2]
                                it += 1
                                for kc in range(8):
                                    P.op("pe", lambda e, kc=kc, w_=w_, jf=jf, t2=t2, bA=bA: e.matmul(
                                        bA.t[:, :], w_.t[:, 0, kc, jf * 128:(jf + 1) * 128], hbt[:, kc, t2 * 512:(t2 + 1) * 512],
                                        start=(kc == 0), stop=(kc == 7)),
                                        reads=[w_, hbb[kc][t2]], writes=[bA], pe_acc=(kc > 0))
                                for kc in range(8):
                                    P.op("pe", lambda e, kc=kc, w_=w_, jf=jf, t2=t2, bB=bB: e.matmul(
                                        bB.t[:, :], w_.t[:, 1, kc, jf * 128:(jf + 1) * 128], hbt[:, kc, t2 * 512:(t2 + 1) * 512],
                                        start=(kc == 0), stop=(kc == 7)),
                                        reads=[w_, hbb[kc][t2]], writes=[bB], pe_acc=(kc > 0))
                                P.op("act", lambda e, s_=s_, bA=bA: e.activation(s_.t[:, :], bA.t[:, :], AF.Silu),
                                     reads=[bA], writes=[s_])
                                P.op("dve", lambda e, s_=s_, bB=bB, fc=fc, t2=t2: e.tensor_tensor(
                                    actt[:, fc, t2 * 512:(t2 + 1) * 512], bB.t[:, :], s_.t[:, :], ALU.mult),
                                    reads=[bB, s_], writes=[actb[fc][t2]])
                    for dc in range(8):
                        w_ = wd[dc % 2]
                        P.dma("pool", w_.t[:, :, :], wdnd[dc].rearrange("p (f d) -> p f d", f=NF), writes=[w_])
                        for t2 in range(2):
                            tt = half * 2 + t2
                            t0 = tt * 512
                            bk = c.bank[4 + (dc * 2 + t2) % 2]
                            for fc in range(NF):
                                P.op("pe", lambda e, fc=fc, w_=w_, t2=t2, bk=bk: e.matmul(
                                    bk.t[:, :], w_.t[:, fc, :], actt[:, fc, t2 * 512:(t2 + 1) * 512],
                                    start=(fc == 0), stop=(fc == NF - 1)),
                                    reads=[w_, actb[fc][t2]], writes=[bk], pe_acc=(fc > 0))
                            P.op("dve", lambda e, dc=dc, bk=bk, t0=t0: e.scalar_tensor_tensor(
                                X[:, dc, t0:t0 + 512], bk.t[:, :], Gcol(l, sub, dc), X[:, dc, t0:t0 + 512], ALU.mult, ALU.add),
                                reads=[bk, c.der, c.Xb[dc][tt]], writes=[c.Xb[dc][tt]])
            P.barrier()

        def proj_fm(wname, hbt, hbb, evac, n_oc=8):
            wd_ = din(wname, [n_oc, 128, 1024])
            with ExitStack() as e3:
                wp = [P.sb(e3, wname + "_sb%d" % i, [128, 8, 128], BF16) for i in range(2)]
                it = 0
                for oc in range(n_oc):
                    w_ = wp[oc % 2]
                    P.dma("pool", w_.t[:, :, :], wd_[oc].rearrange("p (k f) -> p k f", k=8), writes=[w_])
                    for tt in range(4):
                        bk = c.bank[it % 4]
                        it += 1
                        for kc in range(8):
                            P.op("pe", lambda e, kc=kc, w_=w_, tt=tt, bk=bk: e.matmul(
                                bk.t[:, :], w_.t[:, kc, :], hbt[:, kc, tt * 512:(tt + 1) * 512],
                                start=(kc == 0), stop=(kc == 7)),
                                reads=[w_, hbb[kc][tt]], writes=[bk], pe_acc=(kc > 0))
                        evac(oc, tt, bk)
                P.barrier()

        def proj_tm(wname, hbt, hbb, vout, vob, func):
            wd_ = din(wname, [2, 128, 4096])
            with ExitStack() as e3:
                wv = P.sb(e3, wname + "_sb", [128, 2, 8, 512], BF16)
                for cg in range(2):
                    P.dma("pool", wv.t[:, cg, :, :], wd_[cg].rearrange("p (k f) -> p k f", k=8), writes=[wv])
                vt = [P.sb(e3, wname + "vt%d" % i, [128, 512], BF16) for i in range(2)]
                it = 0
                for tk in range(16):
                    for cg in range(2):
                        bk = c.bank[it % 4]
                        v_ = vt[it % 2]
                        it += 1
                        for kc in range(8):
                            P.op("pe", lambda e, kc=kc, tk=tk, cg=cg, bk=bk: e.matmul(
                                bk.t[:, :], hbt[:, kc, tk * 128:(tk + 1) * 128], wv.t[:, cg, kc, :],
                                start=(kc == 0), stop=(kc == 7)),
                                reads=[wv, hbb[kc][tk // 4]], writes=[bk], pe_acc=(kc > 0))
                        P.op("act", lambda e, bk=bk, v_=v_: e.activation(v_.t[:, :], bk.t[:, :], func),
                             reads=[bk], writes=[v_])
                        P.dma("sp", vout[tk * 128:(tk + 1) * 128, cg * 512:(cg + 1) * 512], v_.t[:, :], reads=[v_], ow=vob)
                P.barrier()

        def stage_out(e3, name, shape, dt):
            return [P.sb(e3, name + "%d" % i, shape, dt) for i in range(2)]

        def projections(kind, l):
            R = Ctx()
            with ExitStack() as e2:
                hbt = e2.enter_context(nc.sbuf_tensor(kind + "hb2", [128, 8, T], BF16))
                hbb = [[Buf(hbt) for _ in range(4)] for _ in range(8)]
                for tt in range(4):
                    modnorm_tile(l, 1, tt, lambda kc, tt=tt: hbt[:, kc, tt * 512:(tt + 1) * 512],
                                 lambda kc, tt=tt: hbb[kc][tt])
                kdt = BF16 if kind == "fox" else F32
                R.q, R.qb = idram(kind + "_q", [D, T], BF16), Buf()
                R.k, R.kb = idram(kind + "_k", [D, T], kdt), Buf()
                R.sg, R.sgb = idram(kind + "_sg", [D, T], BF16), Buf()
                R.v, R.vb = idram(kind + "_v", [T, D], BF16), Buf()
                qo, ko, sgo, vo = R.q.ap(), R.k.ap(), R.sg.ap(), R.v.ap()
                qob, kob, sgob, vob = R.qb, R.kb, R.sgb, R.vb
                cnt = [0]

                def simple_evac(od, ob, func, scale, st):
                    def evac(oc, tt, bk):
                        s_ = st[cnt[0] % 2]
                        cnt[0] += 1
                        P.op("act", lambda e, s_=s_, bk=bk: e.activation(s_.t[:, :], bk.t[:, :], func, scale=scale),
                             reads=[bk], writes=[s_])
                        P.dma("sp", od[oc * 128:(oc + 1) * 128, tt * 512:(tt + 1) * 512], s_.t[:, :], reads=[s_], ow=ob)
                    return evac

                stb = stage_out(e2, kind + "stb", [128, 512], BF16)
                if kind == "fox":
                    R.lf, R.lfb = idram("fox_lf", [128, 256], F32), Buf()
                    proj_fm("fox_wq", hbt, hbb, simple_evac(qo, qob, AF.Copy, float(FD ** -0.5), stb))
                    proj_fm("fox_wk", hbt, hbb, simple_evac(ko, kob, AF.Copy, 1.0, stb))
                    proj_fm("fox_wg", hbt, hbb, simple_evac(sgo, sgob, AF.Sigmoid, 1.0, stb))
                    proj_tm("fox_wv", hbt, hbb, vo, vob, AF.Copy)
                    wfd = din("fox_wf", [128, 128])
                    bfd = din("fox_bfb", [128, 256])
                    wf = P.sb(e2, "wf_sb", [128, 8, 16], BF16)
                    P.dma("pool", wf.t[:, :, :], wfd.rearrange("p (k f) -> p k f", k=8), writes=[wf])
                    bfb = P.sb(e2, "bfb", [128, 256], F32)
                    P.dma("sp", bfb.t[:, :], bfd, writes=[bfb])
                    z1 = P.sb(e2, "z1", [128, 256], F32)
                    bk = c.bank[0]
                    for tk in range(16):
                        for kc in range(8):
                            P.op("pe", lambda e, kc=kc, tk=tk: e.matmul(
                                bk.t[:, tk * 16:(tk + 1) * 16], hbt[:, kc, tk * 128:(tk + 1) * 128], wf.t[:, kc, :],
                                start=(kc == 0), stop=(kc == 7)),
                                reads=[wf, hbb[kc][tk // 4]], writes=[bk], pe_acc=not (tk == 0 and kc == 0))
                    P.op("dve", lambda e: e.tensor_tensor(z1.t[:, :], bk.t[:, 0:256], bfb.t[:, :], ALU.add), reads=[bk, bfb], writes=[z1])
                    P.op("act", lambda e: e.activation(z1.t[:, :], z1.t[:, :], AF.Exp, scale=-1.0), reads=[z1], writes=[z1])
                    P.op("act", lambda e: e.activation(z1.t[:, :], z1.t[:, :], AF.Ln, bias=1.0, scale=1.0), reads=[z1], writes=[z1])
                    P.op("dve", lambda e: e.tensor_scalar(z1.t[:, :], z1.t[:, :], -1.0, None, ALU.mult), reads=[z1], writes=[z1])
                    P.dma("sp", R.lf.ap(), z1.t[:, :], reads=[z1], ow=R.lfb)
                else:
                    R.lf, R.lfb = idram("hgrn_lf", [D, T], F32), Buf()
                    lfo, lfob = R.lf.ap(), R.lfb
                    lbd = din("lbl", [128, 16])
                    lbl = P.sb(e2, "lbl_sb", [128, 16], F32)
                    lb = P.sb(e2, "lb", [128, 8], F32)
                    oml = P.sb(e2, "oml", [128, 8], F32)
                    P.dma("sp", lbl.t[:, :], lbd, writes=[lbl])
                    P.op("dve", lambda e: e.tensor_tensor(lb.t[:, :], lbl.t[:, 8:16], lbl.t[:, 0:8], ALU.subtract), reads=[lbl], writes=[lb])
                    P.op("act", lambda e: e.activation(lb.t[:, :], lb.t[:, :], AF.Sigmoid), reads=[lb], writes=[lb])
                    P.op("dve", lambda e: e.tensor_scalar(oml.t[:, :], lb.t[:, :], -1.0, 1.0, ALU.mult, ALU.add), reads=[lb], writes=[oml])
                    proj_fm("hgrn_wq", hbt, hbb, simple_evac(qo, qob, AF.Copy, 1.0, stb))
                    proj_fm("hgrn_wg", hbt, hbb, simple_evac(sgo, sgob, AF.Silu, 1.0, stb))
                    proj_tm("hgrn_wv", hbt, hbb, vo, vob, AF.Silu)
                    sg1 = P.sb(e2, "sg1", [128, 512], F32)
                    ff = stage_out(e2, "ff", [128, 512], F32)
                    lff = stage_out(e2, "lff", [128, 512], F32)
                    kk = stage_out(e2, "kk", [128, 512], F32)

                    def f_evac(oc, tt, bk):
                        i = cnt[0] % 2
                        cnt[0] += 1
                        f_, l_, k_ = ff[i], lff[i], kk[i]
                        P.op("act", lambda e, bk=bk: e.activation(sg1.t[:, :], bk.t[:, :], AF.Sigmoid), reads=[bk], writes=[sg1])
                        P.op("dve", lambda e, f_=f_, oc=oc: e.tensor_scalar(f_.t[:, :], sg1.t[:, :], oml.t[:, oc:oc + 1], lb.t[:, oc:oc + 1], ALU.mult, ALU.add),
                             reads=[sg1, oml, lb], writes=[f_])
                        P.op("act", lambda e, f_=f_, l_=l_: e.activation(l_.t[:, :], f_.t[:, :], AF.Ln), reads=[f_], writes=[l_])
                        P.dma("sp", lfo[oc * 128:(oc + 1) * 128, tt * 512:(tt + 1) * 512], l_.t[:, :], reads=[l_], ow=lfob)
                    proj_fm("hgrn_wf", hbt, hbb, f_evac)
            P.barrier()
            return R

        def gather(name, src, srcb, nch, rows, cols, dt):
            dst = idram(name, [nch, 4 * rows, cols], dt)
            db = Buf()
            sv = src.ap() if len(src.shape) == 2 else None
            for j in range(nch):
                sa = src.ap()[j * rows:(j + 1) * rows, :] if sv is not None else src.ap()[j]
                P.collective("AllGather", G4, sa.opt(), dst.ap()[j].opt(), [srcb], db)
            return dst, db

        def dsel(name, shape, dt, src_dyn, srcb):
            dst = idram(name, shape, dt)
            db = Buf()
            P.dma("sp", dst.ap(), src_dyn, reads=[srcb], writes=[db])
            return dst, db

        def fox_phase(R):
            qG, qGb = gather("fox_qG", R.q, R.qb, 4, 256, T, BF16)
            kG, kGb = gather("fox_kG", R.k, R.kb, 4, 256, T, BF16)
            vG, vGb = gather("fox_vG", R.v, R.vb, 4, 512, D, BF16)
            lG, lGb = gather("fox_lG", R.lf, R.lfb, 1, 128, 256, F32)
            qS, qSb = dsel("fox_qS", [1, D, T], BF16, qG.ap()[bass.ds(g4, 1), :, :], qGb)
            kS, kSb = dsel("fox_kS", [1, D, T], BF16, kG.ap()[bass.ds(g4, 1), :, :], kGb)
            vS, vSb = idram("fox_vS", [S, 256], BF16), Buf()
            for j in range(4):
                P.dma("sp", vS.ap().rearrange("(r j i) c -> j r i c", r=4, j=4)[j],
                      vG.ap()[j].rearrange("(r i) c -> r i c", r=4)[:, :, bass.ds(g4 * 256, 256)], reads=[vGb], writes=[vSb])
            lS, lSb = dsel("fox_lS", [512, 16, 4], F32, lG.ap()[0].rearrange("r (k h) -> r k h", h=16)[:, :, bass.ds(g4 * 4, 4)], lGb)
            o_loc, olb = idram("fox_o", [4, 256, T], BF16), Buf()
            shi, slo = idram("shi", [4, S], BF16), idram("slo", [4, S], BF16)
            shb, slb = Buf(), Buf()
            Ud, seld, mkd, idfd = din("U", [128, 128]), din("sel", [128, 128]), din("mk", [128, 128]), din("identf", [128, 128])
            bank = c.bank
            with ExitStack() as e2:
                U = P.sb(e2, "U_sb", [128, 128], F32)
                sel = P.sb(e2, "sel_sb", [128, 128], F32)
                mk = P.sb(e2, "mk_sb", [128, 128], F32)
                idf = P.sb(e2, "idf_sb", [128, 128], F32)
                onesf = P.sb(e2, "onesf", [128, 128], F32)
                negB = P.sb(e2, "negB", [128, 4 * 16 * 64], F32)
                for t_, d_ in ((U, Ud), (sel, seld), (mk, mkd), (idf, idfd)):
                    P.dma("sp", t_.t[:, :], d_, writes=[t_])
                P.op("pool", lambda e: e.memset(onesf.t[:, :], 1.0), writes=[onesf])
                with ExitStack() as e3:
                    lsel = P.sb(e3, "lsel", [128, 4, 16, 4], F32)
                    lt = P.sb(e3, "lt_sb", [128, 256], F32)
                    within = P.sb(e3, "within", [128, 256], F32)
                    tot = P.sb(e3, "tot", [128, 256], F32)
                    inc = P.sb(e3, "inc", [128, 256], F32)
                    GT = P.sb(e3, "GT", [128, 256], F32)
                    gend = P.sb(e3, "gend", [128, 256], F32)
                    Aa = P.sb(e3, "Aa", [128, 256], F32)
                    AT = P.sb(e3, "AT", [64, 512], F32)
                    ahi = P.sb(e3, "ahi", [64, 512], BF16)
                    ahf = P.sb(e3, "ahf", [64, 512], F32)
                    alo = P.sb(e3, "alo", [64, 512], BF16)
                    for t in range(4):
                        P.dma("sp", lsel.t[:, t, :, :], lS.ap()[t * 128:(t + 1) * 128, :, :], reads=[lSb], writes=[lsel])
                    for hl in range(4):
                        P.op("dve", lambda e, hl=hl: e.tensor_copy(
                            lt.t[:, hl * 64:(hl + 1) * 64].rearrange("p (t k) -> p t k", t=4), lsel.t[:, :, :, hl]),
                            reads=[lsel], writes=[lt])
                    P.op("pe", lambda e: e.matmul(bank[6].t[:, 0:256], U.t[:, :], lt.t[:, :], start=True, stop=True), reads=[U, lt], writes=[bank[6]])
                    P.op("pe", lambda e: e.matmul(bank[5].t[:, 0:256], onesf.t[:, :], lt.t[:, :], start=True, stop=True), reads=[onesf, lt], writes=[bank[5]])
                    P.op("dve", lambda e: e.tensor_copy(within.t[:, :], bank[6].t[:, 0:256]), reads=[bank[6]], writes=[within])
                    P.op("dve", lambda e: e.tensor_copy(tot.t[:, :], bank[5].t[:, 0:256]), reads=[bank[5]], writes=[tot])
                    for h in range(4):
                        P.op("dve", lambda e, h=h: e.tensor_tensor_scan(inc.t[:, h * 64:(h + 1) * 64], onesf.t[:, 0:64], tot.t[:, h * 64:(h + 1) * 64],
                                                                        0.0, ALU.mult, ALU.add), reads=[onesf, tot], writes=[inc])
                    P.op("dve", lambda e: e.tensor_tensor(GT.t[:, :], within.t[:, :], inc.t[:, :], ALU.add), reads=[within, inc], writes=[GT])
                    P.op("dve", lambda e: e.tensor_tensor(GT.t[:, :], GT.t[:, :], tot.t[:, :], ALU.subtract), reads=[GT, tot], writes=[GT])
                    P.op("pe", lambda e: e.matmul(bank[6].t[:, 0:256], sel.t[:, :], GT.t[:, :], start=True, stop=True), reads=[sel, GT], writes=[bank[6]])
                    P.op("dve", lambda e: e.tensor_copy(gend.t[:, :], bank[6].t[:, 0:256]), reads=[bank[6]], writes=[gend])
                    for h in range(4):
                        for Q in range(16):
                            j0 = (h * 16 + Q) * 64
                            gc = h * 64 + 4 * Q + 3
                            P.op("dve", lambda e, h=h, j0=j0, gc=gc: e.tensor_scalar(
                                negB.t[:, j0:j0 + 64], GT.t[:, h * 64:(h + 1) * 64], -1.0, gend.t[:, gc:gc + 1], ALU.mult, ALU.add),
                                reads=[GT, gend], writes=[negB])
                            a0 = h * 64 + 4 * Q
                            P.op("dve", lambda e, a0=a0, gc=gc: e.tensor_scalar(
                                Aa.t[:, a0:a0 + 4], GT.t[:, a0:a0 + 4], gend.t[:, gc:gc + 1], None, ALU.subtract),
                                reads=[GT, gend], writes=[Aa])
                    for h in range(4):
                        P.op("pe", lambda e, h=h: e.matmul(bank[5].t[0:64, h * 128:(h + 1) * 128], Aa.t[:, h * 64:(h + 1) * 64], idf.t[:, :],
                                                           start=True, stop=True), reads=[Aa, idf], writes=[bank[5]], pe_acc=(h > 0))
                    P.op("dve", lambda e: e.tensor_copy(AT.t[:, :], bank[5].t[0:64, :]), reads=[bank[5]], writes=[AT])
                    P.op("dve", lambda e: e.tensor_copy(ahi.t[:, :], AT.t[:, :]), reads=[AT], writes=[ahi])
                    P.op("dve", lambda e: e.tensor_copy(ahf.t[:, :], ahi.t[:, :]), reads=[ahi], writes=[ahf])
                    P.op("dve", lambda e: e.tensor_tensor(alo.t[:, :], AT.t[:, :], ahf.t[:, :], ALU.subtract), reads=[AT, ahf], writes=[alo])
                    P.dma("sp", shi.ap().rearrange("h (k p) -> k h p", p=128), ahi.t[:, :].rearrange("k (h p) -> k h p", h=4), reads=[ahi], writes=[shb])
                    P.dma("sp", slo.ap().rearrange("h (k p) -> k h p", p=128), alo.t[:, :].rearrange("k (h p) -> k h p", h=4), reads=[alo], writes=[slb])
                P.barrier()
                qa = [P.sb(e2, "qa%d" % i, [128, S], BF16) for i in range(2)]
                ka = [P.sb(e2, "ka%d" % i, [128, S], BF16) for i in range(2)]
                va = [P.sb(e2, "va%d" % i, [128, 64 * 65 + 64], BF16) for i in range(2)]
                pt = [P.sb(e2, "pt%d" % i, [128, 512], BF16) for i in range(4)]
                sbanks = [bank[0], bank[1], bank[2], bank[6]]
                drow = P.sb(e2, "drow", [65, 512], F32)
                rec = P.sb(e2, "rec", [64, 512], F32)
                oo = [P.sb(e2, "oo%d" % i, [64, 512], BF16) for i in range(2)]
                vv = lambda v_: v_.t[:, 0:64 * 65].rearrange("p (t d) -> p t d", d=65)
                for i in range(2):
                    P.op("pool", lambda e, i=i: e.memset(ka[i].t[64:128, :], 0.0), writes=[ka[i]])
                    P.op("pool", lambda e, i=i: e.memset(qa[i].t[64:128, :], 0.0), writes=[qa[i]])
                    P.op("pool", lambda e, i=i: e.memset(ka[i].t[64:66, :], 1.0), writes=[ka[i]])
                    P.op("pool", lambda e, i=i: e.memset(va[i].t[:, :], 0.0), writes=[va[i]])
                    P.op("pool", lambda e, i=i: e.memset(vv(va[i])[:, :, 64:65], 1.0), writes=[va[i]])
                vGv = vS.ap().rearrange("(k p) d -> p k d", p=128)

                def load_head(h):
                    q_, k_, v_ = qa[h % 2], ka[h % 2], va[h % 2]
                    for t in range(4):
                        P.dma("sp", q_.t[0:64, t * T:(t + 1) * T], qS.ap()[0, t * 256 + h * 64:t * 256 + (h + 1) * 64, :], reads=[qSb], writes=[q_])
                        P.dma("pool", k_.t[0:64, t * T:(t + 1) * T], kS.ap()[0, t * 256 + h * 64:t * 256 + (h + 1) * 64, :], reads=[kSb], writes=[k_])
                    P.dma("sp", q_.t[64:65, :], shi.ap()[h:h + 1, :], reads=[shb], writes=[q_])
                    P.dma("sp", q_.t[65:66, :], slo.ap()[h:h + 1, :], reads=[slb], writes=[q_])
                    P.dma("pool", vv(v_)[:, :, 0:64], vGv[:, :, h * 64:(h + 1) * 64], reads=[vSb], writes=[v_])

                load_head(0)

                def do_head(h, q_, k_, v_, nit):
                    items = [(Q, kt) for Q in range(16) for kt in range(4 * Q + 4)]

                    def emit_S(idx, it_no):
                        Q, kt = items[idx]
                        d = kt - 4 * Q
                        c0 = 128 * d if d >= 0 else 0
                        bk = sbanks[it_no % 4]
                        p_ = pt[it_no % 4]
                        P.op("pe", lambda e: e.matmul(bk.t[:, c0:512], k_.t[0:128, kt * 128:(kt + 1) * 128],
                                                      q_.t[0:128, Q * 512 + c0:(Q + 1) * 512], start=True, stop=True),
                             reads=[k_, q_], writes=[bk])
                        if d >= 0:
                            P.op("dve", lambda e: e.tensor_tensor(bk.t[:, c0:c0 + 128], bk.t[:, c0:c0 + 128], mk.t[:, :], ALU.add),
                                 reads=[bk, mk], writes=[bk])
                        jb = (h * 16 + Q) * 64 + kt
                        P.op("act", lambda e: e.activation(p_.t[:, c0:512], bk.t[:, c0:512], AF.Exp, bias=negB.t[:, jb:jb + 1], scale=1.0),
                             reads=[bk, negB], writes=[p_])

                    def emit_PV(idx, it_no):
                        Q, kt = items[idx]
                        d = kt - 4 * Q
                        c0 = 128 * d if d >= 0 else 0
                        p_ = pt[it_no % 4]
                        ob_ = bank[3 + Q % 2]
                        last = (kt == 4 * Q + 3)
                        P.op("pe", lambda e: e.matmul(ob_.t[0:128, c0:512], v_.t[:, kt * 65:kt * 65 + 128], p_.t[:, c0:512], start=(kt == 0), stop=last),
                             reads=[v_, p_], writes=[ob_], pe_acc=(kt > 0))
                        if last:
                            o_ = oo[Q % 2]
                            P.op("act", lambda e: e.activation(drow.t[64:65, :], ob_.t[64:65, :], AF.Copy), reads=[ob_], writes=[drow])
                            P.op("pe", lambda e: e.matmul(bank[5].t[0:64, :], onesf.t[64:65, 0:64], drow.t[64:65, :], start=True, stop=True),
                                 reads=[onesf, drow], writes=[bank[5]])
                            P.op("dve", lambda e: e.reciprocal(rec.t[:, :], bank[5].t[0:64, :]), reads=[bank[5]], writes=[rec])
                            P.op("dve", lambda e: e.tensor_tensor(o_.t[:, :], ob_.t[0:64, :], rec.t[:, :], ALU.mult), reads=[ob_, rec], writes=[o_])
                            P.dma("sp", o_loc.ap()[Q // 4][h * 64:(h + 1) * 64, (Q % 4) * 512:(Q % 4 + 1) * 512], o_.t[:, :], reads=[o_], ow=olb)
                            if h == 3 and Q % 4 == 3:
                                P.collective("AllGather", G4, o_loc.ap()[Q // 4].opt(), oGf.ap()[Q // 4].opt(), [olb], oGfb)

                    n = len(items)
                    emit_S(0, nit)
                    emit_S(1, nit + 1)
                    for idx in range(n):
                        if idx + 2 < n:
                            emit_S(idx + 2, nit + idx + 2)
                        emit_PV(idx, nit + idx)
                    return nit + n

                nit = 0
                oGf = idram("fox_oG", [4, 4 * 256, T], BF16)
                oGfb = Buf()
                for h in range(4):
                    if h + 1 < 4:
                        load_head(h + 1)
                    nit = do_head(h, qa[h % 2], ka[h % 2], va[h % 2], nit)
            P.barrier()
            return oGf, oGfb

        def hgrn_phase(R):
            qG, qGb = gather("hg_qG", R.q, R.qb, 4, 256, T, BF16)
            lG, lGb = gather("hg_lG", R.lf, R.lfb, 8, 128, T, F32)
            vG, vGb = gather("hg_vG", R.v, R.vb, 4, 512, D, BF16)
            qS, qSb = dsel("hg_qS", [1, D, T], BF16, qG.ap()[bass.ds(g4, 1), :, :], qGb)
            lS, lSb = dsel("hg_lS", [2, 512, T], F32, lG.ap()[bass.ds(g4 * 2, 2), :, :], lGb)
            vS, vSb = idram("hg_vS", [S, 256], BF16), Buf()
            for j in range(4):
                P.dma("sp", vS.ap().rearrange("(r j i) c -> j r i c", r=4, j=4)[j],
                      vG.ap()[j].rearrange("(r i) c -> r i c", r=4)[:, :, bass.ds(g4 * 256, 256)], reads=[vGb], writes=[vSb])
            o_loc, olb = idram("hg_o", [4, 256, T], BF16), Buf()
            m01d, rmd, idd = din("m01", [128, 64]), din("rm", [128, 2048]), din("ident", [128, 128], BF16)
            NB = 2048
            bankA, bankO, bankU = c.bank[0:2], c.bank[2:4], c.bank[4:6]
            with ExitStack() as e2:
                m01 = P.sb(e2, "m01_sb", [128, 64], F32)
                rm = P.sb(e2, "rm_sb", [128, NB], F32)
                ident = P.sb(e2, "ident_sb", [128, 128], BF16)
                P.dma("sp", m01.t[:, :], m01d, writes=[m01])
                P.dma("sp", rm.t[:, :], rmd, writes=[rm])
                P.dma("sp", ident.t[:, :], idd, writes=[ident])
                sh = Ctx()
                sh.qb = P.sb(e2, "hqb", [128, NB], BF16)
                sh.kb = P.sb(e2, "hkb", [128, NB], F32)
                sh.lf = P.sb(e2, "hlf", [128, NB], F32)
                sh.G = P.sb(e2, "hG", [128, NB], F32)
                sh.tmp = P.sb(e2, "htmp", [128, NB], F32)
                sh.tmp2 = P.sb(e2, "htmp2", [128, NB], F32)
                sh.kend = P.sb(e2, "hkend", [128, NB], BF16)
                hs = []
                for h in range(2):
                    o = Ctx()
                    o.qd = P.sb(e2, "hqd%d" % h, [128, NB], BF16)
                    o.kdd = P.sb(e2, "hkdd%d" % h, [128, NB], BF16)
                    o.kT = P.sb(e2, "hkT%d" % h, [128, 16, 128], BF16)
                    o.vb = P.sb(e2, "hvb%d" % h, [128, 16, 128], BF16)
                    o.egl = P.sb(e2, "hegl%d" % h, [128, 32], F32)
                    o.S32 = P.sb(e2, "hS32_%d" % h, [128, 128], F32)
                    o.Sbf = [P.sb(e2, "hSbf%d_%d" % (h, i), [128, 128], BF16) for i in range(2)]
                    o.am = [P.sb(e2, "ham%d_%d" % (h, i), [128, 64], BF16) for i in range(2)]
                    o.osb = [P.sb(e2, "hosb%d_%d" % (h, i), [128, 512], BF16) for i in range(2)]
                    P.op("pool", lambda e, o=o: e.memset(o.S32.t[:, :], 0.0), writes=[o.S32])
                    P.op("pool", lambda e, o=o: e.memset(o.Sbf[0].t[:, :], 0.0), writes=[o.Sbf[0]])
                    o.si = 0
                    hs.append(o)
                vGv = vS.ap().rearrange("(t p) d -> p t d", p=128)

                def prep(h, blk):
                    o = hs[h]
                    P.dma("sp", sh.qb.t[:, :], qS.ap()[0, blk * 256 + h * 128:blk * 256 + (h + 1) * 128, :], reads=[qSb], writes=[sh.qb])
                    P.dma("sp", sh.lf.t[:, :], lS.ap()[h, blk * 128:(blk + 1) * 128, :], reads=[lSb], writes=[sh.lf])
                    P.dma("pool", o.vb.t[:, :, :], vGv[:, blk * 16:(blk + 1) * 16, h * 128:(h + 1) * 128], reads=[vSb], writes=[o.vb])
                    P.op("act", lambda e: e.activation(sh.kb.t[:, :], sh.lf.t[:, :], AF.Exp), reads=[sh.lf], writes=[sh.kb])
                    P.op("dve", lambda e: e.tensor_scalar(sh.kb.t[:, :], sh.kb.t[:, :], -1.0, 1.0, ALU.mult, ALU.add), reads=[sh.kb], writes=[sh.kb])
                    P.op("dve", lambda e: e.tensor_tensor_scan(sh.G.t[:, :], rm.t[:, :], sh.lf.t[:, :], 0.0, ALU.mult, ALU.add),
                         reads=[rm, sh.lf], writes=[sh.G])
                    P.op("act", lambda e: e.activation(sh.tmp.t[:, :], sh.G.t[:, :], AF.Exp), reads=[sh.G], writes=[sh.tmp])
                    P.op("dve", lambda e: e.tensor_tensor(o.qd.t[:, :], sh.qb.t[:, :], sh.tmp.t[:, :], ALU.mult), reads=[sh.qb, sh.tmp], writes=[o.qd])
                    P.op("act", lambda e: e.activation(sh.tmp2.t[:, :], sh.G.t[:, :], AF.Exp, scale=-1.0), reads=[sh.G], writes=[sh.tmp2])
                    P.op("dve", lambda e: e.tensor_tensor(sh.tmp2.t[:, :], sh.kb.t[:, :], sh.tmp2.t[:, :], ALU.mult), reads=[sh.kb, sh.tmp2], writes=[sh.tmp2])
                    P.op("dve", lambda e: e.tensor_copy(o.kdd.t[:, :], sh.tmp2.t[:, :]), reads=[sh.tmp2], writes=[o.kdd])
                    G3 = sh.G.t[:, :].rearrange("p (c s) -> p c s", s=64)
                    P.op("act", lambda e: e.activation(o.egl.t[:, :], G3[:, :, 63], AF.Exp), reads=[sh.G], writes=[o.egl])
                    for cc in range(32):
                        P.op("dve", lambda e, cc=cc: e.tensor_scalar(sh.kend.t[:, cc * 64:(cc + 1) * 64], sh.tmp2.t[:, cc * 64:(cc + 1) * 64],
                                                                     o.egl.t[:, cc:cc + 1], None, ALU.mult),
                             reads=[sh.tmp2, o.egl], writes=[sh.kend])
                    for grp in range(2):
                        for j in range(8):
                            tk = grp * 8 + j
                            P.op("pe", lambda e, tk=tk, j=j: e.transpose(bankT.t[:, j * 128:(j + 1) * 128], sh.kend.t[:, tk * 128:(tk + 1) * 128], ident.t[:, :]),
                                 reads=[sh.kend, ident], writes=[bankT], pe_acc=(j > 0))
                        P.op("act", lambda e, grp=grp: e.activation(o.kT.t[:, grp * 8:(grp + 1) * 8, :],
                                                                   bankT.t[:, :].rearrange("p (t k) -> p t k", k=128), AF.Copy),
                             reads=[bankT], writes=[o.kT])

                nA = [0]

                def chunk(h, blk, cc):
                    o = hs[h]
                    tk, half = cc // 2, cc % 2
                    pb = 64 * half
                    gc = blk * 32 + cc
                    cs = slice(cc * 64, (cc + 1) * 64)
                    bA = bankA[nA[0] % 2]
                    am = o.am[nA[0] % 2]
                    nA[0] += 1
                    bO = bankO[h]
                    bU = bankU[h]
                    oc0 = (gc % 8) * 64
                    Sb = o.Sbf[o.si % 2]
                    Sn = o.Sbf[(o.si + 1) % 2]
                    o.si += 1
                    P.op("pe", lambda e: e.matmul(bA.t[pb:pb + 64, 0:64], o.kdd.t[:, cs], o.qd.t[:, cs], start=True, stop=True),
                         reads=[o.kdd, o.qd], writes=[bA])
                    P.op("dve", lambda e: e.tensor_tensor(am.t[pb:pb + 64, :], bA.t[pb:pb + 64, 0:64], m01.t[pb:pb + 64, :], ALU.mult),
                         reads=[bA, m01], writes=[am])
                    P.op("pe", lambda e: e.matmul(bO.t[:, oc0:oc0 + 64], Sb.t[:, :], o.qd.t[:, cs], start=True, stop=False),
                         reads=[Sb, o.qd], writes=[bO], pe_acc=(gc % 8 != 0))
                    P.op("pe", lambda e: e.matmul(bO.t[:, oc0:oc0 + 64], o.vb.t[pb:pb + 64, tk, :], am.t[pb:pb + 64, :], start=False, stop=True),
                         reads=[o.vb, am], writes=[bO], pe_acc=True)
                    P.op("pe", lambda e: e.matmul(bU.t[:, 0:128], o.kT.t[pb:pb + 64, tk, :], o.vb.t[pb:pb + 64, tk, :], start=True, stop=True),
                         reads=[o.kT, o.vb], writes=[bU])
                    P.op("dve", lambda e: e.scalar_tensor_tensor(o.S32.t[:, :], o.S32.t[:, :], o.egl.t[:, cc:cc + 1], bU.t[:, 0:128], ALU.mult, ALU.add),
                         reads=[o.S32, o.egl, bU], writes=[o.S32])
                    P.op("act", lambda e: e.activation(Sn.t[:, :], o.S32.t[:, :], AF.Copy), reads=[o.S32], writes=[Sn])
                    if gc % 8 == 7:
                        os_ = o.osb[(gc // 8) % 2]
                        P.op("act", lambda e: e.activation(os_.t[:, :], bO.t[:, :], AF.Copy), reads=[bO], writes=[os_])
                        tok0 = (gc - 7) * 64
                        P.dma("sp", o_loc.ap()[tok0 // T][h * 128:(h + 1) * 128, tok0 % T:tok0 % T + 512], os_.t[:, :], reads=[os_], ow=olb)

                oG = idram("hg_oG", [4, 4 * 256, T], BF16)
                oGb = Buf()
                for blk in range(4):
                    for h in range(2):
                        prep(h, blk)
                    for cc in range(32):
                        for h in range(2):
                            chunk(h, blk, cc)
                    P.collective("AllGather", G4, o_loc.ap()[blk].opt(), oG.ap()[blk].opt(), [olb], oGb)
            P.barrier()
            return oG, oGb

        ffn(0, 0, 0)
        R1 = projections("fox", 0)
        oG1, oG1b = fox_phase(R1)
        epilogue("fox", 0, oG1, oG1b, R1.sg, R1.sgb)
        ffn(1, 0, 2)
        ffn(2, 1, 0)
        R2 = projections("hgrn", 1)
        oG2, oG2b = hgrn_phase(R2)
        epilogue("hgrn", 1, oG2, oG2b, R2.sg, R2.sgb)
        ffn(3, 1, 2)
        xo = dout("xo", [D, T])
        xob = Buf()
        outs.append(xob)
        xov = xo.rearrange("(c p) t -> p c t", p=128)
        fgd = din("fg", [128, 8])
        fg = P.sb(es, "fg_sb", [128, 8], F32)
        P.dma("sp", fg.t[:, :], fgd, writes=[fg])
        P.op("dve", lambda e: e.tensor_scalar(fg.t[:, :], fg.t[:, :], SQD, None, ALU.mult), reads=[fg], writes=[fg])
        yo = [P.sb(es, "yo%d" % i, [128, 512], F32) for i in range(2)]
        it = 0
        for tt in range(4):
            t0 = tt * 512
            rstd_tile(lambda kc, t0=t0: X[:, kc, t0:t0 + 512], [c.Xb[kc][tt] for kc in range(8)], EPS * D)
            for kc in range(8):
                y_ = yo[it % 2]
                it += 1
                P.op("dve", lambda e, kc=kc, y_=y_, t0=t0: e.scalar_tensor_tensor(
                    y_.t[:, :], X[:, kc, t0:t0 + 512], fg.t[:, kc:kc + 1], c.rstd.t[:, :], ALU.mult, ALU.mult),
                    reads=[c.Xb[kc][tt], fg, c.rstd], writes=[y_])
                P.dma("sp", xov[:, kc, t0:t0 + 512], y_.t[:, :], reads=[y_], ow=xob)
        P.finish(outs)
        P.emit()
    return nc


_DBG = {}


def _run(nc, maps):
    return run_bass_kernel_spmd(nc, maps, core_ids=list(range(NCORES))).results


def kernel_unfused(x, c, ada_w, ada_b, norm_g, ffn_w_up, ffn_w_down, fox_w_in, fox_b_f, fox_w_out,
           hgrn_w_in, hgrn_norm_g, hgrn_w_out, hgrn_lb_logits, final_norm_g):
    f32 = lambda a: np.ascontiguousarray(np.asarray(a, dtype=np.float32))
    x, c, ada_w, ada_b, norm_g = f32(x), f32(c), f32(ada_w), f32(ada_b), f32(norm_g)
    ffn_w_up, ffn_w_down, fox_w_in, fox_b_f, fox_w_out = f32(ffn_w_up), f32(ffn_w_down), f32(fox_w_in), f32(fox_b_f), f32(fox_w_out)
    hgrn_w_in, hgrn_norm_g, hgrn_w_out = f32(hgrn_w_in), f32(hgrn_norm_g), f32(hgrn_w_out)
    hgrn_lb_logits, final_norm_g = f32(hgrn_lb_logits), f32(final_norm_g)

    mod = run_mod(c, ada_w, ada_b)
    _DBG["mod"] = mod
    modT = [fm_cols(mod[b].reshape(18, D)) for b in range(B)]
    gT = fm_cols(norm_g.reshape(6, D))
    cores = [(b, t) for b in range(B) for t in range(4)]

    nc1 = build_F({"ffns": [(0, 0)], "proj": ("fox", 0)})
    wi = fox_w_in[0]
    shared = {
        "gT": gT, "wup0": tile_wup(ffn_w_up[0, 0]), "wdn0": tile_wdn(ffn_w_down[0, 0]),
        "wq": tile_w_fm(wi[:, 0:D]), "wk": tile_w_fm(wi[:, D:2 * D]), "wg": tile_w_fm(wi[:, 3 * D:4 * D]),
        "wv": tile_w_tm(wi[:, 2 * D:3 * D]),
        "wf": np.ascontiguousarray(wi[:, 4 * D:4 * D + 16].reshape(8, 128, 16).transpose(1, 0, 2)).reshape(128, 128),
        "bf": np.ascontiguousarray(fox_b_f[0].reshape(16, 1)),
    }
    maps = []
    for (b, t) in cores:
        m = dict(shared)
        m["xT"] = np.ascontiguousarray(x[b, t * T:(t + 1) * T, :].T)
        m["modT"] = modT[b]
        maps.append(m)
    r1 = _run(nc1, maps)
    _DBG["r1"] = r1

    def cat_fm(res, name, b):
        return np.concatenate([res[b * 4 + t][name] for t in range(4)], axis=1)

    def cat_tm(res, name, b):
        return np.concatenate([res[b * 4 + t][name] for t in range(4)], axis=0)

    nc2 = build_fox()
    U, sel, mk = fox_consts()
    maps = []
    for b in range(B):
        qf = cat_fm(r1, "qT", b).reshape(FH, FD, S)
        kf = cat_fm(r1, "kT", b).reshape(FH, FD, S)
        vf = cat_tm(r1, "v", b)
        lf = cat_fm(r1, "lf", b)
        for g in range(4):
            l4 = lf[4 * g:4 * g + 4]
            v4 = np.stack([np.ascontiguousarray(vf[:, hd * FD:(hd + 1) * FD].reshape(64, 128, FD).transpose(1, 0, 2)).reshape(128, 64 * FD)
                           for hd in range(4 * g, 4 * g + 4)])
            maps.append({
                "q": np.ascontiguousarray(qf[4 * g:4 * g + 4]), "k": np.ascontiguousarray(kf[4 * g:4 * g + 4]), "v": v4,
                "lt": np.ascontiguousarray(l4.reshape(4, 64, 128).transpose(2, 0, 1)).reshape(128, 256),
                "lq": np.ascontiguousarray(l4.reshape(4, 16, 512).transpose(1, 0, 2)).reshape(16, 2048),
                "U": U, "sel": sel, "mk": mk,
            })
    r2 = _run(nc2, maps)
    _DBG["r2"] = r2
    ofull = [np.concatenate([r2[b * 4 + g]["o"].reshape(4 * FD, S) for g in range(4)], axis=0) for b in range(B)]

    nc3 = build_F({"epi": ("fox", 0), "ffns": [(0, 2), (1, 0)], "proj": ("hgrn", 1)})
    hi = hgrn_w_in[0]
    shared = {
        "gT": gT, "wo": tile_wo(fox_w_out[0]),
        "wup0": tile_wup(ffn_w_up[0, 1]), "wdn0": tile_wdn(ffn_w_down[0, 1]),
        "wup1": tile_wup(ffn_w_up[1, 0]), "wdn1": tile_wdn(ffn_w_down[1, 0]),
        "wq": tile_w_fm(hi[:, 0:D]), "wf": tile_w_fm(hi[:, D:2 * D]), "wg": tile_w_fm(hi[:, 3 * D:4 * D]),
        "wv": tile_w_tm(hi[:, 2 * D:3 * D]), "lbl": fm_cols(hgrn_lb_logits),
    }
    maps = []
    for i, (b, t) in enumerate(cores):
        m = dict(shared)
        m["xT"] = r1[i]["xo"]
        m["modT"] = modT[b]
        m["oT"] = np.ascontiguousarray(ofull[b][:, t * T:(t + 1) * T])
        m["sg"] = r1[i]["sgo"]
        maps.append(m)
    r3 = _run(nc3, maps)
    _DBG["r3"] = r3

    nc4 = build_hgrn()
    m01, rm, ident = hgrn_consts()
    maps = []
    for b in range(B):
        qf = cat_fm(r3, "qT", b).reshape(HH, 128, S)
        kf = cat_fm(r3, "kT", b).reshape(HH, 128, S)
        lf = cat_fm(r3, "lfT", b).reshape(HH, 128, S)
        vf = cat_tm(r3, "v", b)
        for g in range(4):
            v2 = np.stack([np.ascontiguousarray(vf[:, hd * 128:(hd + 1) * 128].reshape(64, 128, 128).transpose(1, 0, 2)).reshape(128, 64 * 128)
                           for hd in range(2 * g, 2 * g + 2)])
            maps.append({
                "q": np.ascontiguousarray(qf[2 * g:2 * g + 2]), "k": np.ascontiguousarray(kf[2 * g:2 * g + 2]),
                "lf": np.ascontiguousarray(lf[2 * g:2 * g + 2]), "v": v2, "m01": m01, "rm": rm, "ident": ident,
            })
    r4 = _run(nc4, maps)
    _DBG["r4"] = r4
    ofull = [np.concatenate([r4[b * 4 + g]["o"].reshape(256, S) for g in range(4)], axis=0) for b in range(B)]

    nc5 = build_F({"epi": ("hgrn", 1), "ffns": [(1, 2)], "final": True})
    shared = {
        "gT": gT, "wo": tile_wo(hgrn_w_out[0]), "hgn": fm_cols(hgrn_norm_g[0]),
        "wup0": tile_wup(ffn_w_up[1, 1]), "wdn0": tile_wdn(ffn_w_down[1, 1]),
        "fg": fm_cols(final_norm_g),
    }
    maps = []
    for i, (b, t) in enumerate(cores):
        m = dict(shared)
        m["xT"] = r3[i]["xo"]
        m["modT"] = modT[b]
        m["oT"] = np.ascontiguousarray(ofull[b][:, t * T:(t + 1) * T])
        m["sg"] = r3[i]["sgo"]
        maps.append(m)
    r5 = _run(nc5, maps)
    out = np.empty((B, S, D), np.float32)
    for i, (b, t) in enumerate(cores):
        out[b, t * T:(t + 1) * T, :] = r5[i]["xo"].T
    return out


def kernel(x, c, ada_w, ada_b, norm_g, ffn_w_up, ffn_w_down, fox_w_in, fox_b_f, fox_w_out,
           hgrn_w_in, hgrn_norm_g, hgrn_w_out, hgrn_lb_logits, final_norm_g):
    f32 = lambda a: np.ascontiguousarray(np.asarray(a, dtype=np.float32))
    x, c, ada_w, ada_b, norm_g = f32(x), f32(c), f32(ada_w), f32(ada_b), f32(norm_g)
    ffn_w_up, ffn_w_down, fox_w_in, fox_b_f, fox_w_out = f32(ffn_w_up), f32(ffn_w_down), f32(fox_w_in), f32(fox_b_f), f32(fox_w_out)
    hgrn_w_in, hgrn_norm_g, hgrn_w_out = f32(hgrn_w_in), f32(hgrn_norm_g), f32(hgrn_w_out)
    hgrn_lb_logits, final_norm_g = f32(hgrn_lb_logits), f32(final_norm_g)
    nc = build_mega()
    wi, hi = fox_w_in[0], hgrn_w_in[0]
    U, sel, mk = fox_consts()
    m01, rm, ident = hgrn_consts()
    shared = {
        "modb": fm_cols(ada_b.reshape(18, D)),
        "modw": np.ascontiguousarray(ada_w.reshape(2, 8, 128, 9, D).transpose(0, 3, 2, 1, 4)).reshape(18, 128, 8 * D),
        "gT": fm_cols(norm_g.reshape(6, D)),
        "wup0": tile_wup(ffn_w_up[0, 0]), "wdn0": tile_wdn(ffn_w_down[0, 0]),
        "wup1": tile_wup(ffn_w_up[0, 1]), "wdn1": tile_wdn(ffn_w_down[0, 1]),
        "wup2": tile_wup(ffn_w_up[1, 0]), "wdn2": tile_wdn(ffn_w_down[1, 0]),
        "wup3": tile_wup(ffn_w_up[1, 1]), "wdn3": tile_wdn(ffn_w_down[1, 1]),
        "fox_wq": tile_w_fm(wi[:, 0:D]), "fox_wk": tile_w_fm(wi[:, D:2 * D]), "fox_wg": tile_w_fm(wi[:, 3 * D:4 * D]),
        "fox_wv": tile_w_tm(wi[:, 2 * D:3 * D]),
        "fox_wf": np.ascontiguousarray(wi[:, 4 * D:4 * D + 16].reshape(8, 128, 16).transpose(1, 0, 2)).reshape(128, 128),
        "fox_bfb": np.ascontiguousarray(np.broadcast_to(np.tile(fox_b_f[0], 16), (128, 256))),
        "U": U, "sel": sel, "mk": mk, "identf": np.eye(128, dtype=np.float32),
        "fox_wo": tile_wo(fox_w_out[0]),
        "hgrn_wq": tile_w_fm(hi[:, 0:D]), "hgrn_wf": tile_w_fm(hi[:, D:2 * D]), "hgrn_wg": tile_w_fm(hi[:, 3 * D:4 * D]),
        "hgrn_wv": tile_w_tm(hi[:, 2 * D:3 * D]), "lbl": fm_cols(hgrn_lb_logits),
        "m01": m01, "rm": rm, "ident": ident,
        "hgrn_wo": tile_wo(hgrn_w_out[0]), "hgn": fm_cols(hgrn_norm_g[0]),
        "fg": fm_cols(final_norm_g),
    }
    maps = []
    cores = [(b, t) for b in range(B) for t in range(4)]
    for (b, t) in cores:
        m = dict(shared)
        m["xT"] = np.ascontiguousarray(x[b, t * T:(t + 1) * T, :].T)
        m["cT"] = fm_cols(c[b])
        maps.append(m)
    res = _run(nc, maps)
    out = np.empty((B, S, D), np.float32)
    for i, (b, t) in enumerate(cores):
        out[b, t * T:(t + 1) * T, :] = res[i]["xo"].T
    return out
```

```python
from contextlib import ExitStack
import numpy as np
import ml_dtypes
import concourse.bass as bass
import concourse.mybir as mybir
from concourse.bass_utils import run_bass_kernel_spmd

F32 = mybir.dt.float32
BF16 = mybir.dt.bfloat16
AF = mybir.ActivationFunctionType
ALU = mybir.AluOpType
NPBF = ml_dtypes.bfloat16

D = 1024
B = 2
S = 8192
DFF = 2816
NF = 22
EPS = 1e-6
NCORES = 8
T = 2048
FH = 16
FD = 64
HH = 8
CH = 64


class Buf:
    __slots__ = ("w", "r", "t", "key")

    def __init__(self, t=None):
        self.w = None
        self.r = []
        self.t = t
        self.key = None


class Prog:
    ENG = ["pe", "act", "dve", "pool", "sp"]

    def __init__(self, nc):
        self.nc = nc
        self.ops = {e: [] for e in self.ENG}
        self.clock = {e: {} for e in self.ENG}
        self.snaps = {}
        self.count = {}
        self.needed = set()
        self.nkey = 0
        self.final = None
        self.unit_keys = set()

    def sb(self, es, name, shape, dtype):
        self.nname = getattr(self, "nname", 0) + 1
        t = es.enter_context(self.nc.sbuf_tensor("%s_u%d" % (name, self.nname), list(shape), dtype))
        return Buf(t)

    def ps(self, es, name, shape, dtype):
        self.nname = getattr(self, "nname", 0) + 1
        t = es.enter_context(self.nc.psum_tensor("%s_u%d" % (name, self.nname), list(shape), dtype))
        return Buf(t)

    def newkey(self, buf):
        fk = getattr(self, "free_keys", None)
        if fk is None:
            self.free_keys, self.key_owner = [], {}
            fk = self.free_keys
        if fk:
            k = fk.pop()
        else:
            self.nkey += 1
            k = "d%d" % self.nkey
        buf.key = k
        self.key_owner[k] = buf
        return k

    def op(self, eng, fn, reads=(), writes=(), dma=None, pe_acc=False):
        need = {}

        def req(ev):
            if ev is None:
                return
            k, s = ev
            if s > need.get(k, 0):
                need[k] = s

        for b in reads:
            req(b.w)
        for b in writes:
            if not (pe_acc and b.w is not None and b.w[0] == "pe"):
                req(b.w)
            for r in b.r:
                req(r)
        key = dma or eng
        if fn is None:
            idx = 0
        else:
            idx = self.count.get(key, 0) + 1
            self.count[key] = idx
        ck = self.clock[eng]
        waits = []
        for k, s in need.items():
            if ck.get(k, 0) < s:
                waits.append((k, s))
        for k, s in waits:
            sn = self.snaps[(k, s)]
            for kk, ss in sn.items():
                if ck.get(kk, 0) < ss:
                    ck[kk] = ss
            if ck.get(k, 0) < s:
                ck[k] = s
            self.needed.add((k, s))
        self.ops[eng].append((fn, waits, key, idx))
        if fn is None:
            return None
        self.snaps[(key, idx)] = dict(ck)
        ev = (key, idx)
        for b in reads:
            b.r.append(ev)
        for b in writes:
            b.w = ev
            b.r = []
        return ev

    def dma(self, eng, out, in_, reads=(), writes=(), ow=None):
        wb = ow if ow is not None else writes[0]
        if wb.key is None:
            self.newkey(wb)
        def _fn(e, out=out, in_=in_):
            try:
                return e.dma_start(out=out, in_=in_)
            except Exception:
                print("DMA FAIL", out, in_)
                raise
        ev = self.op(eng, _fn, reads=reads, writes=writes, dma=wb.key)
        if ow is not None:
            ow.w = ev
        return ev

    def collective(self, kind, groups, src_ap, dst_ap, reads, wbuf):
        wbuf.key = "cc"
        self.unit_keys.add(wbuf.key)
        return self.op("pool", lambda e: e.collective_compute(kind, ALU.bypass, replica_groups=groups,
                                                              ins=[src_ap], outs=[dst_ap]),
                       reads=reads, writes=[wbuf], dma=wbuf.key)

    def finish(self, outs, eng="sp"):
        self.op(eng, None, reads=list(outs))

    def barrier(self):
        need = dict(self.count)
        for e in self.ENG:
            self.op(e, None, extra=need)
        for k, b in list(getattr(self, "key_owner", {}).items()):
            b.key = None
            self.free_keys.append(k)
        if hasattr(self, "key_owner"):
            self.key_owner.clear()

    def emit(self):
        nc = self.nc
        keys = list(self.count.keys())
        for e in self.ENG:
            if e not in keys:
                keys.append(e)
        rank = {}
        for k in keys:
            if k in self.ENG:
                idxs = sorted(s for (kk, s) in self.needed if kk == k)
                rank[k] = {s: i + 1 for i, s in enumerate(idxs)}
        with ExitStack() as es:
            sems = {k: es.enter_context(nc.semaphore("s_" + k)) for k in keys}
            block = es.enter_context(nc.Block())

            def run(eng_name):
                def body(e):
                    for fn, waits, key, idx in self.ops[eng_name]:
                        for k, s in waits:
                            v = rank[k][s] if k in rank else (s if k in self.unit_keys else 16 * s)
                            e.wait_ge(sems[k], v)
                        if fn is None:
                            continue
                        ins = fn(e)
                        if key in rank:
                            if (key, idx) in self.needed:
                                ins.then_inc(sems[key], 1)
                        elif key in self.unit_keys:
                            ins.then_inc(sems[key], 1)
                        else:
                            ins.then_inc(sems[key], 16)
                return body

            block.tensor(run("pe"))
            block.scalar(run("act"))
            block.vector(run("dve"))
            block.gpsimd(run("pool"))
            block.sync(run("sp"))


def _patch_op():
    base = Prog.op

    def op(self, eng, fn, reads=(), writes=(), dma=None, pe_acc=False, extra=None):
        if extra:
            dummy = []
            for k, s in extra.items():
                if s > 0:
                    b = Buf()
                    b.w = (k, s)
                    dummy.append(b)
            reads = list(reads) + dummy
            ev = base(self, eng, fn, reads=reads, writes=writes, dma=dma, pe_acc=pe_acc)
            return ev
        return base(self, eng, fn, reads=reads, writes=writes, dma=dma, pe_acc=pe_acc)

    Prog.op = op


_patch_op()


SQD = float(np.sqrt(D))


class Ctx:
    pass


def mcol(l, v, ch):
    return (l * 9 + v) * 8 + ch


def build_F(cfg):
    nc = bass.Bass("TRN2", target_bir_lowering=False)
    P = Prog(nc)
    c = Ctx()
    c.P, c.nc = P, nc
    dr = {}

    def din(name, shape, dt=F32):
        dr[name] = nc.dram_tensor(name, list(shape), dt, kind="ExternalInput").ap()
        return dr[name]

    def dout(name, shape, dt=F32):
        dr[name] = nc.dram_tensor(name, list(shape), dt, kind="ExternalOutput").ap()
        return dr[name]

    xT = din("xT", [D, T])
    modT = din("modT", [128, 144])
    gT = din("gT", [128, 48])
    outs = []
    with ExitStack() as es:
        X = es.enter_context(nc.sbuf_tensor("X", [128, 8, T], F32))
        c.X = X
        c.Xb = [[Buf(X) for _ in range(4)] for _ in range(8)]
        c.modt = P.sb(es, "modt", [128, 144], F32)
        c.gt = P.sb(es, "gt", [128, 48], F32)
        c.der = P.sb(es, "der", [128, 96], F32)
        c.ones = P.sb(es, "ones", [128, 128], BF16)
        c.bank = [P.ps(es, "bank%d" % i, [128, 512], F32) for i in range(8)]
        sqt = es.enter_context(nc.sbuf_tensor("sq", [128, 8, 512], BF16))
        c.sq = [Buf(sqt) for _ in range(8)]
        c.rstd = P.sb(es, "rstd", [128, 512], F32)
        c.tmp = [P.sb(es, "tmp%d" % i, [128, 512], F32) for i in range(2)]

        xv = xT.rearrange("(c p) t -> p c t", p=128)
        for kc in range(8):
            P.dma("sp", X[:, kc, :], xv[:, kc, :], writes=[c.Xb[kc][tt] for tt in range(4)])
        P.dma("sp", c.modt.t[:, :], modT, writes=[c.modt])
        P.dma("sp", c.gt.t[:, :], gT, writes=[c.gt])
        P.op("pool", lambda e: e.memset(c.ones.t[:, :], 1.0), writes=[c.ones])
        for l in range(2):
            for sub in range(3):
                base = ((l * 3 + sub) * 2) * 8
                sc0 = mcol(l, sub * 3 + 1, 0)
                g0 = (l * 3 + sub) * 8
                ga0 = mcol(l, sub * 3 + 2, 0)
                P.op("dve", lambda e, base=base, sc0=sc0, g0=g0: e.scalar_tensor_tensor(
                    c.der.t[:, base:base + 8], c.modt.t[:, sc0:sc0 + 8], 1.0, c.gt.t[:, g0:g0 + 8], ALU.add, ALU.mult),
                    reads=[c.modt, c.gt], writes=[c.der])
                P.op("dve", lambda e, base=base: e.tensor_scalar(
                    c.der.t[:, base:base + 8], c.der.t[:, base:base + 8], SQD, None, ALU.mult),
                    reads=[c.der], writes=[c.der])
                P.op("dve", lambda e, base=base, ga0=ga0, sub=sub: e.tensor_scalar(
                    c.der.t[:, base + 8:base + 16], c.modt.t[:, ga0:ga0 + 8], (1.0 if sub == 1 else 0.5), None, ALU.mult),
                    reads=[c.modt], writes=[c.der])

        def Acol(l, sub, ch):
            j = ((l * 3 + sub) * 2) * 8 + ch
            return c.der.t[:, j:j + 1]

        def Gcol(l, sub, ch):
            j = ((l * 3 + sub) * 2 + 1) * 8 + ch
            return c.der.t[:, j:j + 1]

        def Scol(l, sub, ch):
            j = mcol(l, sub * 3 + 0, ch)
            return c.modt.t[:, j:j + 1]

        def rstd_tile(src_fn, src_bufs, epsk):
            for kc in range(8):
                P.op("act", lambda e, kc=kc: e.activation(sqt[:, kc, :], src_fn(kc), AF.Square),
                     reads=[src_bufs[kc]], writes=[c.sq[kc]])
            for kc in range(8):
                P.op("pe", lambda e, kc=kc: e.matmul(c.bank[6].t[:, :], c.ones.t[:, :], sqt[:, kc, :],
                                                      start=(kc == 0), stop=(kc == 7)),
                     reads=[c.ones, c.sq[kc]], writes=[c.bank[6]], pe_acc=(kc > 0))
            P.op("dve", lambda e: e.tensor_scalar(c.rstd.t[:, :], c.bank[6].t[:, :], epsk, None, ALU.add),
                 reads=[c.bank[6]], writes=[c.rstd])
            P.op("act", lambda e: e.activation(c.rstd.t[:, :], c.rstd.t[:, :], AF.Sqrt), reads=[c.rstd], writes=[c.rstd])
            P.op("dve", lambda e: e.reciprocal(c.rstd.t[:, :], c.rstd.t[:, :]), reads=[c.rstd], writes=[c.rstd])

        def modnorm_tile(l, sub, tt, hdst, hbuf):
            t0 = tt * 512
            rstd_tile(lambda kc: X[:, kc, t0:t0 + 512], [c.Xb[kc][tt] for kc in range(8)], EPS * D)
            for kc in range(8):
                tb = c.tmp[kc % 2]
                P.op("dve", lambda e, kc=kc, tb=tb: e.tensor_tensor(tb.t[:, :], X[:, kc, t0:t0 + 512], c.rstd.t[:, :], ALU.mult),
                     reads=[c.Xb[kc][tt], c.rstd], writes=[tb])
                P.op("act", lambda e, kc=kc, tb=tb: e.activation(hdst(kc), tb.t[:, :], AF.Identity,
                                                               bias=Scol(l, sub, kc), scale=Acol(l, sub, kc)),
                     reads=[tb, c.der, c.modt], writes=[hbuf(kc)])

        def epilogue(kind, l):
            oT = din("oT", [D, T])
            sgd = din("sg", [D, T], BF16)
            wod = din("wo", [128, 8192])
            ov = oT.rearrange("(c p) t -> p c t", p=128)
            sv = sgd.rearrange("(c p) t -> p c t", p=128)
            with ExitStack() as e2:
                wo = P.sb(e2, "wo_sb", [128, 8, 1024], BF16)
                P.dma("pool", wo.t[:, :, :], wod.rearrange("p (k d) -> p k d", k=8), writes=[wo])
                ot = [P.sb(e2, "ot%d" % i, [128, 8, 512], F32) for i in range(2)]
                st = [P.sb(e2, "st%d" % i, [128, 8, 512], BF16) for i in range(2)]
                ogt = [e2.enter_context(nc.sbuf_tensor("og%d" % i, [128, 8, 512], BF16)) for i in range(2)]
                ogb = [[Buf(ogt[i]) for _ in range(8)] for i in range(2)]
                if kind == "hgrn":
                    hgd = din("hgn", [128, 8])
                    hg = P.sb(e2, "hg", [128, 8], F32)
                    P.dma("sp", hg.t[:, :], hgd, writes=[hg])
                    P.op("dve", lambda e: e.tensor_scalar(hg.t[:, :], hg.t[:, :], float(np.sqrt(128.0)), None, ALU.mult),
                         reads=[hg], writes=[hg])
                    sq1 = P.sb(e2, "sq1", [128, 512], BF16)
                    r1 = P.sb(e2, "r1", [128, 512], F32)
                    t1 = P.sb(e2, "t1", [128, 512], F32)
                for tt in range(4):
                    t0 = tt * 512
                    o_, s_, og_ = ot[tt % 2], st[tt % 2], ogt[tt % 2]
                    P.dma("sp", o_.t[:, :, :], ov[:, :, t0:t0 + 512], writes=[o_])
                    P.dma("sp", s_.t[:, :, :], sv[:, :, t0:t0 + 512], writes=[s_])
                    if kind == "fox":
                        for kc in range(8):
                            P.op("dve", lambda e, kc=kc, o_=o_, s_=s_, og_=og_: e.tensor_tensor(
                                og_[:, kc, :], o_.t[:, kc, :], s_.t[:, kc, :], ALU.mult),
                                reads=[o_, s_], writes=[ogb[tt % 2][kc]])
                    else:
                        for kc in range(8):
                            P.op("act", lambda e, kc=kc, o_=o_: e.activation(sq1.t[:, :], o_.t[:, kc, :], AF.Square),
                                 reads=[o_], writes=[sq1])
                            P.op("pe", lambda e: e.matmul(c.bank[7].t[:, :], c.ones.t[:, :], sq1.t[:, :], start=True, stop=True),
                                 reads=[c.ones, sq1], writes=[c.bank[7]])
                            P.op("dve", lambda e: e.tensor_scalar(r1.t[:, :], c.bank[7].t[:, :], EPS * 128.0, None, ALU.add),
                                 reads=[c.bank[7]], writes=[r1])
                            P.op("act", lambda e: e.activation(r1.t[:, :], r1.t[:, :], AF.Sqrt), reads=[r1], writes=[r1])
                            P.op("dve", lambda e: e.reciprocal(r1.t[:, :], r1.t[:, :]), reads=[r1], writes=[r1])
                            P.op("dve", lambda e, kc=kc, o_=o_: e.tensor_tensor(t1.t[:, :], o_.t[:, kc, :], r1.t[:, :], ALU.mult),
                                 reads=[o_, r1], writes=[t1])
                            P.op("dve", lambda e, kc=kc, s_=s_, og_=og_: e.scalar_tensor_tensor(
                                og_[:, kc, :], t1.t[:, :], hg.t[:, kc:kc + 1], s_.t[:, kc, :], ALU.mult, ALU.mult),
                                reads=[t1, hg, s_], writes=[ogb[tt % 2][kc]])
                    for dc in range(8):
                        bk = c.bank[4 + dc % 2]
                        for kc in range(8):
                            P.op("pe", lambda e, kc=kc, dc=dc, bk=bk, og_=og_: e.matmul(
                                bk.t[:, :], wo.t[:, kc, dc * 128:(dc + 1) * 128], og_[:, kc, :],
                                start=(kc == 0), stop=(kc == 7)),
                                reads=[wo, ogb[tt % 2][kc]], writes=[bk], pe_acc=(kc > 0))
                        P.op("dve", lambda e, dc=dc, bk=bk, t0=t0: e.scalar_tensor_tensor(
                            X[:, dc, t0:t0 + 512], bk.t[:, :], Gcol(l, 1, dc), X[:, dc, t0:t0 + 512], ALU.mult, ALU.add),
                            reads=[bk, c.der, c.Xb[dc][tt]], writes=[c.Xb[dc][tt]])
            P.barrier()

        def ffn(j, l, sub):
            wupd = din("wup%d" % j, [11, 128, 4096])
            wdnd = din("wdn%d" % j, [8, 128, 2816])
            with ExitStack() as e2:
                hbt = e2.enter_context(nc.sbuf_tensor("hb_%d" % j, [128, 8, 1024], BF16))
                hbb = [[Buf(hbt) for _ in range(2)] for _ in range(8)]
                actt = e2.enter_context(nc.sbuf_tensor("actb_%d" % j, [128, NF, 1024], BF16))
                actb = [[Buf(actt) for _ in range(2)] for _ in range(NF)]
                wu = [P.sb(e2, "wu%d_%d" % (j, i), [128, 2, 8, 256], BF16) for i in range(2)]
                wd = [P.sb(e2, "wd%d_%d" % (j, i), [128, NF, 128], BF16) for i in range(2)]
                sa = [P.sb(e2, "sa%d_%d" % (j, i), [128, 512], F32) for i in range(2)]
                for half in range(2):
                    for t2 in range(2):
                        tt = half * 2 + t2
                        modnorm_tile(l, sub, tt, lambda kc, t2=t2: hbt[:, kc, t2 * 512:(t2 + 1) * 512],
                                     lambda kc, t2=t2: hbb[kc][t2])
                    it = 0
                    for g in range(11):
                        w_ = wu[g % 2]
                        P.dma("pool", w_.t[:, :, :, :], wupd[g].rearrange("p (a k f) -> p a k f", a=2, k=8), writes=[w_])
                        for jf in range(2):
                            fc = 2 * g + jf
                            for t2 in range(2):
                                bA, bB = c.bank[it % 2], c.bank[2 + it % 2]
                                s_ = sa[it % 2]
                                it += 1
                                for kc in range(8):
                                    P.op("pe", lambda e, kc=kc, w_=w_, jf=jf, t2=t2, bA=bA: e.matmul(
                                        bA.t[:, :], w_.t[:, 0, kc, jf * 128:(jf + 1) * 128], hbt[:, kc, t2 * 512:(t2 + 1) * 512],
                                        start=(kc == 0), stop=(kc == 7)),
                                        reads=[w_, hbb[kc][t2]], writes=[bA], pe_acc=(kc > 0))
                                for kc in range(8):
                                    P.op("pe", lambda e, kc=kc, w_=w_, jf=jf, t2=t2, bB=bB: e.matmul(
                                        bB.t[:, :], w_.t[:, 1, kc, jf * 128:(jf + 1) * 128], hbt[:, kc, t2 * 512:(t2 + 1) * 512],
                                        start=(kc == 0), stop=(kc == 7)),
                                        reads=[w_, hbb[kc][t2]], writes=[bB], pe_acc=(kc > 0))
                                P.op("act", lambda e, s_=s_, bA=bA: e.activation(s_.t[:, :], bA.t[:, :], AF.Silu),
                                     reads=[bA], writes=[s_])
                                P.op("dve", lambda e, s_=s_, bB=bB, fc=fc, t2=t2: e.tensor_tensor(
                                    actt[:, fc, t2 * 512:(t2 + 1) * 512], bB.t[:, :], s_.t[:, :], ALU.mult),
                                    reads=[bB, s_], writes=[actb[fc][t2]])
                    for dc in range(8):
                        w_ = wd[dc % 2]
                        P.dma("pool", w_.t[:, :, :], wdnd[dc].rearrange("p (f d) -> p f d", f=NF), writes=[w_])
                        for t2 in range(2):
                            tt = half * 2 + t2
                            t0 = tt * 512
                            bk = c.bank[4 + (dc * 2 + t2) % 2]
                            for fc in range(NF):
                                P.op("pe", lambda e, fc=fc, w_=w_, t2=t2, bk=bk: e.matmul(
                                    bk.t[:, :], w_.t[:, fc, :], actt[:, fc, t2 * 512:(t2 + 1) * 512],
                                    start=(fc == 0), stop=(fc == NF - 1)),
                                    reads=[w_, actb[fc][t2]], writes=[bk], pe_acc=(fc > 0))
                            P.op("dve", lambda e, dc=dc, bk=bk, t0=t0: e.scalar_tensor_tensor(
                                X[:, dc, t0:t0 + 512], bk.t[:, :], Gcol(l, sub, dc), X[:, dc, t0:t0 + 512], ALU.mult, ALU.add),
                                reads=[bk, c.der, c.Xb[dc][tt]], writes=[c.Xb[dc][tt]])
            P.barrier()

        def proj_fm(wname, hbt, hbb, evac, n_oc=8):
            wd_ = din(wname, [n_oc, 128, 1024])
            with ExitStack() as e3:
                wp = [P.sb(e3, wname + "_sb%d" % i, [128, 8, 128], BF16) for i in range(2)]
                it = 0
                for oc in range(n_oc):
                    w_ = wp[oc % 2]
                    P.dma("pool", w_.t[:, :, :], wd_[oc].rearrange("p (k f) -> p k f", k=8), writes=[w_])
                    for tt in range(4):
                        bk = c.bank[it % 4]
                        it += 1
                        for kc in range(8):
                            P.op("pe", lambda e, kc=kc, w_=w_, tt=tt, bk=bk: e.matmul(
                                bk.t[:, :], w_.t[:, kc, :], hbt[:, kc, tt * 512:(tt + 1) * 512],
                                start=(kc == 0), stop=(kc == 7)),
                                reads=[w_, hbb[kc][tt]], writes=[bk], pe_acc=(kc > 0))
                        evac(oc, tt, bk)
                P.barrier()

        def proj_tm(wname, hbt, hbb, vout, vob, func):
            wd_ = din(wname, [2, 128, 4096])
            with ExitStack() as e3:
                wv = P.sb(e3, wname + "_sb", [128, 2, 8, 512], BF16)
                for cg in range(2):
                    P.dma("pool", wv.t[:, cg, :, :], wd_[cg].rearrange("p (k f) -> p k f", k=8), writes=[wv])
                vt = [P.sb(e3, "vt%d" % i, [128, 512], BF16) for i in range(2)]
                it = 0
                for tk in range(16):
                    for cg in range(2):
                        bk = c.bank[it % 4]
                        v_ = vt[it % 2]
                        it += 1
                        for kc in range(8):
                            P.op("pe", lambda e, kc=kc, tk=tk, cg=cg, bk=bk: e.matmul(
                                bk.t[:, :], hbt[:, kc, tk * 128:(tk + 1) * 128], wv.t[:, cg, kc, :],
                                start=(kc == 0), stop=(kc == 7)),
                                reads=[wv, hbb[kc][tk // 4]], writes=[bk], pe_acc=(kc > 0))
                        P.op("act", lambda e, bk=bk, v_=v_: e.activation(v_.t[:, :], bk.t[:, :], func),
                             reads=[bk], writes=[v_])
                        P.dma("sp", vout[tk * 128:(tk + 1) * 128, cg * 512:(cg + 1) * 512], v_.t[:, :], reads=[v_], ow=vob)
                P.barrier()

        def stage_out(e3, name, shape, dt):
            return [P.sb(e3, name + "%d" % i, shape, dt) for i in range(2)]

        def projections(kind, l):
            with ExitStack() as e2:
                hbt = e2.enter_context(nc.sbuf_tensor("hb2", [128, 8, T], BF16))
                hbb = [[Buf(hbt) for _ in range(4)] for _ in range(8)]
                for tt in range(4):
                    modnorm_tile(l, 1, tt, lambda kc, tt=tt: hbt[:, kc, tt * 512:(tt + 1) * 512],
                                 lambda kc, tt=tt: hbb[kc][tt])
                qo = dout("qT", [D, T], BF16)
                qob = Buf()
                outs.append(qob)
                sgo = dout("sgo", [D, T], BF16)
                sgob = Buf()
                outs.append(sgob)
                vo = dout("v", [T, D], BF16)
                vob = Buf()
                outs.append(vob)
                cnt = [0]

                def simple_evac(od, ob, func, scale, st, dt_eng="act"):
                    def evac(oc, tt, bk):
                        s_ = st[cnt[0] % 2]
                        cnt[0] += 1
                        P.op("act", lambda e, s_=s_, bk=bk: e.activation(s_.t[:, :], bk.t[:, :], func, scale=scale),
                             reads=[bk], writes=[s_])
                        P.dma("sp", od[oc * 128:(oc + 1) * 128, tt * 512:(tt + 1) * 512], s_.t[:, :], reads=[s_], ow=ob)
                    return evac

                stb = stage_out(e2, "stb", [128, 512], BF16)
                if kind == "fox":
                    ko = dout("kT", [D, T], BF16)
                    kob = Buf()
                    outs.append(kob)
                    lfo = dout("lf", [16, T], F32)
                    lfob = Buf()
                    outs.append(lfob)
                    proj_fm("wq", hbt, hbb, simple_evac(qo, qob, AF.Copy, float(FD ** -0.5), stb))
                    proj_fm("wk", hbt, hbb, simple_evac(ko, kob, AF.Copy, 1.0, stb))
                    proj_fm("wg", hbt, hbb, simple_evac(sgo, sgob, AF.Sigmoid, 1.0, stb))
                    proj_tm("wv", hbt, hbb, vo, vob, AF.Copy)
                    wfd = din("wf", [128, 128])
                    bfd = din("bf", [16, 1])
                    wf = P.sb(e2, "wf_sb", [128, 8, 16], BF16)
                    P.dma("pool", wf.t[:, :, :], wfd.rearrange("p (k f) -> p k f", k=8), writes=[wf])
                    nbf = P.sb(e2, "nbf", [16, 1], F32)
                    P.dma("sp", nbf.t[:, :], bfd, writes=[nbf])
                    P.op("dve", lambda e: e.tensor_scalar(nbf.t[:, :], nbf.t[:, :], -1.0, None, ALU.mult), reads=[nbf], writes=[nbf])
                    e1 = P.sb(e2, "e1", [16, 512], F32)
                    l1 = [P.sb(e2, "l1_%d" % i, [16, 512], F32) for i in range(2)]
                    for tt in range(4):
                        bk = c.bank[tt % 4]
                        for kc in range(8):
                            P.op("pe", lambda e, kc=kc, tt=tt, bk=bk: e.matmul(
                                bk.t[0:16, :], wf.t[:, kc, :], hbt[:, kc, tt * 512:(tt + 1) * 512],
                                start=(kc == 0), stop=(kc == 7)),
                                reads=[wf, hbb[kc][tt]], writes=[bk], pe_acc=(kc > 0))
                        l_ = l1[tt % 2]
                        P.op("act", lambda e, bk=bk: e.activation(e1.t[:, :], bk.t[0:16, :], AF.Exp, bias=nbf.t[:, 0:1], scale=-1.0),
                             reads=[bk, nbf], writes=[e1])
                        P.op("act", lambda e, l_=l_: e.activation(l_.t[:, :], e1.t[:, :], AF.Ln, bias=1.0, scale=1.0),
                             reads=[e1], writes=[l_])
                        P.op("dve", lambda e, l_=l_: e.tensor_scalar(l_.t[:, :], l_.t[:, :], -1.0, None, ALU.mult),
                             reads=[l_], writes=[l_])
                        P.dma("sp", lfo[:, tt * 512:(tt + 1) * 512], l_.t[:, :], reads=[l_], ow=lfob)
                else:
                    ko = dout("kT", [D, T], F32)
                    kob = Buf()
                    outs.append(kob)
                    lfo = dout("lfT", [D, T], F32)
                    lfob = Buf()
                    outs.append(lfob)
                    lbd = din("lbl", [128, 16])
                    lbl = P.sb(e2, "lbl_sb", [128, 16], F32)
                    lb = P.sb(e2, "lb", [128, 8], F32)
                    oml = P.sb(e2, "oml", [128, 8], F32)
                    P.dma("sp", lbl.t[:, :], lbd, writes=[lbl])
                    P.op("dve", lambda e: e.tensor_tensor(lb.t[:, :], lbl.t[:, 8:16], lbl.t[:, 0:8], ALU.subtract), reads=[lbl], writes=[lb])
                    P.op("act", lambda e: e.activation(lb.t[:, :], lb.t[:, :], AF.Sigmoid), reads=[lb], writes=[lb])
                    P.op("dve", lambda e: e.tensor_scalar(oml.t[:, :], lb.t[:, :], -1.0, 1.0, ALU.mult, ALU.add), reads=[lb], writes=[oml])
                    proj_fm("wq", hbt, hbb, simple_evac(qo, qob, AF.Copy, 1.0, stb))
                    proj_fm("wg", hbt, hbb, simple_evac(sgo, sgob, AF.Silu, 1.0, stb))
                    proj_tm("wv", hbt, hbb, vo, vob, AF.Silu)
                    sg1 = P.sb(e2, "sg1", [128, 512], F32)
                    ff = stage_out(e2, "ff", [128, 512], F32)
                    lff = stage_out(e2, "lff", [128, 512], F32)
                    kk = stage_out(e2, "kk", [128, 512], F32)

                    def f_evac(oc, tt, bk):
                        i = cnt[0] % 2
                        cnt[0] += 1
                        f_, l_, k_ = ff[i], lff[i], kk[i]
                        P.op("act", lambda e, bk=bk: e.activation(sg1.t[:, :], bk.t[:, :], AF.Sigmoid), reads=[bk], writes=[sg1])
                        P.op("dve", lambda e, f_=f_, oc=oc: e.tensor_scalar(f_.t[:, :], sg1.t[:, :], oml.t[:, oc:oc + 1], lb.t[:, oc:oc + 1], ALU.mult, ALU.add),
                             reads=[sg1, oml, lb], writes=[f_])
                        P.op("act", lambda e, f_=f_, l_=l_: e.activation(l_.t[:, :], f_.t[:, :], AF.Ln), reads=[f_], writes=[l_])
                        P.op("dve", lambda e, f_=f_, k_=k_: e.tensor_scalar(k_.t[:, :], f_.t[:, :], -1.0, 1.0, ALU.mult, ALU.add),
                             reads=[f_], writes=[k_])
                        P.dma("sp", lfo[oc * 128:(oc + 1) * 128, tt * 512:(tt + 1) * 512], l_.t[:, :], reads=[l_], ow=lfob)
                        P.dma("sp", ko[oc * 128:(oc + 1) * 128, tt * 512:(tt + 1) * 512], k_.t[:, :], reads=[k_], ow=kob)
                    proj_fm("wf", hbt, hbb, f_evac)
            P.barrier()

        if cfg.get("epi"):
            epilogue(cfg["epi"][0], cfg["epi"][1])
        for j, (l, sub) in enumerate(cfg["ffns"]):
            ffn(j, l, sub)
        if cfg.get("proj"):
            projections(cfg["proj"][0], cfg["proj"][1])
        xo = dout("xo", [D, T])
        xob = Buf()
        outs.append(xob)
        xov = xo.rearrange("(c p) t -> p c t", p=128)
        if cfg.get("final"):
            fgd = din("fg", [128, 8])
            fg = P.sb(es, "fg_sb", [128, 8], F32)
            P.dma("sp", fg.t[:, :], fgd, writes=[fg])
            P.op("dve", lambda e: e.tensor_scalar(fg.t[:, :], fg.t[:, :], SQD, None, ALU.mult), reads=[fg], writes=[fg])
            yo = [P.sb(es, "yo%d" % i, [128, 512], F32) for i in range(2)]
            it = 0
            for tt in range(4):
                t0 = tt * 512
                rstd_tile(lambda kc, t0=t0: X[:, kc, t0:t0 + 512], [c.Xb[kc][tt] for kc in range(8)], EPS * D)
                for kc in range(8):
                    y_ = yo[it % 2]
                    it += 1
                    P.op("dve", lambda e, kc=kc, y_=y_, t0=t0: e.scalar_tensor_tensor(
                        y_.t[:, :], X[:, kc, t0:t0 + 512], fg.t[:, kc:kc + 1], c.rstd.t[:, :], ALU.mult, ALU.mult),
                        reads=[c.Xb[kc][tt], fg, c.rstd], writes=[y_])
                    P.dma("sp", xov[:, kc, t0:t0 + 512], y_.t[:, :], reads=[y_], ow=xob)
        else:
            for kc in range(8):
                P.dma("sp", xov[:, kc, :], X[:, kc, :], reads=[c.Xb[kc][tt] for tt in range(4)], ow=xob)
        P.finish(outs)
        P.emit()
    return nc


MC = 2304


def build_mod():
    nc = bass.Bass("TRN2", target_bir_lowering=False)
    P = Prog(nc)
    cT = nc.dram_tensor("cT", [128, 16], F32, kind="ExternalInput").ap()
    w = nc.dram_tensor("w", [128, 8 * MC], F32, kind="ExternalInput").ap()
    bias = nc.dram_tensor("bias", [2, MC], F32, kind="ExternalInput").ap()
    mo = nc.dram_tensor("mo", [2, MC], F32, kind="ExternalOutput").ap()
    with ExitStack() as es:
        ct = P.sb(es, "ct", [128, 8, 2], F32)
        wt = [P.sb(es, "wt%d" % i, [128, 8, 384], F32) for i in range(6)]
        bt = P.sb(es, "bt", [2, MC], F32)
        ot = P.sb(es, "ot", [2, MC], F32)
        banks = [P.ps(es, "bk%d" % i, [128, 512], F32) for i in range(2)]
        P.dma("sp", ct.t[:, :, :], cT.rearrange("p (k b) -> p k b", k=8), writes=[ct])
        P.dma("sp", bt.t[:, :], bias, writes=[bt])
        wv = w.rearrange("p (k n) -> p k n", k=8)
        for i in range(6):
            P.dma("sp" if i % 2 == 0 else "pool", wt[i].t[:, :, :], wv[:, :, i * 384:(i + 1) * 384], writes=[wt[i]])
        P.op("act", lambda e: e.activation(ct.t[:, :, :], ct.t[:, :, :], AF.Silu), reads=[ct], writes=[ct])
        for i in range(6):
            bk = banks[i % 2]
            for kc in range(8):
                P.op("pe", lambda e, kc=kc, i=i, bk=bk: e.matmul(bk.t[0:2, 0:384], ct.t[:, kc, :], wt[i].t[:, kc, :],
                                                                 start=(kc == 0), stop=(kc == 7)),
                     reads=[ct, wt[i]], writes=[bk], pe_acc=(kc > 0))
            P.op("dve", lambda e, i=i, bk=bk: e.tensor_tensor(ot.t[:, i * 384:(i + 1) * 384], bk.t[0:2, 0:384],
                                                             bt.t[:, i * 384:(i + 1) * 384], ALU.add),
                 reads=[bk, bt], writes=[ot])
        ob = Buf()
        P.dma("sp", mo, ot.t[:, :], reads=[ot], ow=ob)
        P.finish([ob])
        P.emit()
    return nc


def run_mod(c, ada_w, ada_b):
    nc = build_mod()
    cT = np.ascontiguousarray(c.T.reshape(8, 128, B).transpose(1, 0, 2)).reshape(128, 16)
    wall = np.concatenate([ada_w[0], ada_w[1]], axis=1)
    ball = np.concatenate([ada_b[0], ada_b[1]], axis=0)
    maps = []
    for j in range(NCORES):
        wj = wall[:, j * MC:(j + 1) * MC].reshape(8, 128, MC).transpose(1, 0, 2)
        maps.append({"cT": cT, "w": np.ascontiguousarray(wj).reshape(128, 8 * MC),
                     "bias": np.ascontiguousarray(np.broadcast_to(ball[j * MC:(j + 1) * MC], (2, MC)))})
    res = run_bass_kernel_spmd(nc, maps, core_ids=list(range(NCORES)))
    mod = np.concatenate([r["mo"] for r in res.results], axis=1)
    return mod.reshape(B, 2, 9, D)


def fm_cols(v):
    lead = int(np.prod(v.shape[:-1])) if v.ndim > 1 else 1
    a = v.reshape(lead, 8, 128).transpose(2, 0, 1)
    return np.ascontiguousarray(a).reshape(128, lead * 8)


def tile_w_fm(w):
    n = w.shape[1] // 128
    a = w.reshape(8, 128, n, 128).transpose(2, 1, 0, 3)
    return np.ascontiguousarray(a).reshape(n, 128, 1024)


def tile_w_tm(w):
    a = w.reshape(8, 128, 2, 512).transpose(2, 1, 0, 3)
    return np.ascontiguousarray(a).reshape(2, 128, 4096)


def tile_wup(w):
    a = w.reshape(8, 128, 2, 11, 256).transpose(3, 1, 2, 0, 4)
    return np.ascontiguousarray(a).reshape(11, 128, 4096)


def tile_wdn(w):
    a = w.reshape(NF, 128, 8, 128).transpose(2, 1, 0, 3)
    return np.ascontiguousarray(a).reshape(8, 128, NF * 128)


def tile_wo(w):
    a = w.reshape(8, 128, D).transpose(1, 0, 2)
    return np.ascontiguousarray(a).reshape(128, 8 * D)


NEG = -30000.0


def build_fox():
    nc = bass.Bass("TRN2", target_bir_lowering=False)
    P = Prog(nc)
    qd = nc.dram_tensor("q", [4, 64, S], BF16, kind="ExternalInput").ap()
    kd = nc.dram_tensor("k", [4, 64, S], BF16, kind="ExternalInput").ap()
    vd = nc.dram_tensor("v", [4, 128, 64 * 64], BF16, kind="ExternalInput").ap()
    ltd = nc.dram_tensor("lt", [128, 256], F32, kind="ExternalInput").ap()
    lqd = nc.dram_tensor("lq", [16, 2048], F32, kind="ExternalInput").ap()
    Ud = nc.dram_tensor("U", [128, 128], F32, kind="ExternalInput").ap()
    seld = nc.dram_tensor("sel", [128, 128], F32, kind="ExternalInput").ap()
    mkd = nc.dram_tensor("mk", [128, 128], F32, kind="ExternalInput").ap()
    od = nc.dram_tensor("o", [4, 64, S], F32, kind="ExternalOutput").ap()
    shi = nc.dram_tensor("shi", [4, S], BF16).ap()
    slo = nc.dram_tensor("slo", [4, S], BF16).ap()
    with ExitStack() as es:
        bank = [P.ps(es, "bank%d" % i, [128, 512], F32) for i in range(8)]
        U = P.sb(es, "U_sb", [128, 128], F32)
        sel = P.sb(es, "sel_sb", [128, 128], F32)
        mk = P.sb(es, "mk_sb", [128, 128], F32)
        onesf = P.sb(es, "onesf", [128, 128], F32)
        lt = P.sb(es, "lt_sb", [128, 256], F32)
        lq = P.sb(es, "lq_sb", [16, 2048], F32)
        within = P.sb(es, "within", [128, 256], F32)
        tot = P.sb(es, "tot", [128, 256], F32)
        inc = P.sb(es, "inc", [128, 256], F32)
        GT = P.sb(es, "GT", [128, 256], F32)
        gend = P.sb(es, "gend", [128, 256], F32)
        negB = P.sb(es, "negB", [128, 4 * 16 * 64], F32)
        cl = P.sb(es, "cl", [16, 2048], F32)
        Aa = P.sb(es, "Aa", [16, 2048], F32)
        ahi = P.sb(es, "ahi", [16, 2048], BF16)
        ahf = P.sb(es, "ahf", [16, 2048], F32)
        alo = P.sb(es, "alo", [16, 2048], BF16)
        qa = [P.sb(es, "qa%d" % i, [66, S], BF16) for i in range(2)]
        ka = [P.sb(es, "ka%d" % i, [66, S], BF16) for i in range(2)]
        va = [P.sb(es, "va%d" % i, [128, 64, 65], BF16) for i in range(2)]
        pt = [P.sb(es, "pt%d" % i, [128, 512], BF16) for i in range(3)]
        drow = P.sb(es, "drow", [65, 512], F32)
        rec = P.sb(es, "rec", [64, 512], F32)
        oo = [P.sb(es, "oo%d" % i, [64, 512], F32) for i in range(2)]
        ob = Buf()

        for t_, d_ in ((U, Ud), (sel, seld), (mk, mkd), (lt, ltd), (lq, lqd)):
            P.dma("sp", t_.t[:, :], d_, writes=[t_])
        P.op("pool", lambda e: e.memset(onesf.t[:, :], 1.0), writes=[onesf])
        P.op("pe", lambda e: e.matmul(bank[6].t[:, 0:256], U.t[:, :], lt.t[:, :], start=True, stop=True), reads=[U, lt], writes=[bank[6]])
        P.op("pe", lambda e: e.matmul(bank[7].t[:, 0:256], onesf.t[:, :], lt.t[:, :], start=True, stop=True), reads=[onesf, lt], writes=[bank[7]])
        P.op("dve", lambda e: e.tensor_copy(within.t[:, :], bank[6].t[:, 0:256]), reads=[bank[6]], writes=[within])
        P.op("dve", lambda e: e.tensor_copy(tot.t[:, :], bank[7].t[:, 0:256]), reads=[bank[7]], writes=[tot])
        for h in range(4):
            P.op("dve", lambda e, h=h: e.tensor_tensor_scan(inc.t[:, h * 64:(h + 1) * 64], onesf.t[:, 0:64], tot.t[:, h * 64:(h + 1) * 64],
                                                            0.0, ALU.mult, ALU.add), reads=[onesf, tot], writes=[inc])
        P.op("dve", lambda e: e.tensor_tensor(GT.t[:, :], within.t[:, :], inc.t[:, :], ALU.add), reads=[within, inc], writes=[GT])
        P.op("dve", lambda e: e.tensor_tensor(GT.t[:, :], GT.t[:, :], tot.t[:, :], ALU.subtract), reads=[GT, tot], writes=[GT])
        P.op("pe", lambda e: e.matmul(bank[6].t[:, 0:256], sel.t[:, :], GT.t[:, :], start=True, stop=True), reads=[sel, GT], writes=[bank[6]])
        P.op("dve", lambda e: e.tensor_copy(gend.t[:, :], bank[6].t[:, 0:256]), reads=[bank[6]], writes=[gend])
        for h in range(4):
            for Q in range(16):
                j0 = (h * 16 + Q) * 64
                gc = h * 64 + 4 * Q + 3
                P.op("dve", lambda e, h=h, j0=j0, gc=gc: e.tensor_scalar(
                    negB.t[:, j0:j0 + 64], GT.t[:, h * 64:(h + 1) * 64], -1.0, gend.t[:, gc:gc + 1], ALU.mult, ALU.add),
                    reads=[GT, gend], writes=[negB])
        ones16 = P.sb(es, "ones16", [16, 512], F32)
        P.op("pool", lambda e: e.memset(ones16.t[:, :], 1.0), writes=[ones16])
        for h in range(4):
            P.op("dve", lambda e, h=h: e.tensor_tensor_scan(cl.t[:, h * 512:(h + 1) * 512], ones16.t[:, :], lq.t[:, h * 512:(h + 1) * 512],
                                                            0.0, ALU.mult, ALU.add), reads=[lq, ones16], writes=[cl])
        for h in range(4):
            P.op("dve", lambda e, h=h: e.tensor_scalar(Aa.t[:, h * 512:(h + 1) * 512], cl.t[:, h * 512:(h + 1) * 512],
                                                       cl.t[:, h * 512 + 511:h * 512 + 512], None, ALU.subtract),
                 reads=[cl], writes=[Aa])
        P.op("dve", lambda e: e.tensor_copy(ahi.t[:, :], Aa.t[:, :]), reads=[Aa], writes=[ahi])
        P.op("dve", lambda e: e.tensor_copy(ahf.t[:, :], ahi.t[:, :]), reads=[ahi], writes=[ahf])
        P.op("dve", lambda e: e.tensor_tensor(alo.t[:, :], Aa.t[:, :], ahf.t[:, :], ALU.subtract), reads=[Aa, ahf], writes=[alo])
        shb, slb = Buf(), Buf()
        P.dma("sp", shi.rearrange("h (q m) -> q h m", q=16), ahi.t[:, :].rearrange("q (h m) -> q h m", h=4), reads=[ahi], writes=[shb])
        P.dma("sp", slo.rearrange("h (q m) -> q h m", q=16), alo.t[:, :].rearrange("q (h m) -> q h m", h=4), reads=[alo], writes=[slb])

        for i in range(2):
            P.op("pool", lambda e, i=i: e.memset(ka[i].t[64:66, :], 1.0), writes=[ka[i]])
            P.op("pool", lambda e, i=i: e.memset(va[i].t[:, :, 64:65], 1.0), writes=[va[i]])

        def load_head(h):
            q_, k_, v_ = qa[h % 2], ka[h % 2], va[h % 2]
            P.dma("sp", q_.t[0:64, :], qd[h], writes=[q_])
            P.dma("sp", q_.t[64:65, :], shi[h:h + 1, :], reads=[shb], writes=[q_])
            P.dma("sp", q_.t[65:66, :], slo[h:h + 1, :], reads=[slb], writes=[q_])
            P.dma("pool", k_.t[0:64, :], kd[h], writes=[k_])
            P.dma("pool", v_.t[:, :, 0:64], vd[h].rearrange("p (t d) -> p t d", d=64), writes=[v_])

        load_head(0)

        def do_head(h, q_, k_, v_, nit):
            items = [(Q, kt) for Q in range(16) for kt in range(4 * Q + 4)]

            def emit_S(idx, it_no):
                Q, kt = items[idx]
                d = kt - 4 * Q
                c0 = 128 * d if d >= 0 else 0
                bk = bank[it_no % 3]
                p_ = pt[it_no % 3]
                P.op("pe", lambda e: e.matmul(bk.t[:, c0:512], k_.t[0:66, kt * 128:(kt + 1) * 128],
                                              q_.t[0:66, Q * 512 + c0:(Q + 1) * 512], start=True, stop=True),
                     reads=[k_, q_], writes=[bk])
                if d >= 0:
                    P.op("dve", lambda e: e.tensor_tensor(bk.t[:, c0:c0 + 128], bk.t[:, c0:c0 + 128], mk.t[:, :], ALU.add),
                         reads=[bk, mk], writes=[bk])
                jb = (h * 16 + Q) * 64 + kt
                P.op("act", lambda e: e.activation(p_.t[:, c0:512], bk.t[:, c0:512], AF.Exp, bias=negB.t[:, jb:jb + 1], scale=1.0),
                     reads=[bk, negB], writes=[p_])

            def emit_PV(idx, it_no):
                Q, kt = items[idx]
                d = kt - 4 * Q
                c0 = 128 * d if d >= 0 else 0
                p_ = pt[it_no % 3]
                ob_ = bank[3 + Q % 2]
                last = (kt == 4 * Q + 3)
                P.op("pe", lambda e: e.matmul(ob_.t[0:65, c0:512], v_.t[:, kt, :], p_.t[:, c0:512], start=(kt == 0), stop=last),
                     reads=[v_, p_], writes=[ob_], pe_acc=(kt > 0))
                if last:
                    o_ = oo[Q % 2]
                    P.op("act", lambda e: e.activation(drow.t[64:65, :], ob_.t[64:65, :], AF.Copy), reads=[ob_], writes=[drow])
                    P.op("pe", lambda e: e.matmul(bank[5].t[0:64, :], onesf.t[64:65, 0:64], drow.t[64:65, :], start=True, stop=True),
                         reads=[onesf, drow], writes=[bank[5]])
                    P.op("dve", lambda e: e.reciprocal(rec.t[:, :], bank[5].t[0:64, :]), reads=[bank[5]], writes=[rec])
                    P.op("dve", lambda e: e.tensor_tensor(o_.t[:, :], ob_.t[0:64, :], rec.t[:, :], ALU.mult), reads=[ob_, rec], writes=[o_])
                    P.dma("sp", od[h][:, Q * 512:(Q + 1) * 512], o_.t[:, :], reads=[o_], ow=ob)

            n = len(items)
            emit_S(0, nit)
            for idx in range(n):
                if idx + 1 < n:
                    emit_S(idx + 1, nit + idx + 1)
                emit_PV(idx, nit + idx)
            return nit + n

        nit = 0
        for h in range(4):
            if h + 1 < 4:
                load_head(h + 1)
            nit = do_head(h, qa[h % 2], ka[h % 2], va[h % 2], nit)
        P.finish([ob])
        P.emit()
    return nc


def fox_consts():
    k = np.arange(128)
    U = (k[:, None] <= k[None, :]).astype(np.float32)
    sel = np.zeros((128, 128), np.float32)
    sel[127, :] = 1.0
    mk = np.where(k[None, :] >= k[:, None], 0.0, NEG).astype(np.float32)
    return U, sel, mk


def build_hgrn():
    nc = bass.Bass("TRN2", target_bir_lowering=False)
    P = Prog(nc)
    qd = nc.dram_tensor("q", [2, 128, S], BF16, kind="ExternalInput").ap()
    kd = nc.dram_tensor("k", [2, 128, S], F32, kind="ExternalInput").ap()
    lfd = nc.dram_tensor("lf", [2, 128, S], F32, kind="ExternalInput").ap()
    vd = nc.dram_tensor("v", [2, 128, 64 * 128], BF16, kind="ExternalInput").ap()
    m01d = nc.dram_tensor("m01", [128, 64], F32, kind="ExternalInput").ap()
    rmd = nc.dram_tensor("rm", [128, 2048], F32, kind="ExternalInput").ap()
    idd = nc.dram_tensor("ident", [128, 128], BF16, kind="ExternalInput").ap()
    od = nc.dram_tensor("o", [2, 128, S], F32, kind="ExternalOutput").ap()
    NB = 2048
    with ExitStack() as es:
        bankA = [P.ps(es, "bankA%d" % i, [128, 512], F32) for i in range(2)]
        bankO = [P.ps(es, "bankO%d" % i, [128, 512], F32) for i in range(2)]
        bankU = [P.ps(es, "bankU%d" % i, [128, 512], F32) for i in range(2)]
        bankT = P.ps(es, "bankT", [128, 1024], BF16)
        m01 = P.sb(es, "m01_sb", [128, 64], F32)
        rm = P.sb(es, "rm_sb", [128, NB], F32)
        ident = P.sb(es, "ident_sb", [128, 128], BF16)
        P.dma("sp", m01.t[:, :], m01d, writes=[m01])
        P.dma("sp", rm.t[:, :], rmd, writes=[rm])
        P.dma("sp", ident.t[:, :], idd, writes=[ident])
        ob = Buf()
        hs = []
        for h in range(2):
            o = Ctx()
            o.qb = P.sb(es, "qb%d" % h, [128, NB], BF16)
            o.kb = P.sb(es, "kb%d" % h, [128, NB], F32)
            o.lf = P.sb(es, "lf%d" % h, [128, NB], F32)
            o.G = P.sb(es, "G%d" % h, [128, NB], F32)
            o.tmp = P.sb(es, "tmp%d" % h, [128, NB], F32)
            o.tmp2 = P.sb(es, "tmp2%d" % h, [128, NB], F32)
            o.qd = P.sb(es, "qd%d" % h, [128, NB], BF16)
            o.kdd = P.sb(es, "kdd%d" % h, [128, NB], BF16)
            o.kend = P.sb(es, "kend%d" % h, [128, NB], BF16)
            o.kT = P.sb(es, "kT%d" % h, [128, 16, 128], BF16)
            o.vb = P.sb(es, "vb%d" % h, [128, 16, 128], BF16)
            o.egl = P.sb(es, "egl%d" % h, [128, 32], F32)
            o.S32 = P.sb(es, "S32_%d" % h, [128, 128], F32)
            o.Sbf = [P.sb(es, "Sbf%d_%d" % (h, i), [128, 128], BF16) for i in range(2)]
            o.am = [P.sb(es, "am%d_%d" % (h, i), [128, 64], BF16) for i in range(2)]
            o.osb = [P.sb(es, "osb%d_%d" % (h, i), [128, 512], F32) for i in range(2)]
            P.op("pool", lambda e, o=o: e.memset(o.S32.t[:, :], 0.0), writes=[o.S32])
            P.op("pool", lambda e, o=o: e.memset(o.Sbf[0].t[:, :], 0.0), writes=[o.Sbf[0]])
            o.si = 0
            hs.append(o)

        def prep(h, blk):
            o = hs[h]
            t0 = blk * NB
            P.dma("sp", o.qb.t[:, :], qd[h][:, t0:t0 + NB], writes=[o.qb])
            P.dma("sp", o.kb.t[:, :], kd[h][:, t0:t0 + NB], writes=[o.kb])
            P.dma("sp", o.lf.t[:, :], lfd[h][:, t0:t0 + NB], writes=[o.lf])
            P.dma("pool", o.vb.t[:, :, :], vd[h][:, blk * 2048:(blk + 1) * 2048].rearrange("p (t d) -> p t d", d=128), writes=[o.vb])
            P.op("dve", lambda e: e.tensor_tensor_scan(o.G.t[:, :], rm.t[:, :], o.lf.t[:, :], 0.0, ALU.mult, ALU.add),
                 reads=[rm, o.lf], writes=[o.G])
            P.op("act", lambda e: e.activation(o.tmp.t[:, :], o.G.t[:, :], AF.Exp), reads=[o.G], writes=[o.tmp])
            P.op("dve", lambda e: e.tensor_tensor(o.qd.t[:, :], o.qb.t[:, :], o.tmp.t[:, :], ALU.mult), reads=[o.qb, o.tmp], writes=[o.qd])
            P.op("act", lambda e: e.activation(o.tmp2.t[:, :], o.G.t[:, :], AF.Exp, scale=-1.0), reads=[o.G], writes=[o.tmp2])
            P.op("dve", lambda e: e.tensor_tensor(o.tmp2.t[:, :], o.kb.t[:, :], o.tmp2.t[:, :], ALU.mult), reads=[o.kb, o.tmp2], writes=[o.tmp2])
            P.op("dve", lambda e: e.tensor_copy(o.kdd.t[:, :], o.tmp2.t[:, :]), reads=[o.tmp2], writes=[o.kdd])
            G3 = o.G.t[:, :].rearrange("p (c s) -> p c s", s=64)
            P.op("act", lambda e: e.activation(o.egl.t[:, :], G3[:, :, 63], AF.Exp), reads=[o.G], writes=[o.egl])
            for cc in range(32):
                P.op("dve", lambda e, cc=cc: e.tensor_scalar(o.kend.t[:, cc * 64:(cc + 1) * 64], o.tmp2.t[:, cc * 64:(cc + 1) * 64],
                                                             o.egl.t[:, cc:cc + 1], None, ALU.mult),
                     reads=[o.tmp2, o.egl], writes=[o.kend])
            for grp in range(2):
                for j in range(8):
                    tk = grp * 8 + j
                    P.op("pe", lambda e, tk=tk, j=j: e.transpose(bankT.t[:, j * 128:(j + 1) * 128], o.kend.t[:, tk * 128:(tk + 1) * 128], ident.t[:, :]),
                         reads=[o.kend, ident], writes=[bankT], pe_acc=(j > 0))
                P.op("act", lambda e, grp=grp: e.activation(o.kT.t[:, grp * 8:(grp + 1) * 8, :],
                                                           bankT.t[:, :].rearrange("p (t k) -> p t k", k=128), AF.Copy),
                     reads=[bankT], writes=[o.kT])

        nA = [0]

        def chunk(h, blk, cc):
            o = hs[h]
            tk, half = cc // 2, cc % 2
            pb = 64 * half
            gc = blk * 32 + cc
            cs = slice(cc * 64, (cc + 1) * 64)
            bA = bankA[nA[0] % 2]
            am = o.am[nA[0] % 2]
            nA[0] += 1
            bO = bankO[h]
            bU = bankU[h]
            oc0 = (gc % 8) * 64
            Sb = o.Sbf[o.si % 2]
            Sn = o.Sbf[(o.si + 1) % 2]
            o.si += 1
            P.op("pe", lambda e: e.matmul(bA.t[pb:pb + 64, 0:64], o.kdd.t[:, cs], o.qd.t[:, cs], start=True, stop=True),
                 reads=[o.kdd, o.qd], writes=[bA])
            P.op("dve", lambda e: e.tensor_tensor(am.t[pb:pb + 64, :], bA.t[pb:pb + 64, 0:64], m01.t[pb:pb + 64, :], ALU.mult),
                 reads=[bA, m01], writes=[am])
            P.op("pe", lambda e: e.matmul(bO.t[:, oc0:oc0 + 64], Sb.t[:, :], o.qd.t[:, cs], start=True, stop=False),
                 reads=[Sb, o.qd], writes=[bO], pe_acc=(gc % 8 != 0))
            P.op("pe", lambda e: e.matmul(bO.t[:, oc0:oc0 + 64], o.vb.t[pb:pb + 64, tk, :], am.t[pb:pb + 64, :], start=False, stop=True),
                 reads=[o.vb, am], writes=[bO], pe_acc=True)
            P.op("pe", lambda e: e.matmul(bU.t[:, 0:128], o.kT.t[pb:pb + 64, tk, :], o.vb.t[pb:pb + 64, tk, :], start=True, stop=True),
                 reads=[o.kT, o.vb], writes=[bU])
            P.op("dve", lambda e: e.scalar_tensor_tensor(o.S32.t[:, :], o.S32.t[:, :], o.egl.t[:, cc:cc + 1], bU.t[:, 0:128], ALU.mult, ALU.add),
                 reads=[o.S32, o.egl, bU], writes=[o.S32])
            P.op("act", lambda e: e.activation(Sn.t[:, :], o.S32.t[:, :], AF.Copy), reads=[o.S32], writes=[Sn])
            if gc % 8 == 7:
                os_ = o.osb[(gc // 8) % 2]
                P.op("act", lambda e: e.activation(os_.t[:, :], bO.t[:, :], AF.Copy), reads=[bO], writes=[os_])
                tok0 = (gc - 7) * 64
                P.dma("sp", od[h][:, tok0:tok0 + 512], os_.t[:, :], reads=[os_], ow=ob)

        for blk in range(4):
            for h in range(2):
                prep(h, blk)
            for cc in range(32):
                for h in range(2):
                    chunk(h, blk, cc)
        P.finish([ob])
        P.emit()
    return nc


def hgrn_consts():
    p = np.arange(128)
    t = np.arange(64)
    m01 = ((p[:, None] % 64) <= t[None, :]).astype(np.float32)
    rm = np.ones((128, 2048), np.float32)
    rm[:, ::64] = 0.0
    ident = np.eye(128, dtype=np.float32).astype(NPBF)
    return m01, rm, ident


def build_mega():
    nc = bass.Bass("TRN2", target_bir_lowering=False)
    P = Prog(nc)
    c = Ctx()
    c.P, c.nc = P, nc
    dr = {}

    def din(name, shape, dt=F32):
        dr[name] = nc.dram_tensor(name, list(shape), dt, kind="ExternalInput").ap()
        return dr[name]

    def dout(name, shape, dt=F32):
        dr[name] = nc.dram_tensor(name, list(shape), dt, kind="ExternalOutput").ap()
        return dr[name]

    xT = din("xT", [D, T])
    cTd = din("cT", [128, 8])
    modbd = din("modb", [128, 144])
    modwd = din("modw", [18, 128, 8192])
    gT = din("gT", [128, 48])
    outs = []
    pid = nc.partition_id()
    g4 = pid % 4
    G4 = [[0, 1, 2, 3], [4, 5, 6, 7]]
    idram = lambda name, shape, dt: nc.dram_tensor(name, list(shape), dt)
    with ExitStack() as es:
        X = es.enter_context(nc.sbuf_tensor("X", [128, 8, T], F32))
        c.X = X
        c.Xb = [[Buf(X) for _ in range(4)] for _ in range(8)]
        c.modt = P.sb(es, "modt", [128, 144], F32)
        c.gt = P.sb(es, "gt", [128, 48], F32)
        c.der = P.sb(es, "der", [128, 96], F32)
        c.ones = P.sb(es, "ones", [128, 128], BF16)
        c.bank = [P.ps(es, "bank%d" % i, [128, 512], F32) for i in range(7)]
        bankT = P.ps(es, "bankT", [128, 1024], BF16)
        sqt = es.enter_context(nc.sbuf_tensor("sq", [128, 8, 512], BF16))
        c.sq = [Buf(sqt) for _ in range(8)]
        c.rstd = P.sb(es, "rstd", [128, 512], F32)
        c.tmp = [P.sb(es, "tmp%d" % i, [128, 512], F32) for i in range(2)]

        xv = xT.rearrange("(c p) t -> p c t", p=128)
        for kc in range(8):
            P.dma("sp", X[:, kc, :], xv[:, kc, :], writes=[c.Xb[kc][tt] for tt in range(4)])
        P.dma("sp", c.gt.t[:, :], gT, writes=[c.gt])
        P.op("pool", lambda e: e.memset(c.ones.t[:, :], 1.0), writes=[c.ones])
        with ExitStack() as e0:
            ct = P.sb(e0, "ct", [128, 8], F32)
            ctb = P.sb(e0, "ctb", [128, 8], BF16)
            mb = P.sb(e0, "mb", [128, 144], F32)
            mw = [P.sb(e0, "mw%d" % i, [128, 8, 1024], BF16) for i in range(2)]
            P.dma("sp", ct.t[:, :], cTd, writes=[ct])
            P.dma("sp", mb.t[:, :], modbd, writes=[mb])
            P.op("act", lambda e: e.activation(ctb.t[:, :], ct.t[:, :], AF.Silu), reads=[ct], writes=[ctb])
            bm = c.bank[5]
            for v in range(18):
                w_ = mw[v % 2]
                P.dma("pool", w_.t[:, :, :], modwd[v].rearrange("p (k n) -> p k n", k=8), writes=[w_])
                for ch in range(8):
                    col = v * 8 + ch
                    for kc in range(8):
                        P.op("pe", lambda e, w_=w_, ch=ch, kc=kc, col=col: e.matmul(
                            bm.t[:, col:col + 1], w_.t[:, kc, ch * 128:(ch + 1) * 128], ctb.t[:, kc:kc + 1],
                            start=(kc == 0), stop=(kc == 7)),
                            reads=[w_, ctb], writes=[bm], pe_acc=not (v == 0 and ch == 0 and kc == 0))
            P.op("dve", lambda e: e.tensor_tensor(c.modt.t[:, :], bm.t[:, 0:144], mb.t[:, :], ALU.add),
                 reads=[bm, mb], writes=[c.modt])
        P.barrier()
        for l in range(2):
            for sub in range(3):
                base = ((l * 3 + sub) * 2) * 8
                sc0 = mcol(l, sub * 3 + 1, 0)
                g0 = (l * 3 + sub) * 8
                ga0 = mcol(l, sub * 3 + 2, 0)
                P.op("dve", lambda e, base=base, sc0=sc0, g0=g0: e.scalar_tensor_tensor(
                    c.der.t[:, base:base + 8], c.modt.t[:, sc0:sc0 + 8], 1.0, c.gt.t[:, g0:g0 + 8], ALU.add, ALU.mult),
                    reads=[c.modt, c.gt], writes=[c.der])
                P.op("dve", lambda e, base=base: e.tensor_scalar(
                    c.der.t[:, base:base + 8], c.der.t[:, base:base + 8], SQD, None, ALU.mult),
                    reads=[c.der], writes=[c.der])
                P.op("dve", lambda e, base=base, ga0=ga0, sub=sub: e.tensor_scalar(
                    c.der.t[:, base + 8:base + 16], c.modt.t[:, ga0:ga0 + 8], (1.0 if sub == 1 else 0.5), None, ALU.mult),
                    reads=[c.modt], writes=[c.der])

        def Acol(l, sub, ch):
            j = ((l * 3 + sub) * 2) * 8 + ch
            return c.der.t[:, j:j + 1]

        def Gcol(l, sub, ch):
            j = ((l * 3 + sub) * 2 + 1) * 8 + ch
            return c.der.t[:, j:j + 1]

        def Scol(l, sub, ch):
            j = mcol(l, sub * 3 + 0, ch)
            return c.modt.t[:, j:j + 1]

        def rstd_tile(src_fn, src_bufs, epsk):
            for kc in range(8):
                P.op("act", lambda e, kc=kc: e.activation(sqt[:, kc, :], src_fn(kc), AF.Square),
                     reads=[src_bufs[kc]], writes=[c.sq[kc]])
            for kc in range(8):
                P.op("pe", lambda e, kc=kc: e.matmul(c.bank[6].t[:, :], c.ones.t[:, :], sqt[:, kc, :],
                                                      start=(kc == 0), stop=(kc == 7)),
                     reads=[c.ones, c.sq[kc]], writes=[c.bank[6]], pe_acc=(kc > 0))
            P.op("dve", lambda e: e.tensor_scalar(c.rstd.t[:, :], c.bank[6].t[:, :], epsk, None, ALU.add),
                 reads=[c.bank[6]], writes=[c.rstd])
            P.op("act", lambda e: e.activation(c.rstd.t[:, :], c.rstd.t[:, :], AF.Sqrt), reads=[c.rstd], writes=[c.rstd])
            P.op("dve", lambda e: e.reciprocal(c.rstd.t[:, :], c.rstd.t[:, :]), reads=[c.rstd], writes=[c.rstd])

        def modnorm_tile(l, sub, tt, hdst, hbuf):
            t0 = tt * 512
            rstd_tile(lambda kc: X[:, kc, t0:t0 + 512], [c.Xb[kc][tt] for kc in range(8)], EPS * D)
            for kc in range(8):
                tb = c.tmp[kc % 2]
                P.op("dve", lambda e, kc=kc, tb=tb: e.tensor_tensor(tb.t[:, :], X[:, kc, t0:t0 + 512], c.rstd.t[:, :], ALU.mult),
                     reads=[c.Xb[kc][tt], c.rstd], writes=[tb])
                P.op("act", lambda e, kc=kc, tb=tb: e.activation(hdst(kc), tb.t[:, :], AF.Identity,
                                                               bias=Scol(l, sub, kc), scale=Acol(l, sub, kc)),
                     reads=[tb, c.der, c.modt], writes=[hbuf(kc)])

        def epilogue(kind, l, oG, oGb, sgd, sgb):
            wod = din(kind + "_wo", [128, 8192])
            oS, oSb = dsel(kind + "_oS", [1, D, T], BF16, oG.ap()[bass.ds(g4, 1), :, :], oGb)
            sv = sgd.ap().rearrange("(c p) t -> p c t", p=128)
            with ExitStack() as e2:
                wo = P.sb(e2, kind + "wo_sb", [128, 8, 1024], BF16)
                P.dma("pool", wo.t[:, :, :], wod.rearrange("p (k d) -> p k d", k=8), writes=[wo])
                ot = [P.sb(e2, kind + "ot%d" % i, [128, 8, 512], BF16) for i in range(2)]
                st = [P.sb(e2, kind + "st%d" % i, [128, 8, 512], BF16) for i in range(2)]
                ogt = [e2.enter_context(nc.sbuf_tensor(kind + "og%d" % i, [128, 8, 512], BF16)) for i in range(2)]
                ogb = [[Buf(ogt[i]) for _ in range(8)] for i in range(2)]
                if kind == "hgrn":
                    hgd = din("hgn", [128, 8])
                    hg = P.sb(e2, "hg", [128, 8], F32)
                    P.dma("sp", hg.t[:, :], hgd, writes=[hg])
                    P.op("dve", lambda e: e.tensor_scalar(hg.t[:, :], hg.t[:, :], float(np.sqrt(128.0)), None, ALU.mult),
                         reads=[hg], writes=[hg])
                    sq1 = P.sb(e2, "sq1", [128, 512], BF16)
                    r1 = P.sb(e2, "r1", [128, 512], F32)
                    t1 = P.sb(e2, "t1", [128, 512], F32)
                for tt in range(4):
                    t0 = tt * 512
                    o_, s_, og_ = ot[tt % 2], st[tt % 2], ogt[tt % 2]
                    P.dma("sp", o_.t[:, :, :], oS.ap()[0].rearrange("(c p) s -> p c s", p=128)[:, :, t0:t0 + 512], reads=[oSb], writes=[o_])
                    P.dma("sp", s_.t[:, :, :], sv[:, :, t0:t0 + 512], reads=[sgb], writes=[s_])
                    if kind == "fox":
                        for kc in range(8):
                            P.op("dve", lambda e, kc=kc, o_=o_, s_=s_, og_=og_: e.tensor_tensor(
                                og_[:, kc, :], o_.t[:, kc, :], s_.t[:, kc, :], ALU.mult),
                                reads=[o_, s_], writes=[ogb[tt % 2][kc]])
                    else:
                        for kc in range(8):
                            P.op("act", lambda e, kc=kc, o_=o_: e.activation(sq1.t[:, :], o_.t[:, kc, :], AF.Square),
                                 reads=[o_], writes=[sq1])
                            P.op("pe", lambda e: e.matmul(c.bank[5].t[:, :], c.ones.t[:, :], sq1.t[:, :], start=True, stop=True),
                                 reads=[c.ones, sq1], writes=[c.bank[5]])
                            P.op("dve", lambda e: e.tensor_scalar(r1.t[:, :], c.bank[5].t[:, :], EPS * 128.0, None, ALU.add),
                                 reads=[c.bank[5]], writes=[r1])
                            P.op("act", lambda e: e.activation(r1.t[:, :], r1.t[:, :], AF.Sqrt), reads=[r1], writes=[r1])
                            P.op("dve", lambda e: e.reciprocal(r1.t[:, :], r1.t[:, :]), reads=[r1], writes=[r1])
                            P.op("dve", lambda e, kc=kc, o_=o_: e.tensor_tensor(t1.t[:, :], o_.t[:, kc, :], r1.t[:, :], ALU.mult),
                                 reads=[o_, r1], writes=[t1])
                            P.op("dve", lambda e, kc=kc, s_=s_, og_=og_: e.scalar_tensor_tensor(
                                og_[:, kc, :], t1.t[:, :], hg.t[:, kc:kc + 1], s_.t[:, kc, :], ALU.mult, ALU.mult),
                                reads=[t1, hg, s_], writes=[ogb[tt % 2][kc]])
                    for dc in range(8):
                        bk = c.bank[4 + dc % 2]
                        for kc in range(8):
                            P.op("pe", lambda e, kc=kc, dc=dc, bk=bk, og_=og_: e.matmul(
                                bk.t[:, :], wo.t[:, kc, dc * 128:(dc + 1) * 128], og_[:, kc, :],
                                start=(kc == 0), stop=(kc == 7)),
                                reads=[wo, ogb[tt % 2][kc]], writes=[bk], pe_acc=(kc > 0))
                        P.op("dve", lambda e, dc=dc, bk=bk, t0=t0: e.scalar_tensor_tensor(
                            X[:, dc, t0:t0 + 512], bk.t[:, :], Gcol(l, 1, dc), X[:, dc, t0:t0 + 512], ALU.mult, ALU.add),
                            reads=[bk, c.der, c.Xb[dc][tt]], writes=[c.Xb[dc][tt]])
            P.barrier()

        def ffn(j, l, sub):
            wupd = din("wup%d" % j, [11, 128, 4096])
            wdnd = din("wdn%d" % j, [8, 128, 2816])
            with ExitStack() as e2:
                hbt = e2.enter_context(nc.sbuf_tensor("hb_%d" % j, [128, 8, 1024], BF16))
                hbb = [[Buf(hbt) for _ in range(2)] for _ in range(8)]
                actt = e2.enter_context(nc.sbuf_tensor("actb_%d" % j, [128, NF, 1024], BF16))
                actb = [[Buf(actt) for _ in range(2)] for _ in range(NF)]
                wu = [P.sb(e2, "wu%d_%d" % (j, i), [128, 2, 8, 256], BF16) for i in range(2)]
                wd = [P.sb(e2, "wd%d_%d" % (j, i), [128, NF, 128], BF16) for i in range(2)]
                sa = [P.sb(e2, "sa%d_%d" % (j, i), [128, 512], F32) for i in range(2)]
                for half in range(2):
                    for t2 in range(2):
                        tt = half * 2 + t2
                        modnorm_tile(l, sub, tt, lambda kc, t2=t2: hbt[:, kc, t2 * 512:(t2 + 1) * 512],
                                     lambda kc, t2=t2: hbb[kc][t2])
                    it = 0
                    for g in range(11):
                        w_ = wu[g % 2]
                        P.dma("pool", w_.t[:, :, :, :], wupd[g].rearrange("p (a k f) -> p a k f", a=2, k=8), writes=[w_])
                        for jf in range(2):
                            fc = 2 * g + jf
                            for t2 in range(2):
                                bA, bB = c.bank[it % 2], c.bank[2 + it % 2]
                                s_ = sa[it % 2]
                                it += 1
                                for kc in range(8):
                                    P.op("pe", lambda e, kc=kc, w_=w_, jf=jf, t2=t2, bA=bA: e.matmul(
                                        bA.t[:, :], w_.t[:, 0, kc, jf * 128:(jf + 1) * 128], hbt[:, kc, t2 * 512:(t2 + 1) * 512],
                                        start=(kc == 0), stop=(kc == 7)),
                                        reads=[w_, hbb[kc][t2]], writes=[bA], pe_acc=(kc > 0))
                                for kc in range(8):
                                    P.op("pe", lambda e, kc=kc, w_=w_, jf=jf, t2=t2, bB=bB: e.matmul(
                                        bB.t[:, :], w_.t[:, 1, kc, jf * 128:(jf + 1) * 128], hbt[:, kc, t2 * 512:(t2 + 1) * 512],
                                        start=(kc == 0), stop=(kc == 7)),
                                        reads=[w_, hbb[kc][t2]], writes=[bB], pe_acc=(kc > 0))
                                P.op("act", lambda e, s_=s_, bA=bA: e.activation(s_.t[:, :], bA.t[:, :], AF.Silu),
                                     reads=[bA], writes=[s_])
                                P.op("dve", lambda e, s_=s_, bB=bB, fc=fc, t2=t2: e.tensor_tensor(
                                    actt[:, fc, t2 * 512:(t2 + 1) * 512], bB.t[:, :], s_.t[:, :], ALU.mult),
                                    reads=[bB, s_], writes=[actb[fc][t2]])
                    for dc in range(8):
                        w_ = wd[dc % 2]
                        P.dma("pool", w_.t[:, :, :], wdnd[dc].rearrange("p (f d) -> p f d", f=NF), writes=[w_])
                        for t2 in range(2):
                            tt = half * 2 + t2
                            t0 = tt * 512
                            bk = c.bank[4 + (dc * 2 + t2) % 2]
                            for fc in range(NF):
                                P.op("pe", lambda e, fc=fc, w_=w_, t2=t2, bk=bk: e.matmul(
                                    bk.t[:, :], w_.t[:, fc, :], actt[:, fc, t2 * 512:(t2 + 1) * 512],
                                    start=(fc == 0), stop=(fc == NF - 1)),
                                    reads=[w_, actb[fc][t2]], writes=[bk], pe_acc=(fc > 0))
                            P.op("dve", lambda e, dc=dc, bk=bk, t0=t0: e.scalar_tensor_tensor(
                                X[:, dc, t0:t0 + 512], bk.t[:, :], Gcol(l, sub, dc), X[:, dc, t0:t0 + 512], ALU.mult, ALU.add),
                                reads=[bk, c.der, c.Xb[dc][tt]], writes=[c.Xb[dc][tt]])
            P.barrier()

        def proj_fm(wname, hbt, hbb, evac, n_oc=8):
            wd_ = din(wname, [n_oc, 128, 1024])
            with ExitStack() as e3:
                wp = [P.sb(e3, wname + "_sb%d" % i, [128, 8, 128], BF16) for i in range(2)]
                it = 0
                for oc in range(n_oc):
                    w_ = wp[oc % 2]
                    P.dma("pool", w_.t[:, :, :], wd_[oc].rearrange("p (k f) -> p k f", k=8), writes=[w_])
                    for tt in range(4):
                        bk = c.bank[it % 4]
                        it += 1
                        for kc in range(8):
                            P.op("pe", lambda e, kc=kc, w_=w_, tt=tt, bk=bk: e.matmul(
                                bk.t[:, :], w_.t[:, kc, :], hbt[:, kc, tt * 512:(tt + 1) * 512],
                                start=(kc == 0), stop=(kc == 7)),
                                reads=[w_, hbb[kc][tt]], writes=[bk], pe_acc=(kc > 0))
                        evac(oc, tt, bk)
                P.barrier()

        def proj_tm(wname, hbt, hbb, vout, vob, func):
            wd_ = din(wname, [2, 128, 4096])
            with ExitStack() as e3:
                wv = P.sb(e3, wname + "_sb", [128, 2, 8, 512], BF16)
                for cg in range(2):
                    P.dma("pool", wv.t[:, cg, :, :], wd_[cg].rearrange("p (k f) -> p k f", k=8), writes=[wv])
                vt = [P.sb(e3, wname + "vt%d" % i, [128, 512], BF16) for i in range(2)]
                it = 0
                for tk in range(16):
                    for cg in range(2):
                        bk = c.bank[it % 4]
                        v_ = vt[it % 2]
                        it += 1
                        for kc in range(8):
                            P.op("pe", lambda e, kc=kc, tk=tk, cg=cg, bk=bk: e.matmul(
                                bk.t[:, :], hbt[:, kc, tk * 128:(tk + 1) * 128], wv.t[:, cg, kc, :],
                                start=(kc == 0), stop=(kc == 7)),
                                reads=[wv, hbb[kc][tk // 4]], writes=[bk], pe_acc=(kc > 0))
                        P.op("act", lambda e, bk=bk, v_=v_: e.activation(v_.t[:, :], bk.t[:, :], func),
                             reads=[bk], writes=[v_])
                        P.dma("sp", vout[tk * 128:(tk + 1) * 128, cg * 512:(cg + 1) * 512], v_.t[:, :], reads=[v_], ow=vob)
                P.barrier()

        def stage_out(e3, name, shape, dt):
            return [P.sb(e3, name + "%d" % i, shape, dt) for i in range(2)]

        def projections(kind, l):
            R = Ctx()
            with ExitStack() as e2:
                hbt = e2.enter_context(nc.sbuf_tensor(kind + "hb2", [128, 8, T], BF16))
                hbb = [[Buf(hbt) for _ in range(4)] for _ in range(8)]
                for tt in range(4):
                    modnorm_tile(l, 1, tt, lambda kc, tt=tt: hbt[:, kc, tt * 512:(tt + 1) * 512],
                                 lambda kc, tt=tt: hbb[kc][tt])
                kdt = BF16 if kind == "fox" else F32
                R.q, R.qb = idram(kind + "_q", [D, T], BF16), Buf()
                R.k, R.kb = idram(kind + "_k", [D, T], kdt), Buf()
                R.sg, R.sgb = idram(kind + "_sg", [D, T], BF16), Buf()
                R.v, R.vb = idram(kind + "_v", [T, D], BF16), Buf()
                qo, ko, sgo, vo = R.q.ap(), R.k.ap(), R.sg.ap(), R.v.ap()
                qob, kob, sgob, vob = R.qb, R.kb, R.sgb, R.vb
                cnt = [0]

                def simple_evac(od, ob, func, scale, st):
                    def evac(oc, tt, bk):
                        s_ = st[cnt[0] % 2]
                        cnt[0] += 1
                        P.op("act", lambda e, s_=s_, bk=bk: e.activation(s_.t[:, :], bk.t[:, :], func, scale=scale),
                             reads=[bk], writes=[s_])
                        P.dma("sp", od[oc * 128:(oc + 1) * 128, tt * 512:(tt + 1) * 512], s_.t[:, :], reads=[s_], ow=ob)
                    return evac

                stb = stage_out(e2, kind + "stb", [128, 512], BF16)
                if kind == "fox":
                    R.lf, R.lfb = idram("fox_lf", [128, 256], F32), Buf()
                    proj_fm("fox_wq", hbt, hbb, simple_evac(qo, qob, AF.Copy, float(FD ** -0.5), stb))
                    proj_fm("fox_wk", hbt, hbb, simple_evac(ko, kob, AF.Copy, 1.0, stb))
                    proj_fm("fox_wg", hbt, hbb, simple_evac(sgo, sgob, AF.Sigmoid, 1.0, stb))
                    proj_tm("fox_wv", hbt, hbb, vo, vob, AF.Copy)
                    wfd = din("fox_wf", [128, 128])
                    bfd = din("fox_bfb", [128, 256])
                    wf = P.sb(e2, "wf_sb", [128, 8, 16], BF16)
                    P.dma("pool", wf.t[:, :, :], wfd.rearrange("p (k f) -> p k f", k=8), writes=[wf])
                    bfb = P.sb(e2, "bfb", [128, 256], F32)
                    P.dma("sp", bfb.t[:, :], bfd, writes=[bfb])
                    z1 = P.sb(e2, "z1", [128, 256], F32)
                    bk = c.bank[0]
                    for tk in range(16):
                        for kc in range(8):
                            P.op("pe", lambda e, kc=kc, tk=tk: e.matmul(
                                bk.t[:, tk * 16:(tk + 1) * 16], hbt[:, kc, tk * 128:(tk + 1) * 128], wf.t[:, kc, :],
                                start=(kc == 0), stop=(kc == 7)),
                                reads=[wf, hbb[kc][tk // 4]], writes=[bk], pe_acc=not (tk == 0 and kc == 0))
                    P.op("dve", lambda e: e.tensor_tensor(z1.t[:, :], bk.t[:, 0:256], bfb.t[:, :], ALU.add), reads=[bk, bfb], writes=[z1])
                    P.op("act", lambda e: e.activation(z1.t[:, :], z1.t[:, :], AF.Exp, scale=-1.0), reads=[z1], writes=[z1])
                    P.op("act", lambda e: e.activation(z1.t[:, :], z1.t[:, :], AF.Ln, bias=1.0, scale=1.0), reads=[z1], writes=[z1])
                    P.op("dve", lambda e: e.tensor_scalar(z1.t[:, :], z1.t[:, :], -1.0, None, ALU.mult), reads=[z1], writes=[z1])
                    P.dma("sp", R.lf.ap(), z1.t[:, :], reads=[z1], ow=R.lfb)
                else:
                    R.lf, R.lfb = idram("hgrn_lf", [D, T], F32), Buf()
                    lfo, lfob = R.lf.ap(), R.lfb
                    lbd = din("lbl", [128, 16])
                    lbl = P.sb(e2, "lbl_sb", [128, 16], F32)
                    lb = P.sb(e2, "lb", [128, 8], F32)
                    oml = P.sb(e2, "oml", [128, 8], F32)
                    P.dma("sp", lbl.t[:, :], lbd, writes=[lbl])
                    P.op("dve", lambda e: e.tensor_tensor(lb.t[:, :], lbl.t[:, 8:16], lbl.t[:, 0:8], ALU.subtract), reads=[lbl], writes=[lb])
                    P.op("act", lambda e: e.activation(lb.t[:, :], lb.t[:, :], AF.Sigmoid), reads=[lb], writes=[lb])
                    P.op("dve", lambda e: e.tensor_scalar(oml.t[:, :], lb.t[:, :], -1.0, 1.0, ALU.mult, ALU.add), reads=[lb], writes=[oml])
                    proj_fm("hgrn_wq", hbt, hbb, simple_evac(qo, qob, AF.Copy, 1.0, stb))
                    proj_fm("hgrn_wg", hbt, hbb, simple_evac(sgo, sgob, AF.Silu, 1.0, stb))
                    proj_tm("hgrn_wv", hbt, hbb, vo, vob, AF.Silu)
                    sg1 = P.sb(e2, "sg1", [128, 512], F32)
                    ff = stage_out(e2, "ff", [128, 512], F32)
                    lff = stage_out(e2, "lff", [128, 512], F32)
                    kk = stage_out(e2, "kk", [128, 512], F32)

                    def f_evac(oc, tt, bk):
                        i = cnt[0] % 2
                        cnt[0] += 1
                        f_, l_, k_ = ff[i], lff[i], kk[i]
                        P.op("act", lambda e, bk=bk: e.activation(sg1.t[:, :], bk.t[:, :], AF.Sigmoid), reads=[bk], writes=[sg1])
                        P.op("dve", lambda e, f_=f_, oc=oc: e.tensor_scalar(f_.t[:, :], sg1.t[:, :], oml.t[:, oc:oc + 1], lb.t[:, oc:oc + 1], ALU.mult, ALU.add),
                             reads=[sg1, oml, lb], writes=[f_])
                        P.op("act", lambda e, f_=f_, l_=l_: e.activation(l_.t[:, :], f_.t[:, :], AF.Ln), reads=[f_], writes=[l_])
                        P.dma("sp", lfo[oc * 128:(oc + 1) * 128, tt * 512:(tt + 1) * 512], l_.t[:, :], reads=[l_], ow=lfob)
                    proj_fm("hgrn_wf", hbt, hbb, f_evac)
            P.barrier()
            return R

        def gather(name, src, srcb, nch, rows, cols, dt):
            dst = idram(name, [nch, 4 * rows, cols], dt)
            db = Buf()
            sv = src.ap() if len(src.shape) == 2 else None
            for j in range(nch):
                sa = src.ap()[j * rows:(j + 1) * rows, :] if sv is not None else src.ap()[j]
                P.collective("AllGather", G4, sa.opt(), dst.ap()[j].opt(), [srcb], db)
            return dst, db

        def dsel(name, shape, dt, src_dyn, srcb):
            dst = idram(name, shape, dt)
            db = Buf()
            P.dma("sp", dst.ap(), src_dyn, reads=[srcb], writes=[db])
            return dst, db

        def fox_phase(R):
            o_loc, olb = idram("fox_o", [4, 256, T], BF16), Buf()
            shi, slo = idram("shi", [4, S], BF16), idram("slo", [4, S], BF16)
            shb, slb = Buf(), Buf()
            Ud, seld, mkd, idfd = din("U", [128, 128]), din("sel", [128, 128]), din("mk", [128, 128]), din("identf", [128, 128])
            bank = c.bank
            with ExitStack() as e2:
                U = P.sb(e2, "U_sb", [128, 128], F32)
                sel = P.sb(e2, "sel_sb", [128, 128], F32)
                mk = P.sb(e2, "mk_sb", [128, 128], F32)
                idf = P.sb(e2, "idf_sb", [128, 128], F32)
                onesf = P.sb(e2, "onesf", [128, 128], F32)
                negB = P.sb(e2, "negB", [128, 4 * 16 * 64], F32)
                for t_, d_ in ((U, Ud), (sel, seld), (mk, mkd), (idf, idfd)):
                    P.dma("sp", t_.t[:, :], d_, writes=[t_])
                P.op("pool", lambda e: e.memset(onesf.t[:, :], 1.0), writes=[onesf])
                lG, lGb = gather("fox_lG", R.lf, R.lfb, 1, 128, 256, F32)
                qG, qGb = gather("fox_qG", R.q, R.qb, 4, 256, T, BF16)
                kG, kGb = gather("fox_kG", R.k, R.kb, 4, 256, T, BF16)
                vG, vGb = gather("fox_vG", R.v, R.vb, 4, 512, D, BF16)
                lS, lSb = dsel("fox_lS", [512, 16, 4], F32, lG.ap()[0].rearrange("r (k h) -> r k h", h=16)[:, :, bass.ds(g4 * 4, 4)], lGb)
                with ExitStack() as e3:
                    lsel = P.sb(e3, "lsel", [128, 4, 16, 4], F32)
                    lt = P.sb(e3, "lt_sb", [128, 256], F32)
                    within = P.sb(e3, "within", [128, 256], F32)
                    tot = P.sb(e3, "tot", [128, 256], F32)
                    inc = P.sb(e3, "inc", [128, 256], F32)
                    GT = P.sb(e3, "GT", [128, 256], F32)
                    gend = P.sb(e3, "gend", [128, 256], F32)
                    Aa = P.sb(e3, "Aa", [128, 256], F32)
                    AT = P.sb(e3, "AT", [64, 512], F32)
                    ahi = P.sb(e3, "ahi", [64, 512], BF16)
                    ahf = P.sb(e3, "ahf", [64, 512], F32)
                    alo = P.sb(e3, "alo", [64, 512], BF16)
                    for t in range(4):
                        P.dma("sp", lsel.t[:, t, :, :], lS.ap()[t * 128:(t + 1) * 128, :, :], reads=[lSb], writes=[lsel])
                    for hl in range(4):
                        P.op("dve", lambda e, hl=hl: e.tensor_copy(
                            lt.t[:, hl * 64:(hl + 1) * 64].rearrange("p (t k) -> p t k", t=4), lsel.t[:, :, :, hl]),
                            reads=[lsel], writes=[lt])
                    P.op("pe", lambda e: e.matmul(bank[6].t[:, 0:256], U.t[:, :], lt.t[:, :], start=True, stop=True), reads=[U, lt], writes=[bank[6]])
                    P.op("pe", lambda e: e.matmul(bank[5].t[:, 0:256], onesf.t[:, :], lt.t[:, :], start=True, stop=True), reads=[onesf, lt], writes=[bank[5]])
                    P.op("dve", lambda e: e.tensor_copy(within.t[:, :], bank[6].t[:, 0:256]), reads=[bank[6]], writes=[within])
                    P.op("dve", lambda e: e.tensor_copy(tot.t[:, :], bank[5].t[:, 0:256]), reads=[bank[5]], writes=[tot])
                    for h in range(4):
                        P.op("dve", lambda e, h=h: e.tensor_tensor_scan(inc.t[:, h * 64:(h + 1) * 64], onesf.t[:, 0:64], tot.t[:, h * 64:(h + 1) * 64],
                                                                        0.0, ALU.mult, ALU.add), reads=[onesf, tot], writes=[inc])
                    P.op("dve", lambda e: e.tensor_tensor(GT.t[:, :], within.t[:, :], inc.t[:, :], ALU.add), reads=[within, inc], writes=[GT])
                    P.op("dve", lambda e: e.tensor_tensor(GT.t[:, :], GT.t[:, :], tot.t[:, :], ALU.subtract), reads=[GT, tot], writes=[GT])
                    P.op("pe", lambda e: e.matmul(bank[6].t[:, 0:256], sel.t[:, :], GT.t[:, :], start=True, stop=True), reads=[sel, GT], writes=[bank[6]])
                    P.op("dve", lambda e: e.tensor_copy(gend.t[:, :], bank[6].t[:, 0:256]), reads=[bank[6]], writes=[gend])
                    for h in range(4):
                        for Q in range(16):
                            j0 = (h * 16 + Q) * 64
                            gc = h * 64 + 4 * Q + 3
                            P.op("dve", lambda e, h=h, j0=j0, gc=gc: e.tensor_scalar(
                                negB.t[:, j0:j0 + 64], GT.t[:, h * 64:(h + 1) * 64], -1.0, gend.t[:, gc:gc + 1], ALU.mult, ALU.add),
                                reads=[GT, gend], writes=[negB])
                            a0 = h * 64 + 4 * Q
                            P.op("dve", lambda e, a0=a0, gc=gc: e.tensor_scalar(
                                Aa.t[:, a0:a0 + 4], GT.t[:, a0:a0 + 4], gend.t[:, gc:gc + 1], None, ALU.subtract),
                                reads=[GT, gend], writes=[Aa])
                    for h in range(4):
                        P.op("pe", lambda e, h=h: e.matmul(bank[5].t[0:64, h * 128:(h + 1) * 128], Aa.t[:, h * 64:(h + 1) * 64], idf.t[:, :],
                                                           start=True, stop=True), reads=[Aa, idf], writes=[bank[5]], pe_acc=(h > 0))
                    P.op("dve", lambda e: e.tensor_copy(AT.t[:, :], bank[5].t[0:64, :]), reads=[bank[5]], writes=[AT])
                    P.op("dve", lambda e: e.tensor_copy(ahi.t[:, :], AT.t[:, :]), reads=[AT], writes=[ahi])
                    P.op("dve", lambda e: e.tensor_copy(ahf.t[:, :], ahi.t[:, :]), reads=[ahi], writes=[ahf])
                    P.op("dve", lambda e: e.tensor_tensor(alo.t[:, :], AT.t[:, :], ahf.t[:, :], ALU.subtract), reads=[AT, ahf], writes=[alo])
                    P.dma("sp", shi.ap().rearrange("h (k p) -> k h p", p=128), ahi.t[:, :].rearrange("k (h p) -> k h p", h=4), reads=[ahi], writes=[shb])
                    P.dma("sp", slo.ap().rearrange("h (k p) -> k h p", p=128), alo.t[:, :].rearrange("k (h p) -> k h p", h=4), reads=[alo], writes=[slb])
                qS, qSb = dsel("fox_qS", [1, D, T], BF16, qG.ap()[bass.ds(g4, 1), :, :], qGb)
                kS, kSb = dsel("fox_kS", [1, D, T], BF16, kG.ap()[bass.ds(g4, 1), :, :], kGb)
                vS, vSb = idram("fox_vS", [S, 256], BF16), Buf()
                for j in range(4):
                    P.dma("sp", vS.ap().rearrange("(r j i) c -> j r i c", r=4, j=4)[j],
                          vG.ap()[j].rearrange("(r i) c -> r i c", r=4)[:, :, bass.ds(g4 * 256, 256)], reads=[vGb], writes=[vSb])
                P.barrier()
                qa = [P.sb(e2, "qa%d" % i, [128, S], BF16) for i in range(2)]
                ka = [P.sb(e2, "ka%d" % i, [128, S], BF16) for i in range(2)]
                va = [P.sb(e2, "va%d" % i, [128, 64 * 65 + 64], BF16) for i in range(2)]
                pt = [P.sb(e2, "pt%d" % i, [128, 512], BF16) for i in range(4)]
                sbanks = [bank[0], bank[1], bank[2], bank[6]]
                drow = P.sb(e2, "drow", [65, 512], F32)
                rec = P.sb(e2, "rec", [64, 512], F32)
                oo = [P.sb(e2, "oo%d" % i, [64, 512], BF16) for i in range(2)]
                vv = lambda v_: v_.t[:, 0:64 * 65].rearrange("p (t d) -> p t d", d=65)
                for i in range(2):
                    P.op("pool", lambda e, i=i: e.memset(ka[i].t[64:128, :], 0.0), writes=[ka[i]])
                    P.op("pool", lambda e, i=i: e.memset(qa[i].t[64:128, :], 0.0), writes=[qa[i]])
                    P.op("pool", lambda e, i=i: e.memset(ka[i].t[64:66, :], 1.0), writes=[ka[i]])
                    P.op("pool", lambda e, i=i: e.memset(va[i].t[:, :], 0.0), writes=[va[i]])
                    P.op("pool", lambda e, i=i: e.memset(vv(va[i])[:, :, 64:65], 1.0), writes=[va[i]])
                vGv = vS.ap().rearrange("(k p) d -> p k d", p=128)

                def load_head(h):
                    q_, k_, v_ = qa[h % 2], ka[h % 2], va[h % 2]
                    for t in range(4):
                        P.dma("sp", q_.t[0:64, t * T:(t + 1) * T], qS.ap()[0, t * 256 + h * 64:t * 256 + (h + 1) * 64, :], reads=[qSb], writes=[q_])
                        P.dma("pool", k_.t[0:64, t * T:(t + 1) * T], kS.ap()[0, t * 256 + h * 64:t * 256 + (h + 1) * 64, :], reads=[kSb], writes=[k_])
                    P.dma("sp", q_.t[64:65, :], shi.ap()[h:h + 1, :], reads=[shb], writes=[q_])
                    P.dma("sp", q_.t[65:66, :], slo.ap()[h:h + 1, :], reads=[slb], writes=[q_])
                    P.dma("pool", vv(v_)[:, :, 0:64], vGv[:, :, h * 64:(h + 1) * 64], reads=[vSb], writes=[v_])

                load_head(0)

                def do_head(h, q_, k_, v_, nit):
                    items = [(Q, kt) for Q in range(16) for kt in range(4 * Q + 4)]

                    def emit_S(idx, it_no):
                        Q, kt = items[idx]
                        d = kt - 4 * Q
                        c0 = 128 * d if d >= 0 else 0
                        bk = sbanks[it_no % 4]
                        p_ = pt[it_no % 4]
                        P.op("pe", lambda e: e.matmul(bk.t[:, c0:512], k_.t[0:128, kt * 128:(kt + 1) * 128],
                                                      q_.t[0:128, Q * 512 + c0:(Q + 1) * 512], start=True, stop=True),
                             reads=[k_, q_], writes=[bk])
                        if d >= 0:
                            P.op("dve", lambda e: e.tensor_tensor(bk.t[:, c0:c0 + 128], bk.t[:, c0:c0 + 128], mk.t[:, :], ALU.add),
                                 reads=[bk, mk], writes=[bk])
                        jb = (h * 16 + Q) * 64 + kt
                        P.op("act", lambda e: e.activation(p_.t[:, c0:512], bk.t[:, c0:512], AF.Exp, bias=negB.t[:, jb:jb + 1], scale=1.0),
                             reads=[bk, negB], writes=[p_])

                    def emit_PV(idx, it_no):
                        Q, kt = items[idx]
                        d = kt - 4 * Q
                        c0 = 128 * d if d >= 0 else 0
                        p_ = pt[it_no % 4]
                        ob_ = bank[3 + Q % 2]
                        last = (kt == 4 * Q + 3)
                        P.op("pe", lambda e: e.matmul(ob_.t[0:128, c0:512], v_.t[:, kt * 65:kt * 65 + 128], p_.t[:, c0:512], start=(kt == 0), stop=last),
                             reads=[v_, p_], writes=[ob_], pe_acc=(kt > 0))
                        if last:
                            o_ = oo[Q % 2]
                            P.op("act", lambda e: e.activation(drow.t[64:65, :], ob_.t[64:65, :], AF.Copy), reads=[ob_], writes=[drow])
                            P.op("pe", lambda e: e.matmul(bank[5].t[0:64, :], onesf.t[64:65, 0:64], drow.t[64:65, :], start=True, stop=True),
                                 reads=[onesf, drow], writes=[bank[5]])
                            P.op("dve", lambda e: e.reciprocal(rec.t[:, :], bank[5].t[0:64, :]), reads=[bank[5]], writes=[rec])
                            P.op("dve", lambda e: e.tensor_tensor(o_.t[:, :], ob_.t[0:64, :], rec.t[:, :], ALU.mult), reads=[ob_, rec], writes=[o_])
                            P.dma("sp", o_loc.ap()[Q // 4][h * 64:(h + 1) * 64, (Q % 4) * 512:(Q % 4 + 1) * 512], o_.t[:, :], reads=[o_], ow=olb)
                            if h == 3 and Q % 4 == 3:
                                P.collective("AllGather", G4, o_loc.ap()[Q // 4].opt(), oGf.ap()[Q // 4].opt(), [olb], oGfb)

                    n = len(items)
                    emit_S(0, nit)
                    emit_S(1, nit + 1)
                    for idx in range(n):
                        if idx + 2 < n:
                            emit_S(idx + 2, nit + idx + 2)
                        emit_PV(idx, nit + idx)
                    return nit + n

                nit = 0
                oGf = idram("fox_oG", [4, 4 * 256, T], BF16)
                oGfb = Buf()
                for h in range(4):
                    if h + 1 < 4:
                        load_head(h + 1)
                    nit = do_head(h, qa[h % 2], ka[h % 2], va[h % 2], nit)
            P.barrier()
            return oGf, oGfb

        def hgrn_phase(R):
            qG, qGb = gather("hg_qG", R.q, R.qb, 4, 256, T, BF16)
            lG, lGb = gather("hg_lG", R.lf, R.lfb, 8, 128, T, F32)
            vG, vGb = gather("hg_vG", R.v, R.vb, 4, 512, D, BF16)
            qS, qSb = dsel("hg_qS", [1, D, T], BF16, qG.ap()[bass.ds(g4, 1), :, :], qGb)
            lS, lSb = dsel("hg_lS", [2, 512, T], F32, lG.ap()[bass.ds(g4 * 2, 2), :, :], lGb)
            vS, vSb = idram("hg_vS", [S, 256], BF16), Buf()
            for j in range(4):
                P.dma("sp", vS.ap().rearrange("(r j i) c -> j r i c", r=4, j=4)[j],
                      vG.ap()[j].rearrange("(r i) c -> r i c", r=4)[:, :, bass.ds(g4 * 256, 256)], reads=[vGb], writes=[vSb])
            o_loc, olb = idram("hg_o", [4, 256, T], BF16), Buf()
            m01d, rmd, idd = din("m01", [128, 64]), din("rm", [128, 2048]), din("ident", [128, 128], BF16)
            NB = 2048
            bankA, bankO, bankU = c.bank[0:2], c.bank[2:4], c.bank[4:6]
            with ExitStack() as e2:
                m01 = P.sb(e2, "m01_sb", [128, 64], F32)
                rm = P.sb(e2, "rm_sb", [128, NB], F32)
                ident = P.sb(e2, "ident_sb", [128, 128], BF16)
                P.dma("sp", m01.t[:, :], m01d, writes=[m01])
                P.dma("sp", rm.t[:, :], rmd, writes=[rm])
                P.dma("sp", ident.t[:, :], idd, writes=[ident])
                sh = Ctx()
                sh.qb = P.sb(e2, "hqb", [128, NB], BF16)
                sh.kb = P.sb(e2, "hkb", [128, NB], F32)
                sh.lf = P.sb(e2, "hlf", [128, NB], F32)
                sh.G = P.sb(e2, "hG", [128, NB], F32)
                sh.tmp = P.sb(e2, "htmp", [128, NB], F32)
                sh.tmp2 = P.sb(e2, "htmp2", [128, NB], F32)
                sh.kend = P.sb(e2, "hkend", [128, NB], BF16)
                hs = []
                for h in range(2):
                    o = Ctx()
                    o.qd = P.sb(e2, "hqd%d" % h, [128, NB], BF16)
                    o.kdd = P.sb(e2, "hkdd%d" % h, [128, NB], BF16)
                    o.kT = P.sb(e2, "hkT%d" % h, [128, 16, 128], BF16)
                    o.vb = P.sb(e2, "hvb%d" % h, [128, 16, 128], BF16)
                    o.egl = P.sb(e2, "hegl%d" % h, [128, 32], F32)
                    o.S32 = P.sb(e2, "hS32_%d" % h, [128, 128], F32)
                    o.Sbf = [P.sb(e2, "hSbf%d_%d" % (h, i), [128, 128], BF16) for i in range(2)]
                    o.am = [P.sb(e2, "ham%d_%d" % (h, i), [128, 64], BF16) for i in range(2)]
                    o.osb = [P.sb(e2, "hosb%d_%d" % (h, i), [128, 512], BF16) for i in range(2)]
                    P.op("pool", lambda e, o=o: e.memset(o.S32.t[:, :], 0.0), writes=[o.S32])
                    P.op("pool", lambda e, o=o: e.memset(o.Sbf[0].t[:, :], 0.0), writes=[o.Sbf[0]])
                    o.si = 0
                    hs.append(o)
                vGv = vS.ap().rearrange("(t p) d -> p t d", p=128)

                def prep(h, blk):
                    o = hs[h]
                    P.dma("sp", sh.qb.t[:, :], qS.ap()[0, blk * 256 + h * 128:blk * 256 + (h + 1) * 128, :], reads=[qSb], writes=[sh.qb])
                    P.dma("sp", sh.lf.t[:, :], lS.ap()[h, blk * 128:(blk + 1) * 128, :], reads=[lSb], writes=[sh.lf])
                    P.dma("pool", o.vb.t[:, :, :], vGv[:, blk * 16:(blk + 1) * 16, h * 128:(h + 1) * 128], reads=[vSb], writes=[o.vb])
                    P.op("act", lambda e: e.activation(sh.kb.t[:, :], sh.lf.t[:, :], AF.Exp), reads=[sh.lf], writes=[sh.kb])
                    P.op("dve", lambda e: e.tensor_scalar(sh.kb.t[:, :], sh.kb.t[:, :], -1.0, 1.0, ALU.mult, ALU.add), reads=[sh.kb], writes=[sh.kb])
                    P.op("dve", lambda e: e.tensor_tensor_scan(sh.G.t[:, :], rm.t[:, :], sh.lf.t[:, :], 0.0, ALU.mult, ALU.add),
                         reads=[rm, sh.lf], writes=[sh.G])
                    P.op("act", lambda e: e.activation(sh.tmp.t[:, :], sh.G.t[:, :], AF.Exp), reads=[sh.G], writes=[sh.tmp])
                    P.op("dve", lambda e: e.tensor_tensor(o.qd.t[:, :], sh.qb.t[:, :], sh.tmp.t[:, :], ALU.mult), reads=[sh.qb, sh.tmp], writes=[o.qd])
                    P.op("act", lambda e: e.activation(sh.tmp2.t[:, :], sh.G.t[:, :], AF.Exp, scale=-1.0), reads=[sh.G], writes=[sh.tmp2])
                    P.op("dve", lambda e: e.tensor_tensor(sh.tmp2.t[:, :], sh.kb.t[:, :], sh.tmp2.t[:, :], ALU.mult), reads=[sh.kb, sh.tmp2], writes=[sh.tmp2])
                    P.op("dve", lambda e: e.tensor_copy(o.kdd.t[:, :], sh.tmp2.t[:, :]), reads=[sh.tmp2], writes=[o.kdd])
                    G3 = sh.G.t[:, :].rearrange("p (c s) -> p c s", s=64)
                    P.op("act", lambda e: e.activation(o.egl.t[:, :], G3[:, :, 63], AF.Exp), reads=[sh.G], writes=[o.egl])
                    for cc in range(32):
                        P.op("dve", lambda e, cc=cc: e.tensor_scalar(sh.kend.t[:, cc * 64:(cc + 1) * 64], sh.tmp2.t[:, cc * 64:(cc + 1) * 64],
                                                                     o.egl.t[:, cc:cc + 1], None, ALU.mult),
                             reads=[sh.tmp2, o.egl], writes=[sh.kend])
                    for grp in range(2):
                        for j in range(8):
                            tk = grp * 8 + j
                            P.op("pe", lambda e, tk=tk, j=j: e.transpose(bankT.t[:, j * 128:(j + 1) * 128], sh.kend.t[:, tk * 128:(tk + 1) * 128], ident.t[:, :]),
                                 reads=[sh.kend, ident], writes=[bankT], pe_acc=(j > 0))
                        P.op("act", lambda e, grp=grp: e.activation(o.kT.t[:, grp * 8:(grp + 1) * 8, :],
                                                                   bankT.t[:, :].rearrange("p (t k) -> p t k", k=128), AF.Copy),
                             reads=[bankT], writes=[o.kT])

                nA = [0]

                def chunk(h, blk, cc):
                    o = hs[h]
                    tk, half = cc // 2, cc % 2
                    pb = 64 * half
                    gc = blk * 32 + cc
                    cs = slice(cc * 64, (cc + 1) * 64)
                    bA = bankA[nA[0] % 2]
                    am = o.am[nA[0] % 2]
                    nA[0] += 1
                    bO = bankO[h]
                    bU = bankU[h]
                    oc0 = (gc % 8) * 64
                    Sb = o.Sbf[o.si % 2]
                    Sn = o.Sbf[(o.si + 1) % 2]
                    o.si += 1
                    P.op("pe", lambda e: e.matmul(bA.t[pb:pb + 64, 0:64], o.kdd.t[:, cs], o.qd.t[:, cs], start=True, stop=True),
                         reads=[o.kdd, o.qd], writes=[bA])
                    P.op("dve", lambda e: e.tensor_tensor(am.t[pb:pb + 64, :], bA.t[pb:pb + 64, 0:64], m01.t[pb:pb + 64, :], ALU.mult),
                         reads=[bA, m01], writes=[am])
                    P.op("pe", lambda e: e.matmul(bO.t[:, oc0:oc0 + 64], Sb.t[:, :], o.qd.t[:, cs], start=True, stop=False),
                         reads=[Sb, o.qd], writes=[bO], pe_acc=(gc % 8 != 0))
                    P.op("pe", lambda e: e.matmul(bO.t[:, oc0:oc0 + 64], o.vb.t[pb:pb + 64, tk, :], am.t[pb:pb + 64, :], start=False, stop=True),
                         reads=[o.vb, am], writes=[bO], pe_acc=True)
                    P.op("pe", lambda e: e.matmul(bU.t[:, 0:128], o.kT.t[pb:pb + 64, tk, :], o.vb.t[pb:pb + 64, tk, :], start=True, stop=True),
                         reads=[o.kT, o.vb], writes=[bU])
                    P.op("dve", lambda e: e.scalar_tensor_tensor(o.S32.t[:, :], o.S32.t[:, :], o.egl.t[:, cc:cc + 1], bU.t[:, 0:128], ALU.mult, ALU.add),
                         reads=[o.S32, o.egl, bU], writes=[o.S32])
                    P.op("act", lambda e: e.activation(Sn.t[:, :], o.S32.t[:, :], AF.Copy), reads=[o.S32], writes=[Sn])
                    if gc % 8 == 7:
                        os_ = o.osb[(gc // 8) % 2]
                        P.op("act", lambda e: e.activation(os_.t[:, :], bO.t[:, :], AF.Copy), reads=[bO], writes=[os_])
                        tok0 = (gc - 7) * 64
                        P.dma("sp", o_loc.ap()[tok0 // T][h * 128:(h + 1) * 128, tok0 % T:tok0 % T + 512], os_.t[:, :], reads=[os_], ow=olb)

                oG = idram("hg_oG", [4, 4 * 256, T], BF16)
                oGb = Buf()
                for blk in range(4):
                    for h in range(2):
                        prep(h, blk)
                    for cc in range(32):
                        for h in range(2):
                            chunk(h, blk, cc)
                    P.collective("AllGather", G4, o_loc.ap()[blk].opt(), oG.ap()[blk].opt(), [olb], oGb)
            P.barrier()
            return oG, oGb

        ffn(0, 0, 0)
        R1 = projections("fox", 0)
        oG1, oG1b = fox_phase(R1)
        epilogue("fox", 0, oG1, oG1b, R1.sg, R1.sgb)
        ffn(1, 0, 2)
        ffn(2, 1, 0)
        R2 = projections("hgrn", 1)
        oG2, oG2b = hgrn_phase(R2)
        epilogue("hgrn", 1, oG2, oG2b, R2.sg, R2.sgb)
        ffn(3, 1, 2)
        xo = dout("xo", [D, T])
        xob = Buf()
        outs.append(xob)
        xov = xo.rearrange("(c p) t -> p c t", p=128)
        fgd = din("fg", [128, 8])
        fg = P.sb(es, "fg_sb", [128, 8], F32)
        P.dma("sp", fg.t[:, :], fgd, writes=[fg])
        P.op("dve", lambda e: e.tensor_scalar(fg.t[:, :], fg.t[:, :], SQD, None, ALU.mult), reads=[fg], writes=[fg])
        yo = [P.sb(es, "yo%d" % i, [128, 512], F32) for i in range(2)]
        it = 0
        for tt in range(4):
            t0 = tt * 512
            rstd_tile(lambda kc, t0=t0: X[:, kc, t0:t0 + 512], [c.Xb[kc][tt] for kc in range(8)], EPS * D)
            for kc in range(8):
                y_ = yo[it % 2]
                it += 1
                P.op("dve", lambda e, kc=kc, y_=y_, t0=t0: e.scalar_tensor_tensor(
                    y_.t[:, :], X[:, kc, t0:t0 + 512], fg.t[:, kc:kc + 1], c.rstd.t[:, :], ALU.mult, ALU.mult),
                    reads=[c.Xb[kc][tt], fg, c.rstd], writes=[y_])
                P.dma("sp", xov[:, kc, t0:t0 + 512], y_.t[:, :], reads=[y_], ow=xob)
        P.finish(outs)
        P.emit()
    return nc


_DBG = {}


def _run(nc, maps):
    return run_bass_kernel_spmd(nc, maps, core_ids=list(range(NCORES))).results


def kernel_unfused(x, c, ada_w, ada_b, norm_g, ffn_w_up, ffn_w_down, fox_w_in, fox_b_f, fox_w_out,
           hgrn_w_in, hgrn_norm_g, hgrn_w_out, hgrn_lb_logits, final_norm_g):
    f32 = lambda a: np.ascontiguousarray(np.asarray(a, dtype=np.float32))
    x, c, ada_w, ada_b, norm_g = f32(x), f32(c), f32(ada_w), f32(ada_b), f32(norm_g)
    ffn_w_up, ffn_w_down, fox_w_in, fox_b_f, fox_w_out = f32(ffn_w_up), f32(ffn_w_down), f32(fox_w_in), f32(fox_b_f), f32(fox_w_out)
    hgrn_w_in, hgrn_norm_g, hgrn_w_out = f32(hgrn_w_in), f32(hgrn_norm_g), f32(hgrn_w_out)
    hgrn_lb_logits, final_norm_g = f32(hgrn_lb_logits), f32(final_norm_g)

    mod = run_mod(c, ada_w, ada_b)
    _DBG["mod"] = mod
    modT = [fm_cols(mod[b].reshape(18, D)) for b in range(B)]
    gT = fm_cols(norm_g.reshape(6, D))
    cores = [(b, t) for b in range(B) for t in range(4)]

    nc1 = build_F({"ffns": [(0, 0)], "proj": ("fox", 0)})
    wi = fox_w_in[0]
    shared = {
        "gT": gT, "wup0": tile_wup(ffn_w_up[0, 0]), "wdn0": tile_wdn(ffn_w_down[0, 0]),
        "wq": tile_w_fm(wi[:, 0:D]), "wk": tile_w_fm(wi[:, D:2 * D]), "wg": tile_w_fm(wi[:, 3 * D:4 * D]),
        "wv": tile_w_tm(wi[:, 2 * D:3 * D]),
        "wf": np.ascontiguousarray(wi[:, 4 * D:4 * D + 16].reshape(8, 128, 16).transpose(1, 0, 2)).reshape(128, 128),
        "bf": np.ascontiguousarray(fox_b_f[0].reshape(16, 1)),
    }
    maps = []
    for (b, t) in cores:
        m = dict(shared)
        m["xT"] = np.ascontiguousarray(x[b, t * T:(t + 1) * T, :].T)
        m["modT"] = modT[b]
        maps.append(m)
    r1 = _run(nc1, maps)
    _DBG["r1"] = r1

    def cat_fm(res, name, b):
        return np.concatenate([res[b * 4 + t][name] for t in range(4)], axis=1)

    def cat_tm(res, name, b):
        return np.concatenate([res[b * 4 + t][name] for t in range(4)], axis=0)

    nc2 = build_fox()
    U, sel, mk = fox_consts()
    maps = []
    for b in range(B):
        qf = cat_fm(r1, "qT", b).reshape(FH, FD, S)
        kf = cat_fm(r1, "kT", b).reshape(FH, FD, S)
        vf = cat_tm(r1, "v", b)
        lf = cat_fm(r1, "lf", b)
        for g in range(4):
            l4 = lf[4 * g:4 * g + 4]
            v4 = np.stack([np.ascontiguousarray(vf[:, hd * FD:(hd + 1) * FD].reshape(64, 128, FD).transpose(1, 0, 2)).reshape(128, 64 * FD)
                           for hd in range(4 * g, 4 * g + 4)])
            maps.append({
                "q": np.ascontiguousarray(qf[4 * g:4 * g + 4]), "k": np.ascontiguousarray(kf[4 * g:4 * g + 4]), "v": v4,
                "lt": np.ascontiguousarray(l4.reshape(4, 64, 128).transpose(2, 0, 1)).reshape(128, 256),
                "lq": np.ascontiguousarray(l4.reshape(4, 16, 512).transpose(1, 0, 2)).reshape(16, 2048),
                "U": U, "sel": sel, "mk": mk,
            })
    r2 = _run(nc2, maps)
    _DBG["r2"] = r2
    ofull = [np.concatenate([r2[b * 4 + g]["o"].reshape(4 * FD, S) for g in range(4)], axis=0) for b in range(B)]

    nc3 = build_F({"epi": ("fox", 0), "ffns": [(0, 2), (1, 0)], "proj": ("hgrn", 1)})
    hi = hgrn_w_in[0]
    shared = {
        "gT": gT, "wo": tile_wo(fox_w_out[0]),
        "wup0": tile_wup(ffn_w_up[0, 1]), "wdn0": tile_wdn(ffn_w_down[0, 1]),
        "wup1": tile_wup(ffn_w_up[1, 0]), "wdn1": tile_wdn(ffn_w_down[1, 0]),
        "wq": tile_w_fm(hi[:, 0:D]), "wf": tile_w_fm(hi[:, D:2 * D]), "wg": tile_w_fm(hi[:, 3 * D:4 * D]),
        "wv": tile_w_tm(hi[:, 2 * D:3 * D]), "lbl": fm_cols(hgrn_lb_logits),
    }
    maps = []
    for i, (b, t) in enumerate(cores):
        m = dict(shared)
        m["xT"] = r1[i]["xo"]
        m["modT"] = modT[b]
        m["oT"] = np.ascontiguousarray(ofull[b][:, t * T:(t + 1) * T])
        m["sg"] = r1[i]["sgo"]
        maps.append(m)
    r3 = _run(nc3, maps)
    _DBG["r3"] = r3

    nc4 = build_hgrn()
    m01, rm, ident = hgrn_consts()
    maps = []
    for b in range(B):
        qf = cat_fm(r3, "qT", b).reshape(HH, 128, S)
        kf = cat_fm(r3, "kT", b).reshape(HH, 128, S)
        lf = cat_fm(r3, "lfT", b).reshape(HH, 128, S)
        vf = cat_tm(r3, "v", b)
        for g in range(4):
            v2 = np.stack([np.ascontiguousarray(vf[:, hd * 128:(hd + 1) * 128].reshape(64, 128, 128).transpose(1, 0, 2)).reshape(128, 64 * 128)
                           for hd in range(2 * g, 2 * g + 2)])
            maps.append({
                "q": np.ascontiguousarray(qf[2 * g:2 * g + 2]), "k": np.ascontiguousarray(kf[2 * g:2 * g + 2]),
                "lf": np.ascontiguousarray(lf[2 * g:2 * g + 2]), "v": v2, "m01": m01, "rm": rm, "ident": ident,
            })
    r4 = _run(nc4, maps)
    _DBG["r4"] = r4
    ofull = [np.concatenate([r4[b * 4 + g]["o"].reshape(256, S) for g in range(4)], axis=0) for b in range(B)]

    nc5 = build_F({"epi": ("hgrn", 1), "ffns": [(1, 2)], "final": True})
    shared = {
        "gT": gT, "wo": tile_wo(hgrn_w_out[0]), "hgn": fm_cols(hgrn_norm_g[0]),
        "wup0": tile_wup(ffn_w_up[1, 1]), "wdn0": tile_wdn(ffn_w_down[1, 1]),
        "fg": fm_cols(final_norm_g),
    }
    maps = []
    for i, (b, t) in enumerate(cores):
        m = dict(shared)
        m["xT"] = r3[i]["xo"]
        m["modT"] = modT[b]
        m["oT"] = np.ascontiguousarray(ofull[b][:, t * T:(t + 1) * T])
        m["sg"] = r3[i]["sgo"]
        maps.append(m)
    r5 = _run(nc5, maps)
    out = np.empty((B, S, D), np.float32)
    for i, (b, t) in enumerate(cores):
        out[b, t * T:(t + 1) * T, :] = r5[i]["xo"].T
    return out


def kernel(x, c, ada_w, ada_b, norm_g, ffn_w_up, ffn_w_down, fox_w_in, fox_b_f, fox_w_out,
           hgrn_w_in, hgrn_norm_g, hgrn_w_out, hgrn_lb_logits, final_norm_g):
    f32 = lambda a: np.ascontiguousarray(np.asarray(a, dtype=np.float32))
    x, c, ada_w, ada_b, norm_g = f32(x), f32(c), f32(ada_w), f32(ada_b), f32(norm_g)
    ffn_w_up, ffn_w_down, fox_w_in, fox_b_f, fox_w_out = f32(ffn_w_up), f32(ffn_w_down), f32(fox_w_in), f32(fox_b_f), f32(fox_w_out)
    hgrn_w_in, hgrn_norm_g, hgrn_w_out = f32(hgrn_w_in), f32(hgrn_norm_g), f32(hgrn_w_out)
    hgrn_lb_logits, final_norm_g = f32(hgrn_lb_logits), f32(final_norm_g)
    nc = build_mega()
    wi, hi = fox_w_in[0], hgrn_w_in[0]
    U, sel, mk = fox_consts()
    m01, rm, ident = hgrn_consts()
    shared = {
        "modb": fm_cols(ada_b.reshape(18, D)),
        "modw": np.ascontiguousarray(ada_w.reshape(2, 8, 128, 9, D).transpose(0, 3, 2, 1, 4)).reshape(18, 128, 8 * D),
        "gT": fm_cols(norm_g.reshape(6, D)),
        "wup0": tile_wup(ffn_w_up[0, 0]), "wdn0": tile_wdn(ffn_w_down[0, 0]),
        "wup1": tile_wup(ffn_w_up[0, 1]), "wdn1": tile_wdn(ffn_w_down[0, 1]),
        "wup2": tile_wup(ffn_w_up[1, 0]), "wdn2": tile_wdn(ffn_w_down[1, 0]),
        "wup3": tile_wup(ffn_w_up[1, 1]), "wdn3": tile_wdn(ffn_w_down[1, 1]),
        "fox_wq": tile_w_fm(wi[:, 0:D]), "fox_wk": tile_w_fm(wi[:, D:2 * D]), "fox_wg": tile_w_fm(wi[:, 3 * D:4 * D]),
        "fox_wv": tile_w_tm(wi[:, 2 * D:3 * D]),
        "fox_wf": np.ascontiguousarray(wi[:, 4 * D:4 * D + 16].reshape(8, 128, 16).transpose(1, 0, 2)).reshape(128, 128),
        "fox_bfb": np.ascontiguousarray(np.broadcast_to(np.tile(fox_b_f[0], 16), (128, 256))),
        "U": U, "sel": sel, "mk": mk, "identf": np.eye(128, dtype=np.float32),
        "fox_wo": tile_wo(fox_w_out[0]),
        "hgrn_wq": tile_w_fm(hi[:, 0:D]), "hgrn_wf": tile_w_fm(hi[:, D:2 * D]), "hgrn_wg": tile_w_fm(hi[:, 3 * D:4 * D]),
        "hgrn_wv": tile_w_tm(hi[:, 2 * D:3 * D]), "lbl": fm_cols(hgrn_lb_logits),
        "m01": m01, "rm": rm, "ident": ident,
        "hgrn_wo": tile_wo(hgrn_w_out[0]), "hgn": fm_cols(hgrn_norm_g[0]),
        "fg": fm_cols(final_norm_g),
    }
    maps = []
    cores = [(b, t) for b in range(B) for t in range(4)]
    for (b, t) in cores:
        m = dict(shared)
        m["xT"] = np.ascontiguousarray(x[b, t * T:(t + 1) * T, :].T)
        m["cT"] = fm_cols(c[b])
        maps.append(m)
    res = _run(nc, maps)
    out = np.empty((B, S, D), np.float32)
    for i, (b, t) in enumerate(cores):
        out[b, t * T:(t + 1) * T, :] = res[i]["xo"].T
    return out
```

```python
from contextlib import ExitStack
import numpy as np
import ml_dtypes
import concourse.bass as bass
import concourse.mybir as mybir
from concourse.bass_utils import run_bass_kernel_spmd

F32 = mybir.dt.float32
BF16 = mybir.dt.bfloat16
AF = mybir.ActivationFunctionType
ALU = mybir.AluOpType
NPBF = ml_dtypes.bfloat16

D = 1024
B = 2
S = 8192
DFF = 2816
NF = 22
EPS = 1e-6
NCORES = 8
T = 2048
FH = 16
FD = 64
HH = 8
CH = 64


class Buf:
    __slots__ = ("w", "r", "t", "key")

    def __init__(self, t=None):
        self.w = None
        self.r = []
        self.t = t
        self.key = None


class Prog:
    ENG = ["pe", "act", "dve", "pool", "sp"]

    def __init__(self, nc):
        self.nc = nc
        self.ops = {e: [] for e in self.ENG}
        self.clock = {e: {} for e in self.ENG}
        self.snaps = {}
        self.count = {}
        self.needed = set()
        self.nkey = 0
        self.final = None
        self.unit_keys = set()

    def sb(self, es, name, shape, dtype):
        self.nname = getattr(self, "nname", 0) + 1
        t = es.enter_context(self.nc.sbuf_tensor("%s_u%d" % (name, self.nname), list(shape), dtype))
        return Buf(t)

    def ps(self, es, name, shape, dtype):
        self.nname = getattr(self, "nname", 0) + 1
        t = es.enter_context(self.nc.psum_tensor("%s_u%d" % (name, self.nname), list(shape), dtype))
        return Buf(t)

    def newkey(self, buf):
        fk = getattr(self, "free_keys", None)
        if fk is None:
            self.free_keys, self.key_owner = [], {}
            fk = self.free_keys
        if fk:
            k = fk.pop()
        else:
            self.nkey += 1
            k = "d%d" % self.nkey
        buf.key = k
        self.key_owner[k] = buf
        return k

    def op(self, eng, fn, reads=(), writes=(), dma=None, pe_acc=False):
        need = {}

        def req(ev):
            if ev is None:
                return
            k, s = ev
            if s > need.get(k, 0):
                need[k] = s

        for b in reads:
            req(b.w)
        for b in writes:
            if not (pe_acc and b.w is not None and b.w[0] == "pe"):
                req(b.w)
            for r in b.r:
                req(r)
        key = dma or eng
        if fn is None:
            idx = 0
        else:
            idx = self.count.get(key, 0) + 1
            self.count[key] = idx
        ck = self.clock[eng]
        waits = []
        for k, s in need.items():
            if ck.get(k, 0) < s:
                waits.append((k, s))
        for k, s in waits:
            sn = self.snaps[(k, s)]
            for kk, ss in sn.items():
                if ck.get(kk, 0) < ss:
                    ck[kk] = ss
            if ck.get(k, 0) < s:
                ck[k] = s
            self.needed.add((k, s))
        self.ops[eng].append((fn, waits, key, idx))
        if fn is None:
            return None
        self.snaps[(key, idx)] = dict(ck)
        ev = (key, idx)
        for b in reads:
            b.r.append(ev)
        for b in writes:
            b.w = ev
            b.r = []
        return ev

    def dma(self, eng, out, in_, reads=(), writes=(), ow=None):
        wb = ow if ow is not None else writes[0]
        if wb.key is None:
            self.newkey(wb)
        def _fn(e, out=out, in_=in_):
            try:
                return e.dma_start(out=out, in_=in_)
            except Exception:
                print("DMA FAIL", out, in_)
                raise
        ev = self.op(eng, _fn, reads=reads, writes=writes, dma=wb.key)
        if ow is not None:
            ow.w = ev
        return ev

    def collective(self, kind, groups, src_ap, dst_ap, reads, wbuf):
        wbuf.key = "cc"
        self.unit_keys.add(wbuf.key)
        return self.op("pool", lambda e: e.collective_compute(kind, ALU.bypass, replica_groups=groups,
                                                              ins=[src_ap], outs=[dst_ap]),
                       reads=reads, writes=[wbuf], dma=wbuf.key)

    def finish(self, outs, eng="sp"):
        self.op(eng, None, reads=list(outs))

    def barrier(self):
        need = dict(self.count)
        for e in self.ENG:
            self.op(e, None, extra=need)
        for k, b in list(getattr(self, "key_owner", {}).items()):
            b.key = None
            self.free_keys.append(k)
        if hasattr(self, "key_owner"):
            self.key_owner.clear()

    def emit(self):
        nc = self.nc
        keys = list(self.count.keys())
        for e in self.ENG:
            if e not in keys:
                keys.append(e)
        rank = {}
        for k in keys:
            if k in self.ENG:
                idxs = sorted(s for (kk, s) in self.needed if kk == k)
                rank[k] = {s: i + 1 for i, s in enumerate(idxs)}
        with ExitStack() as es:
            sems = {k: es.enter_context(nc.semaphore("s_" + k)) for k in keys}
            block = es.enter_context(nc.Block())

            def run(eng_name):
                def body(e):
                    for fn, waits, key, idx in self.ops[eng_name]:
                        for k, s in waits:
                            v = rank[k][s] if k in rank else (s if k in self.unit_keys else 16 * s)
                            e.wait_ge(sems[k], v)
                        if fn is None:
                            continue
                        ins = fn(e)
                        if key in rank:
                            if (key, idx) in self.needed:
                                ins.then_inc(sems[key], 1)
                        elif key in self.unit_keys:
                            ins.then_inc(sems[key], 1)
                        else:
                            ins.then_inc(sems[key], 16)
                return body

            block.tensor(run("pe"))
            block.scalar(run("act"))
            block.vector(run("dve"))
            block.gpsimd(run("pool"))
            block.sync(run("sp"))


def _patch_op():
    base = Prog.op

    def op(self, eng, fn, reads=(), writes=(), dma=None, pe_acc=False, extra=None):
        if extra:
            dummy = []
            for k, s in extra.items():
                if s > 0:
                    b = Buf()
                    b.w = (k, s)
                    dummy.append(b)
            reads = list(reads) + dummy
            ev = base(self, eng, fn, reads=reads, writes=writes, dma=dma, pe_acc=pe_acc)
            return ev
        return base(self, eng, fn, reads=reads, writes=writes, dma=dma, pe_acc=pe_acc)

    Prog.op = op


_patch_op()


SQD = float(np.sqrt(D))


class Ctx:
    pass


def mcol(l, v, ch):
    return (l * 9 + v) * 8 + ch


def build_F(cfg):
    nc = bass.Bass("TRN2", target_bir_lowering=False)
    P = Prog(nc)
    c = Ctx()
    c.P, c.nc = P, nc
    dr = {}

    def din(name, shape, dt=F32):
        dr[name] = nc.dram_tensor(name, list(shape), dt, kind="ExternalInput").ap()
        return dr[name]

    def dout(name, shape, dt=F32):
        dr[name] = nc.dram_tensor(name, list(shape), dt, kind="ExternalOutput").ap()
        return dr[name]

    xT = din("xT", [D, T])
    modT = din("modT", [128, 144])
    gT = din("gT", [128, 48])
    outs = []
    with ExitStack() as es:
        X = es.enter_context(nc.sbuf_tensor("X", [128, 8, T], F32))
        c.X = X
        c.Xb = [[Buf(X) for _ in range(4)] for _ in range(8)]
        c.modt = P.sb(es, "modt", [128, 144], F32)
        c.gt = P.sb(es, "gt", [128, 48], F32)
        c.der = P.sb(es, "der", [128, 96], F32)
        c.ones = P.sb(es, "ones", [128, 128], BF16)
        c.bank = [P.ps(es, "bank%d" % i, [128, 512], F32) for i in range(8)]
        sqt = es.enter_context(nc.sbuf_tensor("sq", [128, 8, 512], BF16))
        c.sq = [Buf(sqt) for _ in range(8)]
        c.rstd = P.sb(es, "rstd", [128, 512], F32)
        c.tmp = [P.sb(es, "tmp%d" % i, [128, 512], F32) for i in range(2)]

        xv = xT.rearrange("(c p) t -> p c t", p=128)
        for kc in range(8):
            P.dma("sp", X[:, kc, :], xv[:, kc, :], writes=[c.Xb[kc][tt] for tt in range(4)])
        P.dma("sp", c.modt.t[:, :], modT, writes=[c.modt])
        P.dma("sp", c.gt.t[:, :], gT, writes=[c.gt])
        P.op("pool", lambda e: e.memset(c.ones.t[:, :], 1.0), writes=[c.ones])
        for l in range(2):
            for sub in range(3):
                base = ((l * 3 + sub) * 2) * 8
                sc0 = mcol(l, sub * 3 + 1, 0)
                g0 = (l * 3 + sub) * 8
                ga0 = mcol(l, sub * 3 + 2, 0)
                P.op("dve", lambda e, base=base, sc0=sc0, g0=g0: e.scalar_tensor_tensor(
                    c.der.t[:, base:base + 8], c.modt.t[:, sc0:sc0 + 8], 1.0, c.gt.t[:, g0:g0 + 8], ALU.add, ALU.mult),
                    reads=[c.modt, c.gt], writes=[c.der])
                P.op("dve", lambda e, base=base: e.tensor_scalar(
                    c.der.t[:, base:base + 8], c.der.t[:, base:base + 8], SQD, None, ALU.mult),
                    reads=[c.der], writes=[c.der])
                P.op("dve", lambda e, base=base, ga0=ga0, sub=sub: e.tensor_scalar(
                    c.der.t[:, base + 8:base + 16], c.modt.t[:, ga0:ga0 + 8], (1.0 if sub == 1 else 0.5), None, ALU.mult),
                    reads=[c.modt], writes=[c.der])

        def Acol(l, sub, ch):
            j = ((l * 3 + sub) * 2) * 8 + ch
            return c.der.t[:, j:j + 1]

        def Gcol(l, sub, ch):
            j = ((l * 3 + sub) * 2 + 1) * 8 + ch
            return c.der.t[:, j:j + 1]

        def Scol(l, sub, ch):
            j = mcol(l, sub * 3 + 0, ch)
            return c.modt.t[:, j:j + 1]

        def rstd_tile(src_fn, src_bufs, epsk):
            for kc in range(8):
                P.op("act", lambda e, kc=kc: e.activation(sqt[:, kc, :], src_fn(kc), AF.Square),
                     reads=[src_bufs[kc]], writes=[c.sq[kc]])
            for kc in range(8):
                P.op("pe", lambda e, kc=kc: e.matmul(c.bank[6].t[:, :], c.ones.t[:, :], sqt[:, kc, :],
                                                      start=(kc == 0), stop=(kc == 7)),
                     reads=[c.ones, c.sq[kc]], writes=[c.bank[6]], pe_acc=(kc > 0))
            P.op("dve", lambda e: e.tensor_scalar(c.rstd.t[:, :], c.bank[6].t[:, :], epsk, None, ALU.add),
                 reads=[c.bank[6]], writes=[c.rstd])
            P.op("act", lambda e: e.activation(c.rstd.t[:, :], c.rstd.t[:, :], AF.Sqrt), reads=[c.rstd], writes=[c.rstd])
            P.op("dve", lambda e: e.reciprocal(c.rstd.t[:, :], c.rstd.t[:, :]), reads=[c.rstd], writes=[c.rstd])

        def modnorm_tile(l, sub, tt, hdst, hbuf):
            t0 = tt * 512
            rstd_tile(lambda kc: X[:, kc, t0:t0 + 512], [c.Xb[kc][tt] for kc in range(8)], EPS * D)
            for kc in range(8):
                tb = c.tmp[kc % 2]
                P.op("dve", lambda e, kc=kc, tb=tb: e.tensor_tensor(tb.t[:, :], X[:, kc, t0:t0 + 512], c.rstd.t[:, :], ALU.mult),
                     reads=[c.Xb[kc][tt], c.rstd], writes=[tb])
                P.op("act", lambda e, kc=kc, tb=tb: e.activation(hdst(kc), tb.t[:, :], AF.Identity,
                                                               bias=Scol(l, sub, kc), scale=Acol(l, sub, kc)),
                     reads=[tb, c.der, c.modt], writes=[hbuf(kc)])

        def epilogue(kind, l):
            oT = din("oT", [D, T])
            sgd = din("sg", [D, T], BF16)
            wod = din("wo", [128, 8192])
            ov = oT.rearrange("(c p) t -> p c t", p=128)
            sv = sgd.rearrange("(c p) t -> p c t", p=128)
            with ExitStack() as e2:
                wo = P.sb(e2, "wo_sb", [128, 8, 1024], BF16)
                P.dma("pool", wo.t[:, :, :], wod.rearrange("p (k d) -> p k d", k=8), writes=[wo])
                ot = [P.sb(e2, "ot%d" % i, [128, 8, 512], F32) for i in range(2)]
                st = [P.sb(e2, "st%d" % i, [128, 8, 512], BF16) for i in range(2)]
                ogt = [e2.enter_context(nc.sbuf_tensor("og%d" % i, [128, 8, 512], BF16)) for i in range(2)]
                ogb = [[Buf(ogt[i]) for _ in range(8)] for i in range(2)]
                if kind == "hgrn":
                    hgd = din("hgn", [128, 8])
                    hg = P.sb(e2, "hg", [128, 8], F32)
                    P.dma("sp", hg.t[:, :], hgd, writes=[hg])
                    P.op("dve", lambda e: e.tensor_scalar(hg.t[:, :], hg.t[:, :], float(np.sqrt(128.0)), None, ALU.mult),
                         reads=[hg], writes=[hg])
                    sq1 = P.sb(e2, "sq1", [128, 512], BF16)
                    r1 = P.sb(e2, "r1", [128, 512], F32)
                    t1 = P.sb(e2, "t1", [128, 512], F32)
                for tt in range(4):
                    t0 = tt * 512
                    o_, s_, og_ = ot[tt % 2], st[tt % 2], ogt[tt % 2]
                    P.dma("sp", o_.t[:, :, :], ov[:, :, t0:t0 + 512], writes=[o_])
                    P.dma("sp", s_.t[:, :, :], sv[:, :, t0:t0 + 512], writes=[s_])
                    if kind == "fox":
                        for kc in range(8):
                            P.op("dve", lambda e, kc=kc, o_=o_, s_=s_, og_=og_: e.tensor_tensor(
                                og_[:, kc, :], o_.t[:, kc, :], s_.t[:, kc, :], ALU.mult),
                                reads=[o_, s_], writes=[ogb[tt % 2][kc]])
                    else:
                        for kc in range(8):
                            P.op("act", lambda e, kc=kc, o_=o_: e.activation(sq1.t[:, :], o_.t[:, kc, :], AF.Square),
                                 reads=[o_], writes=[sq1])
                            P.op("pe", lambda e: e.matmul(c.bank[7].t[:, :], c.ones.t[:, :], sq1.t[:, :], start=True, stop=True),
                                 reads=[c.ones, sq1], writes=[c.bank[7]])
                            P.op("dve", lambda e: e.tensor_scalar(r1.t[:, :], c.bank[7].t[:, :], EPS * 128.0, None, ALU.add),
                                 reads=[c.bank[7]], writes=[r1])
                            P.op("act", lambda e: e.activation(r1.t[:, :], r1.t[:, :], AF.Sqrt), reads=[r1], writes=[r1])
                            P.op("dve", lambda e: e.reciprocal(r1.t[:, :], r1.t[:, :]), reads=[r1], writes=[r1])
                            P.op("dve", lambda e, kc=kc, o_=o_: e.tensor_tensor(t1.t[:, :], o_.t[:, kc, :], r1.t[:, :], ALU.mult),
                                 reads=[o_, r1], writes=[t1])
                            P.op("dve", lambda e, kc=kc, s_=s_, og_=og_: e.scalar_tensor_tensor(
                                og_[:, kc, :], t1.t[:, :], hg.t[:, kc:kc + 1], s_.t[:, kc, :], ALU.mult, ALU.mult),
                                reads=[t1, hg, s_], writes=[ogb[tt % 2][kc]])
                    for dc in range(8):
                        bk = c.bank[4 + dc % 2]
                        for kc in range(8):
                            P.op("pe", lambda e, kc=kc, dc=dc, bk=bk, og_=og_: e.matmul(
                                bk.t[:, :], wo.t[:, kc, dc * 128:(dc + 1) * 128], og_[:, kc, :],
                                start=(kc == 0), stop=(kc == 7)),
                                reads=[wo, ogb[tt % 2][kc]], writes=[bk], pe_acc=(kc > 0))
                        P.op("dve", lambda e, dc=dc, bk=bk, t0=t0: e.scalar_tensor_tensor(
                            X[:, dc, t0:t0 + 512], bk.t[:, :], Gcol(l, 1, dc), X[:, dc, t0:t0 + 512], ALU.mult, ALU.add),
                            reads=[bk, c.der, c.Xb[dc][tt]], writes=[c.Xb[dc][tt]])
            P.barrier()

        def ffn(j, l, sub):
            wupd = din("wup%d" % j, [11, 128, 4096])
            wdnd = din("wdn%d" % j, [8, 128, 2816])
            with ExitStack() as e2:
                hbt = e2.enter_context(nc.sbuf_tensor("hb_%d" % j, [128, 8, 1024], BF16))
                hbb = [[Buf(hbt) for _ in range(2)] for _ in range(8)]
                actt = e2.enter_context(nc.sbuf_tensor("actb_%d" % j, [128, NF, 1024], BF16))
                actb = [[Buf(actt) for _ in range(2)] for _ in range(NF)]
                wu = [P.sb(e2, "wu%d_%d" % (j, i), [128, 2, 8, 256], BF16) for i in range(2)]
                wd = [P.sb(e2, "wd%d_%d" % (j, i), [128, NF, 128], BF16) for i in range(2)]
                sa = [P.sb(e2, "sa%d_%d" % (j, i), [128, 512], F32) for i in range(2)]
                for half in range(2):
                    for t2 in range(2):
                        tt = half * 2 + t2
                        modnorm_tile(l, sub, tt, lambda kc, t2=t2: hbt[:, kc, t2 * 512:(t2 + 1) * 512],
                                     lambda kc, t2=t2: hbb[kc][t2])
                    it = 0
                    for g in range(11):
                        w_ = wu[g % 2]
                        P.dma("pool", w_.t[:, :, :, :], wupd[g].rearrange("p (a k f) -> p a k f", a=2, k=8), writes=[w_])
                        for jf in range(2):
                            fc = 2 * g + jf
                            for t2 in range(2):
                                bA, bB = c.bank[it % 2], c.bank[2 + it % 2]
                                s_ = sa[it % 2]
                                it += 1
                                for kc in range(8):
                                    P.op("pe", lambda e, kc=kc, w_=w_, jf=jf, t2=t2, bA=bA: e.matmul(
                                        bA.t[:, :], w_.t[:, 0, kc, jf * 128:(jf + 1) * 128], hbt[:, kc, t2 * 512:(t2 + 1) * 512],
                                        start=(kc == 0), stop=(kc == 7)),
                                        reads=[w_, hbb[kc][t2]], writes=[bA], pe_acc=(kc > 0))
                                for kc in range(8):
                                    P.op("pe", lambda e, kc=kc, w_=w_, jf=jf, t2=t2, bB=bB: e.matmul(
                                        bB.t[:, :], w_.t[:, 1, kc, jf * 128:(jf + 1) * 128], hbt[:, kc, t2 * 512:(t2 + 1) * 512],
                                        start=(kc == 0), stop=(kc == 7)),
                                        reads=[w_, hbb[kc][t2]], writes=[bB], pe_acc=(kc > 0))
                                P.op("act", lambda e, s_=s_, bA=bA: e.activation(s_.t[:, :], bA.t[:, :], AF.Silu),
                                     reads=[bA], writes=[s_])
                                P.op("dve", lambda e, s_=s_, bB=bB, fc=fc, t2=t2: e.tensor_tensor(
                                    actt[:, fc, t2 * 512:(t2 + 1) * 512], bB.t[:, :], s_.t[:, :], ALU.mult),
                                    reads=[bB, s_], writes=[actb[fc][t2]])
                    for dc in range(8):
                        w_ = wd[dc % 2]
                        P.dma("pool", w_.t[:, :, :], wdnd[dc].rearrange("p (f d) -> p f d", f=NF), writes=[w_])
                        for t2 in range(2):
                            tt = half * 2 + t2
                            t0 = tt * 512
                            bk = c.bank[4 + (dc * 2 + t2) % 2]
                            for fc in range(NF):
                                P.op("pe", lambda e, fc=fc, w_=w_, t2=t2, bk=bk: e.matmul(
                                    bk.t[:, :], w_.t[:, fc, :], actt[:, fc, t2 * 512:(t2 + 1) * 512],
                                    start=(fc == 0), stop=(fc == NF - 1)),
                                    reads=[w_, actb[fc][t2]], writes=[bk], pe_acc=(fc > 0))
                            P.op("dve", lambda e, dc=dc, bk=bk, t0=t0: e.scalar_tensor_tensor(
                                X[:, dc, t0:t0 + 512], bk.t[:, :], Gcol(l, sub, dc), X[:, dc, t0:t0 + 512], ALU.mult, ALU.add),
                                reads=[bk, c.der, c.Xb[dc][tt]], writes=[c.Xb[dc][tt]])
            P.barrier()

        def proj_fm(wname, hbt, hbb, evac, n_oc=8):
            wd_ = din(wname, [n_oc, 128, 1024])
            with ExitStack() as e3:
                wp = [P.sb(e3, wname + "_sb%d" % i, [128, 8, 128], BF16) for i in range(2)]
                it = 0
                for oc in range(n_oc):
                    w_ = wp[oc % 2]
                    P.dma("pool", w_.t[:, :, :], wd_[oc].rearrange("p (k f) -> p k f", k=8), writes=[w_])
                    for tt in range(4):
                        bk = c.bank[it % 4]
                        it += 1
                        for kc in range(8):
                            P.op("pe", lambda e, kc=kc, w_=w_, tt=tt, bk=bk: e.matmul(
                                bk.t[:, :], w_.t[:, kc, :], hbt[:, kc, tt * 512:(tt + 1) * 512],
                                start=(kc == 0), stop=(kc == 7)),
                                reads=[w_, hbb[kc][tt]], writes=[bk], pe_acc=(kc > 0))
                        evac(oc, tt, bk)
                P.barrier()

        def proj_tm(wname, hbt, hbb, vout, vob, func):
            wd_ = din(wname, [2, 128, 4096])
            with ExitStack() as e3:
                wv = P.sb(e3, wname + "_sb", [128, 2, 8, 512], BF16)
                for cg in range(2):
                    P.dma("pool", wv.t[:, cg, :, :], wd_[cg].rearrange("p (k f) -> p k f", k=8), writes=[wv])
                vt = [P.sb(e3, "vt%d" % i, [128, 512], BF16) for i in range(2)]
                it = 0
                for tk in range(16):
                    for cg in range(2):
                        bk = c.bank[it % 4]
                        v_ = vt[it % 2]
                        it += 1
                        for kc in range(8):
                            P.op("pe", lambda e, kc=kc, tk=tk, cg=cg, bk=bk: e.matmul(
                                bk.t[:, :], hbt[:, kc, tk * 128:(tk + 1) * 128], wv.t[:, cg, kc, :],
                                start=(kc == 0), stop=(kc == 7)),
                                reads=[wv, hbb[kc][tk // 4]], writes=[bk], pe_acc=(kc > 0))
                        P.op("act", lambda e, bk=bk, v_=v_: e.activation(v_.t[:, :], bk.t[:, :], func),
                             reads=[bk], writes=[v_])
                        P.dma("sp", vout[tk * 128:(tk + 1) * 128, cg * 512:(cg + 1) * 512], v_.t[:, :], reads=[v_], ow=vob)
                P.barrier()

        def stage_out(e3, name, shape, dt):
            return [P.sb(e3, name + "%d" % i, shape, dt) for i in range(2)]

        def projections(kind, l):
            with ExitStack() as e2:
                hbt = e2.enter_context(nc.sbuf_tensor("hb2", [128, 8, T], BF16))
                hbb = [[Buf(hbt) for _ in range(4)] for _ in range(8)]
                for tt in range(4):
                    modnorm_tile(l, 1, tt, lambda kc, tt=tt: hbt[:, kc, tt * 512:(tt + 1) * 512],
                                 lambda kc, tt=tt: hbb[kc][tt])
                qo = dout("qT", [D, T], BF16)
                qob = Buf()
                outs.append(qob)
                sgo = dout("sgo", [D, T], BF16)
                sgob = Buf()
                outs.append(sgob)
                vo = dout("v", [T, D], BF16)
                vob = Buf()
                outs.append(vob)
                cnt = [0]

                def simple_evac(od, ob, func, scale, st, dt_eng="act"):
                    def evac(oc, tt, bk):
                        s_ = st[cnt[0] % 2]
                        cnt[0] += 1
                        P.op("act", lambda e, s_=s_, bk=bk: e.activation(s_.t[:, :], bk.t[:, :], func, scale=scale),
                             reads=[bk], writes=[s_])
                        P.dma("sp", od[oc * 128:(oc + 1) * 128, tt * 512:(tt + 1) * 512], s_.t[:, :], reads=[s_], ow=ob)
                    return evac

                stb = stage_out(e2, "stb", [128, 512], BF16)
                if kind == "fox":
                    ko = dout("kT", [D, T], BF16)
                    kob = Buf()
                    outs.append(kob)
                    lfo = dout("lf", [16, T], F32)
                    lfob = Buf()
                    outs.append(lfob)
                    proj_fm("wq", hbt, hbb, simple_evac(qo, qob, AF.Copy, float(FD ** -0.5), stb))
                    proj_fm("wk", hbt, hbb, simple_evac(ko, kob, AF.Copy, 1.0, stb))
                    proj_fm("wg", hbt, hbb, simple_evac(sgo, sgob, AF.Sigmoid, 1.0, stb))
                    proj_tm("wv", hbt, hbb, vo, vob, AF.Copy)
                    wfd = din("wf", [128, 128])
                    bfd = din("bf", [16, 1])
                    wf = P.sb(e2, "wf_sb", [128, 8, 16], BF16)
                    P.dma("pool", wf.t[:, :, :], wfd.rearrange("p (k f) -> p k f", k=8), writes=[wf])
                    nbf = P.sb(e2, "nbf", [16, 1], F32)
                    P.dma("sp", nbf.t[:, :], bfd, writes=[nbf])
                    P.op("dve", lambda e: e.tensor_scalar(nbf.t[:, :], nbf.t[:, :], -1.0, None, ALU.mult), reads=[nbf], writes=[nbf])
                    e1 = P.sb(e2, "e1", [16, 512], F32)
                    l1 = [P.sb(e2, "l1_%d" % i, [16, 512], F32) for i in range(2)]
                    for tt in range(4):
                        bk = c.bank[tt % 4]
                        for kc in range(8):
                            P.op("pe", lambda e, kc=kc, tt=tt, bk=bk: e.matmul(
                                bk.t[0:16, :], wf.t[:, kc, :], hbt[:, kc, tt * 512:(tt + 1) * 512],
                                start=(kc == 0), stop=(kc == 7)),
                                reads=[wf, hbb[kc][tt]], writes=[bk], pe_acc=(kc > 0))
                        l_ = l1[tt % 2]
                        P.op("act", lambda e, bk=bk: e.activation(e1.t[:, :], bk.t[0:16, :], AF.Exp, bias=nbf.t[:, 0:1], scale=-1.0),
                             reads=[bk, nbf], writes=[e1])
                        P.op("act", lambda e, l_=l_: e.activation(l_.t[:, :], e1.t[:, :], AF.Ln, bias=1.0, scale=1.0),
                             reads=[e1], writes=[l_])
                        P.op("dve", lambda e, l_=l_: e.tensor_scalar(l_.t[:, :], l_.t[:, :], -1.0, None, ALU.mult),
                             reads=[l_], writes=[l_])
                        P.dma("sp", lfo[:, tt * 512:(tt + 1) * 512], l_.t[:, :], reads=[l_], ow=lfob)
                else:
                    ko = dout("kT", [D, T], F32)
                    kob = Buf()
                    outs.append(kob)
                    lfo = dout("lfT", [D, T], F32)
                    lfob = Buf()
                    outs.append(lfob)
                    lbd = din("lbl", [128, 16])
                    lbl = P.sb(e2, "lbl_sb", [128, 16], F32)
                    lb = P.sb(e2, "lb", [128, 8], F32)
                    oml = P.sb(e2, "oml", [128, 8], F32)
                    P.dma("sp", lbl.t[:, :], lbd, writes=[lbl])
                    P.op("dve", lambda e: e.tensor_tensor(lb.t[:, :], lbl.t[:, 8:16], lbl.t[:, 0:8], ALU.subtract), reads=[lbl], writes=[lb])
                    P.op("act", lambda e: e.activation(lb.t[:, :], lb.t[:, :], AF.Sigmoid), reads=[lb], writes=[lb])
                    P.op("dve", lambda e: e.tensor_scalar(oml.t[:, :], lb.t[:, :], -1.0, 1.0, ALU.mult, ALU.add), reads=[lb], writes=[oml])
                    proj_fm("wq", hbt, hbb, simple_evac(qo, qob, AF.Copy, 1.0, stb))
                    proj_fm("wg", hbt, hbb, simple_evac(sgo, sgob, AF.Silu, 1.0, stb))
                    proj_tm("wv", hbt, hbb, vo, vob, AF.Silu)
                    sg1 = P.sb(e2, "sg1", [128, 512], F32)
                    ff = stage_out(e2, "ff", [128, 512], F32)
                    lff = stage_out(e2, "lff", [128, 512], F32)
                    kk = stage_out(e2, "kk", [128, 512], F32)

                    def f_evac(oc, tt, bk):
                        i = cnt[0] % 2
                        cnt[0] += 1
                        f_, l_, k_ = ff[i], lff[i], kk[i]
                        P.op("act", lambda e, bk=bk: e.activation(sg1.t[:, :], bk.t[:, :], AF.Sigmoid), reads=[bk], writes=[sg1])
                        P.op("dve", lambda e, f_=f_, oc=oc: e.tensor_scalar(f_.t[:, :], sg1.t[:, :], oml.t[:, oc:oc + 1], lb.t[:, oc:oc + 1], ALU.mult, ALU.add),
                             reads=[sg1, oml, lb], writes=[f_])
                        P.op("act", lambda e, f_=f_, l_=l_: e.activation(l_.t[:, :], f_.t[:, :], AF.Ln), reads=[f_], writes=[l_])
                        P.op("dve", lambda e, f_=f_, k_=k_: e.tensor_scalar(k_.t[:, :], f_.t[:, :], -1.0, 1.0, ALU.mult, ALU.add),
                             reads=[f_], writes=[k_])
                        P.dma("sp", lfo[oc * 128:(oc + 1) * 128, tt * 512:(tt + 1) * 512], l_.t[:, :], reads=[l_], ow=lfob)
                        P.dma("sp", ko[oc * 128:(oc + 1) * 128, tt * 512:(tt + 1) * 512], k_.t[:, :], reads=[k_], ow=kob)
                    proj_fm("wf", hbt, hbb, f_evac)
            P.barrier()

        if cfg.get("epi"):
            epilogue(cfg["epi"][0], cfg["epi"][1])
        for j, (l, sub) in enumerate(cfg["ffns"]):
            ffn(j, l, sub)
        if cfg.get("proj"):
            projections(cfg["proj"][0], cfg["proj"][1])
        xo = dout("xo", [D, T])
        xob = Buf()
        outs.append(xob)
        xov = xo.rearrange("(c p) t -> p c t", p=128)
        if cfg.get("final"):
            fgd = din("fg", [128, 8])
            fg = P.sb(es, "fg_sb", [128, 8], F32)
            P.dma("sp", fg.t[:, :], fgd, writes=[fg])
            P.op("dve", lambda e: e.tensor_scalar(fg.t[:, :], fg.t[:, :], SQD, None, ALU.mult), reads=[fg], writes=[fg])
            yo = [P.sb(es, "yo%d" % i, [128, 512], F32) for i in range(2)]
            it = 0
            for tt in range(4):
                t0 = tt * 512
                rstd_tile(lambda kc, t0=t0: X[:, kc, t0:t0 + 512], [c.Xb[kc][tt] for kc in range(8)], EPS * D)
                for kc in range(8):
                    y_ = yo[it % 2]
                    it += 1
                    P.op("dve", lambda e, kc=kc, y_=y_, t0=t0: e.scalar_tensor_tensor(
                        y_.t[:, :], X[:, kc, t0:t0 + 512], fg.t[:, kc:kc + 1], c.rstd.t[:, :], ALU.mult, ALU.mult),
                        reads=[c.Xb[kc][tt], fg, c.rstd], writes=[y_])
                    P.dma("sp", xov[:, kc, t0:t0 + 512], y_.t[:, :], reads=[y_], ow=xob)
        else:
            for kc in range(8):
                P.dma("sp", xov[:, kc, :], X[:, kc, :], reads=[c.Xb[kc][tt] for tt in range(4)], ow=xob)
        P.finish(outs)
        P.emit()
    return nc


MC = 2304


def build_mod():
    nc = bass.Bass("TRN2", target_bir_lowering=False)
    P = Prog(nc)
    cT = nc.dram_tensor("cT", [128, 16], F32, kind="ExternalInput").ap()
    w = nc.dram_tensor("w", [128, 8 * MC], F32, kind="ExternalInput").ap()
    bias = nc.dram_tensor("bias", [2, MC], F32, kind="ExternalInput").ap()
    mo = nc.dram_tensor("mo", [2, MC], F32, kind="ExternalOutput").ap()
    with ExitStack() as es:
        ct = P.sb(es, "ct", [128, 8, 2], F32)
        wt = [P.sb(es, "wt%d" % i, [128, 8, 384], F32) for i in range(6)]
        bt = P.sb(es, "bt", [2, MC], F32)
        ot = P.sb(es, "ot", [2, MC], F32)
        banks = [P.ps(es, "bk%d" % i, [128, 512], F32) for i in range(2)]
        P.dma("sp", ct.t[:, :, :], cT.rearrange("p (k b) -> p k b", k=8), writes=[ct])
        P.dma("sp", bt.t[:, :], bias, writes=[bt])
        wv = w.rearrange("p (k n) -> p k n", k=8)
        for i in range(6):
            P.dma("sp" if i % 2 == 0 else "pool", wt[i].t[:, :, :], wv[:, :, i * 384:(i + 1) * 384], writes=[wt[i]])
        P.op("act", lambda e: e.activation(ct.t[:, :, :], ct.t[:, :, :], AF.Silu), reads=[ct], writes=[ct])
        for i in range(6):
            bk = banks[i % 2]
            for kc in range(8):
                P.op("pe", lambda e, kc=kc, i=i, bk=bk: e.matmul(bk.t[0:2, 0:384], ct.t[:, kc, :], wt[i].t[:, kc, :],
                                                                 start=(kc == 0), stop=(kc == 7)),
                     reads=[ct, wt[i]], writes=[bk], pe_acc=(kc > 0))
            P.op("dve", lambda e, i=i, bk=bk: e.tensor_tensor(ot.t[:, i * 384:(i + 1) * 384], bk.t[0:2, 0:384],
                                                             bt.t[:, i * 384:(i + 1) * 384], ALU.add),
                 reads=[bk, bt], writes=[ot])
        ob = Buf()
        P.dma("sp", mo, ot.t[:, :], reads=[ot], ow=ob)
        P.finish([ob])
        P.emit()
    return nc


def run_mod(c, ada_w, ada_b):
    nc = build_mod()
    cT = np.ascontiguousarray(c.T.reshape(8, 128, B).transpose(1, 0, 2)).reshape(128, 16)
    wall = np.concatenate([ada_w[0], ada_w[1]], axis=1)
    ball = np.concatenate([ada_b[0], ada_b[1]], axis=0)
    maps = []
    for j in range(NCORES):
        wj = wall[:, j * MC:(j + 1) * MC].reshape(8, 128, MC).transpose(1, 0, 2)
        maps.append({"cT": cT, "w": np.ascontiguousarray(wj).reshape(128, 8 * MC),
                     "bias": np.ascontiguousarray(np.broadcast_to(ball[j * MC:(j + 1) * MC], (2, MC)))})
    res = run_bass_kernel_spmd(nc, maps, core_ids=list(range(NCORES)))
    mod = np.concatenate([r["mo"] for r in res.results], axis=1)
    return mod.reshape(B, 2, 9, D)


def fm_cols(v):
    lead = int(np.prod(v.shape[:-1])) if v.ndim > 1 else 1
    a = v.reshape(lead, 8, 128).transpose(2, 0, 1)
    return np.ascontiguousarray(a).reshape(128, lead * 8)


def tile_w_fm(w):
    n = w.shape[1] // 128
    a = w.reshape(8, 128, n, 128).transpose(2, 1, 0, 3)
    return np.ascontiguousarray(a).reshape(n, 128, 1024)


def tile_w_tm(w):
    a = w.reshape(8, 128, 2, 512).transpose(2, 1, 0, 3)
    return np.ascontiguousarray(a).reshape(2, 128, 4096)


def tile_wup(w):
    a = w.reshape(8, 128, 2, 11, 256).transpose(3, 1, 2, 0, 4)
    return np.ascontiguousarray(a).reshape(11, 128, 4096)


def tile_wdn(w):
    a = w.reshape(NF, 128, 8, 128).transpose(2, 1, 0, 3)
    return np.ascontiguousarray(a).reshape(8, 128, NF * 128)


def tile_wo(w):
    a = w.reshape(8, 128, D).transpose(1, 0, 2)
    return np.ascontiguousarray(a).reshape(128, 8 * D)


NEG = -30000.0


def build_fox():
    nc = bass.Bass("TRN2", target_bir_lowering=False)
    P = Prog(nc)
    qd = nc.dram_tensor("q", [4, 64, S], BF16, kind="ExternalInput").ap()
    kd = nc.dram_tensor("k", [4, 64, S], BF16, kind="ExternalInput").ap()
    vd = nc.dram_tensor("v", [4, 128, 64 * 64], BF16, kind="ExternalInput").ap()
    ltd = nc.dram_tensor("lt", [128, 256], F32, kind="ExternalInput").ap()
    lqd = nc.dram_tensor("lq", [16, 2048], F32, kind="ExternalInput").ap()
    Ud = nc.dram_tensor("U", [128, 128], F32, kind="ExternalInput").ap()
    seld = nc.dram_tensor("sel", [128, 128], F32, kind="ExternalInput").ap()
    mkd = nc.dram_tensor("mk", [128, 128], F32, kind="ExternalInput").ap()
    od = nc.dram_tensor("o", [4, 64, S], F32, kind="ExternalOutput").ap()
    shi = nc.dram_tensor("shi", [4, S], BF16).ap()
    slo = nc.dram_tensor("slo", [4, S], BF16).ap()
    with ExitStack() as es:
        bank = [P.ps(es, "bank%d" % i, [128, 512], F32) for i in range(8)]
        U = P.sb(es, "U_sb", [128, 128], F32)
        sel = P.sb(es, "sel_sb", [128, 128], F32)
        mk = P.sb(es, "mk_sb", [128, 128], F32)
        onesf = P.sb(es, "onesf", [128, 128], F32)
        lt = P.sb(es, "lt_sb", [128, 256], F32)
        lq = P.sb(es, "lq_sb", [16, 2048], F32)
        within = P.sb(es, "within", [128, 256], F32)
        tot = P.sb(es, "tot", [128, 256], F32)
        inc = P.sb(es, "inc", [128, 256], F32)
        GT = P.sb(es, "GT", [128, 256], F32)
        gend = P.sb(es, "gend", [128, 256], F32)
        negB = P.sb(es, "negB", [128, 4 * 16 * 64], F32)
        cl = P.sb(es, "cl", [16, 2048], F32)
        Aa = P.sb(es, "Aa", [16, 2048], F32)
        ahi = P.sb(es, "ahi", [16, 2048], BF16)
        ahf = P.sb(es, "ahf", [16, 2048], F32)
        alo = P.sb(es, "alo", [16, 2048], BF16)
        qa = [P.sb(es, "qa%d" % i, [66, S], BF16) for i in range(2)]
        ka = [P.sb(es, "ka%d" % i, [66, S], BF16) for i in range(2)]
        va = [P.sb(es, "va%d" % i, [128, 64, 65], BF16) for i in range(2)]
        pt = [P.sb(es, "pt%d" % i, [128, 512], BF16) for i in range(3)]
        drow = P.sb(es, "drow", [65, 512], F32)
        rec = P.sb(es, "rec", [64, 512], F32)
        oo = [P.sb(es, "oo%d" % i, [64, 512], F32) for i in range(2)]
        ob = Buf()

        for t_, d_ in ((U, Ud), (sel, seld), (mk, mkd), (lt, ltd), (lq, lqd)):
            P.dma("sp", t_.t[:, :], d_, writes=[t_])
        P.op("pool", lambda e: e.memset(onesf.t[:, :], 1.0), writes=[onesf])
        P.op("pe", lambda e: e.matmul(bank[6].t[:, 0:256], U.t[:, :], lt.t[:, :], start=True, stop=True), reads=[U, lt], writes=[bank[6]])
        P.op("pe", lambda e: e.matmul(bank[7].t[:, 0:256], onesf.t[:, :], lt.t[:, :], start=True, stop=True), reads=[onesf, lt], writes=[bank[7]])
        P.op("dve", lambda e: e.tensor_copy(within.t[:, :], bank[6].t[:, 0:256]), reads=[bank[6]], writes=[within])
        P.op("dve", lambda e: e.tensor_copy(tot.t[:, :], bank[7].t[:, 0:256]), reads=[bank[7]], writes=[tot])
        for h in range(4):
            P.op("dve", lambda e, h=h: e.tensor_tensor_scan(inc.t[:, h * 64:(h + 1) * 64], onesf.t[:, 0:64], tot.t[:, h * 64:(h + 1) * 64],
                                                            0.0, ALU.mult, ALU.add), reads=[onesf, tot], writes=[inc])
        P.op("dve", lambda e: e.tensor_tensor(GT.t[:, :], within.t[:, :], inc.t[:, :], ALU.add), reads=[within, inc], writes=[GT])
        P.op("dve", lambda e: e.tensor_tensor(GT.t[:, :], GT.t[:, :], tot.t[:, :], ALU.subtract), reads=[GT, tot], writes=[GT])
        P.op("pe", lambda e: e.matmul(bank[6].t[:, 0:256], sel.t[:, :], GT.t[:, :], start=True, stop=True), reads=[sel, GT], writes=[bank[6]])
        P.op("dve", lambda e: e.tensor_copy(gend.t[:, :], bank[6].t[:, 0:256]), reads=[bank[6]], writes=[gend])
        for h in range(4):
            for Q in range(16):
                j0 = (h * 16 + Q) * 64
                gc = h * 64 + 4 * Q + 3
                P.op("dve", lambda e, h=h, j0=j0, gc=gc: e.tensor_scalar(
                    negB.t[:, j0:j0 + 64], GT.t[:, h * 64:(h + 1) * 64], -1.0, gend.t[:, gc:gc + 1], ALU.mult, ALU.add),
                    reads=[GT, gend], writes=[negB])
        ones16 = P.sb(es, "ones16", [16, 512], F32)
        P.op("pool", lambda e: e.memset(ones16.t[:, :], 1.0), writes=[ones16])
        for h in range(4):
            P.op("dve", lambda e, h=h: e.tensor_tensor_scan(cl.t[:, h * 512:(h + 1) * 512], ones16.t[:, :], lq.t[:, h * 512:(h + 1) * 512],
                                                            0.0, ALU.mult, ALU.add), reads=[lq, ones16], writes=[cl])
        for h in range(4):
            P.op("dve", lambda e, h=h: e.tensor_scalar(Aa.t[:, h * 512:(h + 1) * 512], cl.t[:, h * 512:(h + 1) * 512],
                                                       cl.t[:, h * 512 + 511:h * 512 + 512], None, ALU.subtract),
                 reads=[cl], writes=[Aa])
        P.op("dve", lambda e: e.tensor_copy(ahi.t[:, :], Aa.t[:, :]), reads=[Aa], writes=[ahi])
        P.op("dve", lambda e: e.tensor_copy(ahf.t[:, :], ahi.t[:, :]), reads=[ahi], writes=[ahf])
        P.op("dve", lambda e: e.tensor_tensor(alo.t[:, :], Aa.t[:, :], ahf.t[:, :], ALU.subtract), reads=[Aa, ahf], writes=[alo])
        shb, slb = Buf(), Buf()
        P.dma("sp", shi.rearrange("h (q m) -> q h m", q=16), ahi.t[:, :].rearrange("q (h m) -> q h m", h=4), reads=[ahi], writes=[shb])
        P.dma("sp", slo.rearrange("h (q m) -> q h m", q=16), alo.t[:, :].rearrange("q (h m) -> q h m", h=4), reads=[alo], writes=[slb])

        for i in range(2):
            P.op("pool", lambda e, i=i: e.memset(ka[i].t[64:66, :], 1.0), writes=[ka[i]])
            P.op("pool", lambda e, i=i: e.memset(va[i].t[:, :, 64:65], 1.0), writes=[va[i]])

        def load_head(h):
            q_, k_, v_ = qa[h % 2], ka[h % 2], va[h % 2]
            P.dma("sp", q_.t[0:64, :], qd[h], writes=[q_])
            P.dma("sp", q_.t[64:65, :], shi[h:h + 1, :], reads=[shb], writes=[q_])
            P.dma("sp", q_.t[65:66, :], slo[h:h + 1, :], reads=[slb], writes=[q_])
            P.dma("pool", k_.t[0:64, :], kd[h], writes=[k_])
            P.dma("pool", v_.t[:, :, 0:64], vd[h].rearrange("p (t d) -> p t d", d=64), writes=[v_])

        load_head(0)

        def do_head(h, q_, k_, v_, nit):
            items = [(Q, kt) for Q in range(16) for kt in range(4 * Q + 4)]

            def emit_S(idx, it_no):
                Q, kt = items[idx]
                d = kt - 4 * Q
                c0 = 128 * d if d >= 0 else 0
                bk = bank[it_no % 3]
                p_ = pt[it_no % 3]
                P.op("pe", lambda e: e.matmul(bk.t[:, c0:512], k_.t[0:66, kt * 128:(kt + 1) * 128],
                                              q_.t[0:66, Q * 512 + c0:(Q + 1) * 512], start=True, stop=True),
                     reads=[k_, q_], writes=[bk])
                if d >= 0:
                    P.op("dve", lambda e: e.tensor_tensor(bk.t[:, c0:c0 + 128], bk.t[:, c0:c0 + 128], mk.t[:, :], ALU.add),
                         reads=[bk, mk], writes=[bk])
                jb = (h * 16 + Q) * 64 + kt
                P.op("act", lambda e: e.activation(p_.t[:, c0:512], bk.t[:, c0:512], AF.Exp, bias=negB.t[:, jb:jb + 1], scale=1.0),
                     reads=[bk, negB], writes=[p_])

            def emit_PV(idx, it_no):
                Q, kt = items[idx]
                d = kt - 4 * Q
                c0 = 128 * d if d >= 0 else 0
                p_ = pt[it_no % 3]
                ob_ = bank[3 + Q % 2]
                last = (kt == 4 * Q + 3)
                P.op("pe", lambda e: e.matmul(ob_.t[0:65, c0:512], v_.t[:, kt, :], p_.t[:, c0:512], start=(kt == 0), stop=last),
                     reads=[v_, p_], writes=[ob_], pe_acc=(kt > 0))
                if last:
                    o_ = oo[Q % 2]
                    P.op("act", lambda e: e.activation(drow.t[64:65, :], ob_.t[64:65, :], AF.Copy), reads=[ob_], writes=[drow])
                    P.op("pe", lambda e: e.matmul(bank[5].t[0:64, :], onesf.t[64:65, 0:64], drow.t[64:65, :], start=True, stop=True),
                         reads=[onesf, drow], writes=[bank[5]])
                    P.op("dve", lambda e: e.reciprocal(rec.t[:, :], bank[5].t[0:64, :]), reads=[bank[5]], writes=[rec])
                    P.op("dve", lambda e: e.tensor_tensor(o_.t[:, :], ob_.t[0:64, :], rec.t[:, :], ALU.mult), reads=[ob_, rec], writes=[o_])
                    P.dma("sp", od[h][:, Q * 512:(Q + 1) * 512], o_.t[:, :], reads=[o_], ow=ob)

            n = len(items)
            emit_S(0, nit)
            for idx in range(n):
                if idx + 1 < n:
                    emit_S(idx + 1, nit + idx + 1)
                emit_PV(idx, nit + idx)
            return nit + n

        nit = 0
        for h in range(4):
            if h + 1 < 4:
                load_head(h + 1)
            nit = do_head(h, qa[h % 2], ka[h % 2], va[h % 2], nit)
        P.finish([ob])
        P.emit()
    return nc


def fox_consts():
    k = np.arange(128)
    U = (k[:, None] <= k[None, :]).astype(np.float32)
    sel = np.zeros((128, 128), np.float32)
    sel[127, :] = 1.0
    mk = np.where(k[None, :] >= k[:, None], 0.0, NEG).astype(np.float32)
    return U, sel, mk


def build_hgrn():
    nc = bass.Bass("TRN2", target_bir_lowering=False)
    P = Prog(nc)
    qd = nc.dram_tensor("q", [2, 128, S], BF16, kind="ExternalInput").ap()
    kd = nc.dram_tensor("k", [2, 128, S], F32, kind="ExternalInput").ap()
    lfd = nc.dram_tensor("lf", [2, 128, S], F32, kind="ExternalInput").ap()
    vd = nc.dram_tensor("v", [2, 128, 64 * 128], BF16, kind="ExternalInput").ap()
    m01d = nc.dram_tensor("m01", [128, 64], F32, kind="ExternalInput").ap()
    rmd = nc.dram_tensor("rm", [128, 2048], F32, kind="ExternalInput").ap()
    idd = nc.dram_tensor("ident", [128, 128], BF16, kind="ExternalInput").ap()
    od = nc.dram_tensor("o", [2, 128, S], F32, kind="ExternalOutput").ap()
    NB = 2048
    with ExitStack() as es:
        bankA = [P.ps(es, "bankA%d" % i, [128, 512], F32) for i in range(2)]
        bankO = [P.ps(es, "bankO%d" % i, [128, 512], F32) for i in range(2)]
        bankU = [P.ps(es, "bankU%d" % i, [128, 512], F32) for i in range(2)]
        bankT = P.ps(es, "bankT", [128, 1024], BF16)
        m01 = P.sb(es, "m01_sb", [128, 64], F32)
        rm = P.sb(es, "rm_sb", [128, NB], F32)
        ident = P.sb(es, "ident_sb", [128, 128], BF16)
        P.dma("sp", m01.t[:, :], m01d, writes=[m01])
        P.dma("sp", rm.t[:, :], rmd, writes=[rm])
        P.dma("sp", ident.t[:, :], idd, writes=[ident])
        ob = Buf()
        hs = []
        for h in range(2):
            o = Ctx()
            o.qb = P.sb(es, "qb%d" % h, [128, NB], BF16)
            o.kb = P.sb(es, "kb%d" % h, [128, NB], F32)
            o.lf = P.sb(es, "lf%d" % h, [128, NB], F32)
            o.G = P.sb(es, "G%d" % h, [128, NB], F32)
            o.tmp = P.sb(es, "tmp%d" % h, [128, NB], F32)
            o.tmp2 = P.sb(es, "tmp2%d" % h, [128, NB], F32)
            o.qd = P.sb(es, "qd%d" % h, [128, NB], BF16)
            o.kdd = P.sb(es, "kdd%d" % h, [128, NB], BF16)
            o.kend = P.sb(es, "kend%d" % h, [128, NB], BF16)
            o.kT = P.sb(es, "kT%d" % h, [128, 16, 128], BF16)
            o.vb = P.sb(es, "vb%d" % h, [128, 16, 128], BF16)
            o.egl = P.sb(es, "egl%d" % h, [128, 32], F32)
            o.S32 = P.sb(es, "S32_%d" % h, [128, 128], F32)
            o.Sbf = [P.sb(es, "Sbf%d_%d" % (h, i), [128, 128], BF16) for i in range(2)]
            o.am = [P.sb(es, "am%d_%d" % (h, i), [128, 64], BF16) for i in range(2)]
            o.osb = [P.sb(es, "osb%d_%d" % (h, i), [128, 512], F32) for i in range(2)]
            P.op("pool", lambda e, o=o: e.memset(o.S32.t[:, :], 0.0), writes=[o.S32])
            P.op("pool", lambda e, o=o: e.memset(o.Sbf[0].t[:, :], 0.0), writes=[o.Sbf[0]])
            o.si = 0
            hs.append(o)

        def prep(h, blk):
            o = hs[h]
            t0 = blk * NB
            P.dma("sp", o.qb.t[:, :], qd[h][:, t0:t0 + NB], writes=[o.qb])
            P.dma("sp", o.kb.t[:, :], kd[h][:, t0:t0 + NB], writes=[o.kb])
            P.dma("sp", o.lf.t[:, :], lfd[h][:, t0:t0 + NB], writes=[o.lf])
            P.dma("pool", o.vb.t[:, :, :], vd[h][:, blk * 2048:(blk + 1) * 2048].rearrange("p (t d) -> p t d", d=128), writes=[o.vb])
            P.op("dve", lambda e: e.tensor_tensor_scan(o.G.t[:, :], rm.t[:, :], o.lf.t[:, :], 0.0, ALU.mult, ALU.add),
                 reads=[rm, o.lf], writes=[o.G])
            P.op("act", lambda e: e.activation(o.tmp.t[:, :], o.G.t[:, :], AF.Exp), reads=[o.G], writes=[o.tmp])
            P.op("dve", lambda e: e.tensor_tensor(o.qd.t[:, :], o.qb.t[:, :], o.tmp.t[:, :], ALU.mult), reads=[o.qb, o.tmp], writes=[o.qd])
            P.op("act", lambda e: e.activation(o.tmp2.t[:, :], o.G.t[:, :], AF.Exp, scale=-1.0), reads=[o.G], writes=[o.tmp2])
            P.op("dve", lambda e: e.tensor_tensor(o.tmp2.t[:, :], o.kb.t[:, :], o.tmp2.t[:, :], ALU.mult), reads=[o.kb, o.tmp2], writes=[o.tmp2])
            P.op("dve", lambda e: e.tensor_copy(o.kdd.t[:, :], o.tmp2.t[:, :]), reads=[o.tmp2], writes=[o.kdd])
            G3 = o.G.t[:, :].rearrange("p (c s) -> p c s", s=64)
            P.op("act", lambda e: e.activation(o.egl.t[:, :], G3[:, :, 63], AF.Exp), reads=[o.G], writes=[o.egl])
            for cc in range(32):
                P.op("dve", lambda e, cc=cc: e.tensor_scalar(o.kend.t[:, cc * 64:(cc + 1) * 64], o.tmp2.t[:, cc * 64:(cc + 1) * 64],
                                                             o.egl.t[:, cc:cc + 1], None, ALU.mult),
                     reads=[o.tmp2, o.egl], writes=[o.kend])
            for grp in range(2):
                for j in range(8):
                    tk = grp * 8 + j
                    P.op("pe", lambda e, tk=tk, j=j: e.transpose(bankT.t[:, j * 128:(j + 1) * 128], o.kend.t[:, tk * 128:(tk + 1) * 128], ident.t[:, :]),
                         reads=[o.kend, ident], writes=[bankT], pe_acc=(j > 0))
                P.op("act", lambda e, grp=grp: e.activation(o.kT.t[:, grp * 8:(grp + 1) * 8, :],
                                                           bankT.t[:, :].rearrange("p (t k) -> p t k", k=128), AF.Copy),
                     reads=[bankT], writes=[o.kT])

        nA = [0]

        def chunk(h, blk, cc):
            o = hs[h]
            tk, half = cc // 2, cc % 2
            pb = 64 * half
            gc = blk * 32 + cc
            cs = slice(cc * 64, (cc + 1) * 64)
            bA = bankA[nA[0] % 2]
            am = o.am[nA[0] % 2]
            nA[0] += 1
            bO = bankO[h]
            bU = bankU[h]
            oc0 = (gc % 8) * 64
            Sb = o.Sbf[o.si % 2]
            Sn = o.Sbf[(o.si + 1) % 2]
            o.si += 1
            P.op("pe", lambda e: e.matmul(bA.t[pb:pb + 64, 0:64], o.kdd.t[:, cs], o.qd.t[:, cs], start=True, stop=True),
                 reads=[o.kdd, o.qd], writes=[bA])
            P.op("dve", lambda e: e.tensor_tensor(am.t[pb:pb + 64, :], bA.t[pb:pb + 64, 0:64], m01.t[pb:pb + 64, :], ALU.mult),
                 reads=[bA, m01], writes=[am])
            P.op("pe", lambda e: e.matmul(bO.t[:, oc0:oc0 + 64], Sb.t[:, :], o.qd.t[:, cs], start=True, stop=False),
                 reads=[Sb, o.qd], writes=[bO], pe_acc=(gc % 8 != 0))
            P.op("pe", lambda e: e.matmul(bO.t[:, oc0:oc0 + 64], o.vb.t[pb:pb + 64, tk, :], am.t[pb:pb + 64, :], start=False, stop=True),
                 reads=[o.vb, am], writes=[bO], pe_acc=True)
            P.op("pe", lambda e: e.matmul(bU.t[:, 0:128], o.kT.t[pb:pb + 64, tk, :], o.vb.t[pb:pb + 64, tk, :], start=True, stop=True),
                 reads=[o.kT, o.vb], writes=[bU])
            P.op("dve", lambda e: e.scalar_tensor_tensor(o.S32.t[:, :], o.S32.t[:, :], o.egl.t[:, cc:cc + 1], bU.t[:, 0:128], ALU.mult, ALU.add),
                 reads=[o.S32, o.egl, bU], writes=[o.S32])
            P.op("act", lambda e: e.activation(Sn.t[:, :], o.S32.t[:, :], AF.Copy), reads=[o.S32], writes=[Sn])
            if gc % 8 == 7:
                os_ = o.osb[(gc // 8) % 2]
                P.op("act", lambda e: e.activation(os_.t[:, :], bO.t[:, :], AF.Copy), reads=[bO], writes=[os_])
                tok0 = (gc - 7) * 64
                P.dma("sp", od[h][:, tok0:tok0 + 512], os_.t[:, :], reads=[os_], ow=ob)

        for blk in range(4):
            for h in range(2):
                prep(h, blk)
            for cc in range(32):
                for h in range(2):
                    chunk(h, blk, cc)
        P.finish([ob])
        P.emit()
    return nc


def hgrn_consts():
    p = np.arange(128)
    t = np.arange(64)
    m01 = ((p[:, None] % 64) <= t[None, :]).astype(np.float32)
    rm = np.ones((128, 2048), np.float32)
    rm[:, ::64] = 0.0
    ident = np.eye(128, dtype=np.float32).astype(NPBF)
    return m01, rm, ident


def build_mega():
    nc = bass.Bass("TRN2", target_bir_lowering=False)
    P = Prog(nc)
    c = Ctx()
    c.P, c.nc = P, nc
    dr = {}

    def din(name, shape, dt=F32):
        dr[name] = nc.dram_tensor(name, list(shape), dt, kind="ExternalInput").ap()
        return dr[name]

    def dout(name, shape, dt=F32):
        dr[name] = nc.dram_tensor(name, list(shape), dt, kind="ExternalOutput").ap()
        return dr[name]

    xT = din("xT", [D, T])
    cTd = din("cT", [128, 8])
    modbd = din("modb", [128, 144])
    modwd = din("modw", [18, 128, 8192])
    gT = din("gT", [128, 48])
    outs = []
    pid = nc.partition_id()
    g4 = pid % 4
    G4 = [[0, 1, 2, 3], [4, 5, 6, 7]]
    idram = lambda name, shape, dt: nc.dram_tensor(name, list(shape), dt)
    with ExitStack() as es:
        X = es.enter_context(nc.sbuf_tensor("X", [128, 8, T], F32))
        c.X = X
        c.Xb = [[Buf(X) for _ in range(4)] for _ in range(8)]
        c.modt = P.sb(es, "modt", [128, 144], F32)
        c.gt = P.sb(es, "gt", [128, 48], F32)
        c.der = P.sb(es, "der", [128, 96], F32)
        c.ones = P.sb(es, "ones", [128, 128], BF16)
        c.bank = [P.ps(es, "bank%d" % i, [128, 512], F32) for i in range(7)]
        bankT = P.ps(es, "bankT", [128, 1024], BF16)
        sqt = es.enter_context(nc.sbuf_tensor("sq", [128, 8, 512], BF16))
        c.sq = [Buf(sqt) for _ in range(8)]
        c.rstd = P.sb(es, "rstd", [128, 512], F32)
        c.tmp = [P.sb(es, "tmp%d" % i, [128, 512], F32) for i in range(2)]

        xv = xT.rearrange("(c p) t -> p c t", p=128)
        for kc in range(8):
            P.dma("sp", X[:, kc, :], xv[:, kc, :], writes=[c.Xb[kc][tt] for tt in range(4)])
        P.dma("sp", c.gt.t[:, :], gT, writes=[c.gt])
        P.op("pool", lambda e: e.memset(c.ones.t[:, :], 1.0), writes=[c.ones])
        with ExitStack() as e0:
            ct = P.sb(e0, "ct", [128, 8], F32)
            ctb = P.sb(e0, "ctb", [128, 8], BF16)
            mb = P.sb(e0, "mb", [128, 144], F32)
            mw = [P.sb(e0, "mw%d" % i, [128, 8, 1024], BF16) for i in range(2)]
            P.dma("sp", ct.t[:, :], cTd, writes=[ct])
            P.dma("sp", mb.t[:, :], modbd, writes=[mb])
            P.op("act", lambda e: e.activation(ctb.t[:, :], ct.t[:, :], AF.Silu), reads=[ct], writes=[ctb])
            bm = c.bank[5]
            for v in range(18):
                w_ = mw[v % 2]
                P.dma("pool", w_.t[:, :, :], modwd[v].rearrange("p (k n) -> p k n", k=8), writes=[w_])
                for ch in range(8):
                    col = v * 8 + ch
                    for kc in range(8):
                        P.op("pe", lambda e, w_=w_, ch=ch, kc=kc, col=col: e.matmul(
                            bm.t[:, col:col + 1], w_.t[:, kc, ch * 128:(ch + 1) * 128], ctb.t[:, kc:kc + 1],
                            start=(kc == 0), stop=(kc == 7)),
                            reads=[w_, ctb], writes=[bm], pe_acc=not (v == 0 and ch == 0 and kc == 0))
            P.op("dve", lambda e: e.tensor_tensor(c.modt.t[:, :], bm.t[:, 0:144], mb.t[:, :], ALU.add),
                 reads=[bm, mb], writes=[c.modt])
        P.barrier()
        for l in range(2):
            for sub in range(3):
                base = ((l * 3 + sub) * 2) * 8
                sc0 = mcol(l, sub * 3 + 1, 0)
                g0 = (l * 3 + sub) * 8
                ga0 = mcol(l, sub * 3 + 2, 0)
                P.op("dve", lambda e, base=base, sc0=sc0, g0=g0: e.scalar_tensor_tensor(
                    c.der.t[:, base:base + 8], c.modt.t[:, sc0:sc0 + 8], 1.0, c.gt.t[:, g0:g0 + 8], ALU.add, ALU.mult),
                    reads=[c.modt, c.gt], writes=[c.der])
                P.op("dve", lambda e, base=base: e.tensor_scalar(
                    c.der.t[:, base:base + 8], c.der.t[:, base:base + 8], SQD, None, ALU.mult),
                    reads=[c.der], writes=[c.der])
                P.op("dve", lambda e, base=base, ga0=ga0, sub=sub: e.tensor_scalar(
                    c.der.t[:, base + 8:base + 16], c.modt.t[:, ga0:ga0 + 8], (1.0 if sub == 1 else 0.5), None, ALU.mult),
                    reads=[c.modt], writes=[c.der])

        def Acol(l, sub, ch):
            j = ((l * 3 + sub) * 2) * 8 + ch
            return c.der.t[:, j:j + 1]

        def Gcol(l, sub, ch):
            j = ((l * 3 + sub) * 2 + 1) * 8 + ch
            return c.der.t[:, j:j + 1]

        def Scol(l, sub, ch):
            j = mcol(l, sub * 3 + 0, ch)
            return c.modt.t[:, j:j + 1]

        def rstd_tile(src_fn, src_bufs, epsk):
            for kc in range(8):
                P.op("act", lambda e, kc=kc: e.activation(sqt[:, kc, :], src_fn(kc), AF.Square),
                     reads=[src_bufs[kc]], writes=[c.sq[kc]])
            for kc in range(8):
                P.op("pe", lambda e, kc=kc: e.matmul(c.bank[6].t[:, :], c.ones.t[:, :], sqt[:, kc, :],
                                                      start=(kc == 0), stop=(kc == 7)),
                     reads=[c.ones, c.sq[kc]], writes=[c.bank[6]], pe_acc=(kc > 0))
            P.op("dve", lambda e: e.tensor_scalar(c.rstd.t[:, :], c.bank[6].t[:, :], epsk, None, ALU.add),
                 reads=[c.bank[6]], writes=[c.rstd])
            P.op("act", lambda e: e.activation(c.rstd.t[:, :], c.rstd.t[:, :], AF.Sqrt), reads=[c.rstd], writes=[c.rstd])
            P.op("dve", lambda e: e.reciprocal(c.rstd.t[:, :], c.rstd.t[:, :]), reads=[c.rstd], writes=[c.rstd])

        def modnorm_tile(l, sub, tt, hdst, hbuf):
            t0 = tt * 512
            rstd_tile(lambda kc: X[:, kc, t0:t0 + 512], [c.Xb[kc][tt] for kc in range(8)], EPS * D)
            for kc in range(8):
                tb = c.tmp[kc % 2]
                P.op("dve", lambda e, kc=kc, tb=tb: e.tensor_tensor(tb.t[:, :], X[:, kc, t0:t0 + 512], c.rstd.t[:, :], ALU.mult),
                     reads=[c.Xb[kc][tt], c.rstd], writes=[tb])
                P.op("act", lambda e, kc=kc, tb=tb: e.activation(hdst(kc), tb.t[:, :], AF.Identity,
                                                               bias=Scol(l, sub, kc), scale=Acol(l, sub, kc)),
                     reads=[tb, c.der, c.modt], writes=[hbuf(kc)])

        def epilogue(kind, l, oG, oGb, sgd, sgb):
            wod = din(kind + "_wo", [128, 8192])
            oS, oSb = dsel(kind + "_oS", [1, D, T], BF16, oG.ap()[bass.ds(g4, 1), :, :], oGb)
            sv = sgd.ap().rearrange("(c p) t -> p c t", p=128)
            with ExitStack() as e2:
                wo = P.sb(e2, kind + "wo_sb", [128, 8, 1024], BF16)
                P.dma("pool", wo.t[:, :, :], wod.rearrange("p (k d) -> p k d", k=8), writes=[wo])
                ot = [P.sb(e2, kind + "ot%d" % i, [128, 8, 512], BF16) for i in range(2)]
                st = [P.sb(e2, kind + "st%d" % i, [128, 8, 512], BF16) for i in range(2)]
                ogt = [e2.enter_context(nc.sbuf_tensor(kind + "og%d" % i, [128, 8, 512], BF16)) for i in range(2)]
                ogb = [[Buf(ogt[i]) for _ in range(8)] for i in range(2)]
                if kind == "hgrn":
                    hgd = din("hgn", [128, 8])
                    hg = P.sb(e2, "hg", [128, 8], F32)
                    P.dma("sp", hg.t[:, :], hgd, writes=[hg])
                    P.op("dve", lambda e: e.tensor_scalar(hg.t[:, :], hg.t[:, :], float(np.sqrt(128.0)), None, ALU.mult),
                         reads=[hg], writes=[hg])
                    sq1 = P.sb(e2, "sq1", [128, 512], BF16)
                    r1 = P.sb(e2, "r1", [128, 512], F32)
                    t1 = P.sb(e2, "t1", [128, 512], F32)
                for tt in range(4):
                    t0 = tt * 512
                    o_, s_, og_ = ot[tt % 2], st[tt % 2], ogt[tt % 2]
                    P.dma("sp", o_.t[:, :, :], oS.ap()[0].rearrange("(c p) s -> p c s", p=128)[:, :, t0:t0 + 512], reads=[oSb], writes=[o_])
                    P.dma("sp", s_.t[:, :, :], sv[:, :, t0:t0 + 512], reads=[sgb], writes=[s_])
                    if kind == "fox":
                        for kc in range(8):
                            P.op("dve", lambda e, kc=kc, o_=o_, s_=s_, og_=og_: e.tensor_tensor(
                                og_[:, kc, :], o_.t[:, kc, :], s_.t[:, kc, :], ALU.mult),
                                reads=[o_, s_], writes=[ogb[tt % 2][kc]])
                    else:
                        for kc in range(8):
                            P.op("act", lambda e, kc=kc, o_=o_: e.activation(sq1.t[:, :], o_.t[:, kc, :], AF.Square),
                                 reads=[o_], writes=[sq1])
                            P.op("pe", lambda e: e.matmul(c.bank[5].t[:, :], c.ones.t[:, :], sq1.t[:, :], start=True, stop=True),
                                 reads=[c.ones, sq1], writes=[c.bank[5]])
                            P.op("dve", lambda e: e.tensor_scalar(r1.t[:, :], c.bank[5].t[:, :], EPS * 128.0, None, ALU.add),
                                 reads=[c.bank[5]], writes=[r1])
                            P.op("act", lambda e: e.activation(r1.t[:, :], r1.t[:, :], AF.Sqrt), reads=[r1], writes=[r1])
                            P.op("dve", lambda e: e.reciprocal(r1.t[:, :], r1.t[:, :]), reads=[r1], writes=[r1])
                            P.op("dve", lambda e, kc=kc, o_=o_: e.tensor_tensor(t1.t[:, :], o_.t[:, kc, :], r1.t[:, :], ALU.mult),
                                 reads=[o_, r1], writes=[t1])
                            P.op("dve", lambda e, kc=kc, s_=s_, og_=og_: e.scalar_tensor_tensor(
                                og_[:, kc, :], t1.t[:, :], hg.t[:, kc:kc + 1], s_.t[:, kc, :], ALU.mult, ALU.mult),
                                reads=[t1, hg, s_], writes=[ogb[tt % 2][kc]])
                    for dc in range(8):
                        bk = c.bank[4 + dc % 2]
                        for kc in range(8):
                            P.op("pe", lambda e, kc=kc, dc=dc, bk=bk, og_=og_: e.matmul(
                                bk.t[:, :], wo.t[:, kc, dc * 128:(dc + 1) * 128], og_[:, kc, :],
                                start=(kc == 0), stop=(kc == 7)),
                                reads=[wo, ogb[tt % 2][kc]], writes=[bk], pe_acc=(kc > 0))
                        P.op("dve", lambda e, dc=dc, bk=bk, t0=t0: e.scalar_tensor_tensor(
                            X[:, dc, t0:t0 + 512], bk.t[:, :], Gcol(l, 1, dc), X[:, dc, t0:t0 + 512], ALU.mult, ALU.add),
                            reads=[bk, c.der, c.Xb[dc][tt]], writes=[c.Xb[dc][tt]])
            P.barrier()

        def ffn(j, l, sub):
            wupd = din("wup%d" % j, [11, 128, 4096])
            wdnd = din("wdn%d" % j, [8, 128, 2816])
            with ExitStack() as e2:
                hbt = e2.enter_context(nc.sbuf_tensor("hb_%d" % j, [128, 8, 1024], BF16))
                hbb = [[Buf(hbt) for _ in range(2)] for _ in range(8)]
                actt = e2.enter_context(nc.sbuf_tensor("actb_%d" % j, [128, NF, 1024], BF16))
                actb = [[Buf(actt) for _ in range(2)] for _ in range(NF)]
                wu = [P.sb(e2, "wu%d_%d" % (j, i), [128, 2, 8, 256], BF16) for i in range(2)]
                wd = [P.sb(e2, "wd%d_%d" % (j, i), [128, NF, 128], BF16) for i in range(2)]
                sa = [P.sb(e2, "sa%d_%d" % (j, i), [128, 512], F32) for i in range(2)]
                for half in range(2):
                    for t2 in range(2):
                        tt = half * 2 + t2
                        modnorm_tile(l, sub, tt, lambda kc, t2=t2: hbt[:, kc, t2 * 512:(t2 + 1) * 512],
                                     lambda kc, t2=t2: hbb[kc][t2])
                    it = 0
                    for g in range(11):
                        w_ = wu[g % 2]
                        P.dma("pool", w_.t[:, :, :, :], wupd[g].rearrange("p (a k f) -> p a k f", a=2, k=8), writes=[w_])
                        for jf in range(2):
                            fc = 2 * g + jf
                            for t2 in range(2):
                                bA, bB = c.bank[it % 2], c.bank[2 + it % 2]
                                s_ = sa[it % 2]
                                it += 1
                                for kc in range(8):
                                    P.op("pe", lambda e, kc=kc, w_=w_, jf=jf, t2=t2, bA=bA: e.matmul(
                                        bA.t[:, :], w_.t[:, 0, kc, jf * 128:(jf + 1) * 128], hbt[:, kc, t2 * 512:(t2 + 1) * 512],
                                        start=(kc == 0), stop=(kc == 7)),
                                        reads=[w_, hbb[kc][t2]], writes=[bA], pe_acc=(kc > 0))
                                for kc in range(8):
                                    P.op("pe", lambda e, kc=kc, w_=w_, jf=jf, t2=t2, bB=bB: e.matmul(
                                        bB.t[:, :], w_.t[:, 1, kc, jf * 128:(jf + 1) * 128], hbt[:, kc, t2 * 512:(t2 + 1) * 512],
                                        start=(kc == 0), stop=(kc == 7)),
                                        reads=[w_, hbb[kc][t2]], writes=[bB], pe_acc=(kc > 0))
                                P.op("act", lambda e, s_=s_, bA=bA: e.activation(s_.t[:, :], bA.t[:, :], AF.Silu),
                                     reads=[bA], writes=[s_])
                                P.op("dve", lambda e, s_=s_, bB=bB, fc=fc, t2=t2: e.tensor_tensor(
                                    actt[:, fc, t2 * 512:(t2 + 1) * 512], bB.t[:, :], s_.t[:, :], ALU.mult),
                                    reads=[bB, s_], writes=[actb[fc][t2]])
                    for dc in range(8):
                        w_ = wd[dc % 2]
                        P.dma("pool", w_.t[:, :, :], wdnd[dc].rearrange("p (f d) -> p f d", f=NF), writes=[w_])
                        for t2 in range(2):
                            tt = half * 2 + t2
                            t0 = tt * 512
                            bk = c.bank[4 + (dc * 2 + t2) % 2]
                            for fc in range(NF):
                                P.op("pe", lambda e, fc=fc, w_=w_, t2=t2, bk=bk: e.matmul(
                                    bk.t[:, :], w_.t[:, fc, :], actt[:, fc, t2 * 512:(t2 + 1) * 512],
                                    start=(fc == 0), stop=(fc == NF - 1)),
                                    reads=[w_, actb[fc][t2]], writes=[bk], pe_acc=(fc > 0))
                            P.op("dve", lambda e, dc=dc, bk=bk, t0=t0: e.scalar_tensor_tensor(
                                X[:, dc, t0:t0 + 512], bk.t[:, :], Gcol(l, sub, dc), X[:, dc, t0:t0 + 512], ALU.mult, ALU.add),
                                reads=[bk, c.der, c.Xb[dc][tt]], writes=[c.Xb[dc][tt]])
            P.barrier()

        def proj_fm(wname, hbt, hbb, evac, n_oc=8):
            wd_ = din(wname, [n_oc, 128, 1024])
            with ExitStack() as e3:
                wp = c.wp
                it = 0
                for oc in range(n_oc):
                    w_ = wp[oc % 2]
                    P.dma("pool", w_.t[:, :, :], wd_[oc].rearrange("p (k f) -> p k f", k=8), writes=[w_])
                    for tt in range(4):
                        bk = c.bank[it % 4]
                        it += 1
                        for kc in range(8):
                            P.op("pe", lambda e, kc=kc, w_=w_, tt=tt, bk=bk: e.matmul(
                                bk.t[:, :], w_.t[:, kc, :], hbt[:, kc, tt * 512:(tt + 1) * 512],
                                start=(kc == 0), stop=(kc == 7)),
                                reads=[w_, hbb[kc][tt]], writes=[bk], pe_acc=(kc > 0))
                        evac(oc, tt, bk)

        def proj_tm(wname, hbt, hbb, vout, vob, func):
            wd_ = din(wname, [2, 128, 4096])
            with ExitStack() as e3:
                wv = c.wv
                for cg in range(2):
                    P.dma("pool", wv.t[:, cg, :, :], wd_[cg].rearrange("p (k f) -> p k f", k=8), writes=[wv])
                vt = c.vt
                it = 0
                for tk in range(16):
                    for cg in range(2):
                        bk = c.bank[it % 4]
                        v_ = vt[it % 2]
                        it += 1
                        for kc in range(8):
                            P.op("pe", lambda e, kc=kc, tk=tk, cg=cg, bk=bk: e.matmul(
                                bk.t[:, :], hbt[:, kc, tk * 128:(tk + 1) * 128], wv.t[:, cg, kc, :],
                                start=(kc == 0), stop=(kc == 7)),
                                reads=[wv, hbb[kc][tk // 4]], writes=[bk], pe_acc=(kc > 0))
                        P.op("act", lambda e, bk=bk, v_=v_: e.activation(v_.t[:, :], bk.t[:, :], func),
                             reads=[bk], writes=[v_])
                        P.dma("sp", vout[tk * 128:(tk + 1) * 128, cg * 512:(cg + 1) * 512], v_.t[:, :], reads=[v_], ow=vob)

        def stage_out(e3, name, shape, dt):
            return [P.sb(e3, name + "%d" % i, shape, dt) for i in range(2)]

        def projections(kind, l):
            R = Ctx()
            with ExitStack() as e2:
                hbt = e2.enter_context(nc.sbuf_tensor(kind + "hb2", [128, 8, T], BF16))
                hbb = [[Buf(hbt) for _ in range(4)] for _ in range(8)]
                c.wp = [P.sb(e2, kind + "wp%d" % i, [128, 8, 128], BF16) for i in range(2)]
                c.wv = P.sb(e2, kind + "wvsb", [128, 2, 8, 512], BF16)
                c.vt = [P.sb(e2, kind + "vt%d" % i, [128, 512], BF16) for i in range(2)]
                for tt in range(4):
                    modnorm_tile(l, 1, tt, lambda kc, tt=tt: hbt[:, kc, tt * 512:(tt + 1) * 512],
                                 lambda kc, tt=tt: hbb[kc][tt])
                kdt = BF16 if kind == "fox" else F32
                R.q, R.qb = idram(kind + "_q", [D, T], BF16), Buf()
                R.k, R.kb = idram(kind + "_k", [D, T], kdt), Buf()
                R.sg, R.sgb = idram(kind + "_sg", [D, T], BF16), Buf()
                R.v, R.vb = idram(kind + "_v", [T, D], BF16), Buf()
                qo, ko, sgo, vo = R.q.ap(), R.k.ap(), R.sg.ap(), R.v.ap()
                qob, kob, sgob, vob = R.qb, R.kb, R.sgb, R.vb
                cnt = [0]

                def simple_evac(od, ob, func, scale, st):
                    def evac(oc, tt, bk):
                        s_ = st[cnt[0] % 2]
                        cnt[0] += 1
                        P.op("act", lambda e, s_=s_, bk=bk: e.activation(s_.t[:, :], bk.t[:, :], func, scale=scale),
                             reads=[bk], writes=[s_])
                        P.dma("sp", od[oc * 128:(oc + 1) * 128, tt * 512:(tt + 1) * 512], s_.t[:, :], reads=[s_], ow=ob)
                    return evac

                stb = stage_out(e2, kind + "stb", [128, 512], BF16)
                if kind == "fox":
                    R.lf, R.lfb = idram("fox_lf", [128, 256], F32), Buf()
                    proj_fm("fox_wq", hbt, hbb, simple_evac(qo, qob, AF.Copy, float(FD ** -0.5), stb))
                    proj_fm("fox_wk", hbt, hbb, simple_evac(ko, kob, AF.Copy, 1.0, stb))
                    proj_fm("fox_wg", hbt, hbb, simple_evac(sgo, sgob, AF.Sigmoid, 1.0, stb))
                    proj_tm("fox_wv", hbt, hbb, vo, vob, AF.Copy)
                    wfd = din("fox_wf", [128, 128])
                    bfd = din("fox_bfb", [128, 256])
                    wf = P.sb(e2, "wf_sb", [128, 8, 16], BF16)
                    P.dma("pool", wf.t[:, :, :], wfd.rearrange("p (k f) -> p k f", k=8), writes=[wf])
                    bfb = P.sb(e2, "bfb", [128, 256], F32)
                    P.dma("sp", bfb.t[:, :], bfd, writes=[bfb])
                    z1 = P.sb(e2, "z1", [128, 256], F32)
                    bk = c.bank[0]
                    for tk in range(16):
                        for kc in range(8):
                            P.op("pe", lambda e, kc=kc, tk=tk: e.matmul(
                                bk.t[:, tk * 16:(tk + 1) * 16], hbt[:, kc, tk * 128:(tk + 1) * 128], wf.t[:, kc, :],
                                start=(kc == 0), stop=(kc == 7)),
                                reads=[wf, hbb[kc][tk // 4]], writes=[bk], pe_acc=not (tk == 0 and kc == 0))
                    P.op("dve", lambda e: e.tensor_tensor(z1.t[:, :], bk.t[:, 0:256], bfb.t[:, :], ALU.add), reads=[bk, bfb], writes=[z1])
                    P.op("act", lambda e: e.activation(z1.t[:, :], z1.t[:, :], AF.Exp, scale=-1.0), reads=[z1], writes=[z1])
                    P.op("act", lambda e: e.activation(z1.t[:, :], z1.t[:, :], AF.Ln, bias=1.0, scale=1.0), reads=[z1], writes=[z1])
                    P.op("dve", lambda e: e.tensor_scalar(z1.t[:, :], z1.t[:, :], -1.0, None, ALU.mult), reads=[z1], writes=[z1])
                    P.dma("sp", R.lf.ap(), z1.t[:, :], reads=[z1], ow=R.lfb)
                else:
                    R.lf, R.lfb = idram("hgrn_lf", [D, T], F32), Buf()
                    lfo, lfob = R.lf.ap(), R.lfb
                    lbd = din("lbl", [128, 16])
                    lbl = P.sb(e2, "lbl_sb", [128, 16], F32)
                    lb = P.sb(e2, "lb", [128, 8], F32)
                    oml = P.sb(e2, "oml", [128, 8], F32)
                    P.dma("sp", lbl.t[:, :], lbd, writes=[lbl])
                    P.op("dve", lambda e: e.tensor_tensor(lb.t[:, :], lbl.t[:, 8:16], lbl.t[:, 0:8], ALU.subtract), reads=[lbl], writes=[lb])
                    P.op("act", lambda e: e.activation(lb.t[:, :], lb.t[:, :], AF.Sigmoid), reads=[lb], writes=[lb])
                    P.op("dve", lambda e: e.tensor_scalar(oml.t[:, :], lb.t[:, :], -1.0, 1.0, ALU.mult, ALU.add), reads=[lb], writes=[oml])
                    proj_fm("hgrn_wq", hbt, hbb, simple_evac(qo, qob, AF.Copy, 1.0, stb))
                    proj_fm("hgrn_wg", hbt, hbb, simple_evac(sgo, sgob, AF.Silu, 1.0, stb))
                    proj_tm("hgrn_wv", hbt, hbb, vo, vob, AF.Silu)
                    sg1 = P.sb(e2, "sg1", [128, 512], F32)
                    ff = stage_out(e2, "ff", [128, 512], F32)
                    lff = stage_out(e2, "lff", [128, 512], F32)
                    kk = stage_out(e2, "kk", [128, 512], F32)

                    def f_evac(oc, tt, bk):
                        i = cnt[0] % 2
                        cnt[0] += 1
                        f_, l_, k_ = ff[i], lff[i], kk[i]
                        P.op("act", lambda e, bk=bk: e.activation(sg1.t[:, :], bk.t[:, :], AF.Sigmoid), reads=[bk], writes=[sg1])
                        P.op("dve", lambda e, f_=f_, oc=oc: e.tensor_scalar(f_.t[:, :], sg1.t[:, :], oml.t[:, oc:oc + 1], lb.t[:, oc:oc + 1], ALU.mult, ALU.add),
                             reads=[sg1, oml, lb], writes=[f_])
                        P.op("act", lambda e, f_=f_, l_=l_: e.activation(l_.t[:, :], f_.t[:, :], AF.Ln), reads=[f_], writes=[l_])
                        P.dma("sp", lfo[oc * 128:(oc + 1) * 128, tt * 512:(tt + 1) * 512], l_.t[:, :], reads=[l_], ow=lfob)
                    proj_fm("hgrn_wf", hbt, hbb, f_evac)
            P.barrier()
            return R

        def gather(name, src, srcb, nch, rows, cols, dt):
            dst = idram(name, [nch, 4 * rows, cols], dt)
            db = Buf()
            sv = src.ap() if len(src.shape) == 2 else None
            for j in range(nch):
                sa = src.ap()[j * rows:(j + 1) * rows, :] if sv is not None else src.ap()[j]
                P.collective("AllGather", G4, sa.opt(), dst.ap()[j].opt(), [srcb], db)
            return dst, db

        def dsel(name, shape, dt, src_dyn, srcb):
            dst = idram(name, shape, dt)
            db = Buf()
            P.dma("sp", dst.ap(), src_dyn, reads=[srcb], writes=[db])
            return dst, db

        def fox_phase(R):
            o_loc, olb = idram("fox_o", [4, 256, T], BF16), Buf()
            shi, slo = idram("shi", [4, S], BF16), idram("slo", [4, S], BF16)
            shb, slb = Buf(), Buf()
            Ud, seld, mkd, idfd = din("U", [128, 128]), din("sel", [128, 128]), din("mk", [128, 128]), din("identf", [128, 128])
            bank = c.bank
            with ExitStack() as e2:
                U = P.sb(e2, "U_sb", [128, 128], F32)
                sel = P.sb(e2, "sel_sb", [128, 128], F32)
                mk = P.sb(e2, "mk_sb", [128, 128], F32)
                idf = P.sb(e2, "idf_sb", [128, 128], F32)
                onesf = P.sb(e2, "onesf", [128, 128], F32)
                negB = P.sb(e2, "negB", [128, 4 * 16 * 64], F32)
                for t_, d_ in ((U, Ud), (sel, seld), (mk, mkd), (idf, idfd)):
                    P.dma("sp", t_.t[:, :], d_, writes=[t_])
                P.op("pool", lambda e: e.memset(onesf.t[:, :], 1.0), writes=[onesf])
                lG, lGb = gather("fox_lG", R.lf, R.lfb, 1, 128, 256, F32)
                qG, qGb = gather("fox_qG", R.q, R.qb, 4, 256, T, BF16)
                kG, kGb = gather("fox_kG", R.k, R.kb, 4, 256, T, BF16)
                vG, vGb = gather("fox_vG", R.v, R.vb, 4, 512, D, BF16)
                lS, lSb = dsel("fox_lS", [512, 16, 4], F32, lG.ap()[0].rearrange("r (k h) -> r k h", h=16)[:, :, bass.ds(g4 * 4, 4)], lGb)
                with ExitStack() as e3:
                    lsel = P.sb(e3, "lsel", [128, 4, 16, 4], F32)
                    lt = P.sb(e3, "lt_sb", [128, 256], F32)
                    within = P.sb(e3, "within", [128, 256], F32)
                    tot = P.sb(e3, "tot", [128, 256], F32)
                    inc = P.sb(e3, "inc", [128, 256], F32)
                    GT = P.sb(e3, "GT", [128, 256], F32)
                    gend = P.sb(e3, "gend", [128, 256], F32)
                    Aa = P.sb(e3, "Aa", [128, 256], F32)
                    AT = P.sb(e3, "AT", [64, 512], F32)
                    ahi = P.sb(e3, "ahi", [64, 512], BF16)
                    ahf = P.sb(e3, "ahf", [64, 512], F32)
                    alo = P.sb(e3, "alo", [64, 512], BF16)
                    for t in range(4):
                        P.dma("sp", lsel.t[:, t, :, :], lS.ap()[t * 128:(t + 1) * 128, :, :], reads=[lSb], writes=[lsel])
                    for hl in range(4):
                        P.op("dve", lambda e, hl=hl: e.tensor_copy(
                            lt.t[:, hl * 64:(hl + 1) * 64].rearrange("p (t k) -> p t k", t=4), lsel.t[:, :, :, hl]),
                            reads=[lsel], writes=[lt])
                    P.op("pe", lambda e: e.matmul(bank[6].t[:, 0:256], U.t[:, :], lt.t[:, :], start=True, stop=True), reads=[U, lt], writes=[bank[6]])
                    P.op("pe", lambda e: e.matmul(bank[5].t[:, 0:256], onesf.t[:, :], lt.t[:, :], start=True, stop=True), reads=[onesf, lt], writes=[bank[5]])
                    P.op("dve", lambda e: e.tensor_copy(within.t[:, :], bank[6].t[:, 0:256]), reads=[bank[6]], writes=[within])
                    P.op("dve", lambda e: e.tensor_copy(tot.t[:, :], bank[5].t[:, 0:256]), reads=[bank[5]], writes=[tot])
                    for h in range(4):
                        P.op("dve", lambda e, h=h: e.tensor_tensor_scan(inc.t[:, h * 64:(h + 1) * 64], onesf.t[:, 0:64], tot.t[:, h * 64:(h + 1) * 64],
                                                                        0.0, ALU.mult, ALU.add), reads=[onesf, tot], writes=[inc])
                    P.op("dve", lambda e: e.tensor_tensor(GT.t[:, :], within.t[:, :], inc.t[:, :], ALU.add), reads=[within, inc], writes=[GT])
                    P.op("dve", lambda e: e.tensor_tensor(GT.t[:, :], GT.t[:, :], tot.t[:, :], ALU.subtract), reads=[GT, tot], writes=[GT])
                    P.op("pe", lambda e: e.matmul(bank[6].t[:, 0:256], sel.t[:, :], GT.t[:, :], start=True, stop=True), reads=[sel, GT], writes=[bank[6]])
                    P.op("dve", lambda e: e.tensor_copy(gend.t[:, :], bank[6].t[:, 0:256]), reads=[bank[6]], writes=[gend])
                    for h in range(4):
                        for Q in range(16):
                            j0 = (h * 16 + Q) * 64
                            gc = h * 64 + 4 * Q + 3
                            P.op("dve", lambda e, h=h, j0=j0, gc=gc: e.tensor_scalar(
                                negB.t[:, j0:j0 + 64], GT.t[:, h * 64:(h + 1) * 64], -1.0, gend.t[:, gc:gc + 1], ALU.mult, ALU.add),
                                reads=[GT, gend], writes=[negB])
                            a0 = h * 64 + 4 * Q
                            P.op("dve", lambda e, a0=a0, gc=gc: e.tensor_scalar(
                                Aa.t[:, a0:a0 + 4], GT.t[:, a0:a0 + 4], gend.t[:, gc:gc + 1], None, ALU.subtract),
                                reads=[GT, gend], writes=[Aa])
                    for h in range(4):
                        P.op("pe", lambda e, h=h: e.matmul(bank[5].t[0:64, h * 128:(h + 1) * 128], Aa.t[:, h * 64:(h + 1) * 64], idf.t[:, :],
                                                           start=True, stop=True), reads=[Aa, idf], writes=[bank[5]], pe_acc=(h > 0))
                    P.op("dve", lambda e: e.tensor_copy(AT.t[:, :], bank[5].t[0:64, :]), reads=[bank[5]], writes=[AT])
                    P.op("dve", lambda e: e.tensor_copy(ahi.t[:, :], AT.t[:, :]), reads=[AT], writes=[ahi])
                    P.op("dve", lambda e: e.tensor_copy(ahf.t[:, :], ahi.t[:, :]), reads=[ahi], writes=[ahf])
                    P.op("dve", lambda e: e.tensor_tensor(alo.t[:, :], AT.t[:, :], ahf.t[:, :], ALU.subtract), reads=[AT, ahf], writes=[alo])
                    P.dma("sp", shi.ap().rearrange("h (k p) -> k h p", p=128), ahi.t[:, :].rearrange("k (h p) -> k h p", h=4), reads=[ahi], writes=[shb])
                    P.dma("sp", slo.ap().rearrange("h (k p) -> k h p", p=128), alo.t[:, :].rearrange("k (h p) -> k h p", h=4), reads=[alo], writes=[slb])
                qS, qSb = dsel("fox_qS", [1, D, T], BF16, qG.ap()[bass.ds(g4, 1), :, :], qGb)
                kS, kSb = dsel("fox_kS", [1, D, T], BF16, kG.ap()[bass.ds(g4, 1), :, :], kGb)
                vS, vSb = idram("fox_vS", [S, 256], BF16), Buf()
                for j in range(4):
                    P.dma("sp", vS.ap().rearrange("(r j i) c -> j r i c", r=4, j=4)[j],
                          vG.ap()[j].rearrange("(r i) c -> r i c", r=4)[:, :, bass.ds(g4 * 256, 256)], reads=[vGb], writes=[vSb])
                P.barrier()
                qa = [P.sb(e2, "qa%d" % i, [128, S], BF16) for i in range(2)]
                ka = [P.sb(e2, "ka%d" % i, [128, S], BF16) for i in range(2)]
                va = [P.sb(e2, "va%d" % i, [128, 64 * 65 + 64], BF16) for i in range(2)]
                pt = [P.sb(e2, "pt%d" % i, [128, 512], BF16) for i in range(4)]
                sbanks = [bank[0], bank[1], bank[2], bank[6]]
                drow = P.sb(e2, "drow", [65, 512], F32)
                rec = P.sb(e2, "rec", [64, 512], F32)
                oo = [P.sb(e2, "oo%d" % i, [64, 512], BF16) for i in range(2)]
                vv = lambda v_: v_.t[:, 0:64 * 65].rearrange("p (t d) -> p t d", d=65)
                for i in range(2):
                    P.op("pool", lambda e, i=i: e.memset(ka[i].t[64:128, :], 0.0), writes=[ka[i]])
                    P.op("pool", lambda e, i=i: e.memset(qa[i].t[64:128, :], 0.0), writes=[qa[i]])
                    P.op("pool", lambda e, i=i: e.memset(ka[i].t[64:66, :], 1.0), writes=[ka[i]])
                    P.op("pool", lambda e, i=i: e.memset(va[i].t[:, :], 0.0), writes=[va[i]])
                    P.op("pool", lambda e, i=i: e.memset(vv(va[i])[:, :, 64:65], 1.0), writes=[va[i]])
                vGv = vS.ap().rearrange("(k p) d -> p k d", p=128)

                def load_head(h):
                    q_, k_, v_ = qa[h % 2], ka[h % 2], va[h % 2]
                    for t in range(4):
                        P.dma("sp", q_.t[0:64, t * T:(t + 1) * T], qS.ap()[0, t * 256 + h * 64:t * 256 + (h + 1) * 64, :], reads=[qSb], writes=[q_])
                        P.dma("pool", k_.t[0:64, t * T:(t + 1) * T], kS.ap()[0, t * 256 + h * 64:t * 256 + (h + 1) * 64, :], reads=[kSb], writes=[k_])
                    P.dma("sp", q_.t[64:65, :], shi.ap()[h:h + 1, :], reads=[shb], writes=[q_])
                    P.dma("sp", q_.t[65:66, :], slo.ap()[h:h + 1, :], reads=[slb], writes=[q_])
                    P.dma("pool", vv(v_)[:, :, 0:64], vGv[:, :, h * 64:(h + 1) * 64], reads=[vSb], writes=[v_])

                load_head(0)

                def do_head(h, q_, k_, v_, nit):
                    items = [(Q, kt) for Q in range(16) for kt in range(4 * Q + 4)]

                    def emit_S(idx, it_no):
                        Q, kt = items[idx]
                        d = kt - 4 * Q
                        c0 = 128 * d if d >= 0 else 0
                        bk = sbanks[it_no % 4]
                        p_ = pt[it_no % 4]
                        P.op("pe", lambda e: e.matmul(bk.t[:, c0:512], k_.t[0:128, kt * 128:(kt + 1) * 128],
                                                      q_.t[0:128, Q * 512 + c0:(Q + 1) * 512], start=True, stop=True),
                             reads=[k_, q_], writes=[bk])
                        if d >= 0:
                            P.op("dve", lambda e: e.tensor_tensor(bk.t[:, c0:c0 + 128], bk.t[:, c0:c0 + 128], mk.t[:, :], ALU.add),
                                 reads=[bk, mk], writes=[bk])
                        jb = (h * 16 + Q) * 64 + kt
                        P.op("act", lambda e: e.activation(p_.t[:, c0:512], bk.t[:, c0:512], AF.Exp, bias=negB.t[:, jb:jb + 1], scale=1.0),
                             reads=[bk, negB], writes=[p_])

                    def emit_PV(idx, it_no):
                        Q, kt = items[idx]
                        d = kt - 4 * Q
                        c0 = 128 * d if d >= 0 else 0
                        p_ = pt[it_no % 4]
                        ob_ = bank[3 + Q % 2]
                        last = (kt == 4 * Q + 3)
                        P.op("pe", lambda e: e.matmul(ob_.t[0:128, c0:512], v_.t[:, kt * 65:kt * 65 + 128], p_.t[:, c0:512], start=(kt == 0), stop=last),
                             reads=[v_, p_], writes=[ob_], pe_acc=(kt > 0))
                        if last:
                            o_ = oo[Q % 2]
                            P.op("act", lambda e: e.activation(drow.t[64:65, :], ob_.t[64:65, :], AF.Copy), reads=[ob_], writes=[drow])
                            P.op("pe", lambda e: e.matmul(bank[5].t[0:64, :], onesf.t[64:65, 0:64], drow.t[64:65, :], start=True, stop=True),
                                 reads=[onesf, drow], writes=[bank[5]])
                            P.op("dve", lambda e: e.reciprocal(rec.t[:, :], bank[5].t[0:64, :]), reads=[bank[5]], writes=[rec])
                            P.op("dve", lambda e: e.tensor_tensor(o_.t[:, :], ob_.t[0:64, :], rec.t[:, :], ALU.mult), reads=[ob_, rec], writes=[o_])
                            P.dma("sp", o_loc.ap()[Q // 4][h * 64:(h + 1) * 64, (Q % 4) * 512:(Q % 4 + 1) * 512], o_.t[:, :], reads=[o_], ow=olb)
                            if h == 3 and Q % 4 == 3:
                                P.collective("AllGather", G4, o_loc.ap()[Q // 4].opt(), oGf.ap()[Q // 4].opt(), [olb], oGfb)

                    n = len(items)
                    emit_S(0, nit)
                    emit_S(1, nit + 1)
                    for idx in range(n):
                        if idx + 2 < n:
                            emit_S(idx + 2, nit + idx + 2)
                        emit_PV(idx, nit + idx)
                    return nit + n

                nit = 0
                oGf = idram("fox_oG", [4, 4 * 256, T], BF16)
                oGfb = Buf()
                for h in range(4):
                    if h + 1 < 4:
                        load_head(h + 1)
                    nit = do_head(h, qa[h % 2], ka[h % 2], va[h % 2], nit)
            P.barrier()
            return oGf, oGfb

        def hgrn_phase(R):
            qG, qGb = gather("hg_qG", R.q, R.qb, 4, 256, T, BF16)
            lG, lGb = gather("hg_lG", R.lf, R.lfb, 8, 128, T, F32)
            vG, vGb = gather("hg_vG", R.v, R.vb, 4, 512, D, BF16)
            qS, qSb = dsel("hg_qS", [1, D, T], BF16, qG.ap()[bass.ds(g4, 1), :, :], qGb)
            lS, lSb = dsel("hg_lS", [2, 512, T], F32, lG.ap()[bass.ds(g4 * 2, 2), :, :], lGb)
            vS, vSb = idram("hg_vS", [S, 256], BF16), Buf()
            for j in range(4):
                P.dma("sp", vS.ap().rearrange("(r j i) c -> j r i c", r=4, j=4)[j],
                      vG.ap()[j].rearrange("(r i) c -> r i c", r=4)[:, :, bass.ds(g4 * 256, 256)], reads=[vGb], writes=[vSb])
            o_loc, olb = idram("hg_o", [4, 256, T], BF16), Buf()
            m01d, rmd, idd = din("m01", [128, 64]), din("rm", [128, 2048]), din("ident", [128, 128], BF16)
            NB = 2048
            bankA, bankO, bankU = c.bank[0:2], c.bank[2:4], c.bank[4:6]
            with ExitStack() as e2:
                m01 = P.sb(e2, "m01_sb", [128, 64], F32)
                rm = P.sb(e2, "rm_sb", [128, NB], F32)
                ident = P.sb(e2, "ident_sb", [128, 128], BF16)
                P.dma("sp", m01.t[:, :], m01d, writes=[m01])
                P.dma("sp", rm.t[:, :], rmd, writes=[rm])
                P.dma("sp", ident.t[:, :], idd, writes=[ident])
                sh = Ctx()
                sh.qb = P.sb(e2, "hqb", [128, NB], BF16)
                sh.kb = P.sb(e2, "hkb", [128, NB], F32)
                sh.lf = P.sb(e2, "hlf", [128, NB], F32)
                sh.G = P.sb(e2, "hG", [128, NB], F32)
                sh.tmp = P.sb(e2, "htmp", [128, NB], F32)
                sh.tmp2 = P.sb(e2, "htmp2", [128, NB], F32)
                sh.kend = P.sb(e2, "hkend", [128, NB], BF16)
                hs = []
                for h in range(2):
                    o = Ctx()
                    o.qd = P.sb(e2, "hqd%d" % h, [128, NB], BF16)
                    o.kdd = P.sb(e2, "hkdd%d" % h, [128, NB], BF16)
                    o.kT = P.sb(e2, "hkT%d" % h, [128, 16, 128], BF16)
                    o.vb = P.sb(e2, "hvb%d" % h, [128, 16, 128], BF16)
                    o.egl = P.sb(e2, "hegl%d" % h, [128, 32], F32)
                    o.S32 = P.sb(e2, "hS32_%d" % h, [128, 128], F32)
                    o.Sbf = [P.sb(e2, "hSbf%d_%d" % (h, i), [128, 128], BF16) for i in range(2)]
                    o.am = [P.sb(e2, "ham%d_%d" % (h, i), [128, 64], BF16) for i in range(2)]
                    o.osb = [P.sb(e2, "hosb%d_%d" % (h, i), [128, 512], BF16) for i in range(2)]
                    P.op("pool", lambda e, o=o: e.memset(o.S32.t[:, :], 0.0), writes=[o.S32])
                    P.op("pool", lambda e, o=o: e.memset(o.Sbf[0].t[:, :], 0.0), writes=[o.Sbf[0]])
                    o.si = 0
                    hs.append(o)
                vGv = vS.ap().rearrange("(t p) d -> p t d", p=128)

                def prep(h, blk):
                    o = hs[h]
                    P.dma("sp", sh.qb.t[:, :], qS.ap()[0, blk * 256 + h * 128:blk * 256 + (h + 1) * 128, :], reads=[qSb], writes=[sh.qb])
                    P.dma("sp", sh.lf.t[:, :], lS.ap()[h, blk * 128:(blk + 1) * 128, :], reads=[lSb], writes=[sh.lf])
                    P.dma("pool", o.vb.t[:, :, :], vGv[:, blk * 16:(blk + 1) * 16, h * 128:(h + 1) * 128], reads=[vSb], writes=[o.vb])
                    P.op("act", lambda e: e.activation(sh.kb.t[:, :], sh.lf.t[:, :], AF.Exp), reads=[sh.lf], writes=[sh.kb])
                    P.op("dve", lambda e: e.tensor_scalar(sh.kb.t[:, :], sh.kb.t[:, :], -1.0, 1.0, ALU.mult, ALU.add), reads=[sh.kb], writes=[sh.kb])
                    P.op("dve", lambda e: e.tensor_tensor_scan(sh.G.t[:, :], rm.t[:, :], sh.lf.t[:, :], 0.0, ALU.mult, ALU.add),
                         reads=[rm, sh.lf], writes=[sh.G])
                    P.op("act", lambda e: e.activation(sh.tmp.t[:, :], sh.G.t[:, :], AF.Exp), reads=[sh.G], writes=[sh.tmp])
                    P.op("dve", lambda e: e.tensor_tensor(o.qd.t[:, :], sh.qb.t[:, :], sh.tmp.t[:, :], ALU.mult), reads=[sh.qb, sh.tmp], writes=[o.qd])
                    P.op("act", lambda e: e.activation(sh.tmp2.t[:, :], sh.G.t[:, :], AF.Exp, scale=-1.0), reads=[sh.G], writes=[sh.tmp2])
                    P.op("dve", lambda e: e.tensor_tensor(sh.tmp2.t[:, :], sh.kb.t[:, :], sh.tmp2.t[:, :], ALU.mult), reads=[sh.kb, sh.tmp2], writes=[sh.tmp2])
                    P.op("dve", lambda e: e.tensor_copy(o.kdd.t[:, :], sh.tmp2.t[:, :]), reads=[sh.tmp2], writes=[o.kdd])
                    G3 = sh.G.t[:, :].rearrange("p (c s) -> p c s", s=64)
                    P.op("act", lambda e: e.activation(o.egl.t[:, :], G3[:, :, 63], AF.Exp), reads=[sh.G], writes=[o.egl])
                    for cc in range(32):
                        P.op("dve", lambda e, cc=cc: e.tensor_scalar(sh.kend.t[:, cc * 64:(cc + 1) * 64], sh.tmp2.t[:, cc * 64:(cc + 1) * 64],
                                                                     o.egl.t[:, cc:cc + 1], None, ALU.mult),
                             reads=[sh.tmp2, o.egl], writes=[sh.kend])
                    for grp in range(2):
                        for j in range(8):
                            tk = grp * 8 + j
                            P.op("pe", lambda e, tk=tk, j=j: e.transpose(bankT.t[:, j * 128:(j + 1) * 128], sh.kend.t[:, tk * 128:(tk + 1) * 128], ident.t[:, :]),
                                 reads=[sh.kend, ident], writes=[bankT], pe_acc=(j > 0))
                        P.op("act", lambda e, grp=grp: e.activation(o.kT.t[:, grp * 8:(grp + 1) * 8, :],
                                                                   bankT.t[:, :].rearrange("p (t k) -> p t k", k=128), AF.Copy),
                             reads=[bankT], writes=[o.kT])

                nA = [0]

                def chunk(h, blk, cc):
                    o = hs[h]
                    tk, half = cc // 2, cc % 2
                    pb = 64 * half
                    gc = blk * 32 + cc
                    cs = slice(cc * 64, (cc + 1) * 64)
                    bA = bankA[nA[0] % 2]
                    am = o.am[nA[0] % 2]
                    nA[0] += 1
                    bO = bankO[h]
                    bU = bankU[h]
                    oc0 = (gc % 8) * 64
                    Sb = o.Sbf[o.si % 2]
                    Sn = o.Sbf[(o.si + 1) % 2]
                    o.si += 1
                    P.op("pe", lambda e: e.matmul(bA.t[pb:pb + 64, 0:64], o.kdd.t[:, cs], o.qd.t[:, cs], start=True, stop=True),
                         reads=[o.kdd, o.qd], writes=[bA])
                    P.op("dve", lambda e: e.tensor_tensor(am.t[pb:pb + 64, :], bA.t[pb:pb + 64, 0:64], m01.t[pb:pb + 64, :], ALU.mult),
                         reads=[bA, m01], writes=[am])
                    P.op("pe", lambda e: e.matmul(bO.t[:, oc0:oc0 + 64], Sb.t[:, :], o.qd.t[:, cs], start=True, stop=False),
                         reads=[Sb, o.qd], writes=[bO], pe_acc=(gc % 8 != 0))
                    P.op("pe", lambda e: e.matmul(bO.t[:, oc0:oc0 + 64], o.vb.t[pb:pb + 64, tk, :], am.t[pb:pb + 64, :], start=False, stop=True),
                         reads=[o.vb, am], writes=[bO], pe_acc=True)
                    P.op("pe", lambda e: e.matmul(bU.t[:, 0:128], o.kT.t[pb:pb + 64, tk, :], o.vb.t[pb:pb + 64, tk, :], start=True, stop=True),
                         reads=[o.kT, o.vb], writes=[bU])
                    P.op("dve", lambda e: e.scalar_tensor_tensor(o.S32.t[:, :], o.S32.t[:, :], o.egl.t[:, cc:cc + 1], bU.t[:, 0:128], ALU.mult, ALU.add),
                         reads=[o.S32, o.egl, bU], writes=[o.S32])
                    P.op("act", lambda e: e.activation(Sn.t[:, :], o.S32.t[:, :], AF.Copy), reads=[o.S32], writes=[Sn])
                    if gc % 8 == 7:
                        os_ = o.osb[(gc // 8) % 2]
                        P.op("act", lambda e: e.activation(os_.t[:, :], bO.t[:, :], AF.Copy), reads=[bO], writes=[os_])
                        tok0 = (gc - 7) * 64
                        P.dma("sp", o_loc.ap()[tok0 // T][h * 128:(h + 1) * 128, tok0 % T:tok0 % T + 512], os_.t[:, :], reads=[os_], ow=olb)

                oG = idram("hg_oG", [4, 4 * 256, T], BF16)
                oGb = Buf()
                for blk in range(4):
                    for h in range(2):
                        prep(h, blk)
                    for cc in range(32):
                        for h in range(2):
                            chunk(h, blk, cc)
                    P.collective("AllGather", G4, o_loc.ap()[blk].opt(), oG.ap()[blk].opt(), [olb], oGb)
            P.barrier()
            return oG, oGb

        ffn(0, 0, 0)
        R1 = projections("fox", 0)
        oG1, oG1b = fox_phase(R1)
        epilogue("fox", 0, oG1, oG1b, R1.sg, R1.sgb)
        ffn(1, 0, 2)
        ffn(2, 1, 0)
        R2 = projections("hgrn", 1)
        oG2, oG2b = hgrn_phase(R2)
        epilogue("hgrn", 1, oG2, oG2b, R2.sg, R2.sgb)
        ffn(3, 1, 2)
        xo = dout("xo", [D, T])
        xob = Buf()
        outs.append(xob)
        xov = xo.rearrange("(c p) t -> p c t", p=128)
        fgd = din("fg", [128, 8])
        fg = P.sb(es, "fg_sb", [128, 8], F32)
        P.dma("sp", fg.t[:, :], fgd, writes=[fg])
        P.op("dve", lambda e: e.tensor_scalar(fg.t[:, :], fg.t[:, :], SQD, None, ALU.mult), reads=[fg], writes=[fg])
        yo = [P.sb(es, "yo%d" % i, [128, 512], F32) for i in range(2)]
        it = 0
        for tt in range(4):
            t0 = tt * 512
            rstd_tile(lambda kc, t0=t0: X[:, kc, t0:t0 + 512], [c.Xb[kc][tt] for kc in range(8)], EPS * D)
            for kc in range(8):
                y_ = yo[it % 2]
                it += 1
                P.op("dve", lambda e, kc=kc, y_=y_, t0=t0: e.scalar_tensor_tensor(
                    y_.t[:, :], X[:, kc, t0:t0 + 512], fg.t[:, kc:kc + 1], c.rstd.t[:, :], ALU.mult, ALU.mult),
                    reads=[c.Xb[kc][tt], fg, c.rstd], writes=[y_])
                P.dma("sp", xov[:, kc, t0:t0 + 512], y_.t[:, :], reads=[y_], ow=xob)
        P.finish(outs)
        P.emit()
    return nc


_DBG = {}


def _run(nc, maps):
    return run_bass_kernel_spmd(nc, maps, core_ids=list(range(NCORES))).results


def kernel_unfused(x, c, ada_w, ada_b, norm_g, ffn_w_up, ffn_w_down, fox_w_in, fox_b_f, fox_w_out,
           hgrn_w_in, hgrn_norm_g, hgrn_w_out, hgrn_lb_logits, final_norm_g):
    f32 = lambda a: np.ascontiguousarray(np.asarray(a, dtype=np.float32))
    x, c, ada_w, ada_b, norm_g = f32(x), f32(c), f32(ada_w), f32(ada_b), f32(norm_g)
    ffn_w_up, ffn_w_down, fox_w_in, fox_b_f, fox_w_out = f32(ffn_w_up), f32(ffn_w_down), f32(fox_w_in), f32(fox_b_f), f32(fox_w_out)
    hgrn_w_in, hgrn_norm_g, hgrn_w_out = f32(hgrn_w_in), f32(hgrn_norm_g), f32(hgrn_w_out)
    hgrn_lb_logits, final_norm_g = f32(hgrn_lb_logits), f32(final_norm_g)

    mod = run_mod(c, ada_w, ada_b)
    _DBG["mod"] = mod
    modT = [fm_cols(mod[b].reshape(18, D)) for b in range(B)]
    gT = fm_cols(norm_g.reshape(6, D))
    cores = [(b, t) for b in range(B) for t in range(4)]

    nc1 = build_F({"ffns": [(0, 0)], "proj": ("fox", 0)})
    wi = fox_w_in[0]
    shared = {
        "gT": gT, "wup0": tile_wup(ffn_w_up[0, 0]), "wdn0": tile_wdn(ffn_w_down[0, 0]),
        "wq": tile_w_fm(wi[:, 0:D]), "wk": tile_w_fm(wi[:, D:2 * D]), "wg": tile_w_fm(wi[:, 3 * D:4 * D]),
        "wv": tile_w_tm(wi[:, 2 * D:3 * D]),
        "wf": np.ascontiguousarray(wi[:, 4 * D:4 * D + 16].reshape(8, 128, 16).transpose(1, 0, 2)).reshape(128, 128),
        "bf": np.ascontiguousarray(fox_b_f[0].reshape(16, 1)),
    }
    maps = []
    for (b, t) in cores:
        m = dict(shared)
        m["xT"] = np.ascontiguousarray(x[b, t * T:(t + 1) * T, :].T)
        m["modT"] = modT[b]
        maps.append(m)
    r1 = _run(nc1, maps)
    _DBG["r1"] = r1

    def cat_fm(res, name, b):
        return np.concatenate([res[b * 4 + t][name] for t in range(4)], axis=1)

    def cat_tm(res, name, b):
        return np.concatenate([res[b * 4 + t][name] for t in range(4)], axis=0)

    nc2 = build_fox()
    U, sel, mk = fox_consts()
    maps = []
    for b in range(B):
        qf = cat_fm(r1, "qT", b).reshape(FH, FD, S)
        kf = cat_fm(r1, "kT", b).reshape(FH, FD, S)
        vf = cat_tm(r1, "v", b)
        lf = cat_fm(r1, "lf", b)
        for g in range(4):
            l4 = lf[4 * g:4 * g + 4]
            v4 = np.stack([np.ascontiguousarray(vf[:, hd * FD:(hd + 1) * FD].reshape(64, 128, FD).transpose(1, 0, 2)).reshape(128, 64 * FD)
                           for hd in range(4 * g, 4 * g + 4)])
            maps.append({
                "q": np.ascontiguousarray(qf[4 * g:4 * g + 4]), "k": np.ascontiguousarray(kf[4 * g:4 * g + 4]), "v": v4,
                "lt": np.ascontiguousarray(l4.reshape(4, 64, 128).transpose(2, 0, 1)).reshape(128, 256),
                "lq": np.ascontiguousarray(l4.reshape(4, 16, 512).transpose(1, 0, 2)).reshape(16, 2048),
                "U": U, "sel": sel, "mk": mk,
            })
    r2 = _run(nc2, maps)
    _DBG["r2"] = r2
    ofull = [np.concatenate([r2[b * 4 + g]["o"].reshape(4 * FD, S) for g in range(4)], axis=0) for b in range(B)]

    nc3 = build_F({"epi": ("fox", 0), "ffns": [(0, 2), (1, 0)], "proj": ("hgrn", 1)})
    hi = hgrn_w_in[0]
    shared = {
        "gT": gT, "wo": tile_wo(fox_w_out[0]),
        "wup0": tile_wup(ffn_w_up[0, 1]), "wdn0": tile_wdn(ffn_w_down[0, 1]),
        "wup1": tile_wup(ffn_w_up[1, 0]), "wdn1": tile_wdn(ffn_w_down[1, 0]),
        "wq": tile_w_fm(hi[:, 0:D]), "wf": tile_w_fm(hi[:, D:2 * D]), "wg": tile_w_fm(hi[:, 3 * D:4 * D]),
        "wv": tile_w_tm(hi[:, 2 * D:3 * D]), "lbl": fm_cols(hgrn_lb_logits),
    }
    maps = []
    for i, (b, t) in enumerate(cores):
        m = dict(shared)
        m["xT"] = r1[i]["xo"]
        m["modT"] = modT[b]
        m["oT"] = np.ascontiguousarray(ofull[b][:, t * T:(t + 1) * T])
        m["sg"] = r1[i]["sgo"]
        maps.append(m)
    r3 = _run(nc3, maps)
    _DBG["r3"] = r3

    nc4 = build_hgrn()
    m01, rm, ident = hgrn_consts()
    maps = []
    for b in range(B):
        qf = cat_fm(r3, "qT", b).reshape(HH, 128, S)
        kf = cat_fm(r3, "kT", b).reshape(HH, 128, S)
        lf = cat_fm(r3, "lfT", b).reshape(HH, 128, S)
        vf = cat_tm(r3, "v", b)
        for g in range(4):
            v2 = np.stack([np.ascontiguousarray(vf[:, hd * 128:(hd + 1) * 128].reshape(64, 128, 128).transpose(1, 0, 2)).reshape(128, 64 * 128)
                           for hd in range(2 * g, 2 * g + 2)])
            maps.append({
                "q": np.ascontiguousarray(qf[2 * g:2 * g + 2]), "k": np.ascontiguousarray(kf[2 * g:2 * g + 2]),
                "lf": np.ascontiguousarray(lf[2 * g:2 * g + 2]), "v": v2, "m01": m01, "rm": rm, "ident": ident,
            })
    r4 = _run(nc4, maps)
    _DBG["r4"] = r4
    ofull = [np.concatenate([r4[b * 4 + g]["o"].reshape(256, S) for g in range(4)], axis=0) for b in range(B)]

    nc5 = build_F({"epi": ("hgrn", 1), "ffns": [(1, 2)], "final": True})
    shared = {
        "gT": gT, "wo": tile_wo(hgrn_w_out[0]), "hgn": fm_cols(hgrn_norm_g[0]),
        "wup0": tile_wup(ffn_w_up[1, 1]), "wdn0": tile_wdn(ffn_w_down[1, 1]),
        "fg": fm_cols(final_norm_g),
    }
    maps = []
    for i, (b, t) in enumerate(cores):
        m = dict(shared)
        m["xT"] = r3[i]["xo"]
        m["modT"] = modT[b]
        m["oT"] = np.ascontiguousarray(ofull[b][:, t * T:(t + 1) * T])
        m["sg"] = r3[i]["sgo"]
        maps.append(m)
    r5 = _run(nc5, maps)
    out = np.empty((B, S, D), np.float32)
    for i, (b, t) in enumerate(cores):
        out[b, t * T:(t + 1) * T, :] = r5[i]["xo"].T
    return out


def kernel(x, c, ada_w, ada_b, norm_g, ffn_w_up, ffn_w_down, fox_w_in, fox_b_f, fox_w_out,
           hgrn_w_in, hgrn_norm_g, hgrn_w_out, hgrn_lb_logits, final_norm_g):
    f32 = lambda a: np.ascontiguousarray(np.asarray(a, dtype=np.float32))
    x, c, ada_w, ada_b, norm_g = f32(x), f32(c), f32(ada_w), f32(ada_b), f32(norm_g)
    ffn_w_up, ffn_w_down, fox_w_in, fox_b_f, fox_w_out = f32(ffn_w_up), f32(ffn_w_down), f32(fox_w_in), f32(fox_b_f), f32(fox_w_out)
    hgrn_w_in, hgrn_norm_g, hgrn_w_out = f32(hgrn_w_in), f32(hgrn_norm_g), f32(hgrn_w_out)
    hgrn_lb_logits, final_norm_g = f32(hgrn_lb_logits), f32(final_norm_g)
    nc = build_mega()
    wi, hi = fox_w_in[0], hgrn_w_in[0]
    U, sel, mk = fox_consts()
    m01, rm, ident = hgrn_consts()
    shared = {
        "modb": fm_cols(ada_b.reshape(18, D)),
        "modw": np.ascontiguousarray(ada_w.reshape(2, 8, 128, 9, D).transpose(0, 3, 2, 1, 4)).reshape(18, 128, 8 * D),
        "gT": fm_cols(norm_g.reshape(6, D)),
        "wup0": tile_wup(ffn_w_up[0, 0]), "wdn0": tile_wdn(ffn_w_down[0, 0]),
        "wup1": tile_wup(ffn_w_up[0, 1]), "wdn1": tile_wdn(ffn_w_down[0, 1]),
        "wup2": tile_wup(ffn_w_up[1, 0]), "wdn2": tile_wdn(ffn_w_down[1, 0]),
        "wup3": tile_wup(ffn_w_up[1, 1]), "wdn3": tile_wdn(ffn_w_down[1, 1]),
        "fox_wq": tile_w_fm(wi[:, 0:D]), "fox_wk": tile_w_fm(wi[:, D:2 * D]), "fox_wg": tile_w_fm(wi[:, 3 * D:4 * D]),
        "fox_wv": tile_w_tm(wi[:, 2 * D:3 * D]),
        "fox_wf": np.ascontiguousarray(wi[:, 4 * D:4 * D + 16].reshape(8, 128, 16).transpose(1, 0, 2)).reshape(128, 128),
        "fox_bfb": np.ascontiguousarray(np.broadcast_to(np.tile(fox_b_f[0], 16), (128, 256))),
        "U": U, "sel": sel, "mk": mk, "identf": np.eye(128, dtype=np.float32),
        "fox_wo": tile_wo(fox_w_out[0]),
        "hgrn_wq": tile_w_fm(hi[:, 0:D]), "hgrn_wf": tile_w_fm(hi[:, D:2 * D]), "hgrn_wg": tile_w_fm(hi[:, 3 * D:4 * D]),
        "hgrn_wv": tile_w_tm(hi[:, 2 * D:3 * D]), "lbl": fm_cols(hgrn_lb_logits),
        "m01": m01, "rm": rm, "ident": ident,
        "hgrn_wo": tile_wo(hgrn_w_out[0]), "hgn": fm_cols(hgrn_norm_g[0]),
        "fg": fm_cols(final_norm_g),
    }
    maps = []
    cores = [(b, t) for b in range(B) for t in range(4)]
    for (b, t) in cores:
        m = dict(shared)
        m["xT"] = np.ascontiguousarray(x[b, t * T:(t + 1) * T, :].T)
        m["cT"] = fm_cols(c[b])
        maps.append(m)
    res = _run(nc, maps)
    out = np.empty((B, S, D), np.float32)
    for i, (b, t) in enumerate(cores):
        out[b, t * T:(t + 1) * T, :] = res[i]["xo"].T
    return out
```
